# Optimizing a Trainium2 kernel written in Bass

```python
import jax, jax.numpy as jnp
from jax import lax
import numpy as np

D_MODEL = 1024
BATCH = 8
SEQ = 4096
DEPTH = 4

EPS = 1e-6
BLOCK = 128
HEAD_DIM = 128
N_HEADS_A = 4
N_HEADS_B = 4
N_IDX_HEADS = 8
IDX_DIM = 64
TOPK_MAX = 256
N_HEADS_C = 4
QK_NOPE_DIM = 128
QK_ROPE_DIM = 64
V_DIM_C = 128
Q_LORA = 384
KV_LORA = 256
ROPE_THETA = 10000.0
SSM_D_INNER = 1024
SSM_HEAD_DIM = 64
SSM_HEADS = SSM_D_INNER // SSM_HEAD_DIM
SSM_GROUPS = 4
SSM_STATE = 128
CONV_WIDTH = 4
SSM_CHUNK = 128
CONV_DIM = SSM_D_INNER + 2 * SSM_GROUPS * SSM_STATE
D_FF = 4 * D_MODEL

N_EVEN = (DEPTH + 1) // 2
N_ODD = DEPTH // 2

EVEN_COLS = (N_HEADS_A * HEAD_DIM, HEAD_DIM, HEAD_DIM,
             N_IDX_HEADS * IDX_DIM, IDX_DIM, N_IDX_HEADS,
             N_HEADS_B * HEAD_DIM, N_HEADS_B * HEAD_DIM, N_HEADS_B * HEAD_DIM, N_HEADS_B)
ODD_COLS = (Q_LORA, KV_LORA, QK_ROPE_DIM,
            SSM_D_INNER, CONV_DIM, SSM_HEADS)
W_IN_EVEN = sum(EVEN_COLS)
W_IN_ODD = sum(ODD_COLS)
W_OUT_EVEN = N_HEADS_A * HEAD_DIM + N_HEADS_B * HEAD_DIM
W_OUT_ODD = N_HEADS_C * V_DIM_C + SSM_D_INNER

kernel_name = 'hybrid_dsa_fox_mla_mamba2_trunk'


def split_cols(t, sizes):
    out, off = [], 0
    for n in sizes:
        out.append(t[..., off:off + n])
        off += n
    return out


def rms_norm(x, g):
    x32 = x.astype(jnp.float32)
    y = x32 * lax.rsqrt(jnp.mean(x32 * x32, axis=-1, keepdims=True) + EPS)
    return (y * g.astype(jnp.float32)).astype(x.dtype)


def rope(x, positions):
    half = QK_ROPE_DIM // 2
    inv_freq = ROPE_THETA ** (-jnp.arange(half, dtype=jnp.float32) / half)
    ang = positions.astype(jnp.float32)[..., None] * inv_freq
    cos, sin = jnp.cos(ang)[:, :, None, :], jnp.sin(ang)[:, :, None, :]
    x32 = x.astype(jnp.float32)
    x1, x2 = x32[..., :half], x32[..., half:]
    return jnp.concatenate([x1 * cos - x2 * sin, x2 * cos + x1 * sin], axis=-1).astype(x.dtype)


def to_blocks(t, nb):
    return t.reshape(t.shape[0], nb, BLOCK, *t.shape[2:]).swapaxes(0, 1)


def causal_softmax_attention(q, k, v, log_decay=None):
    B, S, H, Dq = q.shape
    nb = S // BLOCK
    scale = Dq ** -0.5
    kpos = jnp.arange(S)
    ck = None if log_decay is None else log_decay.transpose(0, 2, 1)

    def block(args):
        qi, start = args
        logits = jnp.einsum('bqhd,bkhd->bhqk', qi, k).astype(jnp.float32) * scale
        if ck is not None:
            ci = lax.dynamic_slice_in_dim(ck, start, BLOCK, axis=2)
            logits = logits + ci[..., None] - ck[:, :, None, :]
        qpos = start + jnp.arange(BLOCK)
        mask = kpos[None, :] <= qpos[:, None]
        logits = jnp.where(mask, logits, -jnp.inf)
        p = jax.nn.softmax(logits, axis=-1).astype(v.dtype)
        return jnp.einsum('bhqk,bkhd->bqhd', p, v)

    out = lax.map(block, (to_blocks(q, nb), jnp.arange(nb) * BLOCK))
    return out.swapaxes(0, 1).reshape(B, S, H, v.shape[-1])


def dsa_attention(q, k, v, q_idx, k_idx, w_idx, topk):
    B, S, H, D = q.shape
    nb = S // BLOCK
    scale = D ** -0.5
    kpos = jnp.arange(S)
    gather = jax.vmap(lambda t, i: t[i])

    def block(args):
        qi, qii, wi, start = args
        s_h = jnp.einsum('bqjd,bkd->bjqk', qii, k_idx).astype(jnp.float32)
        score = jnp.einsum('bqj,bjqk->bqk', wi.astype(jnp.float32), jax.nn.relu(s_h))
        qpos = start + jnp.arange(BLOCK)
        admissible = kpos[None, :] <= qpos[:, None]
        score = jnp.where(admissible[None], score, -jnp.inf)
        _, idx = lax.top_k(score, topk)
        valid = idx <= qpos[None, :, None]
        k_sel = gather(k, idx)
        v_sel = gather(v, idx)
        logits = jnp.einsum('bqhd,bqkd->bhqk', qi, k_sel).astype(jnp.float32) * scale
        logits = jnp.where(valid[:, None], logits, -jnp.inf)
        p = jax.nn.softmax(logits, axis=-1).astype(v.dtype)
        return jnp.einsum('bhqk,bqkd->bqhd', p, v_sel)

    xs = (to_blocks(q, nb), to_blocks(q_idx, nb), to_blocks(w_idx, nb), jnp.arange(nb) * BLOCK)
    out = lax.map(block, xs)
    return out.swapaxes(0, 1).reshape(B, S, H, D)


def causal_depthwise_conv(x, w, b):
    C = x.shape[-1]
    xp = jnp.pad(x, ((0, 0), (CONV_WIDTH - 1, 0), (0, 0)))
    y = lax.conv_general_dilated(xp, w[:, None, :].astype(x.dtype), window_strides=(1,), padding='VALID',
                                 dimension_numbers=('NWC', 'WIO', 'NWC'), feature_group_count=C)
    return y + b.astype(x.dtype)


def ssd_chunked(x, dt, A, Bm, Cm):
    Bsz, S, H, P = x.shape
    G, N = Bm.shape[2], Bm.shape[3]
    R = H // G
    Q = SSM_CHUNK
    nc = S // Q
    xd = (x.astype(jnp.float32) * dt[..., None]).reshape(Bsz, nc, Q, G, R, P)
    a = (dt * A).reshape(Bsz, nc, Q, G, R)
    Bc = Bm.astype(jnp.float32).reshape(Bsz, nc, Q, G, N)
    Cc = Cm.astype(jnp.float32).reshape(Bsz, nc, Q, G, N)
    a_cum = jnp.cumsum(a, axis=2)
    acT = jnp.moveaxis(a_cum, 2, -1)
    causal = jnp.tril(jnp.ones((Q, Q), dtype=bool))
    Lmat = jnp.exp(jnp.where(causal, acT[..., :, None] - acT[..., None, :], -jnp.inf))
    CB = jnp.einsum('bclgn,bcsgn->bcgls', Cc, Bc)
    y_diag = jnp.einsum('bcgrls,bcsgrp->bclgrp', CB[:, :, :, None] * Lmat, xd)
    decay_to_end = jnp.exp(a_cum[:, :, -1:] - a_cum)
    states = jnp.einsum('bclgn,bclgrp->bcgrpn', Bc, xd * decay_to_end[..., None])
    chunk_decay = jnp.exp(a_cum[:, :, -1])

    def step(h, inp):
        st, dec = inp
        return h * dec[..., None, None] + st, h

    h0 = jnp.zeros((Bsz, G, R, P, N), jnp.float32)
    _, h_start = lax.scan(step, h0, (jnp.moveaxis(states, 1, 0), jnp.moveaxis(chunk_decay, 1, 0)))
    h_start = jnp.moveaxis(h_start, 0, 1)
    y_off = jnp.einsum('bclgn,bcgrpn->bclgrp', Cc, h_start) * jnp.exp(a_cum)[..., None]
    return (y_diag + y_off).reshape(Bsz, S, H, P)


def even_mixer(h, w_in, b_f, qn_a, kn_a, qn_b, kn_b, w_out):
    B, S, _ = h.shape
    topk = min(TOPK_MAX, S // 4)
    qa, ka, va, qi, ki, wi, qb, kb, vb, fb = split_cols(h @ w_in, EVEN_COLS)
    qa = rms_norm(qa.reshape(B, S, N_HEADS_A, HEAD_DIM), qn_a)
    ka = rms_norm(ka, kn_a)
    qi = qi.reshape(B, S, N_IDX_HEADS, IDX_DIM)
    oa = dsa_attention(qa, ka, va, qi, ki, wi, topk)
    qb = rms_norm(qb.reshape(B, S, N_HEADS_B, HEAD_DIM), qn_b)
    kb = rms_norm(kb.reshape(B, S, N_HEADS_B, HEAD_DIM), kn_b)
    vb = vb.reshape(B, S, N_HEADS_B, HEAD_DIM)
    log_f = jax.nn.log_sigmoid((fb + b_f).astype(jnp.float32))
    ob = causal_softmax_attention(qb, kb, vb, jnp.cumsum(log_f, axis=1))
    o = jnp.concatenate([oa.reshape(B, S, -1), ob.reshape(B, S, -1)], axis=-1)
    return o @ w_out


def odd_mixer(h, positions, w_in, cq_norm, ckv_norm, w_uq, w_ukv, qn_c, kn_c,
              conv_w, conv_b, dt_bias, a_log, d_skip, gate_norm, w_out):
    B, S, _ = h.shape
    cq, ckv, kr, z, xbc, dt = split_cols(h @ w_in, ODD_COLS)
    q = (rms_norm(cq, cq_norm) @ w_uq).reshape(B, S, N_HEADS_C, QK_NOPE_DIM + QK_ROPE_DIM)
    kv = (rms_norm(ckv, ckv_norm) @ w_ukv).reshape(B, S, N_HEADS_C, QK_NOPE_DIM + V_DIM_C)
    k_nope, v = kv[..., :QK_NOPE_DIM], kv[..., QK_NOPE_DIM:]
    k = jnp.concatenate([k_nope, jnp.broadcast_to(kr[:, :, None, :], (B, S, N_HEADS_C, QK_ROPE_DIM))], axis=-1)
    q = rms_norm(q, qn_c)
    k = rms_norm(k, kn_c)
    q = jnp.concatenate([q[..., :QK_NOPE_DIM], rope(q[..., QK_NOPE_DIM:], positions)], axis=-1)
    k = jnp.concatenate([k[..., :QK_NOPE_DIM], rope(k[..., QK_NOPE_DIM:], positions)], axis=-1)
    oc = causal_softmax_attention(q, k, v)
    xbc = jax.nn.silu(causal_depthwise_conv(xbc, conv_w, conv_b))
    xs, Bm, Cm = split_cols(xbc, (SSM_D_INNER, SSM_GROUPS * SSM_STATE, SSM_GROUPS * SSM_STATE))
    xs = xs.reshape(B, S, SSM_HEADS, SSM_HEAD_DIM)
    Bm = Bm.reshape(B, S, SSM_GROUPS, SSM_STATE)
    Cm = Cm.reshape(B, S, SSM_GROUPS, SSM_STATE)
    dt = jax.nn.softplus((dt + dt_bias).astype(jnp.float32))
    A = -jnp.exp(a_log.astype(jnp.float32))
    y = ssd_chunked(xs, dt, A, Bm, Cm) + d_skip.astype(jnp.float32)[:, None] * xs.astype(jnp.float32)
    y = y.reshape(B, S, SSM_D_INNER) * jax.nn.silu(z.astype(jnp.float32))
    y = rms_norm(y.reshape(B, S, SSM_GROUPS, -1), gate_norm.reshape(SSM_GROUPS, -1))
    y = y.reshape(B, S, SSM_D_INNER).astype(h.dtype)
    o = jnp.concatenate([oc.reshape(B, S, -1), y], axis=-1)
    return o @ w_out


def squared_relu_mlp(h, w1, w2):
    return jnp.square(jax.nn.relu(h @ w1)) @ w2


def setup_inputs(seed: int = 0) -> dict:
    key = jax.random.key(seed)
    ks = jax.random.split(key, 32)
    f32 = jnp.float32

    def nrm(k, shape, scale):
        return jax.random.normal(k, shape, f32) * scale

    def gain(k, shape):
        return 1.0 + 0.02 * jax.random.normal(k, shape, f32)

    dt_init = jnp.exp(jax.random.uniform(ks[20], (N_ODD, SSM_HEADS), f32, np.log(1e-3), np.log(1e-1)))
    return {
        'x': nrm(ks[0], (BATCH, SEQ, D_MODEL), 1.0),
        'positions': jnp.tile(jnp.arange(SEQ, dtype=jnp.int32)[None, :], (BATCH, 1)),
        'ev_norm': gain(ks[1], (N_EVEN, D_MODEL)),
        'ev_w_in': nrm(ks[2], (N_EVEN, D_MODEL, W_IN_EVEN), D_MODEL ** -0.5),
        'ev_b_f': jax.random.uniform(ks[3], (N_EVEN, N_HEADS_B), f32, 1.0, 4.0),
        'ev_qn_a': gain(ks[4], (N_EVEN, HEAD_DIM)),
        'ev_kn_a': gain(ks[5], (N_EVEN, HEAD_DIM)),
        'ev_qn_b': gain(ks[6], (N_EVEN, HEAD_DIM)),
        'ev_kn_b': gain(ks[7], (N_EVEN, HEAD_DIM)),
        'ev_w_out': nrm(ks[8], (N_EVEN, W_OUT_EVEN, D_MODEL), W_OUT_EVEN ** -0.5),
        'od_norm': gain(ks[9], (N_ODD, D_MODEL)),
        'od_w_in': nrm(ks[10], (N_ODD, D_MODEL, W_IN_ODD), D_MODEL ** -0.5),
        'od_cq_norm': gain(ks[11], (N_ODD, Q_LORA)),
        'od_ckv_norm': gain(ks[12], (N_ODD, KV_LORA)),
        'od_w_uq': nrm(ks[13], (N_ODD, Q_LORA, N_HEADS_C * (QK_NOPE_DIM + QK_ROPE_DIM)), Q_LORA ** -0.5),
        'od_w_ukv': nrm(ks[14], (N_ODD, KV_LORA, N_HEADS_C * (QK_NOPE_DIM + V_DIM_C)), KV_LORA ** -0.5),
        'od_qn_c': gain(ks[15], (N_ODD, QK_NOPE_DIM + QK_ROPE_DIM)),
        'od_kn_c': gain(ks[16], (N_ODD, QK_NOPE_DIM + QK_ROPE_DIM)),
        'od_conv_w': nrm(ks[17], (N_ODD, CONV_WIDTH, CONV_DIM), CONV_WIDTH ** -0.5),
        'od_conv_b': nrm(ks[18], (N_ODD, CONV_DIM), 0.02),
        'od_dt_bias': dt_init + jnp.log(-jnp.expm1(-dt_init)),
        'od_a_log': jnp.log(jax.random.uniform(ks[21], (N_ODD, SSM_HEADS), f32, 1.0, 16.0)),
        'od_d_skip': 1.0 + 0.1 * jax.random.normal(ks[22], (N_ODD, SSM_HEADS), f32),
        'od_gate_norm': gain(ks[23], (N_ODD, SSM_D_INNER)),
        'od_w_out': nrm(ks[24], (N_ODD, W_OUT_ODD, D_MODEL), W_OUT_ODD ** -0.5),
        'mlp_norm': gain(ks[25], (DEPTH, D_MODEL)),
        'mlp_w1': nrm(ks[26], (DEPTH, D_MODEL, D_FF), D_MODEL ** -0.5),
        'mlp_w2': nrm(ks[27], (DEPTH, D_FF, D_MODEL), D_FF ** -0.5),
    }


def reference(x, positions, ev_norm, ev_w_in, ev_b_f, ev_qn_a, ev_kn_a, ev_qn_b, ev_kn_b, ev_w_out,
              od_norm, od_w_in, od_cq_norm, od_ckv_norm, od_w_uq, od_w_ukv, od_qn_c, od_kn_c,
              od_conv_w, od_conv_b, od_dt_bias, od_a_log, od_d_skip, od_gate_norm, od_w_out,
              mlp_norm, mlp_w1, mlp_w2):
    for layer in range(DEPTH):
        i = layer // 2
        if layer % 2 == 0:
            x = x + even_mixer(rms_norm(x, ev_norm[i]), ev_w_in[i], ev_b_f[i], ev_qn_a[i], ev_kn_a[i],
                               ev_qn_b[i], ev_kn_b[i], ev_w_out[i])
        else:
            x = x + odd_mixer(rms_norm(x, od_norm[i]), positions, od_w_in[i], od_cq_norm[i], od_ckv_norm[i],
                              od_w_uq[i], od_w_ukv[i], od_qn_c[i], od_kn_c[i], od_conv_w[i], od_conv_b[i],
                              od_dt_bias[i], od_a_log[i], od_d_skip[i], od_gate_norm[i], od_w_out[i])
        x = x + squared_relu_mlp(rms_norm(x, mlp_norm[layer]), mlp_w1[layer], mlp_w2[layer])
    return x
```

```python
import contextlib
import numpy as np
import ml_dtypes
import concourse.bass as bass
import concourse.mybir as mybir
from concourse.bass_utils import run_bass_kernel_spmd

F32 = mybir.dt.float32
BF16 = mybir.dt.bfloat16
I32 = mybir.dt.int32
AF = mybir.ActivationFunctionType
ALU = mybir.AluOpType
AX = mybir.AxisListType

S = 4096
D = 1024
NT = S // 128
DFF = 4096
EPS = 1e-6
N_CORES = 8
WRITE_KEYS = ("out", "accum_out", "ap")


class V:
    def __init__(self, ap, bufs):
        self.ap = ap
        self.bufs = bufs

    def __getitem__(self, idx):
        return V(self.ap[idx], self.bufs)

    def bc(self, shape):
        return V(self.ap.to_broadcast(shape), self.bufs)

    def re(self, pat, **kw):
        return V(self.ap.rearrange(pat, **kw), self.bufs)

    def bitcast(self, dt):
        return V(self.ap.bitcast(dt), self.bufs)


class Buf:
    def __init__(self, ap):
        self.ap = ap
        self.w = None
        self.r = {}
        self.excl = False

    def __getitem__(self, idx):
        return V(self.ap[idx], (self,))

    def v(self):
        return V(self.ap, (self,))


def multi(*views):
    bufs = []
    for v in views:
        bufs.extend(v.bufs)
    return V(views[0].ap, tuple(bufs))


class Eng:
    def __init__(self, k, name, raw, self_sync):
        self.name = name
        self.raw = raw
        self.sem = k.new_sem("e_" + name)
        self.cnt = 0
        self.seen = {}
        self.self_sync = self_sync


class Slot:
    def __init__(self, k, key):
        self.key = key
        self.sem = k.new_sem(key)
        self.val = 0


class K:
    NSLOT = 12

    def __init__(self, nc):
        self.nc = nc
        self.es = contextlib.ExitStack()
        self.pe = Eng(self, "pe", nc.tensor, False)
        self.act = Eng(self, "act", nc.scalar, True)
        self.dve = Eng(self, "dve", nc.vector, True)
        self.pool = Eng(self, "pool", nc.gpsimd, True)
        self.sp = Eng(self, "sp", nc.sync, False)
        self.engs = [self.pe, self.act, self.dve, self.pool, self.sp]
        self.queues = {}
        for q in (self.sp, self.pool):
            self.queues[q.name] = [Slot(self, "d_%s_%d" % (q.name, i)) for i in range(self.NSLOT)]
        self.qnext = {q: 0 for q in self.queues}
        self.nph = 0

    def new_sem(self, name):
        return self.es.enter_context(self.nc.semaphore(name))

    def _wait(self, eng, tok):
        key, sem, val = tok
        if key == eng.name and not eng.self_sync:
            return
        if eng.seen.get(key, 0) >= val:
            return
        eng.raw.wait_ge(sem, val)
        eng.seen[key] = val

    def _deps(self, eng, reads, writes):
        for v in reads:
            for b in v.bufs:
                if b.w is not None:
                    self._wait(eng, b.w)
                if b.excl:
                    for t in b.r.values():
                        if t[0] != eng.name:
                            self._wait(eng, t)
        for v in writes:
            for b in v.bufs:
                if b.w is not None:
                    self._wait(eng, b.w)
                for t in b.r.values():
                    self._wait(eng, t)

    def _mark(self, tok, reads, writes):
        for v in reads:
            for b in v.bufs:
                b.r[tok[0]] = tok
        for v in writes:
            for b in v.bufs:
                b.w = tok
                b.r = {}

    def call(self, eng, method, **kw):
        reads, writes, args = [], [], {}
        for key, v in kw.items():
            if isinstance(v, V):
                (writes if key in WRITE_KEYS else reads).append(v)
                args[key] = v.ap
            else:
                args[key] = v
        self._deps(eng, reads, writes)
        inst = getattr(eng.raw, method)(**args)
        eng.cnt += 1
        inst.then_inc(eng.sem, 1)
        self._mark((eng.name, eng.sem, eng.cnt), reads, writes)
        return inst

    def dma(self, out, in_, q=None, **kw):
        q = q or self.sp
        slots = self.queues[q.name]
        slot = slots[self.qnext[q.name] % len(slots)]
        self.qnext[q.name] += 1
        if slot.val > 0:
            self._wait(q, (slot.key, slot.sem, slot.val))
        self._deps(q, [in_], [out])
        slot.val += 16
        q.raw.dma_start(out=out.ap, in_=in_.ap, **kw).then_inc(slot.sem, 16)
        self._mark((slot.key, slot.sem, slot.val), [in_], [out])

    def barrier(self):
        toks = [(e.name, e.sem, e.cnt) for e in self.engs if e.cnt > 0]
        for sl in self.queues.values():
            toks += [(s.key, s.sem, s.val) for s in sl if s.val > 0]
        for e in self.engs:
            for t in toks:
                self._wait(e, t)

    def mm(self, out, lhsT, rhs, start=True, stop=True):
        return self.call(self.pe, "matmul", out=out, lhsT=lhsT, rhs=rhs, start=start, stop=stop)

    def tr(self, out, in_, ident):
        return self.call(self.pe, "transpose", out=out, in_=in_, identity=ident)

    def actf(self, out, in_, func, **kw):
        return self.call(self.act, "activation", out=out, in_=in_, func=func, **kw)

    def tt(self, out, in0, in1, op, eng=None):
        return self.call(eng or self.dve, "tensor_tensor", out=out, in0=in0, in1=in1, op=op)

    def ts(self, out, in0, s1, op0, s2=None, op1=None, eng=None, **kw):
        if op1 is None:
            return self.call(eng or self.dve, "tensor_scalar", out=out, in0=in0, scalar1=s1, scalar2=None,
                             op0=op0, **kw)
        return self.call(eng or self.dve, "tensor_scalar", out=out, in0=in0, scalar1=s1, scalar2=s2,
                         op0=op0, op1=op1, **kw)

    def stt(self, out, in0, scalar, in1, op0, op1, **kw):
        return self.call(self.dve, "scalar_tensor_tensor", out=out, in0=in0, scalar=scalar, in1=in1,
                         op0=op0, op1=op1, **kw)

    def copy(self, out, in_, eng=None):
        eng = eng or self.dve
        if eng is self.act:
            return self.call(eng, "copy", out=out, in_=in_)
        return self.call(eng, "tensor_copy", out=out, in_=in_)

    def memset(self, ap, val, eng=None):
        return self.call(eng or self.dve, "memset", ap=ap, constant=val)

    @contextlib.contextmanager
    def phase(self):
        self.barrier()
        self.nph += 1
        ph = Phase(self, "p%d" % self.nph)
        with ph.es:
            yield ph
            self.barrier()


class Phase:
    def __init__(self, k, name):
        self.k = k
        self.name = name
        self.es = contextlib.ExitStack()
        self.n = 0

    def sbt(self, shape, dtype):
        self.n += 1
        return self.es.enter_context(self.k.nc.sbuf_tensor("%s_s%d" % (self.name, self.n), list(shape), dtype))

    def sb(self, shape, dtype):
        t = self.sbt(shape, dtype)
        return Buf(t[tuple(slice(None) for _ in shape)])

    def sbs(self, shape, dtype, n):
        return [self.sb(shape, dtype) for _ in range(n)]

    def split(self, shape, dtype, axis, step=1):
        t = self.sbt(shape, dtype)
        out = []
        for i in range(0, shape[axis], step):
            idx = [slice(None)] * len(shape)
            idx[axis] = slice(i, i + step) if step > 1 else i
            out.append(Buf(t[tuple(idx)]))
        return out

    def psum(self, n=8):
        out = []
        for i in range(n):
            self.n += 1
            t = self.es.enter_context(self.k.nc.psum_tensor("%s_ps%d" % (self.name, self.n), [128, 512], F32))
            b = Buf(t[:, :])
            b.excl = True
            out.append(b)
        return out


class Rot:
    def __init__(self, items):
        self.items = items
        self.i = 0

    def next(self):
        it = self.items[self.i % len(self.items)]
        self.i += 1
        return it


def dram_buf(ap):
    return Buf(ap)


class Ctx:
    pass


def setup_consts(k, cd):
    nc = k.nc
    c = Ctx()
    es = k.es
    def sb(name, shape, dt):
        t = es.enter_context(nc.sbuf_tensor(name, list(shape), dt))
        return Buf(t[tuple(slice(None) for _ in shape)])
    c.ident = sb("k_ident", [128, 128], BF16)
    c.mhalf = sb("k_mhalf", [128, 1], F32)
    tmp = sb("k_tmp", [128, 128], F32)
    k.dma(tmp.v(), V(cd["ident"], (Buf(cd["ident"]),)))
    k.copy(c.ident.v(), tmp.v(), eng=k.dve)
    k.memset(c.mhalf.v(), -0.5, eng=k.dve)
    c.epscol = sb("k_epscol", [128, 1], F32)
    k.memset(c.epscol.v(), EPS, eng=k.dve)
    c.identf = sb("k_identf", [128, 128], F32)
    k.copy(c.identf.v(), tmp.v(), eng=k.dve)
    c.ones = sb("k_ones", [128, 128], BF16)
    k.memset(c.ones.v(), 1.0, eng=k.dve)
    c.onesf = sb("k_onesf", [128, 128], F32)
    k.memset(c.onesf.v(), 1.0, eng=k.dve)
    c.tri = sb("k_tri", [128, 128], BF16)
    k.dma(tmp.v(), V(cd["tri"], (Buf(cd["tri"]),)))
    k.copy(c.tri.v(), tmp.v(), eng=k.dve)
    c.trif = sb("k_trif", [128, 128], F32)
    k.copy(c.trif.v(), tmp.v(), eng=k.dve)
    c.invf = sb("k_invf", [64, 1], F32)
    k.dma(c.invf.v(), V(cd["invf"], (Buf(cd["invf"]),)))
    c.rotm = sb("k_rotm", [64, 64], BF16)
    k.dma(tmp[0:64, 0:64], V(cd["rotm"], (Buf(cd["rotm"]),)))
    k.copy(c.rotm.v(), tmp[0:64, 0:64], eng=k.dve)
    c.negtril_d = V(cd["negtril"], (Buf(cd["negtril"]),))
    c.negbig = sb("k_negbig", [128, 1], F32)
    k.memset(c.negbig.v(), -1e29, eng=k.dve)
    c.pow2 = sb("k_pow2", [128, 32], F32)
    k.dma(c.pow2.v(), V(cd["pow2"], (Buf(cd["pow2"]),)))
    c.negtri = sb("k_negtri", [128, 128], F32)
    k.dma(c.negtri.v(), V(cd["negtri"], (Buf(cd["negtri"]),)))
    return c


def rmsnorm_tile(k, c, ph, xt, gt, hn, scr, st):
    k.actf(scr.v(), xt.v(), AF.Square, accum_out=st[:, 0:1])
    k.ts(st[:, 1:2], st[:, 0:1], 1.0 / D, ALU.mult, EPS, ALU.add)
    k.tt(st[:, 2:3], st[:, 1:2], c.mhalf.v(), ALU.pow, eng=k.pool)
    k.stt(hn.v(), xt.v(), st[:, 2:3], gt.v(), ALU.mult, ALU.mult)


def phase_mlp(k, c, x_d, xo_d, g_row, w1_d, w2_d):
    G = 256
    NG = S // G
    with k.phase() as ph:
        w1b = ph.split([128, 8, DFF], BF16, 1)
        w2b = ph.split([128, 32, D], BF16, 1)
        stg = Rot(ph.sbs([128, 2048], F32, 2))
        gt = ph.sb([128, D], F32)
        xin = Rot(ph.sbs([128, D], F32, 3))
        scr = ph.sb([128, D], BF16)
        stats = Rot(ph.sbs([128, 4], F32, 4))
        hn = Rot(ph.sbs([128, D], BF16, 2))
        hT = Rot(ph.sbs([128, 8, G], BF16, 2))
        rl = Rot(ph.sbs([128, G], BF16, 3))
        hid = Rot(ph.sbs([128, G], BF16, 3))
        xres = Rot(ph.sbs([128, D], F32, 3))
        ps = ph.psum(8)
        ps_y = ps[0:4]
        ps_h = Rot(ps[4:6])
        ps_t = Rot(ps[6:8])
        xd = Buf(x_d)
        xod = Buf(xo_d)
        w1d = Buf(w1_d)
        w2d = Buf(w2_d)
        k.dma(gt.v(), V(g_row.to_broadcast([128, D]), (Buf(g_row),)))
        for kk in range(8):
            for hf in range(2):
                s = stg.next()
                k.dma(s.v(), V(w1_d[kk * 128:(kk + 1) * 128, hf * 2048:(hf + 1) * 2048], (w1d,)))
                k.copy(w1b[kk][:, hf * 2048:(hf + 1) * 2048], s.v(), eng=k.pool)
        w2v = w2_d.rearrange("(j p) n -> p j n", p=128)
        for jj in range(0, 32, 2):
            s = stg.next()
            k.dma(s.v().re("p (j n) -> p j n", j=2), V(w2v[:, jj:jj + 2, :], (w2d,)))
            eng = k.pool
            k.copy(w2b[jj][:, :], s[:, 0:1024], eng=eng)
            k.copy(w2b[jj + 1][:, :], s[:, 1024:2048], eng=eng)

        def norm_a(g):
            res = []
            for t in range(G // 128):
                xt = xin.next()
                r0 = g * G + t * 128
                k.dma(xt.v(), V(x_d[r0:r0 + 128, :], (xd,)))
                h = hn.next()
                rmsnorm_tile(k, c, ph, xt, gt, h, scr, stats.next())
                res.append(h)
            return res

        def norm_b(g, hs):
            hTg = hT.next()
            for t, h in enumerate(hs):
                pt = ps_t.next()
                ptb = pt.v().bitcast(BF16)
                for kk in range(8):
                    k.tr(ptb[:, kk * 128:(kk + 1) * 128], h[:, kk * 128:(kk + 1) * 128], c.ident.v())
                k.copy(hTg[:, :, t * 128:(t + 1) * 128], ptb.re("p (k t) -> p k t", k=8), eng=k.act)
            return hTg

        hs = norm_a(0)
        hT_cur = norm_b(0, hs)
        for g in range(NG):
            hs_next = None
            hT_next = None
            pend = None
            for j in range(32):
                ph_ = ps_h.next()
                for kk in range(8):
                    k.mm(ph_[:, 0:G], w1b[kk][:, j * 128:(j + 1) * 128], hT_cur[:, kk, :],
                         start=(kk == 0), stop=(kk == 7))
                r = rl.next()
                k.actf(r.v(), ph_[:, 0:G], AF.Relu)
                hd = hid.next()
                k.tt(hd.v(), r.v(), r.v(), ALU.mult)
                if pend is not None:
                    pj, phd = pend
                    for t in range(2):
                        for cc in range(2):
                            k.mm(ps_y[t * 2 + cc].v(), phd[:, t * 128:(t + 1) * 128],
                                 w2b[pj][:, cc * 512:(cc + 1) * 512], start=(pj == 0), stop=False)
                pend = (j, hd)
                if j == 4 and g + 1 < NG:
                    hs_next = norm_a(g + 1)
                if j == 20 and g + 1 < NG:
                    hT_next = norm_b(g + 1, hs_next)
            pj, phd = pend
            for t in range(2):
                for cc in range(2):
                    k.mm(ps_y[t * 2 + cc].v(), phd[:, t * 128:(t + 1) * 128],
                         w2b[pj][:, cc * 512:(cc + 1) * 512], start=False, stop=True)
            for t in range(2):
                r0 = g * G + t * 128
                xr = xres.next()
                k.dma(xr.v(), V(x_d[r0:r0 + 128, :], (xd,)))
                for cc in range(2):
                    k.tt(xr[:, cc * 512:(cc + 1) * 512], ps_y[t * 2 + cc].v(), xr[:, cc * 512:(cc + 1) * 512], ALU.add)
                k.dma(V(xo_d[r0:r0 + 128, :], (xod,)), xr.v())
            hT_cur = hT_next


def xnorm_group(k, c, x_d, xd, g, gt, xin, hn, scr, stats, hTg, ps_t, ntile=4):
    G = ntile * 128
    for t in range(ntile):
        xt = xin.next()
        r0 = g * G + t * 128
        k.dma(xt.v(), V(x_d[r0:r0 + 128, :], (xd,)))
        h = hn.next()
        rmsnorm_tile(k, c, None, xt, gt, h, scr, stats.next())
        pt = ps_t.next()
        ptb = pt.v().bitcast(BF16)
        for kk in range(8):
            k.tr(ptb[:, kk * 128:(kk + 1) * 128], h[:, kk * 128:(kk + 1) * 128], c.ident.v())
        k.copy(hTg[:, :, t * 128:(t + 1) * 128], ptb.re("p (k t) -> p k t", k=8), eng=k.act)


def load_w_bf16(k, ph, w_d, nk, ncols, stg_cols=None):
    wb = ph.split([128, nk, ncols], BF16, 1)
    stg = Rot(ph.sbs([128, ncols], F32, 2))
    wd = Buf(w_d)
    rows = w_d.shape[0]
    for kk in range(nk):
        s = stg.next()
        r = min(128, rows - kk * 128)
        k.dma(s[0:r, :], V(w_d[kk * 128:kk * 128 + r, :], (wd,)))
        k.copy(wb[kk][0:r, :], s[0:r, :], eng=k.pool)
    return wb


def fm_qknorm(k, c, ps, M, gcol, outb, sq, lnb, rstd, ps2, hd):
    N = ps.ap.shape[-1]
    k.actf(sq[0:M, 0:N], ps, AF.Square)
    k.mm(ps2[0:M, 0:N], c.ones[0:M, 0:M], sq[0:M, 0:N])
    k.actf(lnb[0:M, 0:N], ps2[0:M, 0:N], AF.Ln, scale=1.0 / hd, bias=c.epscol[0:M, :])
    k.actf(rstd[0:M, 0:N], lnb[0:M, 0:N], AF.Exp, scale=-0.5)
    k.stt(outb, ps, gcol, rstd[0:M, 0:N], ALU.mult, ALU.mult)


EV = dict(qa=0, ka=512, va=640, qi=768, ki=1280, wi=1344, qb=1352, kb=1864, vb=2376, fb=2888)


def phase_even_proj(k, c, sc, x_d, g_row, w_d, qn_a, kn_a, qn_b, kn_b):
    with k.phase() as ph:
        wb = load_w_bf16(k, ph, w_d, 8, 2892)
        wkd = ph.sb([128, 8, 128], BF16)
        for kk in range(8):
            k.copy(wkd[:, kk, 0:64], wb[kk][:, 1280:1344], eng=k.pool)
            k.copy(wkd[:, kk, 64:128], wb[kk][:, 1280:1344], eng=k.pool)
        gt = ph.sb([128, D], F32)
        k.dma(gt.v(), V(g_row.to_broadcast([128, D]), (Buf(g_row),)))
        gcol = ph.sb([128, 4], F32)
        for i, gn in enumerate((qn_a, kn_a, qn_b, kn_b)):
            k.dma(gcol[:, i:i + 1], V(gn.rearrange("(p o) -> p o", o=1), (Buf(gn),)))
        xin = Rot(ph.sbs([128, D], F32, 3))
        scr = ph.sb([128, D], BF16)
        stats = Rot(ph.sbs([128, 4], F32, 4))
        hn = Rot(ph.sbs([128, D], BF16, 2))
        hT = Rot(ph.sbs([128, 8, 512], BF16, 2))
        sq = Rot(ph.sbs([128, 512], BF16, 2))
        lnb = Rot(ph.sbs([128, 512], F32, 2))
        rstd = Rot(ph.sbs([128, 512], F32, 2))
        ob = Rot(ph.sbs([128, 512], BF16, 4))
        of = Rot(ph.sbs([128, 512], F32, 2))
        ps = ph.psum(8)
        psA = Rot(ps[0:3])
        psB = Rot(ps[3:5])
        ps_t = Rot(ps[5:7])
        psC = Rot(ps[7:8])
        xd = Buf(x_d)
        chunks = []
        for h in range(4):
            chunks.append((wb, EV["qa"] + h * 128, 128, 0, sc["qT"][h]))
        chunks.append((wb, EV["ka"], 128, 1, sc["kT"][0]))
        for cc in range(4):
            chunks.append((wb, EV["qi"] + cc * 128, 128, None, sc["qiT"][cc]))
        chunks.append((None, 0, 128, None, sc["kiT"]))
        for h in range(4):
            chunks.append((wb, EV["qb"] + h * 128, 128, 2, sc["qT"][4 + h]))
        for h in range(4):
            chunks.append((wb, EV["kb"] + h * 128, 128, 3, sc["kT"][1 + h]))
        chunks.append((wb, EV["fb"], 4, "f32", sc["fbT"]))
        ci = 0
        for g in range(S // 512):
            hTg = hT.next()
            xnorm_group(k, c, x_d, xd, g, gt, xin, hn, scr, stats, hTg, ps_t)
            tok = slice(g * 512, (g + 1) * 512)
            for (wsrc, c0, M, nrm, dst) in chunks:
                p = psA.next()
                for kk in range(8):
                    lhsT = wkd[:, kk, :] if wsrc is None else wb[kk][:, c0:c0 + M]
                    k.mm(p[0:M, :], lhsT, hTg[:, kk, :], start=(kk == 0), stop=(kk == 7))
                if nrm == "f32":
                    o = of.next()
                    k.copy(o[0:M, :], p[0:M, :], eng=k.dve)
                    k.dma(V(dst.ap[0:M, tok], dst.bufs), o[0:M, :])
                    continue
                o = ob.next()
                if nrm is None:
                    ci += 1
                    k.copy(o[0:M, :], p[0:M, :], eng=(k.act if ci % 2 else k.dve))
                else:
                    fm_qknorm(k, c, p[0:M, :], M, gcol[:, nrm:nrm + 1], o[0:M, :], sq.next(), lnb.next(),
                              rstd.next(), psB.next(), 128)
                k.dma(V(dst.ap[0:M, tok], dst.bufs), o[0:M, :])
            for t in range(4):
                r0 = g * 512 + t * 128
                tk = slice(t * 128, (t + 1) * 128)
                p = psA.next()
                for kk in range(8):
                    k.mm(p[:, :], hTg[:, kk, tk], wb[kk][:, EV["vb"]:EV["vb"] + 512], start=(kk == 0), stop=(kk == 7))
                o = ob.next()
                k.copy(o[:, :], p[:, :], eng=k.act)
                k.dma(V(sc["vb"].ap[r0:r0 + 128, :], sc["vb"].bufs), o[:, :])
                p = psC.next()
                for kk in range(8):
                    k.mm(p[:, 0:128], hTg[:, kk, tk], wb[kk][:, EV["va"]:EV["va"] + 128], start=(kk == 0), stop=(kk == 7))
                for kk in range(8):
                    k.mm(p[:, 128:136], hTg[:, kk, tk], wb[kk][:, EV["wi"]:EV["wi"] + 8], start=(kk == 0), stop=(kk == 7))
                o = ob.next()
                k.copy(o[:, 0:128], p[:, 0:128], eng=k.dve)
                k.dma(V(sc["va"].ap[r0:r0 + 128, :], sc["va"].bufs), o[:, 0:128])
                o2 = of.next()
                k.copy(o2[:, 0:8], p[:, 128:136], eng=k.dve)
                k.dma(V(sc["wi"].ap[r0:r0 + 128, :], sc["wi"].bufs), o2[:, 0:8])


def split3(k, ph, src, n, outs):
    k.copy(outs[0][0:n, :], src[0:n, :])
    k.tt(src[0:n, :], src[0:n, :], outs[0][0:n, :], ALU.subtract)
    k.copy(outs[1][0:n, :], src[0:n, :])
    k.tt(src[0:n, :], src[0:n, :], outs[1][0:n, :], ALU.subtract)
    k.copy(outs[2][0:n, :], src[0:n, :])


def attn_core(k, c, qg, nkt_fn, qk_fn, P_rot, ps_s, ps_o, ps_d, v_fn, scale, finalize):
    nkt = 4 * qg + 4
    po = ps_o
    pd = ps_d
    for kt in range(nkt):
        diag = kt >= 4 * qg
        col0 = (kt - 4 * qg) * 128 if diag else 0
        s = ps_s.next()
        qk_fn(s[:, col0:512], kt, col0)
        P = P_rot.next()
        k.actf(P[:, col0:512], s[:, col0:512], AF.Exp, scale=scale)
        if diag:
            k.tt(P[:, col0:col0 + 128], P[:, col0:col0 + 128], c.tri.v(), ALU.mult, eng=k.pool)
        k.mm(po[:, col0:512], v_fn(kt), P[:, col0:512], start=(kt == 0), stop=(kt == nkt - 1))
        k.mm(pd[:, col0:512], c.ones.v(), P[:, col0:512], start=(kt == 0), stop=(kt == nkt - 1))
    finalize(po, pd)


def phase_fox(k, c, sc, b_f):
    SQ = float(np.sqrt(128.0))
    with k.phase() as ph:
        with contextlib.ExitStack() as es2:
            ph2 = Phase(k, ph.name + "a")
            es2.enter_context(ph2.es)
            f0 = ph2.sb([4, S], F32)
            f1 = ph2.sb([4, S], F32)
            bcol = ph2.sb([4, 2], F32)
            one4 = ph2.sb([4, 1], F32)
            pcs = ph2.sbs([4, S], BF16, 3)
            ones4 = ph2.sb([4, S], BF16)
            k.dma(f0.v(), sc["fbT"])
            k.dma(bcol[:, 0:1], V(b_f.rearrange("(p o) -> p o", o=1), (Buf(b_f),)))
            k.ts(bcol[:, 1:2], bcol[:, 0:1], -1.0, ALU.mult)
            k.memset(one4.v(), 1.0)
            k.memset(ones4.v(), 1.0)
            k.actf(f0.v(), f0.v(), AF.Exp, scale=-1.0, bias=bcol[:, 1:2])
            k.actf(f0.v(), f0.v(), AF.Ln, bias=one4.v())
            k.ts(f0.v(), f0.v(), -SQ, ALU.mult)
            k.call(k.dve, "tensor_tensor_scan", out=f1.v(), data0=one4.v().bc([4, S]), data1=f0.v(),
                   initial=0.0, op0=ALU.mult, op1=ALU.add)
            k.copy(f0.v().re("p (b t) -> p b t", t=128), f1.v().re("p (b t) -> p b t", t=128)[:, :, 127:128].bc([4, 32, 128]))
            aug = sc["aug"]
            split3(k, ph2, f0, 4, pcs)
            for p_ in range(3):
                k.dma(V(aug.ap[:, 0, p_, :], aug.bufs), pcs[p_].v())
            k.ts(f1.v(), f1.v(), -1.0, ALU.mult)
            split3(k, ph2, f1, 4, pcs)
            for p_ in range(3):
                k.dma(V(aug.ap[:, 1, 3 + p_, :], aug.bufs), pcs[p_].v())
                k.dma(V(aug.ap[:, 1, p_, :], aug.bufs), ones4.v())
                k.dma(V(aug.ap[:, 0, 3 + p_, :], aug.bufs), ones4.v())
            k.barrier()
        qT = Rot(ph.sbs([128, S], BF16, 2))
        kT = Rot(ph.sbs([128, S], BF16, 2))
        vv = Rot(ph.sbs([128, 32, 128], BF16, 2))
        aq = Rot(ph.sbs([6, S], BF16, 2))
        ak = Rot(ph.sbs([6, S], BF16, 2))
        P_rot = Rot(ph.sbs([128, 512], BF16, 4))
        rden = Rot(ph.sbs([128, 512], F32, 2))
        ob = Rot(ph.sbs([128, 512], BF16, 2))
        ps = ph.psum(8)
        ps_s = Rot(ps[0:3])
        ps_o = Rot(ps[3:5])
        ps_d = Rot(ps[5:7])
        for h in range(4):
            q_, k_, v_, aq_, ak_ = qT.next(), kT.next(), vv.next(), aq.next(), ak.next()
            k.dma(q_.v(), sc["qT"][4 + h])
            k.dma(k_.v(), sc["kT"][1 + h])
            vsrc = sc["vb"]
            k.dma(v_.v(), V(vsrc.ap.rearrange("(t p) (h d) -> p t h d", p=128, h=4)[:, :, h, :], vsrc.bufs))
            k.dma(aq_.v(), V(sc["aug"].ap[h, 0], sc["aug"].bufs))
            k.dma(ak_.v(), V(sc["aug"].ap[h, 1], sc["aug"].bufs))
            for qg in range(8):
                def qk_fn(sv, kt, col0, q_=q_, k_=k_, aq_=aq_, ak_=ak_, qg=qg):
                    qs = slice(qg * 512 + col0, (qg + 1) * 512)
                    ks = slice(kt * 128, (kt + 1) * 128)
                    k.mm(sv, k_[:, ks], q_[:, qs], start=True, stop=False)
                    k.mm(sv, ak_[:, ks], aq_[:, qs], start=False, stop=True)

                def fin(po, pd, h=h, qg=qg):
                    r = rden.next()
                    k.call(k.dve, "reciprocal", out=r.v(), in_=pd.v())
                    o = ob.next()
                    k.tt(o.v(), po.v(), r.v(), ALU.mult)
                    dst = sc["oT"]
                    k.dma(V(dst.ap[512 + h * 128:512 + (h + 1) * 128, qg * 512:(qg + 1) * 512], dst.bufs), o.v())

                attn_core(k, c, qg, None, qk_fn, P_rot, ps_s, ps_o.next(), ps_d.next(),
                          lambda kt, v_=v_: v_[:, kt, :], 1.0 / SQ, fin)


def phase_outproj(k, c, sc, x_d, xo_d, w_d, nfeat):
    nk = nfeat // 128
    with k.phase() as ph:
        wb = load_w_bf16(k, ph, w_d, nk, D)
        oT = Rot(ph.sbs([128, nk, 512], BF16, 2))
        xres = Rot(ph.sbs([128, D], F32, 3))
        ps = ph.psum(8)
        psr = Rot(ps)
        xd = Buf(x_d)
        xod = Buf(xo_d)
        src = sc["oT"]
        for g in range(S // 512):
            o_ = oT.next()
            k.dma(o_.v(), V(src.ap[0:nfeat, g * 512:(g + 1) * 512].rearrange("(k p) s -> p k s", p=128), src.bufs))
            for t in range(4):
                r0 = g * 512 + t * 128
                xr = xres.next()
                k.dma(xr.v(), V(x_d[r0:r0 + 128, :], (xd,)))
                for cc in range(2):
                    p = psr.next()
                    for kk in range(nk):
                        k.mm(p.v(), o_[:, kk, t * 128:(t + 1) * 128], wb[kk][:, cc * 512:(cc + 1) * 512],
                             start=(kk == 0), stop=(kk == nk - 1))
                    k.tt(xr[:, cc * 512:(cc + 1) * 512], p.v(), xr[:, cc * 512:(cc + 1) * 512], ALU.add)
                k.dma(V(xo_d[r0:r0 + 128, :], (xod,)), xr.v())


def bc1(v, n):
    p, f = v.ap.shape
    return V(v.ap.unsqueeze(1).to_broadcast([p, n, f]), v.bufs)


NBIS = 16


def phase_dsa(k, c, sc):
    SCALE = float(128.0 ** -0.5)
    with k.phase() as ph:
        qi = ph.sb([128, 4, S], BF16)
        ki = ph.sb([128, S], BF16)
        qa = ph.sb([128, 4, S], BF16)
        ka = ph.sb([128, S], BF16)
        va = ph.sb([128, 32, 128], BF16)
        wi = ph.sb([128, 32, 8], F32)
        for h in range(4):
            k.dma(qi[:, h, :], sc["qiT"][h])
            k.dma(qa[:, h, :], sc["qT"][h])
        k.dma(ki.v(), sc["kiT"])
        k.dma(ka.v(), sc["kT"][0])
        k.dma(va.v(), V(sc["va"].ap.rearrange("(t p) d -> p t d", p=128), sc["va"].bufs))
        k.dma(wi.v(), V(sc["wi"].ap.rearrange("(t p) d -> p t d", p=128), sc["wi"].bufs))
        scb = Rot(ph.sbs([128, S], F32, 2))
        junk = ph.sb([128, S], BF16)
        msk = Rot(ph.sbs([128, S], BF16, 2))
        rl = Rot(ph.sbs([128, 512], BF16, 4))
        dg = Rot(ph.sbs([128, 8, 128], BF16, 2))
        E = Rot(ph.sbs([128, 512], BF16, 3))
        P = Rot(ph.sbs([128, 512], BF16, 3))
        mT = Rot(ph.sbs([128, 128], BF16, 4))
        stt_ = Rot(ph.sbs([128, 64], F32, 2))
        rden = Rot(ph.sbs([128, 512], F32, 2))
        ob = Rot(ph.sbs([128, 512], BF16, 2))
        ps = ph.psum(8)
        ps_i = Rot(ps[0:2])
        ps_acc = Rot(ps[2:3])
        ps_m = Rot(ps[3:4])
        ps_s = Rot(ps[4:6])
        po = ps[6]
        pd = ps[7]
        for qt in range(NT):
            W = (qt + 1) * 128
            qs = slice(qt * 128, (qt + 1) * 128)
            dgt = dg.next()
            for h in range(8):
                k.ts(dgt[:, h, :], c.identf.v(), wi[:, qt, h:h + 1], ALU.mult, eng=k.pool)
            scq = scb.next()
            for kg in range((W + 511) // 512):
                cols = min(512, W - kg * 512)
                acc = ps_acc.next()
                for h in range(8):
                    p = ps_i.next()
                    pr = slice(64 * (h % 2), 64 * (h % 2) + 64)
                    k.mm(p[:, 0:cols], qi[pr, h // 2, qs], ki[pr, kg * 512:kg * 512 + cols])
                    r = rl.next()
                    k.actf(r[:, 0:cols], p[:, 0:cols], AF.Relu)
                    k.mm(acc[:, 0:cols], dgt[:, h, :], r[:, 0:cols], start=(h == 0), stop=(h == 7))
                k.copy(scq[:, kg * 512:kg * 512 + cols], acc[:, 0:cols], eng=k.dve)
            k.tt(scq[:, qt * 128:W], scq[:, qt * 128:W], c.negtri.v(), ALU.add)
            st = stt_.next()
            if qt >= 2:
                k.call(k.dve, "tensor_reduce", out=st[:, 0:1], in_=scq[:, 0:W], axis=AX.X, op=ALU.max)
                k.call(k.dve, "tensor_reduce", out=st[:, 1:2], in_=scq[:, 0:qt * 128], axis=AX.X, op=ALU.min)
                k.ts(st[:, 1:2], st[:, 1:2], -1.0, ALU.add)
                k.tt(st[:, 2:3], st[:, 0:1], st[:, 1:2], ALU.subtract)
                k.ts(st[:, 8:8 + NBIS + 1], c.pow2[:, 0:NBIS + 1], st[:, 2:3], ALU.mult)
                k.ts(st[:, 32:32 + NBIS + 1], st[:, 8:8 + NBIS + 1], 2.0, ALU.mult)
                k.tt(st[:, 3:4], st[:, 1:2], st[:, 8:9], ALU.add)
                for it in range(NBIS):
                    k.call(k.dve, "tensor_scalar", out=junk[:, 0:W], in0=scq[:, 0:W], scalar1=st[:, 3:4], scalar2=None,
                           op0=ALU.is_gt, op1=ALU.add, accum_out=st[:, 4:5])
                    k.stt(st[:, 5:6], st[:, 4:5], 255.5, st[:, 32 + it + 1:32 + it + 2], ALU.is_gt, ALU.mult)
                    k.stt(st[:, 3:4], st[:, 5:6], st[:, 8 + it + 1:8 + it + 2], st[:, 3:4], ALU.subtract, ALU.add)
                k.tt(st[:, 6:7], st[:, 3:4], st[:, 8 + NBIS:8 + NBIS + 1], ALU.subtract)
                thr = st[:, 6:7]
            else:
                thr = c.negbig.v()
            m = msk.next()
            k.ts(m[:, 0:W], scq[:, 0:W], thr, ALU.is_gt)
            for kt in range(qt + 1):
                ks = slice(kt * 128, (kt + 1) * 128)
                pm = ps_m.next()
                pmb = pm.v().bitcast(BF16)
                k.tr(pmb[:, 0:128], m[:, ks], c.ident.v())
                mt = mT.next()
                k.copy(mt.v(), pmb[:, 0:128], eng=k.act)
                s = ps_s.next()
                k.mm(s.v().re("p (h q) -> p h q", h=4), ka[:, ks], qa[:, :, qs])
                e = E.next()
                k.actf(e.v(), s.v(), AF.Exp, scale=SCALE)
                p_ = P.next()
                k.tt(p_.v().re("p (h q) -> p h q", h=4), e.v().re("p (h q) -> p h q", h=4), bc1(mt.v(), 4), ALU.mult)
                k.mm(po.v(), va[:, kt, :], p_.v(), start=(kt == 0), stop=(kt == qt))
                k.mm(pd.v(), c.ones.v(), p_.v(), start=(kt == 0), stop=(kt == qt))
            r = rden.next()
            k.call(k.dve, "reciprocal", out=r.v(), in_=pd.v())
            o = ob.next()
            k.tt(o.v(), po.v(), r.v(), ALU.mult)
            dst = sc["oT"]
            k.dma(V(dst.ap[0:512, qs].rearrange("(h d) q -> d h q", d=128), dst.bufs),
                  o.v().re("p (h q) -> p h q", h=4))


OD = dict(cq=0, ckv=384, kr=640, z=704, xs=1728, B=2752, C=3264, dt=3776)
PI = float(np.pi)


def phase_rope_tables(k, c, sc, pos_d):
    with k.phase() as ph:
        pi_ = ph.sb([64, S], I32)
        ang = ph.sb([64, S], F32)
        u = ph.sb([64, S], F32)
        ni = ph.sb([64, S], I32)
        r = ph.sb([64, S], F32)
        k.dma(pi_.v(), V(pos_d.rearrange("(o s) -> o s", o=1).to_broadcast([64, S]), (Buf(pos_d),)))
        k.copy(ang.v(), pi_.v())
        k.ts(ang.v(), ang.v(), c.invf.v(), ALU.mult)
        for name, shift in (("sin", 0.0), ("cos", PI / 2)):
            k.ts(r.v(), ang.v(), shift, ALU.add)
            k.ts(u.v(), r.v(), 1.0 / (2 * PI), ALU.mult)
            k.copy(ni.v(), u.v())
            k.copy(u.v(), ni.v())
            k.stt(r.v(), u.v(), -2 * PI, r.v(), ALU.mult, ALU.add)
            k.ts(u.v(), r.v(), PI, ALU.is_gt, 2 * PI, ALU.mult)
            k.tt(r.v(), r.v(), u.v(), ALU.subtract)
            k.ts(u.v(), r.v(), -PI, ALU.is_lt, 2 * PI, ALU.mult)
            k.tt(r.v(), r.v(), u.v(), ALU.add)
            k.ts(r.v(), r.v(), 3.1415925, ALU.min, -3.1415925, ALU.max)
            k.actf(u.v(), r.v(), AF.Sin)
            k.dma(sc[name], u.v())


def col_load(k, dst, src_ap, n):
    k.dma(dst, V(src_ap.rearrange("(p o) -> p o", o=1), (Buf(src_ap),)))


ODDLVL = [9]


def phase_odd_proj(k, c, sc, x_d, g_row, w_d, cqn, ckvn, wuq_d, wukv_d, qn_c, kn_c, conv_w, conv_b):
    with k.phase() as ph:
        stg = Rot(ph.sbs([128, 1896], F32, 2))

        def loadw(w_ap, nk, ncols):
            wb_ = ph.split([128, nk, ncols], BF16, 1)
            wd = Buf(w_ap)
            for kk in range(nk):
                for c0 in range(0, ncols, 1896):
                    c1 = min(ncols, c0 + 1896)
                    s = stg.next()
                    k.dma(s[:, 0:c1 - c0], V(w_ap[kk * 128:(kk + 1) * 128, c0:c1], (wd,)))
                    k.copy(wb_[kk][:, c0:c1], s[:, 0:c1 - c0], eng=k.pool)
            return wb_
        wb = loadw(w_d, 8, 3792)
        wuq = loadw(wuq_d, 3, 768)
        wukv = loadw(wukv_d, 2, 1024)
        gt = ph.sb([128, D], F32)
        k.dma(gt.v(), V(g_row.to_broadcast([128, D]), (Buf(g_row),)))
        gc = ph.sb([128, 16], F32)
        for i in range(3):
            col_load(k, gc[:, i:i + 1], cqn[i * 128:(i + 1) * 128], 128)
        for i in range(2):
            col_load(k, gc[:, 3 + i:4 + i], ckvn[i * 128:(i + 1) * 128], 128)
        col_load(k, gc[:, 5:6], qn_c[0:128], 128)
        col_load(k, gc[0:64, 6:7], qn_c[128:192], 64)
        col_load(k, gc[:, 7:8], kn_c[0:128], 128)
        col_load(k, gc[0:64, 8:9], kn_c[128:192], 64)
        cw = ph.sb([128, 16, 4], F32)
        cb = ph.sb([128, 16], F32)
        cwd = Buf(conv_w)
        cbd = Buf(conv_b)
        for j in range(16):
            k.dma(cw[:, j, :], V(conv_w[:, j * 128:(j + 1) * 128].rearrange("w p -> p w"), (cwd,)),
                  allow_slow_non_contiguous=True)
            k.dma(cb[:, j:j + 1], V(conv_b[j * 128:(j + 1) * 128].rearrange("(p o) -> p o", o=1), (cbd,)))
        hal = ph.split([128, 16, 3], F32, 1)
        for j in range(16):
            k.memset(hal[j][:, :], 0.0, eng=k.pool)
        xin = Rot(ph.sbs([128, D], F32, 3))
        scr = ph.sb([128, D], BF16)
        stats = Rot(ph.sbs([128, 4], F32, 4))
        hn = Rot(ph.sbs([128, D], BF16, 2))
        hT = Rot(ph.sbs([128, 8, 512], BF16, 2))
        sq = Rot(ph.sbs([128, 512], BF16, 6))
        lnb = Rot(ph.sbs([128, 512], F32, 2))
        rstd = Rot(ph.sbs([128, 512], F32, 2))
        ob = Rot(ph.sbs([128, 512], BF16, 4))
        of = Rot(ph.sbs([128, 16], F32, 3))
        cqraw = ph.sb([128, 3, 512], F32)
        cqn_b = ph.sb([128, 3, 512], BF16)
        ckvraw = ph.sb([128, 2, 512], F32)
        ckvn_b = ph.sb([128, 2, 512], BF16)
        krraw = ph.sb([64, 512], F32)
        sqkr = ph.sb([64, 512], BF16)
        xr = Rot(ph.sbs([128, 515], F32, 2))
        acc = Rot(ph.sbs([128, 512], F32, 2))
        xact = Rot(ph.sbs([128, 512], BF16, 3))
        xtm = Rot(ph.sbs([128, 4, 128], BF16, 2))
        cs = ph.sb([64, 2, 512], F32)
        yb = Rot(ph.sbs([64, 512], BF16, 2))
        t1 = Rot(ph.sbs([64, 512], F32, 2))
        t2 = Rot(ph.sbs([64, 512], F32, 2))
        ps = ph.psum(8)
        psA = Rot(ps[0:4])
        psB = Rot(ps[4:6])
        ps_t = Rot(ps[6:8])
        xd = Buf(x_d)

        def grpnorm(raws, sqs, nch, hd, gcol0, outb):
            p2 = psB.next()
            for i in range(nch):
                k.mm(p2.v(), c.ones.v(), sqs[i].v(), start=(i == 0), stop=(i == nch - 1))
            l_, r_ = lnb.next(), rstd.next()
            k.actf(l_.v(), p2.v(), AF.Ln, scale=1.0 / hd, bias=c.epscol.v())
            k.actf(r_.v(), l_.v(), AF.Exp, scale=-0.5)
            for i in range(nch):
                k.stt(outb[:, i, :], raws[:, i, :], gc[:, gcol0 + i:gcol0 + i + 1], r_.v(), ALU.mult, ALU.mult)

        def rope(ybv, dst, tok):
            p = psB.next()
            k.mm(p[0:64, :], c.rotm.v(), ybv)
            a, b = t1.next(), t2.next()
            k.tt(a.v(), ybv, cs[:, 0, :], ALU.mult)
            k.tt(b.v(), p[0:64, :], cs[:, 1, :], ALU.mult)
            o = ob.next()
            k.tt(o[0:64, :], a.v(), b.v(), ALU.add)
            k.dma(V(dst.ap[:, tok], dst.bufs), o[0:64, :])

        def headnorm(pn, sq_r, gcol_n):
            sqn = sq.next()
            k.actf(sqn.v(), pn.v(), AF.Square)
            p2 = psB.next()
            k.mm(p2.v(), c.ones.v(), sqn.v(), start=True, stop=False)
            k.mm(p2.v(), c.ones[0:64, :], sq_r, start=False, stop=True)
            l_, r_ = lnb.next(), rstd.next()
            k.actf(l_.v(), p2.v(), AF.Ln, scale=1.0 / 192, bias=c.epscol.v())
            k.actf(r_.v(), l_.v(), AF.Exp, scale=-0.5)
            return r_

        for g in range(S // 512):
            if ODDLVL[0] < 1:
                break
            hTg = hT.next()
            xnorm_group(k, c, x_d, xd, g, gt, xin, hn, scr, stats, hTg, ps_t)
            tok = slice(g * 512, (g + 1) * 512)
            if ODDLVL[0] == 11:
                continue
            k.dma(cs[:, 0, :], V(sc["cos"].ap[:, tok], sc["cos"].bufs))
            k.dma(cs[:, 1, :], V(sc["sin"].ap[:, tok], sc["sin"].bufs))
            if ODDLVL[0] == 12:
                continue

            def proj(c0, M):
                p = psA.next()
                for kk in range(8):
                    k.mm(p[0:M, :], wb[kk][:, c0:c0 + M], hTg[:, kk, :], start=(kk == 0), stop=(kk == 7))
                return p
            sqs = []
            for i in range(3):
                p = proj(OD["cq"] + i * 128, 128)
                s_ = sq.next()
                k.actf(s_.v(), p.v(), AF.Square)
                k.copy(cqraw[:, i, :], p.v(), eng=k.dve)
                sqs.append(s_)
            if ODDLVL[0] == 13:
                continue
            grpnorm(cqraw, sqs, 3, 384, 0, cqn_b)
            if ODDLVL[0] == 14:
                continue
            sqs = []
            for i in range(2):
                p = proj(OD["ckv"] + i * 128, 128)
                s_ = sq.next()
                k.actf(s_.v(), p.v(), AF.Square)
                k.copy(ckvraw[:, i, :], p.v(), eng=k.dve)
                sqs.append(s_)
            grpnorm(ckvraw, sqs, 2, 256, 3, ckvn_b)
            p = proj(OD["kr"], 64)
            k.copy(krraw.v(), p[0:64, :], eng=k.dve)
            if ODDLVL[0] < 2:
                continue
            for h in range(4):
                pn = psA.next()
                for kc in range(3):
                    k.mm(pn.v(), wuq[kc][:, h * 192:h * 192 + 128], cqn_b[:, kc, :], start=(kc == 0), stop=(kc == 2))
                pr = psA.next()
                for kc in range(3):
                    k.mm(pr[0:64, :], wuq[kc][:, h * 192 + 128:h * 192 + 192], cqn_b[:, kc, :], start=(kc == 0), stop=(kc == 2))
                sqr = sq.next()
                k.actf(sqr[0:64, :], pr[0:64, :], AF.Square)
                r_ = headnorm(pn, sqr[0:64, :], 5)
                o = ob.next()
                k.stt(o.v(), pn.v(), gc[:, 5:6], r_.v(), ALU.mult, ALU.mult)
                k.dma(V(sc["qT"][h].ap[:, tok], sc["qT"][h].bufs), o.v())
                y_ = yb.next()
                k.stt(y_.v(), pr[0:64, :], gc[0:64, 6:7], r_[0:64, :], ALU.mult, ALU.mult)
                rope(y_.v(), sc["qr"][h], tok)
            k.actf(sqkr.v(), krraw.v(), AF.Square)
            for h in range(4):
                pn = psA.next()
                for kc in range(2):
                    k.mm(pn.v(), wukv[kc][:, h * 256:h * 256 + 128], ckvn_b[:, kc, :], start=(kc == 0), stop=(kc == 1))
                r_ = headnorm(pn, sqkr.v(), 7)
                o = ob.next()
                k.stt(o.v(), pn.v(), gc[:, 7:8], r_.v(), ALU.mult, ALU.mult)
                k.dma(V(sc["kT"][h].ap[:, tok], sc["kT"][h].bufs), o.v())
                y_ = yb.next()
                k.stt(y_.v(), krraw.v(), gc[0:64, 8:9], r_[0:64, :], ALU.mult, ALU.mult)
                rope(y_.v(), sc["kr"][h], tok)
            if ODDLVL[0] < 3:
                continue
            for j in range(16):
                p = proj(OD["xs"] + j * 128, 128)
                x_ = xr.next()
                k.copy(x_[:, 0:3], hal[j][:, :], eng=k.pool)
                k.copy(x_[:, 3:515], p.v(), eng=k.act)
                k.copy(hal[j][:, :], x_[:, 512:515], eng=k.pool)
                a_ = acc.next()
                k.ts(a_.v(), x_[:, 0:512], cw[:, j, 0:1], ALU.mult)
                for w in range(1, 4):
                    k.stt(a_.v(), x_[:, w:w + 512], cw[:, j, w:w + 1], a_.v(), ALU.mult, ALU.add)
                xa_ = xact.next()
                k.actf(xa_.v(), a_.v(), AF.Silu, bias=cb[:, j:j + 1])
                if j >= 8:
                    dst = sc["BCT"]
                    k.dma(V(dst.ap[(j - 8) * 128:(j - 7) * 128, tok], dst.bufs), xa_.v())
                if j < 12:
                    pt = ps_t.next()
                    ptb = pt.v().bitcast(BF16)
                    for t in range(4):
                        k.tr(ptb[:, t * 128:(t + 1) * 128], xa_[:, t * 128:(t + 1) * 128], c.ident.v())
                    xt_ = xtm.next()
                    k.copy(xt_.v(), ptb[:, 0:512].re("p (t c) -> p t c", t=4), eng=k.dve)
                    dst = sc["xsB"]
                    k.dma(V(dst.ap[tok, j * 128:(j + 1) * 128].rearrange("(t p) c -> p t c", p=128), dst.bufs), xt_.v())
            if ODDLVL[0] < 4:
                continue
            for t in range(4):
                r0 = g * 512 + t * 128
                tk = slice(t * 128, (t + 1) * 128)
                for hf in range(2):
                    p = psA.next()
                    for kk in range(8):
                        k.mm(p.v(), hTg[:, kk, tk], wb[kk][:, OD["z"] + hf * 512:OD["z"] + (hf + 1) * 512],
                             start=(kk == 0), stop=(kk == 7))
                    o = ob.next()
                    k.actf(o.v(), p.v(), AF.Silu)
                    k.dma(V(sc["zs"].ap[r0:r0 + 128, hf * 512:(hf + 1) * 512], sc["zs"].bufs), o.v())
                p = psA.next()
                for kk in range(8):
                    k.mm(p[:, 0:16], hTg[:, kk, tk], wb[kk][:, OD["dt"]:OD["dt"] + 16], start=(kk == 0), stop=(kk == 7))
                o2 = of.next()
                k.copy(o2[:, 0:16], p[:, 0:16], eng=k.dve)
                k.dma(V(sc["dt"].ap[r0:r0 + 128, :], sc["dt"].bufs), o2[:, 0:16])
                p = psA.next()
                for kc in range(2):
                    k.mm(p.v().re("p (h d) -> p h d", h=4), ckvn_b[:, kc, tk],
                         wukv[kc][:, :].re("p (h d) -> p h d", h=4)[:, :, 128:256], start=(kc == 0), stop=(kc == 1))
                o = ob.next()
                k.copy(o.v(), p.v(), eng=k.act)
                k.dma(V(sc["vb"].ap[r0:r0 + 128, :], sc["vb"].bufs), o.v())


def phase_mla(k, c, sc):
    SCALE = float(192.0 ** -0.5)
    with k.phase() as ph:
        qT = Rot(ph.sbs([128, S], BF16, 2))
        kT = Rot(ph.sbs([128, S], BF16, 2))
        qr = Rot(ph.sbs([64, S], BF16, 2))
        kr = Rot(ph.sbs([64, S], BF16, 2))
        vv = Rot(ph.sbs([128, 32, 128], BF16, 2))
        P_rot = Rot(ph.sbs([128, 512], BF16, 4))
        rden = Rot(ph.sbs([128, 512], F32, 2))
        ob = Rot(ph.sbs([128, 512], BF16, 2))
        ps = ph.psum(8)
        ps_s = Rot(ps[0:3])
        ps_o = Rot(ps[3:5])
        ps_d = Rot(ps[5:7])
        for h in range(4):
            q_, k_, qr_, kr_, v_ = qT.next(), kT.next(), qr.next(), kr.next(), vv.next()
            k.dma(q_.v(), sc["qT"][h])
            k.dma(k_.v(), sc["kT"][h])
            k.dma(qr_.v(), sc["qr"][h])
            k.dma(kr_.v(), sc["kr"][h])
            vsrc = sc["vb"]
            k.dma(v_.v(), V(vsrc.ap.rearrange("(t p) (h d) -> p t h d", p=128, h=4)[:, :, h, :], vsrc.bufs))
            for qg in range(8):
                def qk_fn(sv, kt, col0, q_=q_, k_=k_, qr_=qr_, kr_=kr_, qg=qg):
                    qs = slice(qg * 512 + col0, (qg + 1) * 512)
                    ks = slice(kt * 128, (kt + 1) * 128)
                    k.mm(sv, k_[:, ks], q_[:, qs], start=True, stop=False)
                    k.mm(sv, kr_[:, ks], qr_[:, qs], start=False, stop=True)

                def fin(po, pd, h=h, qg=qg):
                    r = rden.next()
                    k.call(k.dve, "reciprocal", out=r.v(), in_=pd.v())
                    o = ob.next()
                    k.tt(o.v(), po.v(), r.v(), ALU.mult)
                    dst = sc["oT"]
                    k.dma(V(dst.ap[h * 128:(h + 1) * 128, qg * 512:(qg + 1) * 512], dst.bufs), o.v())

                attn_core(k, c, qg, None, qk_fn, P_rot, ps_s, ps_o.next(), ps_d.next(),
                          lambda kt, v_=v_: v_[:, kt, :], SCALE, fin)


def bcl(v, n):
    p, h = v.ap.shape
    return V(v.ap.unsqueeze(2).to_broadcast([p, h, n]), v.bufs)


def phase_ssd(k, c, sc, dt_bias, a_log, d_skip, gate_norm):
    with k.phase() as ph:
        rep = ph.sb([128, 64], F32)
        for i, src in enumerate((dt_bias, a_log, d_skip)):
            k.dma(rep[:, i * 16:(i + 1) * 16], V(src.rearrange("(o h) -> o h", o=1).to_broadcast([128, 16]), (Buf(src),)))
        k.actf(rep[:, 16:32], rep[:, 16:32], AF.Exp)
        k.ts(rep[:, 16:32], rep[:, 16:32], -1.0, ALU.mult)
        one = ph.sb([128, 1], F32)
        k.memset(one.v(), 1.0)
        gg = ph.sb([128, D], F32)
        k.dma(gg.v(), V(gate_norm.rearrange("(o h) -> o h", o=1).to_broadcast([128, D]), (Buf(gate_norm),)))
        negtril = ph.sb([128, 128], BF16)
        tmpf = ph.sb([128, 128], F32)
        k.dma(tmpf.v(), c.negtril_d)
        k.copy(negtril.v(), tmpf.v())
        hst = ph.sb([128, D], F32)
        hstb = ph.sb([128, D], BF16)
        k.memset(hst.v(), 0.0)
        k.memset(hstb.v(), 0.0)
        xs = Rot(ph.sbs([128, D], BF16, 2))
        Btm = Rot(ph.sbs([128, 4, 128], BF16, 2))
        BT = Rot(ph.sbs([128, 4, 128], BF16, 2))
        CT = Rot(ph.sbs([128, 4, 128], BF16, 2))
        zs = Rot(ph.sbs([128, D], BF16, 2))
        dtr = Rot(ph.sbs([128, 16], F32, 2))
        sm = Rot(ph.sbs([128, 128], F32, 2))
        LT = Rot(ph.sbs([128, 16, 128], BF16, 2))
        MT = Rot(ph.sbs([128, 16, 128], BF16, 2))
        cbt = Rot(ph.sbs([128, 4, 128], BF16, 2))
        xdr = Rot(ph.sbs([128, D], BF16, 2))
        xddr = Rot(ph.sbs([128, D], BF16, 2))
        yr = Rot(ph.sbs([128, D], F32, 2))
        t2r = Rot(ph.sbs([128, D], F32, 2))
        junk = ph.sb([128, 256], BF16)
        ynr = Rot(ph.sbs([128, D], BF16, 2))
        oTt = Rot(ph.sbs([128, 8, 128], BF16, 2))
        ps = Rot(ph.psum(8))
        for ci in range(NT):
            r0 = ci * 128
            tok = slice(r0, r0 + 128)
            xs_, Btm_, BT_, CT_, zs_, dtr_ = xs.next(), Btm.next(), BT.next(), CT.next(), zs.next(), dtr.next()
            xsB = sc["xsB"]
            k.dma(xs_.v(), V(xsB.ap[tok, 0:1024], xsB.bufs))
            k.dma(Btm_.v().re("p g n -> p (g n)"), V(xsB.ap[tok, 1024:1536], xsB.bufs))
            bct = sc["BCT"]
            k.dma(BT_.v(), V(bct.ap[0:512, tok].rearrange("(g n) t -> n g t", n=128), bct.bufs))
            k.dma(CT_.v(), V(bct.ap[512:1024, tok].rearrange("(g n) t -> n g t", n=128), bct.bufs))
            k.dma(zs_.v(), V(sc["zs"].ap[tok, :], sc["zs"].bufs))
            k.dma(dtr_.v(), V(sc["dt"].ap[tok, :], sc["dt"].bufs))
            s_ = sm.next()
            k.tt(s_[:, 0:16], dtr_.v(), rep[:, 0:16], ALU.add)
            k.actf(s_[:, 0:16], s_[:, 0:16], AF.Exp)
            k.actf(s_[:, 0:16], s_[:, 0:16], AF.Ln, bias=one.v())
            k.tt(s_[:, 16:32], s_[:, 0:16], rep[:, 16:32], ALU.mult)
            pc = ps.next()
            k.mm(pc[:, 0:16], c.trif.v(), s_[:, 16:32])
            k.mm(pc[:, 16:32], c.onesf.v(), s_[:, 16:32])
            k.copy(s_[:, 32:48], pc[:, 0:16])
            k.ts(s_[:, 48:64], pc[:, 0:16], -1.0, ALU.mult)
            k.actf(s_[:, 64:80], pc[:, 0:16], AF.Exp)
            k.tt(s_[:, 112:128], pc[:, 16:32], s_[:, 32:48], ALU.subtract)
            k.actf(s_[:, 80:96], s_[:, 112:128], AF.Exp)
            k.actf(s_[:, 96:112], pc[:, 16:32], AF.Exp)
            LT_ = LT.next()
            for q4 in range(4):
                pl = ps.next()
                for i in range(4):
                    h = 4 * q4 + i
                    k.mm(pl[:, i * 128:(i + 1) * 128], s_[:, 16 + h:17 + h].bc([128, 128]), c.trif.v(), start=True, stop=False)
                    k.mm(pl[:, i * 128:(i + 1) * 128], c.ident.v(), negtril.v(), start=False, stop=True)
                    k.actf(LT_[:, h, :], pl[:, i * 128:(i + 1) * 128], AF.Exp, bias=s_[:, 48 + h:49 + h])
            pcb = ps.next()
            for g in range(4):
                k.mm(pcb[:, g * 128:(g + 1) * 128], BT_[:, g, :], CT_[:, g, :])
            cbt_ = cbt.next()
            k.copy(cbt_.v().re("p g n -> p (g n)"), pcb.v(), eng=k.act)
            MT_ = MT.next()
            for g in range(4):
                k.tt(MT_[:, 4 * g:4 * g + 4, :], LT_[:, 4 * g:4 * g + 4, :], bc1(cbt_[:, g, :], 4), ALU.mult)
            xd_, xdd_ = xdr.next(), xddr.next()
            k.tt(xd_.v().re("l (h p) -> l h p", p=64), xs_.v().re("l (h p) -> l h p", p=64), bcl(s_[:, 0:16], 64), ALU.mult)
            k.tt(xdd_.v().re("l (h p) -> l h p", p=64), xd_.v().re("l (h p) -> l h p", p=64), bcl(s_[:, 80:96], 64), ALU.mult)
            y_ = yr.next()
            t2_ = t2r.next()
            k.tt(t2_.v().re("l (h p) -> l h p", p=64), xs_.v().re("l (h p) -> l h p", p=64), bcl(rep[:, 32:48], 64), ALU.mult, eng=k.pool)
            for hf in range(2):
                hs = slice(hf * 512, (hf + 1) * 512)
                py = ps.next()
                for hh in range(8):
                    h = hf * 8 + hh
                    k.mm(py[:, hh * 64:(hh + 1) * 64], MT_[:, h, :], xd_[:, h * 64:(h + 1) * 64])
                po = ps.next()
                for gg_ in range(2):
                    g = hf * 2 + gg_
                    k.mm(po[:, gg_ * 256:(gg_ + 1) * 256], CT_[:, g, :], hstb[:, g * 256:(g + 1) * 256])
                k.tt(y_[:, hs].re("l (h p) -> l h p", p=64), po.v().re("l (h p) -> l h p", p=64),
                     bcl(s_[:, 64 + hf * 8:72 + hf * 8], 64), ALU.mult)
                k.tt(y_[:, hs], y_[:, hs], py.v(), ALU.add)
                k.tt(y_[:, hs], y_[:, hs], t2_[:, hs], ALU.add)
            for hf in range(2):
                hs = slice(hf * 512, (hf + 1) * 512)
                pst = ps.next()
                for gg_ in range(2):
                    g = hf * 2 + gg_
                    k.mm(pst[:, gg_ * 256:(gg_ + 1) * 256], Btm_[:, g, :], xdd_[:, g * 256:(g + 1) * 256])
                k.tt(hst[:, hs].re("l (h p) -> l h p", p=64), hst[:, hs].re("l (h p) -> l h p", p=64),
                     bcl(s_[:, 96 + hf * 8:104 + hf * 8], 64), ALU.mult, eng=k.pool)
                k.tt(hst[:, hs], hst[:, hs], pst.v(), ALU.add)
                k.copy(hstb[:, hs], hst[:, hs], eng=k.act)
            k.tt(y_.v(), y_.v(), zs_.v(), ALU.mult)
            for g in range(4):
                k.actf(junk.v(), y_[:, g * 256:(g + 1) * 256], AF.Square, accum_out=s_[:, 112 + g:113 + g])
            k.ts(s_[:, 116:120], s_[:, 112:116], 1.0 / 256, ALU.mult, EPS, ALU.add)
            k.tt(s_[:, 120:124], s_[:, 116:120], c.mhalf.v().bc([128, 4]), ALU.pow, eng=k.pool)
            yn_ = ynr.next()
            for g in range(4):
                gs = slice(g * 256, (g + 1) * 256)
                k.stt(yn_[:, gs], y_[:, gs], s_[:, 120 + g:121 + g], gg[:, gs], ALU.mult, ALU.mult)
            pt = ps.next()
            ptb = pt.v().bitcast(BF16)
            for j in range(8):
                k.tr(ptb[:, j * 128:(j + 1) * 128], yn_[:, j * 128:(j + 1) * 128], c.ident.v())
            o_ = oTt.next()
            k.copy(o_.v().re("p j t -> p (j t)"), ptb, eng=k.act)
            dst = sc["oT"]
            k.dma(V(dst.ap[512:1536, tok].rearrange("(j p) t -> p j t", p=128), dst.bufs), o_.v())


def const_arrays():
    cd = {}
    cd["ident"] = np.eye(128, dtype=np.float32)
    cd["pow2"] = np.tile((2.0 ** -(np.arange(32) + 1.0)).astype(np.float32)[None, :], (128, 1))
    invf = (10000.0 ** (-np.arange(32, dtype=np.float32) / 32)).astype(np.float32)
    cd["invf"] = np.concatenate([invf, invf])[:, None].astype(np.float32)
    rot = np.zeros((64, 64), np.float32)
    for m in range(32):
        rot[m + 32, m] = -1.0
        rot[m, m + 32] = 1.0
    cd["rotm"] = rot
    cd["negtril"] = (np.tril(np.ones((128, 128), np.float32), -1) * -30000.0).astype(np.float32)
    cd["tri"] = np.triu(np.ones((128, 128), np.float32))
    cd["negtri"] = (np.triu(np.ones((128, 128), np.float32), 1) * -1e30).astype(np.float32)
    return cd


INPUT_NAMES = ["x", "positions", "ev_norm", "ev_w_in", "ev_b_f", "ev_qn_a", "ev_kn_a", "ev_qn_b", "ev_kn_b",
               "ev_w_out", "od_norm", "od_w_in", "od_cq_norm", "od_ckv_norm", "od_w_uq", "od_w_ukv", "od_qn_c",
               "od_kn_c", "od_conv_w", "od_conv_b", "od_dt_bias", "od_a_log", "od_d_skip", "od_gate_norm",
               "od_w_out", "mlp_norm", "mlp_w1", "mlp_w2"]


def build(shapes, cfg):
    nc = bass.Bass("TRN2", target_bir_lowering=False)
    din = {}
    for name, (shp, dt) in shapes.items():
        bdt = I32 if np.dtype(dt) == np.int32 else F32
        din[name] = nc.dram_tensor(name, list(shp), bdt, kind="ExternalInput").ap()
    cds = const_arrays()
    cd = {n: nc.dram_tensor("c_" + n, list(a.shape), F32, kind="ExternalInput").ap() for n, a in cds.items()}
    y = nc.dram_tensor("y", [S, D], F32, kind="ExternalOutput").ap()
    xa = nc.dram_tensor("xa", [S, D], F32, kind="Internal").ap()
    xb = nc.dram_tensor("xb", [S, D], F32, kind="Internal").ap()

    def mk(name, shape, dt):
        return nc.dram_tensor(name, list(shape), dt, kind="Internal").ap()

    def dv(ap):
        return V(ap, (Buf(ap),))
    sc = {}
    t = mk("s_qT", [8, 128, S], BF16)
    sc["qT"] = [dv(t[h]) for h in range(8)]
    t = mk("s_kT", [5, 128, S], BF16)
    sc["kT"] = [dv(t[h]) for h in range(5)]
    t = mk("s_qiT", [4, 128, S], BF16)
    sc["qiT"] = [dv(t[h]) for h in range(4)]
    sc["kiT"] = dv(mk("s_kiT", [128, S], BF16))
    sc["va"] = dv(mk("s_va", [S, 128], BF16))
    sc["vb"] = dv(mk("s_vb", [S, 512], BF16))
    sc["wi"] = dv(mk("s_wi", [S, 8], F32))
    sc["fbT"] = dv(mk("s_fbT", [4, S], F32))
    sc["aug"] = dv(mk("s_aug", [4, 2, 6, S], BF16))
    sc["oT"] = dv(mk("s_oT", [1536, S], BF16))
    t = mk("s_qr", [4, 64, S], BF16)
    sc["qr"] = [dv(t[h]) for h in range(4)]
    t = mk("s_kr", [4, 64, S], BF16)
    sc["kr"] = [dv(t[h]) for h in range(4)]
    sc["cos"] = dv(mk("s_cos", [64, S], F32))
    sc["sin"] = dv(mk("s_sin", [64, S], F32))
    sc["BCT"] = dv(mk("s_BCT", [1024, S], BF16))
    sc["xsB"] = dv(mk("s_xsB", [S, 1536], BF16))
    sc["zs"] = dv(mk("s_zs", [S, 1024], BF16))
    sc["dt"] = dv(mk("s_dt", [S, 16], F32))
    dbg = cfg.get("debug", {})
    k = K(nc)
    with k.es:
        c = setup_consts(k, cd)
        cur = din["x"]
        ropedone = [False]
        steps = cfg["steps"]
        for si, (kind, l) in enumerate(steps):
            last = si == len(steps) - 1
            dst = y if last else (xa if cur is not xa else xb)
            if kind == "mlp":
                phase_mlp(k, c, cur, dst, din["mlp_norm"][l:l + 1, :], din["mlp_w1"][l], din["mlp_w2"][l])
            elif kind == "even":
                phase_even_proj(k, c, sc, cur, din["ev_norm"][l:l + 1, :], din["ev_w_in"][l], din["ev_qn_a"][l],
                                din["ev_kn_a"][l], din["ev_qn_b"][l], din["ev_kn_b"][l])
                if "nodsa" not in dbg:
                    phase_dsa(k, c, sc)
                if "nofox" not in dbg:
                    phase_fox(k, c, sc, din["ev_b_f"][l])
                phase_outproj(k, c, sc, cur, dst, din["ev_w_out"][l], 1024)
            elif kind == "odd":
                if "oddlvl" in dbg:
                    ODDLVL[0] = dbg["oddlvl"]
                if not ropedone[0] and "norope" not in dbg:
                    phase_rope_tables(k, c, sc, din["positions"])
                    ropedone[0] = True
                phase_odd_proj(k, c, sc, cur, din["od_norm"][l:l + 1, :], din["od_w_in"][l], din["od_cq_norm"][l],
                               din["od_ckv_norm"][l], din["od_w_uq"][l], din["od_w_ukv"][l], din["od_qn_c"][l],
                               din["od_kn_c"][l], din["od_conv_w"][l], din["od_conv_b"][l])
                if "nomla" not in dbg:
                    phase_mla(k, c, sc)
                if "nossd" not in dbg:
                    phase_ssd(k, c, sc, din["od_dt_bias"][l], din["od_a_log"][l], din["od_d_skip"][l],
                              din["od_gate_norm"][l])
                phase_outproj(k, c, sc, cur, dst, din["od_w_out"][l], 1536)
            else:
                raise ValueError(kind)
            cur = dst
        k.barrier()
    return nc, cds


FULL_CFG = {"steps": [("even", 0), ("mlp", 0), ("odd", 0), ("mlp", 1), ("even", 1), ("mlp", 2), ("odd", 1), ("mlp", 3)]}


def run(inputs, cfg, cores=N_CORES):
    per_core = []
    for b in range(cores):
        m = {}
        for n in INPUT_NAMES:
            a = np.asarray(inputs[n])
            if n in ("x", "positions"):
                a = a[b]
            m[n] = np.ascontiguousarray(a)
        per_core.append(m)
    shapes = {n: (per_core[0][n].shape, per_core[0][n].dtype) for n in INPUT_NAMES}
    nc, cds = build(shapes, cfg)
    for m in per_core:
        for n, a in cds.items():
            m["c_" + n] = a
    res = run_bass_kernel_spmd(nc, per_core, core_ids=list(range(cores)))
    return np.stack([np.asarray(r["y"]) for r in res.results], axis=0)


def kernel(**inputs):
    out = run(inputs, FULL_CFG)
    return out.astype(np.float32)
```

```python
import contextlib
import numpy as np
import ml_dtypes
import concourse.bass as bass
import concourse.mybir as mybir
from concourse.bass_utils import run_bass_kernel_spmd

F32 = mybir.dt.float32
BF16 = mybir.dt.bfloat16
I32 = mybir.dt.int32
AF = mybir.ActivationFunctionType
ALU = mybir.AluOpType
AX = mybir.AxisListType

S = 4096
D = 1024
NT = S // 128
DFF = 4096
EPS = 1e-6
N_CORES = 8
WRITE_KEYS = ("out", "accum_out", "ap")


class V:
    def __init__(self, ap, bufs):
        self.ap = ap
        self.bufs = bufs

    def __getitem__(self, idx):
        return V(self.ap[idx], self.bufs)

    def bc(self, shape):
        return V(self.ap.to_broadcast(shape), self.bufs)

    def re(self, pat, **kw):
        return V(self.ap.rearrange(pat, **kw), self.bufs)

    def bitcast(self, dt):
        return V(self.ap.bitcast(dt), self.bufs)


class Buf:
    def __init__(self, ap):
        self.ap = ap
        self.w = None
        self.r = {}
        self.excl = False

    def __getitem__(self, idx):
        return V(self.ap[idx], (self,))

    def v(self):
        return V(self.ap, (self,))


def multi(*views):
    bufs = []
    for v in views:
        bufs.extend(v.bufs)
    return V(views[0].ap, tuple(bufs))


class Eng:
    def __init__(self, k, name, raw, self_sync):
        self.name = name
        self.raw = raw
        self.sem = k.new_sem("e_" + name)
        self.cnt = 0
        self.seen = {}
        self.self_sync = self_sync


class Slot:
    def __init__(self, k, key):
        self.key = key
        self.sem = k.new_sem(key)
        self.val = 0


class K:
    NSLOT = 12

    def __init__(self, nc):
        self.nc = nc
        self.es = contextlib.ExitStack()
        self.pe = Eng(self, "pe", nc.tensor, False)
        self.act = Eng(self, "act", nc.scalar, True)
        self.dve = Eng(self, "dve", nc.vector, True)
        self.pool = Eng(self, "pool", nc.gpsimd, True)
        self.sp = Eng(self, "sp", nc.sync, False)
        self.engs = [self.pe, self.act, self.dve, self.pool, self.sp]
        self.queues = {}
        for q in (self.sp, self.pool):
            self.queues[q.name] = [Slot(self, "d_%s_%d" % (q.name, i)) for i in range(self.NSLOT)]
        self.qnext = {q: 0 for q in self.queues}
        self.nph = 0

    def new_sem(self, name):
        return self.es.enter_context(self.nc.semaphore(name))

    def _wait(self, eng, tok):
        key, sem, val = tok
        if key == eng.name and not eng.self_sync:
            return
        if eng.seen.get(key, 0) >= val:
            return
        eng.raw.wait_ge(sem, val)
        eng.seen[key] = val

    def _deps(self, eng, reads, writes):
        for v in reads:
            for b in v.bufs:
                if b.w is not None:
                    self._wait(eng, b.w)
                if b.excl:
                    for t in b.r.values():
                        if t[0] != eng.name:
                            self._wait(eng, t)
        for v in writes:
            for b in v.bufs:
                if b.w is not None:
                    self._wait(eng, b.w)
                for t in b.r.values():
                    self._wait(eng, t)

    def _mark(self, tok, reads, writes):
        for v in reads:
            for b in v.bufs:
                b.r[tok[0]] = tok
        for v in writes:
            for b in v.bufs:
                b.w = tok
                b.r = {}

    def call(self, eng, method, **kw):
        reads, writes, args = [], [], {}
        for key, v in kw.items():
            if isinstance(v, V):
                (writes if key in WRITE_KEYS else reads).append(v)
                args[key] = v.ap
            else:
                args[key] = v
        self._deps(eng, reads, writes)
        inst = getattr(eng.raw, method)(**args)
        eng.cnt += 1
        inst.then_inc(eng.sem, 1)
        self._mark((eng.name, eng.sem, eng.cnt), reads, writes)
        return inst

    def dma(self, out, in_, q=None, **kw):
        q = q or self.sp
        slots = self.queues[q.name]
        slot = slots[self.qnext[q.name] % len(slots)]
        self.qnext[q.name] += 1
        if slot.val > 0:
            self._wait(q, (slot.key, slot.sem, slot.val))
        self._deps(q, [in_], [out])
        slot.val += 16
        q.raw.dma_start(out=out.ap, in_=in_.ap, **kw).then_inc(slot.sem, 16)
        self._mark((slot.key, slot.sem, slot.val), [in_], [out])

    def barrier(self):
        toks = [(e.name, e.sem, e.cnt) for e in self.engs if e.cnt > 0]
        for sl in self.queues.values():
            toks += [(s.key, s.sem, s.val) for s in sl if s.val > 0]
        for e in self.engs:
            for t in toks:
                self._wait(e, t)

    def mm(self, out, lhsT, rhs, start=True, stop=True):
        return self.call(self.pe, "matmul", out=out, lhsT=lhsT, rhs=rhs, start=start, stop=stop)

    def tr(self, out, in_, ident):
        return self.call(self.pe, "transpose", out=out, in_=in_, identity=ident)

    def actf(self, out, in_, func, **kw):
        return self.call(self.act, "activation", out=out, in_=in_, func=func, **kw)

    def tt(self, out, in0, in1, op, eng=None):
        return self.call(eng or self.dve, "tensor_tensor", out=out, in0=in0, in1=in1, op=op)

    def ts(self, out, in0, s1, op0, s2=None, op1=None, eng=None, **kw):
        if op1 is None:
            return self.call(eng or self.dve, "tensor_scalar", out=out, in0=in0, scalar1=s1, scalar2=None,
                             op0=op0, **kw)
        return self.call(eng or self.dve, "tensor_scalar", out=out, in0=in0, scalar1=s1, scalar2=s2,
                         op0=op0, op1=op1, **kw)

    def stt(self, out, in0, scalar, in1, op0, op1, **kw):
        return self.call(self.dve, "scalar_tensor_tensor", out=out, in0=in0, scalar=scalar, in1=in1,
                         op0=op0, op1=op1, **kw)

    def copy(self, out, in_, eng=None):
        eng = eng or self.dve
        if eng is self.act:
            return self.call(eng, "copy", out=out, in_=in_)
        return self.call(eng, "tensor_copy", out=out, in_=in_)

    def memset(self, ap, val, eng=None):
        return self.call(eng or self.dve, "memset", ap=ap, constant=val)

    @contextlib.contextmanager
    def phase(self):
        self.barrier()
        self.nph += 1
        ph = Phase(self, "p%d" % self.nph)
        with ph.es:
            yield ph
            self.barrier()


class Phase:
    def __init__(self, k, name):
        self.k = k
        self.name = name
        self.es = contextlib.ExitStack()
        self.n = 0

    def sbt(self, shape, dtype):
        self.n += 1
        return self.es.enter_context(self.k.nc.sbuf_tensor("%s_s%d" % (self.name, self.n), list(shape), dtype))

    def sb(self, shape, dtype):
        t = self.sbt(shape, dtype)
        return Buf(t[tuple(slice(None) for _ in shape)])

    def sbs(self, shape, dtype, n):
        return [self.sb(shape, dtype) for _ in range(n)]

    def split(self, shape, dtype, axis, step=1):
        t = self.sbt(shape, dtype)
        out = []
        for i in range(0, shape[axis], step):
            idx = [slice(None)] * len(shape)
            idx[axis] = slice(i, i + step) if step > 1 else i
            out.append(Buf(t[tuple(idx)]))
        return out

    def psum(self, n=8):
        out = []
        for i in range(n):
            self.n += 1
            t = self.es.enter_context(self.k.nc.psum_tensor("%s_ps%d" % (self.name, self.n), [128, 512], F32))
            b = Buf(t[:, :])
            b.excl = True
            out.append(b)
        return out


class Rot:
    def __init__(self, items):
        self.items = items
        self.i = 0

    def next(self):
        it = self.items[self.i % len(self.items)]
        self.i += 1
        return it


def dram_buf(ap):
    return Buf(ap)


class Ctx:
    pass


def setup_consts(k, cd):
    nc = k.nc
    c = Ctx()
    es = k.es
    def sb(name, shape, dt):
        t = es.enter_context(nc.sbuf_tensor(name, list(shape), dt))
        return Buf(t[tuple(slice(None) for _ in shape)])
    c.ident = sb("k_ident", [128, 128], BF16)
    c.mhalf = sb("k_mhalf", [128, 1], F32)
    tmp = sb("k_tmp", [128, 128], F32)
    k.dma(tmp.v(), V(cd["ident"], (Buf(cd["ident"]),)))
    k.copy(c.ident.v(), tmp.v(), eng=k.dve)
    k.memset(c.mhalf.v(), -0.5, eng=k.dve)
    c.epscol = sb("k_epscol", [128, 1], F32)
    k.memset(c.epscol.v(), EPS, eng=k.dve)
    c.identf = sb("k_identf", [128, 128], F32)
    k.copy(c.identf.v(), tmp.v(), eng=k.dve)
    c.ones = sb("k_ones", [128, 128], BF16)
    k.memset(c.ones.v(), 1.0, eng=k.dve)
    c.onesf = sb("k_onesf", [128, 128], F32)
    k.memset(c.onesf.v(), 1.0, eng=k.dve)
    c.tri = sb("k_tri", [128, 128], BF16)
    k.dma(tmp.v(), V(cd["tri"], (Buf(cd["tri"]),)))
    k.copy(c.tri.v(), tmp.v(), eng=k.dve)
    c.trif = sb("k_trif", [128, 128], F32)
    k.copy(c.trif.v(), tmp.v(), eng=k.dve)
    c.invf = sb("k_invf", [64, 1], F32)
    k.dma(c.invf.v(), V(cd["invf"], (Buf(cd["invf"]),)))
    c.rotm = sb("k_rotm", [64, 64], BF16)
    k.dma(tmp[0:64, 0:64], V(cd["rotm"], (Buf(cd["rotm"]),)))
    k.copy(c.rotm.v(), tmp[0:64, 0:64], eng=k.dve)
    c.negtril_d = V(cd["negtril"], (Buf(cd["negtril"]),))
    c.negbig = sb("k_negbig", [128, 1], F32)
    k.memset(c.negbig.v(), -1e29, eng=k.dve)
    c.pow2 = sb("k_pow2", [128, 32], F32)
    k.dma(c.pow2.v(), V(cd["pow2"], (Buf(cd["pow2"]),)))
    c.negtri = sb("k_negtri", [128, 128], F32)
    k.dma(c.negtri.v(), V(cd["negtri"], (Buf(cd["negtri"]),)))
    return c


def rmsnorm_tile(k, c, ph, xt, gt, hn, scr, st):
    k.actf(scr.v(), xt.v(), AF.Square, accum_out=st[:, 0:1])
    k.ts(st[:, 1:2], st[:, 0:1], 1.0 / D, ALU.mult, EPS, ALU.add)
    k.tt(st[:, 2:3], st[:, 1:2], c.mhalf.v(), ALU.pow, eng=k.pool)
    k.stt(hn.v(), xt.v(), st[:, 2:3], gt.v(), ALU.mult, ALU.mult)


def phase_mlp(k, c, x_d, xo_d, g_row, w1_d, w2_d):
    G = 256
    NG = S // G
    with k.phase() as ph:
        w1b = ph.split([128, 8, DFF], BF16, 1)
        w2b = ph.split([128, 32, D], BF16, 1)
        stg = Rot(ph.sbs([128, 2048], F32, 2))
        gt = ph.sb([128, D], F32)
        xin = Rot(ph.sbs([128, D], F32, 3))
        scr = ph.sb([128, D], BF16)
        stats = Rot(ph.sbs([128, 4], F32, 4))
        hn = Rot(ph.sbs([128, D], BF16, 2))
        hT = Rot(ph.sbs([128, 8, G], BF16, 2))
        rl = Rot(ph.sbs([128, G], BF16, 3))
        hid = Rot(ph.sbs([128, G], BF16, 3))
        xres = Rot(ph.sbs([128, D], F32, 3))
        ps = ph.psum(8)
        ps_y = ps[0:4]
        ps_h = Rot(ps[4:6])
        ps_t = Rot(ps[6:8])
        xd = Buf(x_d)
        xod = Buf(xo_d)
        w1d = Buf(w1_d)
        w2d = Buf(w2_d)
        k.dma(gt.v(), V(g_row.to_broadcast([128, D]), (Buf(g_row),)))
        for kk in range(8):
            for hf in range(2):
                s = stg.next()
                k.dma(s.v(), V(w1_d[kk * 128:(kk + 1) * 128, hf * 2048:(hf + 1) * 2048], (w1d,)))
                k.copy(w1b[kk][:, hf * 2048:(hf + 1) * 2048], s.v(), eng=k.pool)
        w2v = w2_d.rearrange("(j p) n -> p j n", p=128)
        for jj in range(0, 32, 2):
            s = stg.next()
            k.dma(s.v().re("p (j n) -> p j n", j=2), V(w2v[:, jj:jj + 2, :], (w2d,)))
            eng = k.pool
            k.copy(w2b[jj][:, :], s[:, 0:1024], eng=eng)
            k.copy(w2b[jj + 1][:, :], s[:, 1024:2048], eng=eng)

        def norm_a(g):
            res = []
            for t in range(G // 128):
                xt = xin.next()
                r0 = g * G + t * 128
                k.dma(xt.v(), V(x_d[r0:r0 + 128, :], (xd,)))
                h = hn.next()
                rmsnorm_tile(k, c, ph, xt, gt, h, scr, stats.next())
                res.append(h)
            return res

        def norm_b(g, hs):
            hTg = hT.next()
            for t, h in enumerate(hs):
                pt = ps_t.next()
                ptb = pt.v().bitcast(BF16)
                for kk in range(8):
                    k.tr(ptb[:, kk * 128:(kk + 1) * 128], h[:, kk * 128:(kk + 1) * 128], c.ident.v())
                k.copy(hTg[:, :, t * 128:(t + 1) * 128], ptb.re("p (k t) -> p k t", k=8), eng=k.act)
            return hTg

        hs = norm_a(0)
        hT_cur = norm_b(0, hs)
        for g in range(NG):
            hs_next = None
            hT_next = None
            pend = None
            for j in range(32):
                ph_ = ps_h.next()
                for kk in range(8):
                    k.mm(ph_[:, 0:G], w1b[kk][:, j * 128:(j + 1) * 128], hT_cur[:, kk, :],
                         start=(kk == 0), stop=(kk == 7))
                r = rl.next()
                k.actf(r.v(), ph_[:, 0:G], AF.Relu)
                hd = hid.next()
                k.tt(hd.v(), r.v(), r.v(), ALU.mult)
                if pend is not None:
                    pj, phd = pend
                    for t in range(2):
                        for cc in range(2):
                            k.mm(ps_y[t * 2 + cc].v(), phd[:, t * 128:(t + 1) * 128],
                                 w2b[pj][:, cc * 512:(cc + 1) * 512], start=(pj == 0), stop=False)
                pend = (j, hd)
                if j == 4 and g + 1 < NG:
                    hs_next = norm_a(g + 1)
                if j == 20 and g + 1 < NG:
                    hT_next = norm_b(g + 1, hs_next)
            pj, phd = pend
            for t in range(2):
                for cc in range(2):
                    k.mm(ps_y[t * 2 + cc].v(), phd[:, t * 128:(t + 1) * 128],
                         w2b[pj][:, cc * 512:(cc + 1) * 512], start=False, stop=True)
            for t in range(2):
                r0 = g * G + t * 128
                xr = xres.next()
                k.dma(xr.v(), V(x_d[r0:r0 + 128, :], (xd,)))
                for cc in range(2):
                    k.tt(xr[:, cc * 512:(cc + 1) * 512], ps_y[t * 2 + cc].v(), xr[:, cc * 512:(cc + 1) * 512], ALU.add)
                k.dma(V(xo_d[r0:r0 + 128, :], (xod,)), xr.v())
            hT_cur = hT_next


def xnorm_group(k, c, x_d, xd, g, gt, xin, hn, scr, stats, hTg, ps_t, ntile=4):
    G = ntile * 128
    for t in range(ntile):
        xt = xin.next()
        r0 = g * G + t * 128
        k.dma(xt.v(), V(x_d[r0:r0 + 128, :], (xd,)))
        h = hn.next()
        rmsnorm_tile(k, c, None, xt, gt, h, scr, stats.next())
        pt = ps_t.next()
        ptb = pt.v().bitcast(BF16)
        for kk in range(8):
            k.tr(ptb[:, kk * 128:(kk + 1) * 128], h[:, kk * 128:(kk + 1) * 128], c.ident.v())
        k.copy(hTg[:, :, t * 128:(t + 1) * 128], ptb.re("p (k t) -> p k t", k=8), eng=k.act)


def load_w_bf16(k, ph, w_d, nk, ncols, stg_cols=None):
    wb = ph.split([128, nk, ncols], BF16, 1)
    stg = Rot(ph.sbs([128, ncols], F32, 2))
    wd = Buf(w_d)
    rows = w_d.shape[0]
    for kk in range(nk):
        s = stg.next()
        r = min(128, rows - kk * 128)
        k.dma(s[0:r, :], V(w_d[kk * 128:kk * 128 + r, :], (wd,)))
        k.copy(wb[kk][0:r, :], s[0:r, :], eng=k.pool)
    return wb


def fm_qknorm(k, c, ps, M, gcol, outb, sq, lnb, rstd, ps2, hd):
    N = ps.ap.shape[-1]
    k.actf(sq[0:M, 0:N], ps, AF.Square)
    k.mm(ps2[0:M, 0:N], c.ones[0:M, 0:M], sq[0:M, 0:N])
    k.actf(lnb[0:M, 0:N], ps2[0:M, 0:N], AF.Ln, scale=1.0 / hd, bias=c.epscol[0:M, :])
    k.actf(rstd[0:M, 0:N], lnb[0:M, 0:N], AF.Exp, scale=-0.5)
    k.stt(outb, ps, gcol, rstd[0:M, 0:N], ALU.mult, ALU.mult)


EV = dict(qa=0, ka=512, va=640, qi=768, ki=1280, wi=1344, qb=1352, kb=1864, vb=2376, fb=2888)


def phase_even_proj(k, c, sc, x_d, g_row, w_d, qn_a, kn_a, qn_b, kn_b):
    with k.phase() as ph:
        wb = load_w_bf16(k, ph, w_d, 8, 2892)
        wkd = ph.sb([128, 8, 128], BF16)
        for kk in range(8):
            k.copy(wkd[:, kk, 0:64], wb[kk][:, 1280:1344], eng=k.pool)
            k.copy(wkd[:, kk, 64:128], wb[kk][:, 1280:1344], eng=k.pool)
        gt = ph.sb([128, D], F32)
        k.dma(gt.v(), V(g_row.to_broadcast([128, D]), (Buf(g_row),)))
        gcol = ph.sb([128, 4], F32)
        for i, gn in enumerate((qn_a, kn_a, qn_b, kn_b)):
            k.dma(gcol[:, i:i + 1], V(gn.rearrange("(p o) -> p o", o=1), (Buf(gn),)))
        xin = Rot(ph.sbs([128, D], F32, 3))
        scr = ph.sb([128, D], BF16)
        stats = Rot(ph.sbs([128, 4], F32, 4))
        hn = Rot(ph.sbs([128, D], BF16, 2))
        hT = Rot(ph.sbs([128, 8, 512], BF16, 2))
        sq = Rot(ph.sbs([128, 512], BF16, 2))
        lnb = Rot(ph.sbs([128, 512], F32, 2))
        rstd = Rot(ph.sbs([128, 512], F32, 2))
        ob = Rot(ph.sbs([128, 512], BF16, 4))
        of = Rot(ph.sbs([128, 512], F32, 2))
        ps = ph.psum(8)
        psA = Rot(ps[0:3])
        psB = Rot(ps[3:5])
        ps_t = Rot(ps[5:7])
        psC = Rot(ps[7:8])
        xd = Buf(x_d)
        chunks = []
        for h in range(4):
            chunks.append((wb, EV["qa"] + h * 128, 128, 0, sc["qT"][h]))
        chunks.append((wb, EV["ka"], 128, 1, sc["kT"][0]))
        for cc in range(4):
            chunks.append((wb, EV["qi"] + cc * 128, 128, None, sc["qiT"][cc]))
        chunks.append((None, 0, 128, None, sc["kiT"]))
        for h in range(4):
            chunks.append((wb, EV["qb"] + h * 128, 128, 2, sc["qT"][4 + h]))
        for h in range(4):
            chunks.append((wb, EV["kb"] + h * 128, 128, 3, sc["kT"][1 + h]))
        chunks.append((wb, EV["fb"], 4, "f32", sc["fbT"]))
        ci = 0
        for g in range(S // 512):
            hTg = hT.next()
            xnorm_group(k, c, x_d, xd, g, gt, xin, hn, scr, stats, hTg, ps_t)
            tok = slice(g * 512, (g + 1) * 512)
            for (wsrc, c0, M, nrm, dst) in chunks:
                p = psA.next()
                for kk in range(8):
                    lhsT = wkd[:, kk, :] if wsrc is None else wb[kk][:, c0:c0 + M]
                    k.mm(p[0:M, :], lhsT, hTg[:, kk, :], start=(kk == 0), stop=(kk == 7))
                if nrm == "f32":
                    o = of.next()
                    k.copy(o[0:M, :], p[0:M, :], eng=k.dve)
                    k.dma(V(dst.ap[0:M, tok], dst.bufs), o[0:M, :])
                    continue
                o = ob.next()
                if nrm is None:
                    ci += 1
                    k.copy(o[0:M, :], p[0:M, :], eng=(k.act if ci % 2 else k.dve))
                else:
                    fm_qknorm(k, c, p[0:M, :], M, gcol[:, nrm:nrm + 1], o[0:M, :], sq.next(), lnb.next(),
                              rstd.next(), psB.next(), 128)
                k.dma(V(dst.ap[0:M, tok], dst.bufs), o[0:M, :])
            for t in range(4):
                r0 = g * 512 + t * 128
                tk = slice(t * 128, (t + 1) * 128)
                p = psA.next()
                for kk in range(8):
                    k.mm(p[:, :], hTg[:, kk, tk], wb[kk][:, EV["vb"]:EV["vb"] + 512], start=(kk == 0), stop=(kk == 7))
                o = ob.next()
                k.copy(o[:, :], p[:, :], eng=k.act)
                k.dma(V(sc["vb"].ap[r0:r0 + 128, :], sc["vb"].bufs), o[:, :])
                p = psC.next()
                for kk in range(8):
                    k.mm(p[:, 0:128], hTg[:, kk, tk], wb[kk][:, EV["va"]:EV["va"] + 128], start=(kk == 0), stop=(kk == 7))
                for kk in range(8):
                    k.mm(p[:, 128:136], hTg[:, kk, tk], wb[kk][:, EV["wi"]:EV["wi"] + 8], start=(kk == 0), stop=(kk == 7))
                o = ob.next()
                k.copy(o[:, 0:128], p[:, 0:128], eng=k.dve)
                k.dma(V(sc["va"].ap[r0:r0 + 128, :], sc["va"].bufs), o[:, 0:128])
                o2 = of.next()
                k.copy(o2[:, 0:8], p[:, 128:136], eng=k.dve)
                k.dma(V(sc["wi"].ap[r0:r0 + 128, :], sc["wi"].bufs), o2[:, 0:8])


def split3(k, ph, src, n, outs):
    k.copy(outs[0][0:n, :], src[0:n, :])
    k.tt(src[0:n, :], src[0:n, :], outs[0][0:n, :], ALU.subtract)
    k.copy(outs[1][0:n, :], src[0:n, :])
    k.tt(src[0:n, :], src[0:n, :], outs[1][0:n, :], ALU.subtract)
    k.copy(outs[2][0:n, :], src[0:n, :])


def attn_core(k, c, qg, nkt_fn, qk_fn, P_rot, ps_s, ps_o, ps_d, v_fn, scale, finalize, la=2):
    nkt = 4 * qg + 4
    po = ps_o
    pd = ps_d
    issued = []

    def issue(kt):
        diag = kt >= 4 * qg
        col0 = (kt - 4 * qg) * 128 if diag else 0
        s = ps_s.next()
        qk_fn(s[:, col0:512], kt, col0)
        issued.append((s, col0, diag))
    for kt in range(min(la, nkt)):
        issue(kt)
    for kt in range(nkt):
        if kt + la < nkt:
            issue(kt + la)
        s, col0, diag = issued[kt]
        P = P_rot.next()
        k.actf(P[:, col0:512], s[:, col0:512], AF.Exp, scale=scale)
        if diag:
            k.tt(P[:, col0:col0 + 128], P[:, col0:col0 + 128], c.tri.v(), ALU.mult, eng=k.pool)
        k.mm(po[:, col0:512], v_fn(kt), P[:, col0:512], start=(kt == 0), stop=(kt == nkt - 1))
        k.mm(pd[:, col0:512], c.ones.v(), P[:, col0:512], start=(kt == 0), stop=(kt == nkt - 1))
    finalize(po, pd)


def phase_fox(k, c, sc, b_f):
    SQ = float(np.sqrt(128.0))
    with k.phase() as ph:
        with contextlib.ExitStack() as es2:
            ph2 = Phase(k, ph.name + "a")
            es2.enter_context(ph2.es)
            f0 = ph2.sb([4, S], F32)
            f1 = ph2.sb([4, S], F32)
            bcol = ph2.sb([4, 2], F32)
            one4 = ph2.sb([4, 1], F32)
            pcs = ph2.sbs([4, S], BF16, 3)
            ones4 = ph2.sb([4, S], BF16)
            k.dma(f0.v(), sc["fbT"])
            k.dma(bcol[:, 0:1], V(b_f.rearrange("(p o) -> p o", o=1), (Buf(b_f),)))
            k.ts(bcol[:, 1:2], bcol[:, 0:1], -1.0, ALU.mult)
            k.memset(one4.v(), 1.0)
            k.memset(ones4.v(), 1.0)
            k.actf(f0.v(), f0.v(), AF.Exp, scale=-1.0, bias=bcol[:, 1:2])
            k.actf(f0.v(), f0.v(), AF.Ln, bias=one4.v())
            k.ts(f0.v(), f0.v(), -SQ, ALU.mult)
            k.call(k.dve, "tensor_tensor_scan", out=f1.v(), data0=one4.v().bc([4, S]), data1=f0.v(),
                   initial=0.0, op0=ALU.mult, op1=ALU.add)
            k.copy(f0.v().re("p (b t) -> p b t", t=128), f1.v().re("p (b t) -> p b t", t=128)[:, :, 127:128].bc([4, 32, 128]))
            aug = sc["aug"]
            split3(k, ph2, f0, 4, pcs)
            for p_ in range(3):
                k.dma(V(aug.ap[:, 0, p_, :], aug.bufs), pcs[p_].v())
            k.ts(f1.v(), f1.v(), -1.0, ALU.mult)
            split3(k, ph2, f1, 4, pcs)
            for p_ in range(3):
                k.dma(V(aug.ap[:, 1, 3 + p_, :], aug.bufs), pcs[p_].v())
                k.dma(V(aug.ap[:, 1, p_, :], aug.bufs), ones4.v())
                k.dma(V(aug.ap[:, 0, 3 + p_, :], aug.bufs), ones4.v())
            k.barrier()
        qT = Rot(ph.sbs([128, S], BF16, 2))
        kT = Rot(ph.sbs([128, S], BF16, 2))
        vv = Rot(ph.sbs([128, 32, 128], BF16, 2))
        aq = Rot(ph.sbs([6, S], BF16, 2))
        ak = Rot(ph.sbs([6, S], BF16, 2))
        P_rot = Rot(ph.sbs([128, 512], BF16, 4))
        rden = Rot(ph.sbs([128, 512], F32, 2))
        ob = Rot(ph.sbs([128, 512], BF16, 2))
        ps = ph.psum(8)
        ps_s = Rot(ps[0:3])
        ps_o = Rot(ps[3:5])
        ps_d = Rot(ps[5:7])
        for h in range(4):
            q_, k_, v_, aq_, ak_ = qT.next(), kT.next(), vv.next(), aq.next(), ak.next()
            k.dma(q_.v(), sc["qT"][4 + h])
            k.dma(k_.v(), sc["kT"][1 + h])
            vsrc = sc["vb"]
            k.dma(v_.v(), V(vsrc.ap.rearrange("(t p) (h d) -> p t h d", p=128, h=4)[:, :, h, :], vsrc.bufs))
            k.dma(aq_.v(), V(sc["aug"].ap[h, 0], sc["aug"].bufs))
            k.dma(ak_.v(), V(sc["aug"].ap[h, 1], sc["aug"].bufs))
            for qg in range(8):
                def qk_fn(sv, kt, col0, q_=q_, k_=k_, aq_=aq_, ak_=ak_, qg=qg):
                    qs = slice(qg * 512 + col0, (qg + 1) * 512)
                    ks = slice(kt * 128, (kt + 1) * 128)
                    k.mm(sv, k_[:, ks], q_[:, qs], start=True, stop=False)
                    k.mm(sv, ak_[:, ks], aq_[:, qs], start=False, stop=True)

                def fin(po, pd, h=h, qg=qg):
                    r = rden.next()
                    k.call(k.dve, "reciprocal", out=r.v(), in_=pd.v())
                    o = ob.next()
                    k.tt(o.v(), po.v(), r.v(), ALU.mult)
                    dst = sc["oT"]
                    k.dma(V(dst.ap[512 + h * 128:512 + (h + 1) * 128, qg * 512:(qg + 1) * 512], dst.bufs), o.v())

                attn_core(k, c, qg, None, qk_fn, P_rot, ps_s, ps_o.next(), ps_d.next(),
                          lambda kt, v_=v_: v_[:, kt, :], 1.0 / SQ, fin)


def phase_outproj(k, c, sc, x_d, xo_d, w_d, nfeat):
    nk = nfeat // 128
    with k.phase() as ph:
        wb = load_w_bf16(k, ph, w_d, nk, D)
        oT = Rot(ph.sbs([128, nk, 512], BF16, 2))
        xres = Rot(ph.sbs([128, D], F32, 3))
        ps = ph.psum(8)
        psr = Rot(ps)
        xd = Buf(x_d)
        xod = Buf(xo_d)
        src = sc["oT"]
        for g in range(S // 512):
            o_ = oT.next()
            k.dma(o_.v(), V(src.ap[0:nfeat, g * 512:(g + 1) * 512].rearrange("(k p) s -> p k s", p=128), src.bufs))
            for t in range(4):
                r0 = g * 512 + t * 128
                xr = xres.next()
                k.dma(xr.v(), V(x_d[r0:r0 + 128, :], (xd,)))
                for cc in range(2):
                    p = psr.next()
                    for kk in range(nk):
                        k.mm(p.v(), o_[:, kk, t * 128:(t + 1) * 128], wb[kk][:, cc * 512:(cc + 1) * 512],
                             start=(kk == 0), stop=(kk == nk - 1))
                    k.tt(xr[:, cc * 512:(cc + 1) * 512], p.v(), xr[:, cc * 512:(cc + 1) * 512], ALU.add)
                k.dma(V(xo_d[r0:r0 + 128, :], (xod,)), xr.v())


def bc1(v, n):
    p, f = v.ap.shape
    return V(v.ap.unsqueeze(1).to_broadcast([p, n, f]), v.bufs)


NBIS = 14


def phase_dsa(k, c, sc):
    SCALE = float(128.0 ** -0.5)
    with k.phase() as ph:
        qi = ph.sb([128, 4, S], BF16)
        ki = ph.sb([128, S], BF16)
        qa = ph.sb([128, 4, S], BF16)
        ka = ph.sb([128, S], BF16)
        va = ph.sb([128, 32, 128], BF16)
        wi = ph.sb([128, 32, 8], F32)
        for h in range(4):
            k.dma(qi[:, h, :], sc["qiT"][h])
            k.dma(qa[:, h, :], sc["qT"][h])
        k.dma(ki.v(), sc["kiT"])
        k.dma(ka.v(), sc["kT"][0])
        k.dma(va.v(), V(sc["va"].ap.rearrange("(t p) d -> p t d", p=128), sc["va"].bufs))
        k.dma(wi.v(), V(sc["wi"].ap.rearrange("(t p) d -> p t d", p=128), sc["wi"].bufs))
        scb = ph.sbs([128, S], F32, 2)
        junk = ph.sb([128, S], BF16)
        msk = ph.sbs([128, S], BF16, 2)
        rl = Rot(ph.sbs([128, 512], BF16, 4))
        dg = ph.sbs([128, 8, 128], BF16, 2)
        E = Rot(ph.sbs([128, 512], BF16, 3))
        P = Rot(ph.sbs([128, 512], BF16, 3))
        mT = Rot(ph.sbs([128, 512], BF16, 3))
        stt_ = ph.sbs([128, 64], F32, 2)
        rden = Rot(ph.sbs([128, 512], F32, 2))
        ob = Rot(ph.sbs([128, 512], BF16, 2))
        ps = ph.psum(8)
        ps_i = Rot(ps[0:2])
        ps_acc = Rot(ps[2:3])
        ps_m = Rot(ps[3:4])
        ps_s = Rot(ps[4:6])
        po = ps[6]
        pd = ps[7]
        thr_of = {}

        def gen_index(qt):
            W = (qt + 1) * 128
            qs = slice(qt * 128, (qt + 1) * 128)
            dgt = dg[qt % 2]
            for h in range(8):
                k.ts(dgt[:, h, :], c.identf.v(), wi[:, qt, h:h + 1], ALU.mult, eng=k.pool)
            scq = scb[qt % 2]
            for kg in range((W + 511) // 512):
                cols = min(512, W - kg * 512)
                acc = ps_acc.next()
                for h in range(8):
                    p = ps_i.next()
                    pr = slice(64 * (h % 2), 64 * (h % 2) + 64)
                    k.mm(p[:, 0:cols], qi[pr, h // 2, qs], ki[pr, kg * 512:kg * 512 + cols])
                    r = rl.next()
                    k.actf(r[:, 0:cols], p[:, 0:cols], AF.Relu)
                    k.mm(acc[:, 0:cols], dgt[:, h, :], r[:, 0:cols], start=(h == 0), stop=(h == 7))
                k.copy(scq[:, kg * 512:kg * 512 + cols], acc[:, 0:cols], eng=k.act)
                yield
            k.tt(scq[:, qt * 128:W], scq[:, qt * 128:W], c.negtri.v(), ALU.add, eng=k.pool)
            yield

        def gen_bisect(qt):
            W = (qt + 1) * 128
            scq = scb[qt % 2]
            st = stt_[qt % 2]
            if qt >= 2:
                k.call(k.dve, "tensor_reduce", out=st[:, 0:1], in_=scq[:, 0:W], axis=AX.X, op=ALU.max)
                yield
                k.call(k.dve, "tensor_reduce", out=st[:, 1:2], in_=scq[:, 0:qt * 128], axis=AX.X, op=ALU.min)
                k.ts(st[:, 1:2], st[:, 1:2], -1.0, ALU.add)
                k.tt(st[:, 2:3], st[:, 0:1], st[:, 1:2], ALU.subtract)
                k.ts(st[:, 8:8 + NBIS + 1], c.pow2[:, 0:NBIS + 1], st[:, 2:3], ALU.mult)
                k.ts(st[:, 32:32 + NBIS + 1], st[:, 8:8 + NBIS + 1], 2.0, ALU.mult)
                k.tt(st[:, 3:4], st[:, 1:2], st[:, 8:9], ALU.add)
                yield
                for it in range(NBIS):
                    k.call(k.dve, "tensor_scalar", out=junk[:, 0:W], in0=scq[:, 0:W], scalar1=st[:, 3:4], scalar2=None,
                           op0=ALU.is_gt, op1=ALU.add, accum_out=st[:, 4:5])
                    yield
                    k.stt(st[:, 5:6], st[:, 4:5], 255.5, st[:, 32 + it + 1:32 + it + 2], ALU.is_gt, ALU.mult)
                    k.stt(st[:, 3:4], st[:, 5:6], st[:, 8 + it + 1:8 + it + 2], st[:, 3:4], ALU.subtract, ALU.add)
                k.tt(st[:, 6:7], st[:, 3:4], st[:, 8 + NBIS:8 + NBIS + 1], ALU.subtract)
                thr = st[:, 6:7]
            else:
                thr = c.negbig.v()
            m = msk[qt % 2]
            k.ts(m[:, 0:W], scq[:, 0:W], thr, ALU.is_gt)
            yield

        def gen_attn(qt):
            qs = slice(qt * 128, (qt + 1) * 128)
            m = msk[qt % 2]
            nkt = qt + 1
            issued = {}
            mts = {}

            def issue(kt):
                if kt % 4 == 0:
                    n = min(4, nkt - kt)
                    pm = ps_m.next()
                    pmb = pm.v().bitcast(BF16)
                    for i in range(n):
                        k.tr(pmb[:, i * 128:(i + 1) * 128], m[:, (kt + i) * 128:(kt + i + 1) * 128], c.ident.v())
                    mt = mT.next()
                    k.copy(mt[:, 0:n * 128], pmb[:, 0:n * 128], eng=k.act)
                    mts[kt // 4] = mt
                s = ps_s.next()
                k.mm(s.v().re("p (h q) -> p h q", h=4), ka[:, kt * 128:(kt + 1) * 128], qa[:, :, qs])
                issued[kt] = s
            issue(0)
            for kt in range(nkt):
                if kt + 1 < nkt:
                    issue(kt + 1)
                s = issued.pop(kt)
                e = E.next()
                k.actf(e.v(), s.v(), AF.Exp, scale=SCALE)
                p_ = P.next()
                mt = mts[kt // 4]
                i = kt % 4
                k.tt(p_.v().re("p (h q) -> p h q", h=4), e.v().re("p (h q) -> p h q", h=4),
                     bc1(mt[:, i * 128:(i + 1) * 128], 4), ALU.mult, eng=k.pool)
                k.mm(po.v(), va[:, kt, :], p_.v(), start=(kt == 0), stop=(kt == qt))
                k.mm(pd.v(), c.ones.v(), p_.v(), start=(kt == 0), stop=(kt == qt))
                yield
            r = rden.next()
            k.call(k.dve, "reciprocal", out=r.v(), in_=pd.v())
            o = ob.next()
            k.tt(o.v(), po.v(), r.v(), ALU.mult)
            dst = sc["oT"]
            k.dma(V(dst.ap[0:512, qs].rearrange("(h d) q -> d h q", d=128), dst.bufs),
                  o.v().re("p (h q) -> p h q", h=4))
            yield

        for step in range(-2, NT):
            gens = []
            if 0 <= step + 2 < NT:
                gens.append(gen_index(step + 2))
            if 0 <= step + 1 < NT:
                gens.append(gen_bisect(step + 1))
            if 0 <= step < NT:
                gens.append(gen_attn(step))
            while gens:
                for g_ in list(gens):
                    try:
                        next(g_)
                    except StopIteration:
                        gens.remove(g_)


OD = dict(cq=0, ckv=384, kr=640, z=704, xs=1728, B=2752, C=3264, dt=3776)
PI = float(np.pi)


def phase_rope_tables(k, c, sc, pos_d):
    with k.phase() as ph:
        pi_ = ph.sb([64, S], I32)
        ang = ph.sb([64, S], F32)
        u = ph.sb([64, S], F32)
        ni = ph.sb([64, S], I32)
        r = ph.sb([64, S], F32)
        k.dma(pi_.v(), V(pos_d.rearrange("(o s) -> o s", o=1).to_broadcast([64, S]), (Buf(pos_d),)))
        k.copy(ang.v(), pi_.v())
        k.ts(ang.v(), ang.v(), c.invf.v(), ALU.mult)
        for name, shift in (("sin", 0.0), ("cos", PI / 2)):
            k.ts(r.v(), ang.v(), shift, ALU.add)
            k.ts(u.v(), r.v(), 1.0 / (2 * PI), ALU.mult)
            k.copy(ni.v(), u.v())
            k.copy(u.v(), ni.v())
            k.stt(r.v(), u.v(), -2 * PI, r.v(), ALU.mult, ALU.add)
            k.ts(u.v(), r.v(), PI, ALU.is_gt, 2 * PI, ALU.mult)
            k.tt(r.v(), r.v(), u.v(), ALU.subtract)
            k.ts(u.v(), r.v(), -PI, ALU.is_lt, 2 * PI, ALU.mult)
            k.tt(r.v(), r.v(), u.v(), ALU.add)
            k.ts(r.v(), r.v(), 3.1415925, ALU.min, -3.1415925, ALU.max)
            k.actf(u.v(), r.v(), AF.Sin)
            k.dma(sc[name], u.v())


def col_load(k, dst, src_ap, n):
    k.dma(dst, V(src_ap.rearrange("(p o) -> p o", o=1), (Buf(src_ap),)))


ODDLVL = [9]


def phase_odd_proj(k, c, sc, x_d, g_row, w_d, cqn, ckvn, wuq_d, wukv_d, qn_c, kn_c, conv_w, conv_b):
    with k.phase() as ph:
        stg = Rot(ph.sbs([128, 1896], F32, 2))

        def loadw(w_ap, nk, ncols):
            wb_ = ph.split([128, nk, ncols], BF16, 1)
            wd = Buf(w_ap)
            for kk in range(nk):
                for c0 in range(0, ncols, 1896):
                    c1 = min(ncols, c0 + 1896)
                    s = stg.next()
                    k.dma(s[:, 0:c1 - c0], V(w_ap[kk * 128:(kk + 1) * 128, c0:c1], (wd,)))
                    k.copy(wb_[kk][:, c0:c1], s[:, 0:c1 - c0], eng=k.pool)
            return wb_
        wb = loadw(w_d, 8, 3792)
        wuq = loadw(wuq_d, 3, 768)
        wukv = loadw(wukv_d, 2, 1024)
        gt = ph.sb([128, D], F32)
        k.dma(gt.v(), V(g_row.to_broadcast([128, D]), (Buf(g_row),)))
        gc = ph.sb([128, 16], F32)
        for i in range(3):
            col_load(k, gc[:, i:i + 1], cqn[i * 128:(i + 1) * 128], 128)
        for i in range(2):
            col_load(k, gc[:, 3 + i:4 + i], ckvn[i * 128:(i + 1) * 128], 128)
        col_load(k, gc[:, 5:6], qn_c[0:128], 128)
        col_load(k, gc[0:64, 6:7], qn_c[128:192], 64)
        col_load(k, gc[:, 7:8], kn_c[0:128], 128)
        col_load(k, gc[0:64, 8:9], kn_c[128:192], 64)
        cw = ph.sb([128, 16, 4], F32)
        cb = ph.sb([128, 16], F32)
        cwd = Buf(conv_w)
        cbd = Buf(conv_b)
        for j in range(16):
            k.dma(cw[:, j, :], V(conv_w[:, j * 128:(j + 1) * 128].rearrange("w p -> p w"), (cwd,)),
                  allow_slow_non_contiguous=True)
            k.dma(cb[:, j:j + 1], V(conv_b[j * 128:(j + 1) * 128].rearrange("(p o) -> p o", o=1), (cbd,)))
        hal = ph.split([128, 16, 3], F32, 1)
        for j in range(16):
            k.memset(hal[j][:, :], 0.0, eng=k.pool)
        xin = Rot(ph.sbs([128, D], F32, 3))
        scr = ph.sb([128, D], BF16)
        stats = Rot(ph.sbs([128, 4], F32, 4))
        hn = Rot(ph.sbs([128, D], BF16, 2))
        hT = Rot(ph.sbs([128, 8, 512], BF16, 2))
        sq = Rot(ph.sbs([128, 512], BF16, 6))
        lnb = Rot(ph.sbs([128, 512], F32, 2))
        rstd = Rot(ph.sbs([128, 512], F32, 2))
        ob = Rot(ph.sbs([128, 512], BF16, 4))
        of = Rot(ph.sbs([128, 16], F32, 3))
        cqraw = ph.sb([128, 3, 512], F32)
        cqn_b = ph.sb([128, 3, 512], BF16)
        ckvraw = ph.sb([128, 2, 512], F32)
        ckvn_b = ph.sb([128, 2, 512], BF16)
        krraw = ph.sb([64, 512], F32)
        sqkr = ph.sb([64, 512], BF16)
        xr = Rot(ph.sbs([128, 515], F32, 2))
        acc = Rot(ph.sbs([128, 512], F32, 2))
        xact = Rot(ph.sbs([128, 512], BF16, 3))
        xtm = Rot(ph.sbs([128, 4, 128], BF16, 2))
        cs = ph.sb([64, 2, 512], F32)
        yb = Rot(ph.sbs([64, 512], BF16, 2))
        t1 = Rot(ph.sbs([64, 512], F32, 2))
        t2 = Rot(ph.sbs([64, 512], F32, 2))
        ps = ph.psum(8)
        psA = Rot(ps[0:4])
        psB = Rot(ps[4:6])
        ps_t = Rot(ps[6:8])
        xd = Buf(x_d)

        def grpnorm(raws, sqs, nch, hd, gcol0, outb):
            p2 = psB.next()
            for i in range(nch):
                k.mm(p2.v(), c.ones.v(), sqs[i].v(), start=(i == 0), stop=(i == nch - 1))
            l_, r_ = lnb.next(), rstd.next()
            k.actf(l_.v(), p2.v(), AF.Ln, scale=1.0 / hd, bias=c.epscol.v())
            k.actf(r_.v(), l_.v(), AF.Exp, scale=-0.5)
            for i in range(nch):
                k.stt(outb[:, i, :], raws[:, i, :], gc[:, gcol0 + i:gcol0 + i + 1], r_.v(), ALU.mult, ALU.mult)

        def rope(ybv, dst, tok):
            p = psB.next()
            k.mm(p[0:64, :], c.rotm.v(), ybv)
            a, b = t1.next(), t2.next()
            k.tt(a.v(), ybv, cs[:, 0, :], ALU.mult)
            k.tt(b.v(), p[0:64, :], cs[:, 1, :], ALU.mult)
            o = ob.next()
            k.tt(o[0:64, :], a.v(), b.v(), ALU.add)
            k.dma(V(dst.ap[:, tok], dst.bufs), o[0:64, :])

        def headnorm(pn, sq_r, gcol_n):
            sqn = sq.next()
            k.actf(sqn.v(), pn.v(), AF.Square)
            p2 = psB.next()
            k.mm(p2.v(), c.ones.v(), sqn.v(), start=True, stop=False)
            k.mm(p2.v(), c.ones[0:64, :], sq_r, start=False, stop=True)
            l_, r_ = lnb.next(), rstd.next()
            k.actf(l_.v(), p2.v(), AF.Ln, scale=1.0 / 192, bias=c.epscol.v())
            k.actf(r_.v(), l_.v(), AF.Exp, scale=-0.5)
            return r_

        for g in range(S // 512):
            if ODDLVL[0] < 1:
                break
            hTg = hT.next()
            xnorm_group(k, c, x_d, xd, g, gt, xin, hn, scr, stats, hTg, ps_t)
            tok = slice(g * 512, (g + 1) * 512)
            if ODDLVL[0] == 11:
                continue
            k.dma(cs[:, 0, :], V(sc["cos"].ap[:, tok], sc["cos"].bufs))
            k.dma(cs[:, 1, :], V(sc["sin"].ap[:, tok], sc["sin"].bufs))
            if ODDLVL[0] == 12:
                continue

            def proj(c0, M):
                p = psA.next()
                for kk in range(8):
                    k.mm(p[0:M, :], wb[kk][:, c0:c0 + M], hTg[:, kk, :], start=(kk == 0), stop=(kk == 7))
                return p
            sqs = []
            for i in range(3):
                p = proj(OD["cq"] + i * 128, 128)
                s_ = sq.next()
                k.actf(s_.v(), p.v(), AF.Square)
                k.copy(cqraw[:, i, :], p.v(), eng=k.dve)
                sqs.append(s_)
            if ODDLVL[0] == 13:
                continue
            grpnorm(cqraw, sqs, 3, 384, 0, cqn_b)
            if ODDLVL[0] == 14:
                continue
            sqs = []
            for i in range(2):
                p = proj(OD["ckv"] + i * 128, 128)
                s_ = sq.next()
                k.actf(s_.v(), p.v(), AF.Square)
                k.copy(ckvraw[:, i, :], p.v(), eng=k.dve)
                sqs.append(s_)
            grpnorm(ckvraw, sqs, 2, 256, 3, ckvn_b)
            p = proj(OD["kr"], 64)
            k.copy(krraw.v(), p[0:64, :], eng=k.dve)
            if ODDLVL[0] < 2:
                continue
            for h in range(4):
                pn = psA.next()
                for kc in range(3):
                    k.mm(pn.v(), wuq[kc][:, h * 192:h * 192 + 128], cqn_b[:, kc, :], start=(kc == 0), stop=(kc == 2))
                pr = psA.next()
                for kc in range(3):
                    k.mm(pr[0:64, :], wuq[kc][:, h * 192 + 128:h * 192 + 192], cqn_b[:, kc, :], start=(kc == 0), stop=(kc == 2))
                sqr = sq.next()
                k.actf(sqr[0:64, :], pr[0:64, :], AF.Square)
                r_ = headnorm(pn, sqr[0:64, :], 5)
                o = ob.next()
                k.stt(o.v(), pn.v(), gc[:, 5:6], r_.v(), ALU.mult, ALU.mult)
                k.dma(V(sc["qT"][h].ap[:, tok], sc["qT"][h].bufs), o.v())
                y_ = yb.next()
                k.stt(y_.v(), pr[0:64, :], gc[0:64, 6:7], r_[0:64, :], ALU.mult, ALU.mult)
                rope(y_.v(), sc["qr"][h], tok)
            k.actf(sqkr.v(), krraw.v(), AF.Square)
            for h in range(4):
                pn = psA.next()
                for kc in range(2):
                    k.mm(pn.v(), wukv[kc][:, h * 256:h * 256 + 128], ckvn_b[:, kc, :], start=(kc == 0), stop=(kc == 1))
                r_ = headnorm(pn, sqkr.v(), 7)
                o = ob.next()
                k.stt(o.v(), pn.v(), gc[:, 7:8], r_.v(), ALU.mult, ALU.mult)
                k.dma(V(sc["kT"][h].ap[:, tok], sc["kT"][h].bufs), o.v())
                y_ = yb.next()
                k.stt(y_.v(), krraw.v(), gc[0:64, 8:9], r_[0:64, :], ALU.mult, ALU.mult)
                rope(y_.v(), sc["kr"][h], tok)
            if ODDLVL[0] < 3:
                continue
            for j in range(16):
                p = proj(OD["xs"] + j * 128, 128)
                x_ = xr.next()
                k.copy(x_[:, 0:3], hal[j][:, :], eng=k.pool)
                k.copy(x_[:, 3:515], p.v(), eng=k.act)
                k.copy(hal[j][:, :], x_[:, 512:515], eng=k.pool)
                a_ = acc.next()
                k.ts(a_.v(), x_[:, 0:512], cw[:, j, 0:1], ALU.mult)
                for w in range(1, 4):
                    k.stt(a_.v(), x_[:, w:w + 512], cw[:, j, w:w + 1], a_.v(), ALU.mult, ALU.add)
                xa_ = xact.next()
                k.actf(xa_.v(), a_.v(), AF.Silu, bias=cb[:, j:j + 1])
                if j >= 8:
                    dst = sc["BCT"]
                    k.dma(V(dst.ap[(j - 8) * 128:(j - 7) * 128, tok], dst.bufs), xa_.v())
                if j < 12:
                    pt = ps_t.next()
                    ptb = pt.v().bitcast(BF16)
                    for t in range(4):
                        k.tr(ptb[:, t * 128:(t + 1) * 128], xa_[:, t * 128:(t + 1) * 128], c.ident.v())
                    xt_ = xtm.next()
                    k.copy(xt_.v(), ptb[:, 0:512].re("p (t c) -> p t c", t=4), eng=k.dve)
                    dst = sc["xsB"]
                    k.dma(V(dst.ap[tok, j * 128:(j + 1) * 128].rearrange("(t p) c -> p t c", p=128), dst.bufs), xt_.v())
            if ODDLVL[0] < 4:
                continue
            for t in range(4):
                r0 = g * 512 + t * 128
                tk = slice(t * 128, (t + 1) * 128)
                for hf in range(2):
                    p = psA.next()
                    for kk in range(8):
                        k.mm(p.v(), hTg[:, kk, tk], wb[kk][:, OD["z"] + hf * 512:OD["z"] + (hf + 1) * 512],
                             start=(kk == 0), stop=(kk == 7))
                    o = ob.next()
                    k.actf(o.v(), p.v(), AF.Silu)
                    k.dma(V(sc["zs"].ap[r0:r0 + 128, hf * 512:(hf + 1) * 512], sc["zs"].bufs), o.v())
                p = psA.next()
                for kk in range(8):
                    k.mm(p[:, 0:16], hTg[:, kk, tk], wb[kk][:, OD["dt"]:OD["dt"] + 16], start=(kk == 0), stop=(kk == 7))
                o2 = of.next()
                k.copy(o2[:, 0:16], p[:, 0:16], eng=k.dve)
                k.dma(V(sc["dt"].ap[r0:r0 + 128, :], sc["dt"].bufs), o2[:, 0:16])
                p = psA.next()
                for kc in range(2):
                    k.mm(p.v().re("p (h d) -> p h d", h=4), ckvn_b[:, kc, tk],
                         wukv[kc][:, :].re("p (h d) -> p h d", h=4)[:, :, 128:256], start=(kc == 0), stop=(kc == 1))
                o = ob.next()
                k.copy(o.v(), p.v(), eng=k.act)
                k.dma(V(sc["vb"].ap[r0:r0 + 128, :], sc["vb"].bufs), o.v())


def phase_mla(k, c, sc):
    SCALE = float(192.0 ** -0.5)
    with k.phase() as ph:
        qT = Rot(ph.sbs([128, S], BF16, 2))
        kT = Rot(ph.sbs([128, S], BF16, 2))
        qr = Rot(ph.sbs([64, S], BF16, 2))
        kr = Rot(ph.sbs([64, S], BF16, 2))
        vv = Rot(ph.sbs([128, 32, 128], BF16, 2))
        P_rot = Rot(ph.sbs([128, 512], BF16, 4))
        rden = Rot(ph.sbs([128, 512], F32, 2))
        ob = Rot(ph.sbs([128, 512], BF16, 2))
        ps = ph.psum(8)
        ps_s = Rot(ps[0:3])
        ps_o = Rot(ps[3:5])
        ps_d = Rot(ps[5:7])
        for h in range(4):
            q_, k_, qr_, kr_, v_ = qT.next(), kT.next(), qr.next(), kr.next(), vv.next()
            k.dma(q_.v(), sc["qT"][h])
            k.dma(k_.v(), sc["kT"][h])
            k.dma(qr_.v(), sc["qr"][h])
            k.dma(kr_.v(), sc["kr"][h])
            vsrc = sc["vb"]
            k.dma(v_.v(), V(vsrc.ap.rearrange("(t p) (h d) -> p t h d", p=128, h=4)[:, :, h, :], vsrc.bufs))
            for qg in range(8):
                def qk_fn(sv, kt, col0, q_=q_, k_=k_, qr_=qr_, kr_=kr_, qg=qg):
                    qs = slice(qg * 512 + col0, (qg + 1) * 512)
                    ks = slice(kt * 128, (kt + 1) * 128)
                    k.mm(sv, k_[:, ks], q_[:, qs], start=True, stop=False)
                    k.mm(sv, kr_[:, ks], qr_[:, qs], start=False, stop=True)

                def fin(po, pd, h=h, qg=qg):
                    r = rden.next()
                    k.call(k.dve, "reciprocal", out=r.v(), in_=pd.v())
                    o = ob.next()
                    k.tt(o.v(), po.v(), r.v(), ALU.mult)
                    dst = sc["oT"]
                    k.dma(V(dst.ap[h * 128:(h + 1) * 128, qg * 512:(qg + 1) * 512], dst.bufs), o.v())

                attn_core(k, c, qg, None, qk_fn, P_rot, ps_s, ps_o.next(), ps_d.next(),
                          lambda kt, v_=v_: v_[:, kt, :], SCALE, fin)


def bcl(v, n):
    p, h = v.ap.shape
    return V(v.ap.unsqueeze(2).to_broadcast([p, h, n]), v.bufs)


def phase_ssd(k, c, sc, dt_bias, a_log, d_skip, gate_norm):
    with k.phase() as ph:
        rep = ph.sb([128, 64], F32)
        for i, src in enumerate((dt_bias, a_log, d_skip)):
            k.dma(rep[:, i * 16:(i + 1) * 16], V(src.rearrange("(o h) -> o h", o=1).to_broadcast([128, 16]), (Buf(src),)))
        k.actf(rep[:, 16:32], rep[:, 16:32], AF.Exp)
        k.ts(rep[:, 16:32], rep[:, 16:32], -1.0, ALU.mult)
        one = ph.sb([128, 1], F32)
        k.memset(one.v(), 1.0)
        gg = ph.sb([128, D], F32)
        k.dma(gg.v(), V(gate_norm.rearrange("(o h) -> o h", o=1).to_broadcast([128, D]), (Buf(gate_norm),)))
        negtril = ph.sb([128, 128], BF16)
        tmpf = ph.sb([128, 128], F32)
        k.dma(tmpf.v(), c.negtril_d)
        k.copy(negtril.v(), tmpf.v())
        hst = ph.sb([128, D], F32)
        hstb = ph.sb([128, D], BF16)
        k.memset(hst.v(), 0.0)
        k.memset(hstb.v(), 0.0)
        xs = Rot(ph.sbs([128, D], BF16, 2))
        Btm = Rot(ph.sbs([128, 4, 128], BF16, 2))
        BT = Rot(ph.sbs([128, 4, 128], BF16, 2))
        CT = Rot(ph.sbs([128, 4, 128], BF16, 2))
        zs = Rot(ph.sbs([128, D], BF16, 2))
        dtr = Rot(ph.sbs([128, 16], F32, 2))
        sm = Rot(ph.sbs([128, 128], F32, 2))
        LT = Rot(ph.sbs([128, 16, 128], BF16, 2))
        MT = Rot(ph.sbs([128, 16, 128], BF16, 2))
        cbt = Rot(ph.sbs([128, 4, 128], BF16, 2))
        xdr = Rot(ph.sbs([128, D], BF16, 2))
        xddr = Rot(ph.sbs([128, D], BF16, 2))
        yr = Rot(ph.sbs([128, D], F32, 2))
        t2r = Rot(ph.sbs([128, D], F32, 2))
        junk = ph.sb([128, 256], BF16)
        ynr = Rot(ph.sbs([128, D], BF16, 2))
        oTt = Rot(ph.sbs([128, 8, 128], BF16, 2))
        ps = Rot(ph.psum(8))
        for ci in range(NT):
            r0 = ci * 128
            tok = slice(r0, r0 + 128)
            xs_, Btm_, BT_, CT_, zs_, dtr_ = xs.next(), Btm.next(), BT.next(), CT.next(), zs.next(), dtr.next()
            xsB = sc["xsB"]
            k.dma(xs_.v(), V(xsB.ap[tok, 0:1024], xsB.bufs))
            k.dma(Btm_.v().re("p g n -> p (g n)"), V(xsB.ap[tok, 1024:1536], xsB.bufs))
            bct = sc["BCT"]
            k.dma(BT_.v(), V(bct.ap[0:512, tok].rearrange("(g n) t -> n g t", n=128), bct.bufs))
            k.dma(CT_.v(), V(bct.ap[512:1024, tok].rearrange("(g n) t -> n g t", n=128), bct.bufs))
            k.dma(zs_.v(), V(sc["zs"].ap[tok, :], sc["zs"].bufs))
            k.dma(dtr_.v(), V(sc["dt"].ap[tok, :], sc["dt"].bufs))
            s_ = sm.next()
            k.tt(s_[:, 0:16], dtr_.v(), rep[:, 0:16], ALU.add)
            k.actf(s_[:, 0:16], s_[:, 0:16], AF.Exp)
            k.actf(s_[:, 0:16], s_[:, 0:16], AF.Ln, bias=one.v())
            k.tt(s_[:, 16:32], s_[:, 0:16], rep[:, 16:32], ALU.mult)
            pc = ps.next()
            k.mm(pc[:, 0:16], c.trif.v(), s_[:, 16:32])
            k.mm(pc[:, 16:32], c.onesf.v(), s_[:, 16:32])
            k.copy(s_[:, 32:48], pc[:, 0:16])
            k.ts(s_[:, 48:64], pc[:, 0:16], -1.0, ALU.mult)
            k.actf(s_[:, 64:80], pc[:, 0:16], AF.Exp)
            k.tt(s_[:, 112:128], pc[:, 16:32], s_[:, 32:48], ALU.subtract)
            k.actf(s_[:, 80:96], s_[:, 112:128], AF.Exp)
            k.actf(s_[:, 96:112], pc[:, 16:32], AF.Exp)
            LT_ = LT.next()
            for q4 in range(4):
                pl = ps.next()
                for i in range(4):
                    h = 4 * q4 + i
                    k.mm(pl[:, i * 128:(i + 1) * 128], s_[:, 16 + h:17 + h].bc([128, 128]), c.trif.v(), start=True, stop=False)
                    k.mm(pl[:, i * 128:(i + 1) * 128], c.ident.v(), negtril.v(), start=False, stop=True)
                    k.actf(LT_[:, h, :], pl[:, i * 128:(i + 1) * 128], AF.Exp, bias=s_[:, 48 + h:49 + h])
            pcb = ps.next()
            for g in range(4):
                k.mm(pcb[:, g * 128:(g + 1) * 128], BT_[:, g, :], CT_[:, g, :])
            cbt_ = cbt.next()
            k.copy(cbt_.v().re("p g n -> p (g n)"), pcb.v(), eng=k.act)
            MT_ = MT.next()
            for g in range(4):
                k.tt(MT_[:, 4 * g:4 * g + 4, :], LT_[:, 4 * g:4 * g + 4, :], bc1(cbt_[:, g, :], 4), ALU.mult)
            xd_, xdd_ = xdr.next(), xddr.next()
            k.tt(xd_.v().re("l (h p) -> l h p", p=64), xs_.v().re("l (h p) -> l h p", p=64), bcl(s_[:, 0:16], 64), ALU.mult)
            k.tt(xdd_.v().re("l (h p) -> l h p", p=64), xd_.v().re("l (h p) -> l h p", p=64), bcl(s_[:, 80:96], 64), ALU.mult)
            y_ = yr.next()
            t2_ = t2r.next()
            k.tt(t2_.v().re("l (h p) -> l h p", p=64), xs_.v().re("l (h p) -> l h p", p=64), bcl(rep[:, 32:48], 64), ALU.mult, eng=k.pool)
            for hf in range(2):
                hs = slice(hf * 512, (hf + 1) * 512)
                py = ps.next()
                for hh in range(8):
                    h = hf * 8 + hh
                    k.mm(py[:, hh * 64:(hh + 1) * 64], MT_[:, h, :], xd_[:, h * 64:(h + 1) * 64])
                po = ps.next()
                for gg_ in range(2):
                    g = hf * 2 + gg_
                    k.mm(po[:, gg_ * 256:(gg_ + 1) * 256], CT_[:, g, :], hstb[:, g * 256:(g + 1) * 256])
                k.tt(y_[:, hs].re("l (h p) -> l h p", p=64), po.v().re("l (h p) -> l h p", p=64),
                     bcl(s_[:, 64 + hf * 8:72 + hf * 8], 64), ALU.mult)
                k.tt(y_[:, hs], y_[:, hs], py.v(), ALU.add)
                k.tt(y_[:, hs], y_[:, hs], t2_[:, hs], ALU.add)
            for hf in range(2):
                hs = slice(hf * 512, (hf + 1) * 512)
                pst = ps.next()
                for gg_ in range(2):
                    g = hf * 2 + gg_
                    k.mm(pst[:, gg_ * 256:(gg_ + 1) * 256], Btm_[:, g, :], xdd_[:, g * 256:(g + 1) * 256])
                k.tt(hst[:, hs].re("l (h p) -> l h p", p=64), hst[:, hs].re("l (h p) -> l h p", p=64),
                     bcl(s_[:, 96 + hf * 8:104 + hf * 8], 64), ALU.mult, eng=k.pool)
                k.tt(hst[:, hs], hst[:, hs], pst.v(), ALU.add)
                k.copy(hstb[:, hs], hst[:, hs], eng=k.act)
            k.tt(y_.v(), y_.v(), zs_.v(), ALU.mult)
            for g in range(4):
                k.actf(junk.v(), y_[:, g * 256:(g + 1) * 256], AF.Square, accum_out=s_[:, 112 + g:113 + g])
            k.ts(s_[:, 116:120], s_[:, 112:116], 1.0 / 256, ALU.mult, EPS, ALU.add)
            k.tt(s_[:, 120:124], s_[:, 116:120], c.mhalf.v().bc([128, 4]), ALU.pow, eng=k.pool)
            yn_ = ynr.next()
            for g in range(4):
                gs = slice(g * 256, (g + 1) * 256)
                k.stt(yn_[:, gs], y_[:, gs], s_[:, 120 + g:121 + g], gg[:, gs], ALU.mult, ALU.mult)
            pt = ps.next()
            ptb = pt.v().bitcast(BF16)
            for j in range(8):
                k.tr(ptb[:, j * 128:(j + 1) * 128], yn_[:, j * 128:(j + 1) * 128], c.ident.v())
            o_ = oTt.next()
            k.copy(o_.v().re("p j t -> p (j t)"), ptb, eng=k.act)
            dst = sc["oT"]
            k.dma(V(dst.ap[512:1536, tok].rearrange("(j p) t -> p j t", p=128), dst.bufs), o_.v())


def const_arrays():
    cd = {}
    cd["ident"] = np.eye(128, dtype=np.float32)
    cd["pow2"] = np.tile((2.0 ** -(np.arange(32) + 1.0)).astype(np.float32)[None, :], (128, 1))
    invf = (10000.0 ** (-np.arange(32, dtype=np.float32) / 32)).astype(np.float32)
    cd["invf"] = np.concatenate([invf, invf])[:, None].astype(np.float32)
    rot = np.zeros((64, 64), np.float32)
    for m in range(32):
        rot[m + 32, m] = -1.0
        rot[m, m + 32] = 1.0
    cd["rotm"] = rot
    cd["negtril"] = (np.tril(np.ones((128, 128), np.float32), -1) * -30000.0).astype(np.float32)
    cd["tri"] = np.triu(np.ones((128, 128), np.float32))
    cd["negtri"] = (np.triu(np.ones((128, 128), np.float32), 1) * -1e30).astype(np.float32)
    return cd


INPUT_NAMES = ["x", "positions", "ev_norm", "ev_w_in", "ev_b_f", "ev_qn_a", "ev_kn_a", "ev_qn_b", "ev_kn_b",
               "ev_w_out", "od_norm", "od_w_in", "od_cq_norm", "od_ckv_norm", "od_w_uq", "od_w_ukv", "od_qn_c",
               "od_kn_c", "od_conv_w", "od_conv_b", "od_dt_bias", "od_a_log", "od_d_skip", "od_gate_norm",
               "od_w_out", "mlp_norm", "mlp_w1", "mlp_w2"]


def build(shapes, cfg):
    nc = bass.Bass("TRN2", target_bir_lowering=False)
    din = {}
    for name, (shp, dt) in shapes.items():
        bdt = I32 if np.dtype(dt) == np.int32 else F32
        din[name] = nc.dram_tensor(name, list(shp), bdt, kind="ExternalInput").ap()
    cds = const_arrays()
    cd = {n: nc.dram_tensor("c_" + n, list(a.shape), F32, kind="ExternalInput").ap() for n, a in cds.items()}
    y = nc.dram_tensor("y", [S, D], F32, kind="ExternalOutput").ap()
    xa = nc.dram_tensor("xa", [S, D], F32, kind="Internal").ap()
    xb = nc.dram_tensor("xb", [S, D], F32, kind="Internal").ap()

    def mk(name, shape, dt):
        return nc.dram_tensor(name, list(shape), dt, kind="Internal").ap()

    def dv(ap):
        return V(ap, (Buf(ap),))
    sc = {}
    t = mk("s_qT", [8, 128, S], BF16)
    sc["qT"] = [dv(t[h]) for h in range(8)]
    t = mk("s_kT", [5, 128, S], BF16)
    sc["kT"] = [dv(t[h]) for h in range(5)]
    t = mk("s_qiT", [4, 128, S], BF16)
    sc["qiT"] = [dv(t[h]) for h in range(4)]
    sc["kiT"] = dv(mk("s_kiT", [128, S], BF16))
    sc["va"] = dv(mk("s_va", [S, 128], BF16))
    sc["vb"] = dv(mk("s_vb", [S, 512], BF16))
    sc["wi"] = dv(mk("s_wi", [S, 8], F32))
    sc["fbT"] = dv(mk("s_fbT", [4, S], F32))
    sc["aug"] = dv(mk("s_aug", [4, 2, 6, S], BF16))
    sc["oT"] = dv(mk("s_oT", [1536, S], BF16))
    t = mk("s_qr", [4, 64, S], BF16)
    sc["qr"] = [dv(t[h]) for h in range(4)]
    t = mk("s_kr", [4, 64, S], BF16)
    sc["kr"] = [dv(t[h]) for h in range(4)]
    sc["cos"] = dv(mk("s_cos", [64, S], F32))
    sc["sin"] = dv(mk("s_sin", [64, S], F32))
    sc["BCT"] = dv(mk("s_BCT", [1024, S], BF16))
    sc["xsB"] = dv(mk("s_xsB", [S, 1536], BF16))
    sc["zs"] = dv(mk("s_zs", [S, 1024], BF16))
    sc["dt"] = dv(mk("s_dt", [S, 16], F32))
    dbg = cfg.get("debug", {})
    k = K(nc)
    with k.es:
        c = setup_consts(k, cd)
        cur = din["x"]
        ropedone = [False]
        steps = cfg["steps"]
        for si, (kind, l) in enumerate(steps):
            last = si == len(steps) - 1
            dst = y if last else (xa if cur is not xa else xb)
            if kind == "mlp":
                phase_mlp(k, c, cur, dst, din["mlp_norm"][l:l + 1, :], din["mlp_w1"][l], din["mlp_w2"][l])
            elif kind == "even":
                phase_even_proj(k, c, sc, cur, din["ev_norm"][l:l + 1, :], din["ev_w_in"][l], din["ev_qn_a"][l],
                                din["ev_kn_a"][l], din["ev_qn_b"][l], din["ev_kn_b"][l])
                if "nodsa" not in dbg:
                    phase_dsa(k, c, sc)
                if "nofox" not in dbg:
                    phase_fox(k, c, sc, din["ev_b_f"][l])
                phase_outproj(k, c, sc, cur, dst, din["ev_w_out"][l], 1024)
            elif kind == "odd":
                if "oddlvl" in dbg:
                    ODDLVL[0] = dbg["oddlvl"]
                if not ropedone[0] and "norope" not in dbg:
                    phase_rope_tables(k, c, sc, din["positions"])
                    ropedone[0] = True
                phase_odd_proj(k, c, sc, cur, din["od_norm"][l:l + 1, :], din["od_w_in"][l], din["od_cq_norm"][l],
                               din["od_ckv_norm"][l], din["od_w_uq"][l], din["od_w_ukv"][l], din["od_qn_c"][l],
                               din["od_kn_c"][l], din["od_conv_w"][l], din["od_conv_b"][l])
                if "nomla" not in dbg:
                    phase_mla(k, c, sc)
                if "nossd" not in dbg:
                    phase_ssd(k, c, sc, din["od_dt_bias"][l], din["od_a_log"][l], din["od_d_skip"][l],
                              din["od_gate_norm"][l])
                phase_outproj(k, c, sc, cur, dst, din["od_w_out"][l], 1536)
            else:
                raise ValueError(kind)
            cur = dst
        k.barrier()
    return nc, cds


FULL_CFG = {"steps": [("even", 0), ("mlp", 0), ("odd", 0), ("mlp", 1), ("even", 1), ("mlp", 2), ("odd", 1), ("mlp", 3)]}


def run(inputs, cfg, cores=N_CORES):
    per_core = []
    for b in range(cores):
        m = {}
        for n in INPUT_NAMES:
            a = np.asarray(inputs[n])
            if n in ("x", "positions"):
                a = a[b]
            m[n] = np.ascontiguousarray(a)
        per_core.append(m)
    shapes = {n: (per_core[0][n].shape, per_core[0][n].dtype) for n in INPUT_NAMES}
    nc, cds = build(shapes, cfg)
    for m in per_core:
        for n, a in cds.items():
            m["c_" + n] = a
    res = run_bass_kernel_spmd(nc, per_core, core_ids=list(range(cores)))
    return np.stack([np.asarray(r["y"]) for r in res.results], axis=0)


def kernel(**inputs):
    out = run(inputs, FULL_CFG)
    return out.astype(np.float32)
```

```python
import contextlib
import numpy as np
import ml_dtypes
import concourse.bass as bass
import concourse.mybir as mybir
from concourse.bass_utils import run_bass_kernel_spmd

F32 = mybir.dt.float32
BF16 = mybir.dt.bfloat16
I32 = mybir.dt.int32
AF = mybir.ActivationFunctionType
ALU = mybir.AluOpType
AX = mybir.AxisListType

S = 4096
D = 1024
NT = S // 128
DFF = 4096
EPS = 1e-6
N_CORES = 8
WRITE_KEYS = ("out", "accum_out", "ap")


class V:
    def __init__(self, ap, bufs):
        self.ap = ap
        self.bufs = bufs

    def __getitem__(self, idx):
        return V(self.ap[idx], self.bufs)

    def bc(self, shape):
        return V(self.ap.to_broadcast(shape), self.bufs)

    def re(self, pat, **kw):
        return V(self.ap.rearrange(pat, **kw), self.bufs)

    def bitcast(self, dt):
        return V(self.ap.bitcast(dt), self.bufs)


class Buf:
    def __init__(self, ap):
        self.ap = ap
        self.w = None
        self.r = {}
        self.excl = False

    def __getitem__(self, idx):
        return V(self.ap[idx], (self,))

    def v(self):
        return V(self.ap, (self,))


def multi(*views):
    bufs = []
    for v in views:
        bufs.extend(v.bufs)
    return V(views[0].ap, tuple(bufs))


class Eng:
    def __init__(self, k, name, raw, self_sync):
        self.name = name
        self.raw = raw
        self.sem = k.new_sem("e_" + name)
        self.cnt = 0
        self.seen = {}
        self.self_sync = self_sync


class Slot:
    def __init__(self, k, key):
        self.key = key
        self.sem = k.new_sem(key)
        self.val = 0


class K:
    NSLOT = 12

    def __init__(self, nc):
        self.nc = nc
        self.es = contextlib.ExitStack()
        self.pe = Eng(self, "pe", nc.tensor, False)
        self.act = Eng(self, "act", nc.scalar, True)
        self.dve = Eng(self, "dve", nc.vector, True)
        self.pool = Eng(self, "pool", nc.gpsimd, True)
        self.sp = Eng(self, "sp", nc.sync, False)
        self.engs = [self.pe, self.act, self.dve, self.pool, self.sp]
        self.queues = {}
        for q in (self.sp, self.pool):
            self.queues[q.name] = [Slot(self, "d_%s_%d" % (q.name, i)) for i in range(self.NSLOT)]
        self.qnext = {q: 0 for q in self.queues}
        self.nph = 0

    def new_sem(self, name):
        return self.es.enter_context(self.nc.semaphore(name))

    def _wait(self, eng, tok):
        key, sem, val = tok
        if key == eng.name and not eng.self_sync:
            return
        if eng.seen.get(key, 0) >= val:
            return
        eng.raw.wait_ge(sem, val)
        eng.seen[key] = val

    def _deps(self, eng, reads, writes):
        for v in reads:
            for b in v.bufs:
                if b.w is not None:
                    self._wait(eng, b.w)
                if b.excl:
                    for t in b.r.values():
                        if t[0] != eng.name:
                            self._wait(eng, t)
        for v in writes:
            for b in v.bufs:
                if b.w is not None:
                    self._wait(eng, b.w)
                for t in b.r.values():
                    self._wait(eng, t)

    def _mark(self, tok, reads, writes):
        for v in reads:
            for b in v.bufs:
                b.r[tok[0]] = tok
        for v in writes:
            for b in v.bufs:
                b.w = tok
                b.r = {}

    def call(self, eng, method, **kw):
        reads, writes, args = [], [], {}
        for key, v in kw.items():
            if isinstance(v, V):
                (writes if key in WRITE_KEYS else reads).append(v)
                args[key] = v.ap
            else:
                args[key] = v
        self._deps(eng, reads, writes)
        inst = getattr(eng.raw, method)(**args)
        eng.cnt += 1
        inst.then_inc(eng.sem, 1)
        self._mark((eng.name, eng.sem, eng.cnt), reads, writes)
        return inst

    def dma(self, out, in_, q=None, **kw):
        q = q or self.sp
        slots = self.queues[q.name]
        slot = slots[self.qnext[q.name] % len(slots)]
        self.qnext[q.name] += 1
        if slot.val > 0:
            self._wait(q, (slot.key, slot.sem, slot.val))
        self._deps(q, [in_], [out])
        slot.val += 16
        q.raw.dma_start(out=out.ap, in_=in_.ap, **kw).then_inc(slot.sem, 16)
        self._mark((slot.key, slot.sem, slot.val), [in_], [out])

    def barrier(self):
        toks = [(e.name, e.sem, e.cnt) for e in self.engs if e.cnt > 0]
        for sl in self.queues.values():
            toks += [(s.key, s.sem, s.val) for s in sl if s.val > 0]
        for e in self.engs:
            for t in toks:
                self._wait(e, t)

    def mm(self, out, lhsT, rhs, start=True, stop=True):
        return self.call(self.pe, "matmul", out=out, lhsT=lhsT, rhs=rhs, start=start, stop=stop)

    def tr(self, out, in_, ident):
        return self.call(self.pe, "transpose", out=out, in_=in_, identity=ident)

    def actf(self, out, in_, func, **kw):
        return self.call(self.act, "activation", out=out, in_=in_, func=func, **kw)

    def tt(self, out, in0, in1, op, eng=None):
        return self.call(eng or self.dve, "tensor_tensor", out=out, in0=in0, in1=in1, op=op)

    def ts(self, out, in0, s1, op0, s2=None, op1=None, eng=None, **kw):
        if op1 is None:
            return self.call(eng or self.dve, "tensor_scalar", out=out, in0=in0, scalar1=s1, scalar2=None,
                             op0=op0, **kw)
        return self.call(eng or self.dve, "tensor_scalar", out=out, in0=in0, scalar1=s1, scalar2=s2,
                         op0=op0, op1=op1, **kw)

    def stt(self, out, in0, scalar, in1, op0, op1, **kw):
        return self.call(self.dve, "scalar_tensor_tensor", out=out, in0=in0, scalar=scalar, in1=in1,
                         op0=op0, op1=op1, **kw)

    def copy(self, out, in_, eng=None):
        eng = eng or self.dve
        if eng is self.act:
            return self.call(eng, "copy", out=out, in_=in_)
        return self.call(eng, "tensor_copy", out=out, in_=in_)

    def memset(self, ap, val, eng=None):
        return self.call(eng or self.dve, "memset", ap=ap, constant=val)

    @contextlib.contextmanager
    def phase(self):
        self.barrier()
        self.nph += 1
        ph = Phase(self, "p%d" % self.nph)
        with ph.es:
            yield ph
            self.barrier()


class Phase:
    def __init__(self, k, name):
        self.k = k
        self.name = name
        self.es = contextlib.ExitStack()
        self.n = 0

    def sbt(self, shape, dtype):
        self.n += 1
        return self.es.enter_context(self.k.nc.sbuf_tensor("%s_s%d" % (self.name, self.n), list(shape), dtype))

    def sb(self, shape, dtype):
        t = self.sbt(shape, dtype)
        return Buf(t[tuple(slice(None) for _ in shape)])

    def sbs(self, shape, dtype, n):
        return [self.sb(shape, dtype) for _ in range(n)]

    def split(self, shape, dtype, axis, step=1):
        t = self.sbt(shape, dtype)
        out = []
        for i in range(0, shape[axis], step):
            idx = [slice(None)] * len(shape)
            idx[axis] = slice(i, i + step) if step > 1 else i
            out.append(Buf(t[tuple(idx)]))
        return out

    def psum(self, n=8):
        out = []
        for i in range(n):
            self.n += 1
            t = self.es.enter_context(self.k.nc.psum_tensor("%s_ps%d" % (self.name, self.n), [128, 512], F32))
            b = Buf(t[:, :])
            b.excl = True
            out.append(b)
        return out


class Rot:
    def __init__(self, items):
        self.items = items
        self.i = 0

    def next(self):
        it = self.items[self.i % len(self.items)]
        self.i += 1
        return it


def dram_buf(ap):
    return Buf(ap)


class Ctx:
    pass


def setup_consts(k, cd):
    nc = k.nc
    c = Ctx()
    es = k.es
    def sb(name, shape, dt):
        t = es.enter_context(nc.sbuf_tensor(name, list(shape), dt))
        return Buf(t[tuple(slice(None) for _ in shape)])
    c.ident = sb("k_ident", [128, 128], BF16)
    c.mhalf = sb("k_mhalf", [128, 1], F32)
    tmp = sb("k_tmp", [128, 128], F32)
    k.dma(tmp.v(), V(cd["ident"], (Buf(cd["ident"]),)))
    k.copy(c.ident.v(), tmp.v(), eng=k.dve)
    k.memset(c.mhalf.v(), -0.5, eng=k.dve)
    c.epscol = sb("k_epscol", [128, 1], F32)
    k.memset(c.epscol.v(), EPS, eng=k.dve)
    c.identf = sb("k_identf", [128, 128], F32)
    k.copy(c.identf.v(), tmp.v(), eng=k.dve)
    c.i4 = sb("k_i4", [128, 512], BF16)
    for h_ in range(4):
        k.copy(c.i4[:, h_ * 128:(h_ + 1) * 128], tmp.v(), eng=k.dve)
    c.ones = sb("k_ones", [128, 128], BF16)
    k.memset(c.ones.v(), 1.0, eng=k.dve)
    c.onesf = sb("k_onesf", [128, 128], F32)
    k.memset(c.onesf.v(), 1.0, eng=k.dve)
    c.tri = sb("k_tri", [128, 128], BF16)
    k.dma(tmp.v(), V(cd["tri"], (Buf(cd["tri"]),)))
    k.copy(c.tri.v(), tmp.v(), eng=k.dve)
    c.trif = sb("k_trif", [128, 128], F32)
    k.copy(c.trif.v(), tmp.v(), eng=k.dve)
    c.invf = sb("k_invf", [64, 1], F32)
    k.dma(c.invf.v(), V(cd["invf"], (Buf(cd["invf"]),)))
    c.rotm = sb("k_rotm", [64, 64], BF16)
    k.dma(tmp[0:64, 0:64], V(cd["rotm"], (Buf(cd["rotm"]),)))
    k.copy(c.rotm.v(), tmp[0:64, 0:64], eng=k.dve)
    c.negtril_d = V(cd["negtril"], (Buf(cd["negtril"]),))
    c.negbig = sb("k_negbig", [128, 1], F32)
    k.memset(c.negbig.v(), -1e29, eng=k.dve)
    c.pow2 = sb("k_pow2", [128, 32], F32)
    k.dma(c.pow2.v(), V(cd["pow2"], (Buf(cd["pow2"]),)))
    c.negtri = sb("k_negtri", [128, 128], F32)
    k.dma(c.negtri.v(), V(cd["negtri"], (Buf(cd["negtri"]),)))
    return c


def rmsnorm_tile(k, c, ph, xt, gt, hn, scr, st):
    k.actf(scr.v(), xt.v(), AF.Square, accum_out=st[:, 0:1])
    k.ts(st[:, 1:2], st[:, 0:1], 1.0 / D, ALU.mult, EPS, ALU.add)
    k.tt(st[:, 2:3], st[:, 1:2], c.mhalf.v(), ALU.pow, eng=k.pool)
    k.stt(hn.v(), xt.v(), st[:, 2:3], gt.v(), ALU.mult, ALU.mult)


def phase_mlp(k, c, x_d, xo_d, g_row, w1_d, w2_d):
    G = 256
    NG = S // G
    with k.phase() as ph:
        w1b = ph.split([128, 8, DFF], BF16, 1)
        w2b = ph.split([128, 32, D], BF16, 1)
        stg = Rot(ph.sbs([128, 2048], F32, 2))
        gt = ph.sb([128, D], F32)
        xin = Rot(ph.sbs([128, D], F32, 3))
        scr = ph.sb([128, D], BF16)
        stats = Rot(ph.sbs([128, 4], F32, 4))
        hn = Rot(ph.sbs([128, D], BF16, 2))
        hT = Rot(ph.sbs([128, 8, G], BF16, 2))
        rl = Rot(ph.sbs([128, G], BF16, 4))
        hid = Rot(ph.sbs([128, G], BF16, 5))
        xres = Rot(ph.sbs([128, D], F32, 3))
        ps = ph.psum(8)
        ps_y = ps[0:4]
        ps_h = Rot(ps[4:7])
        ps_t = Rot(ps[7:8])
        xd = Buf(x_d)
        xod = Buf(xo_d)
        w1d = Buf(w1_d)
        w2d = Buf(w2_d)
        k.dma(gt.v(), V(g_row.to_broadcast([128, D]), (Buf(g_row),)))
        for kk in range(8):
            for hf in range(2):
                s = stg.next()
                k.dma(s.v(), V(w1_d[kk * 128:(kk + 1) * 128, hf * 2048:(hf + 1) * 2048], (w1d,)))
                k.copy(w1b[kk][:, hf * 2048:(hf + 1) * 2048], s.v(), eng=k.pool)
        w2v = w2_d.rearrange("(j p) n -> p j n", p=128)
        for jj in range(0, 32, 2):
            s = stg.next()
            k.dma(s.v().re("p (j n) -> p j n", j=2), V(w2v[:, jj:jj + 2, :], (w2d,)))
            eng = k.pool
            k.copy(w2b[jj][:, :], s[:, 0:1024], eng=eng)
            k.copy(w2b[jj + 1][:, :], s[:, 1024:2048], eng=eng)

        def norm_a(g):
            res = []
            for t in range(G // 128):
                xt = xin.next()
                r0 = g * G + t * 128
                k.dma(xt.v(), V(x_d[r0:r0 + 128, :], (xd,)))
                h = hn.next()
                rmsnorm_tile(k, c, ph, xt, gt, h, scr, stats.next())
                res.append(h)
            return res

        def norm_b(g, hs):
            hTg = hT.next()
            for t, h in enumerate(hs):
                pt = ps_t.next()
                ptb = pt.v().bitcast(BF16)
                for kk in range(8):
                    k.tr(ptb[:, kk * 128:(kk + 1) * 128], h[:, kk * 128:(kk + 1) * 128], c.ident.v())
                k.copy(hTg[:, :, t * 128:(t + 1) * 128], ptb.re("p (k t) -> p k t", k=8), eng=k.act)
            return hTg

        hs = norm_a(0)
        hT_cur = norm_b(0, hs)
        for g in range(NG):
            hs_next = None
            hT_next = None
            pend = []

            def w2(pj, phd, last):
                for t in range(2):
                    for cc in range(2):
                        k.mm(ps_y[t * 2 + cc].v(), phd[:, t * 128:(t + 1) * 128],
                             w2b[pj][:, cc * 512:(cc + 1) * 512], start=(pj == 0), stop=last)
            for j in range(32):
                ph_ = ps_h.next()
                for kk in range(8):
                    k.mm(ph_[:, 0:G], w1b[kk][:, j * 128:(j + 1) * 128], hT_cur[:, kk, :],
                         start=(kk == 0), stop=(kk == 7))
                r = rl.next()
                k.actf(r.v(), ph_[:, 0:G], AF.Relu)
                hd = hid.next()
                k.tt(hd.v(), r.v(), r.v(), ALU.mult)
                pend.append((j, hd))
                if len(pend) > 2:
                    pj, phd = pend.pop(0)
                    w2(pj, phd, False)
                if j == 4 and g + 1 < NG:
                    hs_next = norm_a(g + 1)
                if j == 20 and g + 1 < NG:
                    hT_next = norm_b(g + 1, hs_next)
            while pend:
                pj, phd = pend.pop(0)
                w2(pj, phd, pj == 31)
            for t in range(2):
                r0 = g * G + t * 128
                xr = xres.next()
                k.dma(xr.v(), V(x_d[r0:r0 + 128, :], (xd,)))
                for cc in range(2):
                    k.tt(xr[:, cc * 512:(cc + 1) * 512], ps_y[t * 2 + cc].v(), xr[:, cc * 512:(cc + 1) * 512], ALU.add)
                k.dma(V(xo_d[r0:r0 + 128, :], (xod,)), xr.v())
            hT_cur = hT_next


def xnorm_group(k, c, x_d, xd, g, gt, xin, hn, scr, stats, hTg, ps_t, ntile=4):
    G = ntile * 128
    for t in range(ntile):
        xt = xin.next()
        r0 = g * G + t * 128
        k.dma(xt.v(), V(x_d[r0:r0 + 128, :], (xd,)))
        h = hn.next()
        rmsnorm_tile(k, c, None, xt, gt, h, scr, stats.next())
        pt = ps_t.next()
        ptb = pt.v().bitcast(BF16)
        for kk in range(8):
            k.tr(ptb[:, kk * 128:(kk + 1) * 128], h[:, kk * 128:(kk + 1) * 128], c.ident.v())
        k.copy(hTg[:, :, t * 128:(t + 1) * 128], ptb.re("p (k t) -> p k t", k=8), eng=k.act)


def load_w_bf16(k, ph, w_d, nk, ncols, stg_cols=None):
    wb = ph.split([128, nk, ncols], BF16, 1)
    stg = Rot(ph.sbs([128, ncols], F32, 2))
    wd = Buf(w_d)
    rows = w_d.shape[0]
    for kk in range(nk):
        s = stg.next()
        r = min(128, rows - kk * 128)
        k.dma(s[0:r, :], V(w_d[kk * 128:kk * 128 + r, :], (wd,)))
        k.copy(wb[kk][0:r, :], s[0:r, :], eng=k.pool)
    return wb


def fm_qknorm(k, c, ps, M, gcol, outb, sq, lnb, rstd, ps2, hd):
    N = ps.ap.shape[-1]
    k.actf(sq[0:M, 0:N], ps, AF.Square)
    k.mm(ps2[0:M, 0:N], c.ones[0:M, 0:M], sq[0:M, 0:N])
    k.actf(lnb[0:M, 0:N], ps2[0:M, 0:N], AF.Ln, scale=1.0 / hd, bias=c.epscol[0:M, :])
    k.actf(rstd[0:M, 0:N], lnb[0:M, 0:N], AF.Exp, scale=-0.5)
    k.stt(outb, ps, gcol, rstd[0:M, 0:N], ALU.mult, ALU.mult)


EV = dict(qa=0, ka=512, va=640, qi=768, ki=1280, wi=1344, qb=1352, kb=1864, vb=2376, fb=2888)


def phase_even_proj(k, c, sc, x_d, g_row, w_d, qn_a, kn_a, qn_b, kn_b):
    with k.phase() as ph:
        wb = load_w_bf16(k, ph, w_d, 8, 2892)
        wkd = ph.sb([128, 8, 128], BF16)
        for kk in range(8):
            k.copy(wkd[:, kk, 0:64], wb[kk][:, 1280:1344], eng=k.pool)
            k.copy(wkd[:, kk, 64:128], wb[kk][:, 1280:1344], eng=k.pool)
        gt = ph.sb([128, D], F32)
        k.dma(gt.v(), V(g_row.to_broadcast([128, D]), (Buf(g_row),)))
        gcol = ph.sb([128, 4], F32)
        for i, gn in enumerate((qn_a, kn_a, qn_b, kn_b)):
            k.dma(gcol[:, i:i + 1], V(gn.rearrange("(p o) -> p o", o=1), (Buf(gn),)))
        xin = Rot(ph.sbs([128, D], F32, 3))
        scr = ph.sb([128, D], BF16)
        stats = Rot(ph.sbs([128, 4], F32, 4))
        hn = Rot(ph.sbs([128, D], BF16, 2))
        hT = Rot(ph.sbs([128, 8, 512], BF16, 2))
        sq = Rot(ph.sbs([128, 512], BF16, 2))
        lnb = Rot(ph.sbs([128, 512], F32, 2))
        rstd = Rot(ph.sbs([128, 512], F32, 2))
        ob = Rot(ph.sbs([128, 512], BF16, 4))
        of = Rot(ph.sbs([128, 512], F32, 2))
        ps = ph.psum(8)
        psA = Rot(ps[0:3])
        psB = Rot(ps[3:5])
        ps_t = Rot(ps[5:7])
        psC = Rot(ps[7:8])
        xd = Buf(x_d)
        chunks = []
        for h in range(4):
            chunks.append((wb, EV["qa"] + h * 128, 128, 0, sc["qT"][h]))
        chunks.append((wb, EV["ka"], 128, 1, sc["kT"][0]))
        for cc in range(4):
            chunks.append((wb, EV["qi"] + cc * 128, 128, None, sc["qiT"][cc]))
        chunks.append((None, 0, 128, None, sc["kiT"]))
        for h in range(4):
            chunks.append((wb, EV["qb"] + h * 128, 128, 2, sc["qT"][4 + h]))
        for h in range(4):
            chunks.append((wb, EV["kb"] + h * 128, 128, 3, sc["kT"][1 + h]))
        chunks.append((wb, EV["fb"], 4, "f32", sc["fbT"]))
        ci = 0
        for g in range(S // 512):
            hTg = hT.next()
            xnorm_group(k, c, x_d, xd, g, gt, xin, hn, scr, stats, hTg, ps_t)
            tok = slice(g * 512, (g + 1) * 512)
            for (wsrc, c0, M, nrm, dst) in chunks:
                p = psA.next()
                for kk in range(8):
                    lhsT = wkd[:, kk, :] if wsrc is None else wb[kk][:, c0:c0 + M]
                    k.mm(p[0:M, :], lhsT, hTg[:, kk, :], start=(kk == 0), stop=(kk == 7))
                if nrm == "f32":
                    o = of.next()
                    k.copy(o[0:M, :], p[0:M, :], eng=k.dve)
                    k.dma(V(dst.ap[0:M, tok], dst.bufs), o[0:M, :])
                    continue
                o = ob.next()
                if nrm is None:
                    ci += 1
                    k.copy(o[0:M, :], p[0:M, :], eng=(k.act if ci % 2 else k.dve))
                else:
                    fm_qknorm(k, c, p[0:M, :], M, gcol[:, nrm:nrm + 1], o[0:M, :], sq.next(), lnb.next(),
                              rstd.next(), psB.next(), 128)
                k.dma(V(dst.ap[0:M, tok], dst.bufs), o[0:M, :])
            for t in range(4):
                r0 = g * 512 + t * 128
                tk = slice(t * 128, (t + 1) * 128)
                p = psA.next()
                for kk in range(8):
                    k.mm(p[:, :], hTg[:, kk, tk], wb[kk][:, EV["vb"]:EV["vb"] + 512], start=(kk == 0), stop=(kk == 7))
                o = ob.next()
                k.copy(o[:, :], p[:, :], eng=k.act)
                k.dma(V(sc["vb"].ap[r0:r0 + 128, :], sc["vb"].bufs), o[:, :])
                p = psC.next()
                for kk in range(8):
                    k.mm(p[:, 0:128], hTg[:, kk, tk], wb[kk][:, EV["va"]:EV["va"] + 128], start=(kk == 0), stop=(kk == 7))
                for kk in range(8):
                    k.mm(p[:, 128:136], hTg[:, kk, tk], wb[kk][:, EV["wi"]:EV["wi"] + 8], start=(kk == 0), stop=(kk == 7))
                o = ob.next()
                k.copy(o[:, 0:128], p[:, 0:128], eng=k.dve)
                k.dma(V(sc["va"].ap[r0:r0 + 128, :], sc["va"].bufs), o[:, 0:128])
                o2 = of.next()
                k.copy(o2[:, 0:8], p[:, 128:136], eng=k.dve)
                k.dma(V(sc["wi"].ap[r0:r0 + 128, :], sc["wi"].bufs), o2[:, 0:8])


def split3(k, ph, src, n, outs):
    k.copy(outs[0][0:n, :], src[0:n, :])
    k.tt(src[0:n, :], src[0:n, :], outs[0][0:n, :], ALU.subtract)
    k.copy(outs[1][0:n, :], src[0:n, :])
    k.tt(src[0:n, :], src[0:n, :], outs[1][0:n, :], ALU.subtract)
    k.copy(outs[2][0:n, :], src[0:n, :])


def attn_core(k, c, qg, nkt_fn, qk_fn, P_rot, ps_s, ps_o, ps_d, v_fn, scale, finalize, la=2):
    nkt = 4 * qg + 4
    po = ps_o
    pd = ps_d
    issued = []

    def issue(kt):
        diag = kt >= 4 * qg
        col0 = (kt - 4 * qg) * 128 if diag else 0
        s = ps_s.next()
        qk_fn(s[:, col0:512], kt, col0)
        issued.append((s, col0, diag))
    for kt in range(min(la, nkt)):
        issue(kt)
    for kt in range(nkt):
        if kt + la < nkt:
            issue(kt + la)
        s, col0, diag = issued[kt]
        P = P_rot.next()
        k.actf(P[:, col0:512], s[:, col0:512], AF.Exp, scale=scale)
        if diag:
            k.tt(P[:, col0:col0 + 128], P[:, col0:col0 + 128], c.tri.v(), ALU.mult, eng=k.pool)
        k.mm(po[:, col0:512], v_fn(kt), P[:, col0:512], start=(kt == 0), stop=(kt == nkt - 1))
        k.mm(pd[:, col0:512], c.ones.v(), P[:, col0:512], start=(kt == 0), stop=(kt == nkt - 1))
    finalize(po, pd)


def phase_fox(k, c, sc, b_f):
    SQ = float(np.sqrt(128.0))
    with k.phase() as ph:
        with contextlib.ExitStack() as es2:
            ph2 = Phase(k, ph.name + "a")
            es2.enter_context(ph2.es)
            f0 = ph2.sb([4, S], F32)
            f1 = ph2.sb([4, S], F32)
            bcol = ph2.sb([4, 2], F32)
            one4 = ph2.sb([4, 1], F32)
            pcs = ph2.sbs([4, S], BF16, 3)
            ones4 = ph2.sb([4, S], BF16)
            k.dma(f0.v(), sc["fbT"])
            k.dma(bcol[:, 0:1], V(b_f.rearrange("(p o) -> p o", o=1), (Buf(b_f),)))
            k.ts(bcol[:, 1:2], bcol[:, 0:1], -1.0, ALU.mult)
            k.memset(one4.v(), 1.0)
            k.memset(ones4.v(), 1.0)
            k.actf(f0.v(), f0.v(), AF.Exp, scale=-1.0, bias=bcol[:, 1:2])
            k.actf(f0.v(), f0.v(), AF.Ln, bias=one4.v())
            k.ts(f0.v(), f0.v(), -SQ, ALU.mult)
            k.call(k.dve, "tensor_tensor_scan", out=f1.v(), data0=one4.v().bc([4, S]), data1=f0.v(),
                   initial=0.0, op0=ALU.mult, op1=ALU.add)
            k.copy(f0.v().re("p (b t) -> p b t", t=128), f1.v().re("p (b t) -> p b t", t=128)[:, :, 127:128].bc([4, 32, 128]))
            aug = sc["aug"]
            split3(k, ph2, f0, 4, pcs)
            for p_ in range(3):
                k.dma(V(aug.ap[:, 0, p_, :], aug.bufs), pcs[p_].v())
            k.ts(f1.v(), f1.v(), -1.0, ALU.mult)
            split3(k, ph2, f1, 4, pcs)
            for p_ in range(3):
                k.dma(V(aug.ap[:, 1, 3 + p_, :], aug.bufs), pcs[p_].v())
                k.dma(V(aug.ap[:, 1, p_, :], aug.bufs), ones4.v())
                k.dma(V(aug.ap[:, 0, 3 + p_, :], aug.bufs), ones4.v())
            k.barrier()
        qT = Rot(ph.sbs([128, S], BF16, 2))
        kT = Rot(ph.sbs([128, S], BF16, 2))
        vv = Rot(ph.sbs([128, 32, 128], BF16, 2))
        aq = Rot(ph.sbs([6, S], BF16, 2))
        ak = Rot(ph.sbs([6, S], BF16, 2))
        P_rot = Rot(ph.sbs([128, 512], BF16, 4))
        rden = Rot(ph.sbs([128, 512], F32, 2))
        ob = Rot(ph.sbs([128, 512], BF16, 2))
        ps = ph.psum(8)
        ps_s = Rot(ps[0:3])
        ps_o = Rot(ps[3:5])
        ps_d = Rot(ps[5:7])
        for h in range(4):
            q_, k_, v_, aq_, ak_ = qT.next(), kT.next(), vv.next(), aq.next(), ak.next()
            k.dma(q_.v(), sc["qT"][4 + h])
            k.dma(k_.v(), sc["kT"][1 + h])
            vsrc = sc["vb"]
            k.dma(v_.v(), V(vsrc.ap.rearrange("(t p) (h d) -> p t h d", p=128, h=4)[:, :, h, :], vsrc.bufs))
            k.dma(aq_.v(), V(sc["aug"].ap[h, 0], sc["aug"].bufs))
            k.dma(ak_.v(), V(sc["aug"].ap[h, 1], sc["aug"].bufs))
            for qg in range(8):
                def qk_fn(sv, kt, col0, q_=q_, k_=k_, aq_=aq_, ak_=ak_, qg=qg):
                    qs = slice(qg * 512 + col0, (qg + 1) * 512)
                    ks = slice(kt * 128, (kt + 1) * 128)
                    k.mm(sv, k_[:, ks], q_[:, qs], start=True, stop=False)
                    k.mm(sv, ak_[:, ks], aq_[:, qs], start=False, stop=True)

                def fin(po, pd, h=h, qg=qg):
                    r = rden.next()
                    k.call(k.dve, "reciprocal", out=r.v(), in_=pd.v())
                    o = ob.next()
                    k.tt(o.v(), po.v(), r.v(), ALU.mult)
                    dst = sc["oT"]
                    k.dma(V(dst.ap[512 + h * 128:512 + (h + 1) * 128, qg * 512:(qg + 1) * 512], dst.bufs), o.v())

                attn_core(k, c, qg, None, qk_fn, P_rot, ps_s, ps_o.next(), ps_d.next(),
                          lambda kt, v_=v_: v_[:, kt, :], 1.0 / SQ, fin)


def phase_outproj(k, c, sc, x_d, xo_d, w_d, nfeat):
    nk = nfeat // 128
    with k.phase() as ph:
        wb = load_w_bf16(k, ph, w_d, nk, D)
        oT = Rot(ph.sbs([128, nk, 512], BF16, 2))
        xres = Rot(ph.sbs([128, D], F32, 3))
        ps = ph.psum(8)
        psr = Rot(ps)
        xd = Buf(x_d)
        xod = Buf(xo_d)
        src = sc["oT"]
        for g in range(S // 512):
            o_ = oT.next()
            k.dma(o_.v(), V(src.ap[0:nfeat, g * 512:(g + 1) * 512].rearrange("(k p) s -> p k s", p=128), src.bufs))
            for t in range(4):
                r0 = g * 512 + t * 128
                xr = xres.next()
                k.dma(xr.v(), V(x_d[r0:r0 + 128, :], (xd,)))
                for cc in range(2):
                    p = psr.next()
                    for kk in range(nk):
                        k.mm(p.v(), o_[:, kk, t * 128:(t + 1) * 128], wb[kk][:, cc * 512:(cc + 1) * 512],
                             start=(kk == 0), stop=(kk == nk - 1))
                    k.tt(xr[:, cc * 512:(cc + 1) * 512], p.v(), xr[:, cc * 512:(cc + 1) * 512], ALU.add)
                k.dma(V(xo_d[r0:r0 + 128, :], (xod,)), xr.v())


def bc1(v, n):
    p, f = v.ap.shape
    return V(v.ap.unsqueeze(1).to_broadcast([p, n, f]), v.bufs)


NBIS = 14


def phase_dsa(k, c, sc):
    SCALE = float(128.0 ** -0.5)
    with k.phase() as ph:
        qi = ph.sb([128, 4, S], BF16)
        ki = ph.sb([128, S], BF16)
        qa = ph.sb([128, 4, S], BF16)
        ka = ph.sb([128, S], BF16)
        va = ph.sb([128, 32, 128], BF16)
        wi = ph.sb([128, 32, 8], F32)
        for h in range(4):
            k.dma(qi[:, h, :], sc["qiT"][h])
            k.dma(qa[:, h, :], sc["qT"][h])
        k.dma(ki.v(), sc["kiT"])
        k.dma(ka.v(), sc["kT"][0])
        k.dma(va.v(), V(sc["va"].ap.rearrange("(t p) d -> p t d", p=128), sc["va"].bufs))
        k.dma(wi.v(), V(sc["wi"].ap.rearrange("(t p) d -> p t d", p=128), sc["wi"].bufs))
        scb = ph.sbs([128, S], F32, 3)
        junk_t = ph.sbt([128, S], BF16)
        msk = ph.sbs([128, S], BF16, 3)
        rl = Rot(ph.sbs([128, 512], BF16, 6))
        dg = ph.sbs([128, 8, 128], BF16, 3)
        E = Rot(ph.sbs([128, 512], BF16, 3))
        P = Rot(ph.sbs([128, 512], BF16, 3))
        mT = Rot(ph.sbs([128, 512], BF16, 3))
        stt_ = ph.sbs([128, 64], F32, 3)
        rden = Rot(ph.sbs([128, 512], F32, 2))
        ob = Rot(ph.sbs([128, 512], BF16, 2))
        ps = ph.psum(8)
        ps_i = Rot(ps[0:4])
        ps_acc = Rot([ps[4], ps[7]])
        ps_s = Rot(ps[0:3])
        po = ps[5]
        pd = ps[6]
        thr_of = {}

        def gen_index(qt):
            W = (qt + 1) * 128
            qs = slice(qt * 128, (qt + 1) * 128)
            dgt = dg[qt % 3]
            for h in range(8):
                k.actf(dgt[:, h, :], c.identf.v(), AF.Copy, scale=wi[:, qt, h:h + 1])
            scq = scb[qt % 3]
            for kg in range((W + 511) // 512):
                cols = min(512, W - kg * 512)
                acc = ps_acc.next()
                pend = []
                for h in range(8):
                    p = ps_i.next()
                    pr = slice(64 * (h % 2), 64 * (h % 2) + 64)
                    k.mm(p[:, 0:cols], qi[pr, h // 2, qs], ki[pr, kg * 512:kg * 512 + cols])
                    r = rl.next()
                    k.actf(r[:, 0:cols], p[:, 0:cols], AF.Relu)
                    pend.append((h, r))
                    if len(pend) > 2:
                        h0, r0 = pend.pop(0)
                        k.mm(acc[:, 0:cols], dgt[:, h0, :], r0[:, 0:cols], start=(h0 == 0), stop=(h0 == 7))
                for h0, r0 in pend:
                    k.mm(acc[:, 0:cols], dgt[:, h0, :], r0[:, 0:cols], start=(h0 == 0), stop=(h0 == 7))
                k.copy(scq[:, kg * 512:kg * 512 + cols], acc[:, 0:cols], eng=k.act)
                yield
            k.tt(scq[:, qt * 128:W], scq[:, qt * 128:W], c.negtri.v(), ALU.add, eng=k.pool)
            yield

        def gen_bisect(qt):
            W = (qt + 1) * 128
            scq = scb[qt % 3]
            st = stt_[qt % 3]
            if qt >= 2:
                k.call(k.dve, "tensor_reduce", out=st[:, 0:1], in_=scq[:, 0:W], axis=AX.X, op=ALU.max)
                yield
                k.call(k.dve, "tensor_reduce", out=st[:, 1:2], in_=scq[:, 0:qt * 128], axis=AX.X, op=ALU.min)
                k.ts(st[:, 1:2], st[:, 1:2], -1.0, ALU.add)
                k.tt(st[:, 2:3], st[:, 0:1], st[:, 1:2], ALU.subtract)
                k.ts(st[:, 8:8 + NBIS + 1], c.pow2[:, 0:NBIS + 1], st[:, 2:3], ALU.mult)
                k.ts(st[:, 32:32 + NBIS + 1], st[:, 8:8 + NBIS + 1], 2.0, ALU.mult)
                k.tt(st[:, 3:4], st[:, 1:2], st[:, 8:9], ALU.add)
                yield
                for it in range(NBIS):
                    k.call(k.dve, "tensor_scalar", out=junk_t[:, 0:W], in0=scq[:, 0:W], scalar1=st[:, 3:4], scalar2=None,
                           op0=ALU.is_gt, op1=ALU.add, accum_out=st[:, 4:5])
                    yield
                    k.stt(st[:, 5:6], st[:, 4:5], 255.5, st[:, 32 + it + 1:32 + it + 2], ALU.is_gt, ALU.mult)
                    k.stt(st[:, 3:4], st[:, 5:6], st[:, 8 + it + 1:8 + it + 2], st[:, 3:4], ALU.subtract, ALU.add)
                k.tt(st[:, 6:7], st[:, 3:4], st[:, 8 + NBIS:8 + NBIS + 1], ALU.subtract)
                thr = st[:, 6:7]
            else:
                thr = c.negbig.v()
            m = msk[qt % 3]
            k.ts(m[:, 0:W], scq[:, 0:W], thr, ALU.is_le, -30000.0, ALU.mult)
            yield

        def gen_attn(qt):
            qs = slice(qt * 128, (qt + 1) * 128)
            m = msk[qt % 3]
            nkt = qt + 1
            issued = {}

            def issue(kt):
                s = ps_s.next()
                sv = s.v().re("p (h q) -> p h q", h=4)
                k.mm(sv, ka[:, kt * 128:(kt + 1) * 128], qa[:, :, qs], start=True, stop=False)
                k.mm(s.v(), m[:, kt * 128:(kt + 1) * 128], c.i4.v(), start=False, stop=True)
                issued[kt] = s
            for kt in range(min(2, nkt)):
                issue(kt)
            for kt in range(nkt):
                if kt + 2 < nkt:
                    issue(kt + 2)
                s = issued.pop(kt)
                p_ = P.next()
                k.actf(p_.v(), s.v(), AF.Exp, scale=SCALE)
                k.mm(po.v(), va[:, kt, :], p_.v(), start=(kt == 0), stop=(kt == qt))
                k.mm(pd.v(), c.ones.v(), p_.v(), start=(kt == 0), stop=(kt == qt))
                yield
            r = rden.next()
            k.call(k.dve, "reciprocal", out=r.v(), in_=pd.v())
            o = ob.next()
            k.tt(o.v(), po.v(), r.v(), ALU.mult)
            dst = sc["oT"]
            k.dma(V(dst.ap[0:512, qs].rearrange("(h d) q -> d h q", d=128), dst.bufs),
                  o.v().re("p (h q) -> p h q", h=4))
            yield

        def chain(*gs):
            for g_ in gs:
                yield from g_
        HALF = (NBIS + 4) // 2
        bgen = {}
        for step in range(-3, NT):
            tasks = []
            lane1 = []
            if 0 <= step + 3 < NT:
                lane1.append(gen_index(step + 3))
            if 0 <= step < NT:
                lane1.append(gen_attn(step))
            if lane1:
                tasks.append([chain(*lane1), None])
            if 0 <= step + 2 < NT:
                bgen[step + 2] = gen_bisect(step + 2)
                tasks.append([bgen[step + 2], HALF])
            if 0 <= step + 1 < NT:
                tasks.append([bgen.pop(step + 1), None])
            while tasks:
                for tk_ in list(tasks):
                    try:
                        next(tk_[0])
                        if tk_[1] is not None:
                            tk_[1] -= 1
                            if tk_[1] <= 0:
                                tasks.remove(tk_)
                    except StopIteration:
                        tasks.remove(tk_)


OD = dict(cq=0, ckv=384, kr=640, z=704, xs=1728, B=2752, C=3264, dt=3776)
PI = float(np.pi)


def phase_rope_tables(k, c, sc, pos_d):
    with k.phase() as ph:
        pi_ = ph.sb([64, S], I32)
        ang = ph.sb([64, S], F32)
        u = ph.sb([64, S], F32)
        ni = ph.sb([64, S], I32)
        r = ph.sb([64, S], F32)
        k.dma(pi_.v(), V(pos_d.rearrange("(o s) -> o s", o=1).to_broadcast([64, S]), (Buf(pos_d),)))
        k.copy(ang.v(), pi_.v())
        k.ts(ang.v(), ang.v(), c.invf.v(), ALU.mult)
        for name, shift in (("sin", 0.0), ("cos", PI / 2)):
            k.ts(r.v(), ang.v(), shift, ALU.add)
            k.ts(u.v(), r.v(), 1.0 / (2 * PI), ALU.mult)
            k.copy(ni.v(), u.v())
            k.copy(u.v(), ni.v())
            k.stt(r.v(), u.v(), -2 * PI, r.v(), ALU.mult, ALU.add)
            k.ts(u.v(), r.v(), PI, ALU.is_gt, 2 * PI, ALU.mult)
            k.tt(r.v(), r.v(), u.v(), ALU.subtract)
            k.ts(u.v(), r.v(), -PI, ALU.is_lt, 2 * PI, ALU.mult)
            k.tt(r.v(), r.v(), u.v(), ALU.add)
            k.ts(r.v(), r.v(), 3.1415925, ALU.min, -3.1415925, ALU.max)
            k.actf(u.v(), r.v(), AF.Sin)
            k.dma(sc[name], u.v())


def col_load(k, dst, src_ap, n):
    k.dma(dst, V(src_ap.rearrange("(p o) -> p o", o=1), (Buf(src_ap),)))


ODDLVL = [9]


def phase_odd_proj(k, c, sc, x_d, g_row, w_d, cqn, ckvn, wuq_d, wukv_d, qn_c, kn_c, conv_w, conv_b):
    with k.phase() as ph:
        stg = Rot(ph.sbs([128, 1896], F32, 2))

        def loadw(w_ap, nk, ncols):
            wb_ = ph.split([128, nk, ncols], BF16, 1)
            wd = Buf(w_ap)
            for kk in range(nk):
                for c0 in range(0, ncols, 1896):
                    c1 = min(ncols, c0 + 1896)
                    s = stg.next()
                    k.dma(s[:, 0:c1 - c0], V(w_ap[kk * 128:(kk + 1) * 128, c0:c1], (wd,)))
                    k.copy(wb_[kk][:, c0:c1], s[:, 0:c1 - c0], eng=k.pool)
            return wb_
        wb = loadw(w_d, 8, 3792)
        wuq = loadw(wuq_d, 3, 768)
        wukv = loadw(wukv_d, 2, 1024)
        gt = ph.sb([128, D], F32)
        k.dma(gt.v(), V(g_row.to_broadcast([128, D]), (Buf(g_row),)))
        gc = ph.sb([128, 16], F32)
        for i in range(3):
            col_load(k, gc[:, i:i + 1], cqn[i * 128:(i + 1) * 128], 128)
        for i in range(2):
            col_load(k, gc[:, 3 + i:4 + i], ckvn[i * 128:(i + 1) * 128], 128)
        col_load(k, gc[:, 5:6], qn_c[0:128], 128)
        col_load(k, gc[0:64, 6:7], qn_c[128:192], 64)
        col_load(k, gc[:, 7:8], kn_c[0:128], 128)
        col_load(k, gc[0:64, 8:9], kn_c[128:192], 64)
        cw = ph.sb([128, 16, 4], F32)
        cb = ph.sb([128, 16], F32)
        cwd = Buf(conv_w)
        cbd = Buf(conv_b)
        for j in range(16):
            k.dma(cw[:, j, :], V(conv_w[:, j * 128:(j + 1) * 128].rearrange("w p -> p w"), (cwd,)),
                  allow_slow_non_contiguous=True)
            k.dma(cb[:, j:j + 1], V(conv_b[j * 128:(j + 1) * 128].rearrange("(p o) -> p o", o=1), (cbd,)))
        hal = ph.split([128, 16, 3], F32, 1)
        for j in range(16):
            k.memset(hal[j][:, :], 0.0, eng=k.pool)
        xin = Rot(ph.sbs([128, D], F32, 3))
        scr = ph.sb([128, D], BF16)
        stats = Rot(ph.sbs([128, 4], F32, 4))
        hn = Rot(ph.sbs([128, D], BF16, 2))
        hT = Rot(ph.sbs([128, 8, 512], BF16, 2))
        sq = Rot(ph.sbs([128, 512], BF16, 6))
        lnb = Rot(ph.sbs([128, 512], F32, 2))
        rstd = Rot(ph.sbs([128, 512], F32, 2))
        ob = Rot(ph.sbs([128, 512], BF16, 4))
        of = Rot(ph.sbs([128, 16], F32, 3))
        cqraw = ph.sb([128, 3, 512], F32)
        cqn_b = ph.sb([128, 3, 512], BF16)
        ckvraw = ph.sb([128, 2, 512], F32)
        ckvn_b = ph.sb([128, 2, 512], BF16)
        krraw = ph.sb([64, 512], F32)
        sqkr = ph.sb([64, 512], BF16)
        xr = Rot(ph.sbs([128, 515], F32, 2))
        acc = Rot(ph.sbs([128, 512], F32, 2))
        xact = Rot(ph.sbs([128, 512], BF16, 3))
        xtm = Rot(ph.sbs([128, 4, 128], BF16, 2))
        cs = ph.sb([64, 2, 512], F32)
        yb = Rot(ph.sbs([64, 512], BF16, 2))
        t1 = Rot(ph.sbs([64, 512], F32, 2))
        t2 = Rot(ph.sbs([64, 512], F32, 2))
        ps = ph.psum(8)
        psA = Rot(ps[0:4])
        psB = Rot(ps[4:6])
        ps_t = Rot(ps[6:8])
        xd = Buf(x_d)

        def grpnorm(raws, sqs, nch, hd, gcol0, outb):
            p2 = psB.next()
            for i in range(nch):
                k.mm(p2.v(), c.ones.v(), sqs[i].v(), start=(i == 0), stop=(i == nch - 1))
            l_, r_ = lnb.next(), rstd.next()
            k.actf(l_.v(), p2.v(), AF.Ln, scale=1.0 / hd, bias=c.epscol.v())
            k.actf(r_.v(), l_.v(), AF.Exp, scale=-0.5)
            for i in range(nch):
                k.stt(outb[:, i, :], raws[:, i, :], gc[:, gcol0 + i:gcol0 + i + 1], r_.v(), ALU.mult, ALU.mult)

        def rope(ybv, dst, tok):
            p = psB.next()
            k.mm(p[0:64, :], c.rotm.v(), ybv)
            a, b = t1.next(), t2.next()
            k.tt(a.v(), ybv, cs[:, 0, :], ALU.mult)
            k.tt(b.v(), p[0:64, :], cs[:, 1, :], ALU.mult)
            o = ob.next()
            k.tt(o[0:64, :], a.v(), b.v(), ALU.add)
            k.dma(V(dst.ap[:, tok], dst.bufs), o[0:64, :])

        def headnorm(pn, sq_r, gcol_n):
            sqn = sq.next()
            k.actf(sqn.v(), pn.v(), AF.Square)
            p2 = psB.next()
            k.mm(p2.v(), c.ones.v(), sqn.v(), start=True, stop=False)
            k.mm(p2.v(), c.ones[0:64, :], sq_r, start=False, stop=True)
            l_, r_ = lnb.next(), rstd.next()
            k.actf(l_.v(), p2.v(), AF.Ln, scale=1.0 / 192, bias=c.epscol.v())
            k.actf(r_.v(), l_.v(), AF.Exp, scale=-0.5)
            return r_

        for g in range(S // 512):
            if ODDLVL[0] < 1:
                break
            hTg = hT.next()
            xnorm_group(k, c, x_d, xd, g, gt, xin, hn, scr, stats, hTg, ps_t)
            tok = slice(g * 512, (g + 1) * 512)
            if ODDLVL[0] == 11:
                continue
            k.dma(cs[:, 0, :], V(sc["cos"].ap[:, tok], sc["cos"].bufs))
            k.dma(cs[:, 1, :], V(sc["sin"].ap[:, tok], sc["sin"].bufs))
            if ODDLVL[0] == 12:
                continue

            def proj(c0, M):
                p = psA.next()
                for kk in range(8):
                    k.mm(p[0:M, :], wb[kk][:, c0:c0 + M], hTg[:, kk, :], start=(kk == 0), stop=(kk == 7))
                return p
            sqs = []
            for i in range(3):
                p = proj(OD["cq"] + i * 128, 128)
                s_ = sq.next()
                k.actf(s_.v(), p.v(), AF.Square)
                k.copy(cqraw[:, i, :], p.v(), eng=k.dve)
                sqs.append(s_)
            if ODDLVL[0] == 13:
                continue
            grpnorm(cqraw, sqs, 3, 384, 0, cqn_b)
            if ODDLVL[0] == 14:
                continue
            sqs = []
            for i in range(2):
                p = proj(OD["ckv"] + i * 128, 128)
                s_ = sq.next()
                k.actf(s_.v(), p.v(), AF.Square)
                k.copy(ckvraw[:, i, :], p.v(), eng=k.dve)
                sqs.append(s_)
            grpnorm(ckvraw, sqs, 2, 256, 3, ckvn_b)
            p = proj(OD["kr"], 64)
            k.copy(krraw.v(), p[0:64, :], eng=k.dve)
            if ODDLVL[0] < 2:
                continue
            for h in range(4):
                pn = psA.next()
                for kc in range(3):
                    k.mm(pn.v(), wuq[kc][:, h * 192:h * 192 + 128], cqn_b[:, kc, :], start=(kc == 0), stop=(kc == 2))
                pr = psA.next()
                for kc in range(3):
                    k.mm(pr[0:64, :], wuq[kc][:, h * 192 + 128:h * 192 + 192], cqn_b[:, kc, :], start=(kc == 0), stop=(kc == 2))
                sqr = sq.next()
                k.actf(sqr[0:64, :], pr[0:64, :], AF.Square)
                r_ = headnorm(pn, sqr[0:64, :], 5)
                o = ob.next()
                k.stt(o.v(), pn.v(), gc[:, 5:6], r_.v(), ALU.mult, ALU.mult)
                k.dma(V(sc["qT"][h].ap[:, tok], sc["qT"][h].bufs), o.v())
                y_ = yb.next()
                k.stt(y_.v(), pr[0:64, :], gc[0:64, 6:7], r_[0:64, :], ALU.mult, ALU.mult)
                rope(y_.v(), sc["qr"][h], tok)
            k.actf(sqkr.v(), krraw.v(), AF.Square)
            for h in range(4):
                pn = psA.next()
                for kc in range(2):
                    k.mm(pn.v(), wukv[kc][:, h * 256:h * 256 + 128], ckvn_b[:, kc, :], start=(kc == 0), stop=(kc == 1))
                r_ = headnorm(pn, sqkr.v(), 7)
                o = ob.next()
                k.stt(o.v(), pn.v(), gc[:, 7:8], r_.v(), ALU.mult, ALU.mult)
                k.dma(V(sc["kT"][h].ap[:, tok], sc["kT"][h].bufs), o.v())
                y_ = yb.next()
                k.stt(y_.v(), krraw.v(), gc[0:64, 8:9], r_[0:64, :], ALU.mult, ALU.mult)
                rope(y_.v(), sc["kr"][h], tok)
            if ODDLVL[0] < 3:
                continue
            for j in range(16):
                p = proj(OD["xs"] + j * 128, 128)
                x_ = xr.next()
                k.copy(x_[:, 0:3], hal[j][:, :], eng=k.pool)
                k.copy(x_[:, 3:515], p.v(), eng=k.act)
                k.copy(hal[j][:, :], x_[:, 512:515], eng=k.pool)
                a_ = acc.next()
                k.ts(a_.v(), x_[:, 0:512], cw[:, j, 0:1], ALU.mult)
                for w in range(1, 4):
                    k.stt(a_.v(), x_[:, w:w + 512], cw[:, j, w:w + 1], a_.v(), ALU.mult, ALU.add)
                xa_ = xact.next()
                k.actf(xa_.v(), a_.v(), AF.Silu, bias=cb[:, j:j + 1])
                if j >= 8:
                    dst = sc["BCT"]
                    k.dma(V(dst.ap[(j - 8) * 128:(j - 7) * 128, tok], dst.bufs), xa_.v())
                if j < 12:
                    pt = ps_t.next()
                    ptb = pt.v().bitcast(BF16)
                    for t in range(4):
                        k.tr(ptb[:, t * 128:(t + 1) * 128], xa_[:, t * 128:(t + 1) * 128], c.ident.v())
                    xt_ = xtm.next()
                    k.copy(xt_.v(), ptb[:, 0:512].re("p (t c) -> p t c", t=4), eng=k.dve)
                    dst = sc["xsB"]
                    k.dma(V(dst.ap[tok, j * 128:(j + 1) * 128].rearrange("(t p) c -> p t c", p=128), dst.bufs), xt_.v())
            if ODDLVL[0] < 4:
                continue
            for t in range(4):
                r0 = g * 512 + t * 128
                tk = slice(t * 128, (t + 1) * 128)
                for hf in range(2):
                    p = psA.next()
                    for kk in range(8):
                        k.mm(p.v(), hTg[:, kk, tk], wb[kk][:, OD["z"] + hf * 512:OD["z"] + (hf + 1) * 512],
                             start=(kk == 0), stop=(kk == 7))
                    o = ob.next()
                    k.actf(o.v(), p.v(), AF.Silu)
                    k.dma(V(sc["zs"].ap[r0:r0 + 128, hf * 512:(hf + 1) * 512], sc["zs"].bufs), o.v())
                p = psA.next()
                for kk in range(8):
                    k.mm(p[:, 0:16], hTg[:, kk, tk], wb[kk][:, OD["dt"]:OD["dt"] + 16], start=(kk == 0), stop=(kk == 7))
                o2 = of.next()
                k.copy(o2[:, 0:16], p[:, 0:16], eng=k.dve)
                k.dma(V(sc["dt"].ap[r0:r0 + 128, :], sc["dt"].bufs), o2[:, 0:16])
                p = psA.next()
                for kc in range(2):
                    k.mm(p.v().re("p (h d) -> p h d", h=4), ckvn_b[:, kc, tk],
                         wukv[kc][:, :].re("p (h d) -> p h d", h=4)[:, :, 128:256], start=(kc == 0), stop=(kc == 1))
                o = ob.next()
                k.copy(o.v(), p.v(), eng=k.act)
                k.dma(V(sc["vb"].ap[r0:r0 + 128, :], sc["vb"].bufs), o.v())


def phase_mla(k, c, sc):
    SCALE = float(192.0 ** -0.5)
    with k.phase() as ph:
        qT = Rot(ph.sbs([128, S], BF16, 2))
        kT = Rot(ph.sbs([128, S], BF16, 2))
        qr = Rot(ph.sbs([64, S], BF16, 2))
        kr = Rot(ph.sbs([64, S], BF16, 2))
        vv = Rot(ph.sbs([128, 32, 128], BF16, 2))
        P_rot = Rot(ph.sbs([128, 512], BF16, 4))
        rden = Rot(ph.sbs([128, 512], F32, 2))
        ob = Rot(ph.sbs([128, 512], BF16, 2))
        ps = ph.psum(8)
        ps_s = Rot(ps[0:3])
        ps_o = Rot(ps[3:5])
        ps_d = Rot(ps[5:7])
        for h in range(4):
            q_, k_, qr_, kr_, v_ = qT.next(), kT.next(), qr.next(), kr.next(), vv.next()
            k.dma(q_.v(), sc["qT"][h])
            k.dma(k_.v(), sc["kT"][h])
            k.dma(qr_.v(), sc["qr"][h])
            k.dma(kr_.v(), sc["kr"][h])
            vsrc = sc["vb"]
            k.dma(v_.v(), V(vsrc.ap.rearrange("(t p) (h d) -> p t h d", p=128, h=4)[:, :, h, :], vsrc.bufs))
            for qg in range(8):
                def qk_fn(sv, kt, col0, q_=q_, k_=k_, qr_=qr_, kr_=kr_, qg=qg):
                    qs = slice(qg * 512 + col0, (qg + 1) * 512)
                    ks = slice(kt * 128, (kt + 1) * 128)
                    k.mm(sv, k_[:, ks], q_[:, qs], start=True, stop=False)
                    k.mm(sv, kr_[:, ks], qr_[:, qs], start=False, stop=True)

                def fin(po, pd, h=h, qg=qg):
                    r = rden.next()
                    k.call(k.dve, "reciprocal", out=r.v(), in_=pd.v())
                    o = ob.next()
                    k.tt(o.v(), po.v(), r.v(), ALU.mult)
                    dst = sc["oT"]
                    k.dma(V(dst.ap[h * 128:(h + 1) * 128, qg * 512:(qg + 1) * 512], dst.bufs), o.v())

                attn_core(k, c, qg, None, qk_fn, P_rot, ps_s, ps_o.next(), ps_d.next(),
                          lambda kt, v_=v_: v_[:, kt, :], SCALE, fin)


def bcl(v, n):
    p, h = v.ap.shape
    return V(v.ap.unsqueeze(2).to_broadcast([p, h, n]), v.bufs)


def phase_ssd(k, c, sc, dt_bias, a_log, d_skip, gate_norm):
    with k.phase() as ph:
        rep = ph.sb([128, 64], F32)
        for i, src in enumerate((dt_bias, a_log, d_skip)):
            k.dma(rep[:, i * 16:(i + 1) * 16], V(src.rearrange("(o h) -> o h", o=1).to_broadcast([128, 16]), (Buf(src),)))
        k.actf(rep[:, 16:32], rep[:, 16:32], AF.Exp)
        k.ts(rep[:, 16:32], rep[:, 16:32], -1.0, ALU.mult)
        one = ph.sb([128, 1], F32)
        k.memset(one.v(), 1.0)
        gg = ph.sb([128, D], F32)
        k.dma(gg.v(), V(gate_norm.rearrange("(o h) -> o h", o=1).to_broadcast([128, D]), (Buf(gate_norm),)))
        negtril = ph.sb([128, 128], BF16)
        tmpf = ph.sb([128, 128], F32)
        k.dma(tmpf.v(), c.negtril_d)
        k.copy(negtril.v(), tmpf.v())
        hst = ph.sb([128, D], F32)
        hstb = ph.sb([128, D], BF16)
        k.memset(hst.v(), 0.0)
        k.memset(hstb.v(), 0.0)
        xs = Rot(ph.sbs([128, D], BF16, 2))
        Btm = Rot(ph.sbs([128, 4, 128], BF16, 2))
        BT = Rot(ph.sbs([128, 4, 128], BF16, 2))
        CT = Rot(ph.sbs([128, 4, 128], BF16, 2))
        zs = Rot(ph.sbs([128, D], BF16, 2))
        dtr = Rot(ph.sbs([128, 16], F32, 2))
        sm = Rot(ph.sbs([128, 128], F32, 2))
        LT = Rot(ph.sbs([128, 16, 128], BF16, 2))
        MT = Rot(ph.sbs([128, 16, 128], BF16, 2))
        cbt = Rot(ph.sbs([128, 4, 128], BF16, 2))
        xdr = Rot(ph.sbs([128, D], BF16, 2))
        xddr = Rot(ph.sbs([128, D], BF16, 2))
        yr = Rot(ph.sbs([128, D], F32, 2))
        t2r = Rot(ph.sbs([128, D], F32, 2))
        junk = ph.sb([128, 256], BF16)
        ynr = Rot(ph.sbs([128, D], BF16, 2))
        oTt = Rot(ph.sbs([128, 8, 128], BF16, 2))
        ps = Rot(ph.psum(8))
        for ci in range(NT):
            r0 = ci * 128
            tok = slice(r0, r0 + 128)
            xs_, Btm_, BT_, CT_, zs_, dtr_ = xs.next(), Btm.next(), BT.next(), CT.next(), zs.next(), dtr.next()
            xsB = sc["xsB"]
            k.dma(xs_.v(), V(xsB.ap[tok, 0:1024], xsB.bufs))
            k.dma(Btm_.v().re("p g n -> p (g n)"), V(xsB.ap[tok, 1024:1536], xsB.bufs))
            bct = sc["BCT"]
            k.dma(BT_.v(), V(bct.ap[0:512, tok].rearrange("(g n) t -> n g t", n=128), bct.bufs))
            k.dma(CT_.v(), V(bct.ap[512:1024, tok].rearrange("(g n) t -> n g t", n=128), bct.bufs))
            k.dma(zs_.v(), V(sc["zs"].ap[tok, :], sc["zs"].bufs))
            k.dma(dtr_.v(), V(sc["dt"].ap[tok, :], sc["dt"].bufs))
            s_ = sm.next()
            k.tt(s_[:, 0:16], dtr_.v(), rep[:, 0:16], ALU.add)
            k.actf(s_[:, 0:16], s_[:, 0:16], AF.Exp)
            k.actf(s_[:, 0:16], s_[:, 0:16], AF.Ln, bias=one.v())
            k.tt(s_[:, 16:32], s_[:, 0:16], rep[:, 16:32], ALU.mult)
            pc = ps.next()
            k.mm(pc[:, 0:16], c.trif.v(), s_[:, 16:32])
            k.mm(pc[:, 16:32], c.onesf.v(), s_[:, 16:32])
            k.copy(s_[:, 32:48], pc[:, 0:16])
            k.ts(s_[:, 48:64], pc[:, 0:16], -1.0, ALU.mult)
            k.actf(s_[:, 64:80], pc[:, 0:16], AF.Exp)
            k.tt(s_[:, 112:128], pc[:, 16:32], s_[:, 32:48], ALU.subtract)
            k.actf(s_[:, 80:96], s_[:, 112:128], AF.Exp)
            k.actf(s_[:, 96:112], pc[:, 16:32], AF.Exp)
            LT_ = LT.next()
            for q4 in range(4):
                pl = ps.next()
                for i in range(4):
                    h = 4 * q4 + i
                    k.mm(pl[:, i * 128:(i + 1) * 128], s_[:, 16 + h:17 + h].bc([128, 128]), c.trif.v(), start=True, stop=False)
                    k.mm(pl[:, i * 128:(i + 1) * 128], c.ident.v(), negtril.v(), start=False, stop=True)
                    k.actf(LT_[:, h, :], pl[:, i * 128:(i + 1) * 128], AF.Exp, bias=s_[:, 48 + h:49 + h])
            pcb = ps.next()
            for g in range(4):
                k.mm(pcb[:, g * 128:(g + 1) * 128], BT_[:, g, :], CT_[:, g, :])
            cbt_ = cbt.next()
            k.copy(cbt_.v().re("p g n -> p (g n)"), pcb.v(), eng=k.act)
            MT_ = MT.next()
            for g in range(4):
                k.tt(MT_[:, 4 * g:4 * g + 4, :], LT_[:, 4 * g:4 * g + 4, :], bc1(cbt_[:, g, :], 4), ALU.mult)
            xd_, xdd_ = xdr.next(), xddr.next()
            k.tt(xd_.v().re("l (h p) -> l h p", p=64), xs_.v().re("l (h p) -> l h p", p=64), bcl(s_[:, 0:16], 64), ALU.mult)
            k.tt(xdd_.v().re("l (h p) -> l h p", p=64), xd_.v().re("l (h p) -> l h p", p=64), bcl(s_[:, 80:96], 64), ALU.mult)
            y_ = yr.next()
            t2_ = t2r.next()
            k.tt(t2_.v().re("l (h p) -> l h p", p=64), xs_.v().re("l (h p) -> l h p", p=64), bcl(rep[:, 32:48], 64), ALU.mult, eng=k.pool)
            for hf in range(2):
                hs = slice(hf * 512, (hf + 1) * 512)
                py = ps.next()
                for hh in range(8):
                    h = hf * 8 + hh
                    k.mm(py[:, hh * 64:(hh + 1) * 64], MT_[:, h, :], xd_[:, h * 64:(h + 1) * 64])
                po = ps.next()
                for gg_ in range(2):
                    g = hf * 2 + gg_
                    k.mm(po[:, gg_ * 256:(gg_ + 1) * 256], CT_[:, g, :], hstb[:, g * 256:(g + 1) * 256])
                k.tt(y_[:, hs].re("l (h p) -> l h p", p=64), po.v().re("l (h p) -> l h p", p=64),
                     bcl(s_[:, 64 + hf * 8:72 + hf * 8], 64), ALU.mult)
                k.tt(y_[:, hs], y_[:, hs], py.v(), ALU.add)
                k.tt(y_[:, hs], y_[:, hs], t2_[:, hs], ALU.add)
            for hf in range(2):
                hs = slice(hf * 512, (hf + 1) * 512)
                pst = ps.next()
                for gg_ in range(2):
                    g = hf * 2 + gg_
                    k.mm(pst[:, gg_ * 256:(gg_ + 1) * 256], Btm_[:, g, :], xdd_[:, g * 256:(g + 1) * 256])
                k.tt(hst[:, hs].re("l (h p) -> l h p", p=64), hst[:, hs].re("l (h p) -> l h p", p=64),
                     bcl(s_[:, 96 + hf * 8:104 + hf * 8], 64), ALU.mult, eng=k.pool)
                k.tt(hst[:, hs], hst[:, hs], pst.v(), ALU.add)
                k.copy(hstb[:, hs], hst[:, hs], eng=k.act)
            k.tt(y_.v(), y_.v(), zs_.v(), ALU.mult)
            for g in range(4):
                k.actf(junk.v(), y_[:, g * 256:(g + 1) * 256], AF.Square, accum_out=s_[:, 112 + g:113 + g])
            k.ts(s_[:, 116:120], s_[:, 112:116], 1.0 / 256, ALU.mult, EPS, ALU.add)
            k.tt(s_[:, 120:124], s_[:, 116:120], c.mhalf.v().bc([128, 4]), ALU.pow, eng=k.pool)
            yn_ = ynr.next()
            for g in range(4):
                gs = slice(g * 256, (g + 1) * 256)
                k.stt(yn_[:, gs], y_[:, gs], s_[:, 120 + g:121 + g], gg[:, gs], ALU.mult, ALU.mult)
            pt = ps.next()
            ptb = pt.v().bitcast(BF16)
            for j in range(8):
                k.tr(ptb[:, j * 128:(j + 1) * 128], yn_[:, j * 128:(j + 1) * 128], c.ident.v())
            o_ = oTt.next()
            k.copy(o_.v().re("p j t -> p (j t)"), ptb, eng=k.act)
            dst = sc["oT"]
            k.dma(V(dst.ap[512:1536, tok].rearrange("(j p) t -> p j t", p=128), dst.bufs), o_.v())


def const_arrays():
    cd = {}
    cd["ident"] = np.eye(128, dtype=np.float32)
    cd["pow2"] = np.tile((2.0 ** -(np.arange(32) + 1.0)).astype(np.float32)[None, :], (128, 1))
    invf = (10000.0 ** (-np.arange(32, dtype=np.float32) / 32)).astype(np.float32)
    cd["invf"] = np.concatenate([invf, invf])[:, None].astype(np.float32)
    rot = np.zeros((64, 64), np.float32)
    for m in range(32):
        rot[m + 32, m] = -1.0
        rot[m, m + 32] = 1.0
    cd["rotm"] = rot
    cd["negtril"] = (np.tril(np.ones((128, 128), np.float32), -1) * -30000.0).astype(np.float32)
    cd["tri"] = np.triu(np.ones((128, 128), np.float32))
    cd["negtri"] = (np.triu(np.ones((128, 128), np.float32), 1) * -1e30).astype(np.float32)
    return cd


INPUT_NAMES = ["x", "positions", "ev_norm", "ev_w_in", "ev_b_f", "ev_qn_a", "ev_kn_a", "ev_qn_b", "ev_kn_b",
               "ev_w_out", "od_norm", "od_w_in", "od_cq_norm", "od_ckv_norm", "od_w_uq", "od_w_ukv", "od_qn_c",
               "od_kn_c", "od_conv_w", "od_conv_b", "od_dt_bias", "od_a_log", "od_d_skip", "od_gate_norm",
               "od_w_out", "mlp_norm", "mlp_w1", "mlp_w2"]


def build(shapes, cfg):
    nc = bass.Bass("TRN2", target_bir_lowering=False)
    din = {}
    for name, (shp, dt) in shapes.items():
        bdt = I32 if np.dtype(dt) == np.int32 else F32
        din[name] = nc.dram_tensor(name, list(shp), bdt, kind="ExternalInput").ap()
    cds = const_arrays()
    cd = {n: nc.dram_tensor("c_" + n, list(a.shape), F32, kind="ExternalInput").ap() for n, a in cds.items()}
    y = nc.dram_tensor("y", [S, D], F32, kind="ExternalOutput").ap()
    xa = nc.dram_tensor("xa", [S, D], F32, kind="Internal").ap()
    xb = nc.dram_tensor("xb", [S, D], F32, kind="Internal").ap()

    def mk(name, shape, dt):
        return nc.dram_tensor(name, list(shape), dt, kind="Internal").ap()

    def dv(ap):
        return V(ap, (Buf(ap),))
    sc = {}
    t = mk("s_qT", [8, 128, S], BF16)
    sc["qT"] = [dv(t[h]) for h in range(8)]
    t = mk("s_kT", [5, 128, S], BF16)
    sc["kT"] = [dv(t[h]) for h in range(5)]
    t = mk("s_qiT", [4, 128, S], BF16)
    sc["qiT"] = [dv(t[h]) for h in range(4)]
    sc["kiT"] = dv(mk("s_kiT", [128, S], BF16))
    sc["va"] = dv(mk("s_va", [S, 128], BF16))
    sc["vb"] = dv(mk("s_vb", [S, 512], BF16))
    sc["wi"] = dv(mk("s_wi", [S, 8], F32))
    sc["fbT"] = dv(mk("s_fbT", [4, S], F32))
    sc["aug"] = dv(mk("s_aug", [4, 2, 6, S], BF16))
    sc["oT"] = dv(mk("s_oT", [1536, S], BF16))
    t = mk("s_qr", [4, 64, S], BF16)
    sc["qr"] = [dv(t[h]) for h in range(4)]
    t = mk("s_kr", [4, 64, S], BF16)
    sc["kr"] = [dv(t[h]) for h in range(4)]
    sc["cos"] = dv(mk("s_cos", [64, S], F32))
    sc["sin"] = dv(mk("s_sin", [64, S], F32))
    sc["BCT"] = dv(mk("s_BCT", [1024, S], BF16))
    sc["xsB"] = dv(mk("s_xsB", [S, 1536], BF16))
    sc["zs"] = dv(mk("s_zs", [S, 1024], BF16))
    sc["dt"] = dv(mk("s_dt", [S, 16], F32))
    dbg = cfg.get("debug", {})
    k = K(nc)
    with k.es:
        c = setup_consts(k, cd)
        cur = din["x"]
        ropedone = [False]
        steps = cfg["steps"]
        for si, (kind, l) in enumerate(steps):
            last = si == len(steps) - 1
            dst = y if last else (xa if cur is not xa else xb)
            if kind == "mlp":
                phase_mlp(k, c, cur, dst, din["mlp_norm"][l:l + 1, :], din["mlp_w1"][l], din["mlp_w2"][l])
            elif kind == "even":
                phase_even_proj(k, c, sc, cur, din["ev_norm"][l:l + 1, :], din["ev_w_in"][l], din["ev_qn_a"][l],
                                din["ev_kn_a"][l], din["ev_qn_b"][l], din["ev_kn_b"][l])
                if "nodsa" not in dbg:
                    phase_dsa(k, c, sc)
                if "nofox" not in dbg:
                    phase_fox(k, c, sc, din["ev_b_f"][l])
                phase_outproj(k, c, sc, cur, dst, din["ev_w_out"][l], 1024)
            elif kind == "odd":
                if "oddlvl" in dbg:
                    ODDLVL[0] = dbg["oddlvl"]
                if not ropedone[0] and "norope" not in dbg:
                    phase_rope_tables(k, c, sc, din["positions"])
                    ropedone[0] = True
                phase_odd_proj(k, c, sc, cur, din["od_norm"][l:l + 1, :], din["od_w_in"][l], din["od_cq_norm"][l],
                               din["od_ckv_norm"][l], din["od_w_uq"][l], din["od_w_ukv"][l], din["od_qn_c"][l],
                               din["od_kn_c"][l], din["od_conv_w"][l], din["od_conv_b"][l])
                if "nomla" not in dbg:
                    phase_mla(k, c, sc)
                if "nossd" not in dbg:
                    phase_ssd(k, c, sc, din["od_dt_bias"][l], din["od_a_log"][l], din["od_d_skip"][l],
                              din["od_gate_norm"][l])
                phase_outproj(k, c, sc, cur, dst, din["od_w_out"][l], 1536)
            else:
                raise ValueError(kind)
            cur = dst
        k.barrier()
    return nc, cds


FULL_CFG = {"steps": [("even", 0), ("mlp", 0), ("odd", 0), ("mlp", 1), ("even", 1), ("mlp", 2), ("odd", 1), ("mlp", 3)]}


def run(inputs, cfg, cores=N_CORES):
    per_core = []
    for b in range(cores):
        m = {}
        for n in INPUT_NAMES:
            a = np.asarray(inputs[n])
            if n in ("x", "positions"):
                a = a[b]
            m[n] = np.ascontiguousarray(a)
        per_core.append(m)
    shapes = {n: (per_core[0][n].shape, per_core[0][n].dtype) for n in INPUT_NAMES}
    nc, cds = build(shapes, cfg)
    for m in per_core:
        for n, a in cds.items():
            m["c_" + n] = a
    res = run_bass_kernel_spmd(nc, per_core, core_ids=list(range(cores)))
    return np.stack([np.asarray(r["y"]) for r in res.results], axis=0)


def kernel(**inputs):
    out = run(inputs, FULL_CFG)
    return out.astype(np.float32)
```

```python
import contextlib
import numpy as np
import ml_dtypes
import concourse.bass as bass
import concourse.mybir as mybir
from concourse.bass_utils import run_bass_kernel_spmd

F32 = mybir.dt.float32
BF16 = mybir.dt.bfloat16
I32 = mybir.dt.int32
AF = mybir.ActivationFunctionType
ALU = mybir.AluOpType
AX = mybir.AxisListType

S = 4096
D = 1024
NT = S // 128
DFF = 4096
EPS = 1e-6
N_CORES = 8
WRITE_KEYS = ("out", "accum_out", "ap")


class V:
    def __init__(self, ap, bufs):
        self.ap = ap
        self.bufs = bufs

    def __getitem__(self, idx):
        return V(self.ap[idx], self.bufs)

    def bc(self, shape):
        return V(self.ap.to_broadcast(shape), self.bufs)

    def re(self, pat, **kw):
        return V(self.ap.rearrange(pat, **kw), self.bufs)

    def bitcast(self, dt):
        return V(self.ap.bitcast(dt), self.bufs)


class Buf:
    def __init__(self, ap):
        self.ap = ap
        self.w = None
        self.r = {}
        self.excl = False

    def __getitem__(self, idx):
        return V(self.ap[idx], (self,))

    def v(self):
        return V(self.ap, (self,))


def multi(*views):
    bufs = []
    for v in views:
        bufs.extend(v.bufs)
    return V(views[0].ap, tuple(bufs))


class Eng:
    def __init__(self, k, name, raw, self_sync):
        self.name = name
        self.raw = raw
        self.sem = k.new_sem("e_" + name)
        self.cnt = 0
        self.seen = {}
        self.self_sync = self_sync


class Slot:
    def __init__(self, k, key):
        self.key = key
        self.sem = k.new_sem(key)
        self.val = 0


class K:
    NSLOT = 12

    def __init__(self, nc):
        self.nc = nc
        self.es = contextlib.ExitStack()
        self.pe = Eng(self, "pe", nc.tensor, False)
        self.act = Eng(self, "act", nc.scalar, True)
        self.dve = Eng(self, "dve", nc.vector, True)
        self.pool = Eng(self, "pool", nc.gpsimd, True)
        self.sp = Eng(self, "sp", nc.sync, False)
        self.engs = [self.pe, self.act, self.dve, self.pool, self.sp]
        self.queues = {}
        for q in (self.sp, self.pool):
            self.queues[q.name] = [Slot(self, "d_%s_%d" % (q.name, i)) for i in range(self.NSLOT)]
        self.qnext = {q: 0 for q in self.queues}
        self.nph = 0

    def new_sem(self, name):
        return self.es.enter_context(self.nc.semaphore(name))

    def _wait(self, eng, tok):
        key, sem, val = tok
        if key == eng.name and not eng.self_sync:
            return
        if eng.seen.get(key, 0) >= val:
            return
        eng.raw.wait_ge(sem, val)
        eng.seen[key] = val

    def _deps(self, eng, reads, writes):
        for v in reads:
            for b in v.bufs:
                if b.w is not None:
                    self._wait(eng, b.w)
                if b.excl:
                    for t in b.r.values():
                        if t[0] != eng.name:
                            self._wait(eng, t)
        for v in writes:
            for b in v.bufs:
                if b.w is not None:
                    self._wait(eng, b.w)
                for t in b.r.values():
                    self._wait(eng, t)

    def _mark(self, tok, reads, writes):
        for v in reads:
            for b in v.bufs:
                b.r[tok[0]] = tok
        for v in writes:
            for b in v.bufs:
                b.w = tok
                b.r = {}

    def call(self, eng, method, **kw):
        reads, writes, args = [], [], {}
        for key, v in kw.items():
            if isinstance(v, V):
                (writes if key in WRITE_KEYS else reads).append(v)
                args[key] = v.ap
            else:
                args[key] = v
        self._deps(eng, reads, writes)
        inst = getattr(eng.raw, method)(**args)
        eng.cnt += 1
        inst.then_inc(eng.sem, 1)
        self._mark((eng.name, eng.sem, eng.cnt), reads, writes)
        return inst

    def dma(self, out, in_, q=None, **kw):
        q = q or self.sp
        slots = self.queues[q.name]
        slot = slots[self.qnext[q.name] % len(slots)]
        self.qnext[q.name] += 1
        if slot.val > 0:
            self._wait(q, (slot.key, slot.sem, slot.val))
        self._deps(q, [in_], [out])
        slot.val += 16
        q.raw.dma_start(out=out.ap, in_=in_.ap, **kw).then_inc(slot.sem, 16)
        self._mark((slot.key, slot.sem, slot.val), [in_], [out])

    def barrier(self):
        toks = [(e.name, e.sem, e.cnt) for e in self.engs if e.cnt > 0]
        for sl in self.queues.values():
            toks += [(s.key, s.sem, s.val) for s in sl if s.val > 0]
        for e in self.engs:
            for t in toks:
                self._wait(e, t)

    def mm(self, out, lhsT, rhs, start=True, stop=True):
        return self.call(self.pe, "matmul", out=out, lhsT=lhsT, rhs=rhs, start=start, stop=stop)

    def tr(self, out, in_, ident):
        return self.call(self.pe, "transpose", out=out, in_=in_, identity=ident)

    def actf(self, out, in_, func, **kw):
        return self.call(self.act, "activation", out=out, in_=in_, func=func, **kw)

    def tt(self, out, in0, in1, op, eng=None):
        return self.call(eng or self.dve, "tensor_tensor", out=out, in0=in0, in1=in1, op=op)

    def ts(self, out, in0, s1, op0, s2=None, op1=None, eng=None, **kw):
        if op1 is None:
            return self.call(eng or self.dve, "tensor_scalar", out=out, in0=in0, scalar1=s1, scalar2=None,
                             op0=op0, **kw)
        return self.call(eng or self.dve, "tensor_scalar", out=out, in0=in0, scalar1=s1, scalar2=s2,
                         op0=op0, op1=op1, **kw)

    def stt(self, out, in0, scalar, in1, op0, op1, **kw):
        return self.call(self.dve, "scalar_tensor_tensor", out=out, in0=in0, scalar=scalar, in1=in1,
                         op0=op0, op1=op1, **kw)

    def copy(self, out, in_, eng=None):
        eng = eng or self.dve
        if eng is self.act:
            return self.call(eng, "copy", out=out, in_=in_)
        return self.call(eng, "tensor_copy", out=out, in_=in_)

    def memset(self, ap, val, eng=None):
        return self.call(eng or self.dve, "memset", ap=ap, constant=val)

    @contextlib.contextmanager
    def phase(self):
        self.barrier()
        self.nph += 1
        ph = Phase(self, "p%d" % self.nph)
        with ph.es:
            yield ph
            self.barrier()


class Phase:
    def __init__(self, k, name):
        self.k = k
        self.name = name
        self.es = contextlib.ExitStack()
        self.n = 0

    def sbt(self, shape, dtype):
        self.n += 1
        return self.es.enter_context(self.k.nc.sbuf_tensor("%s_s%d" % (self.name, self.n), list(shape), dtype))

    def sb(self, shape, dtype):
        t = self.sbt(shape, dtype)
        return Buf(t[tuple(slice(None) for _ in shape)])

    def sbs(self, shape, dtype, n):
        return [self.sb(shape, dtype) for _ in range(n)]

    def split(self, shape, dtype, axis, step=1):
        t = self.sbt(shape, dtype)
        out = []
        for i in range(0, shape[axis], step):
            idx = [slice(None)] * len(shape)
            idx[axis] = slice(i, i + step) if step > 1 else i
            out.append(Buf(t[tuple(idx)]))
        return out

    def psum(self, n=8):
        out = []
        for i in range(n):
            self.n += 1
            t = self.es.enter_context(self.k.nc.psum_tensor("%s_ps%d" % (self.name, self.n), [128, 512], F32))
            b = Buf(t[:, :])
            b.excl = True
            out.append(b)
        return out


class Rot:
    def __init__(self, items):
        self.items = items
        self.i = 0

    def next(self):
        it = self.items[self.i % len(self.items)]
        self.i += 1
        return it


def dram_buf(ap):
    return Buf(ap)


class Ctx:
    pass


def setup_consts(k, cd):
    nc = k.nc
    c = Ctx()
    es = k.es
    def sb(name, shape, dt):
        t = es.enter_context(nc.sbuf_tensor(name, list(shape), dt))
        return Buf(t[tuple(slice(None) for _ in shape)])
    c.ident = sb("k_ident", [128, 128], BF16)
    c.mhalf = sb("k_mhalf", [128, 1], F32)
    tmp = sb("k_tmp", [128, 128], F32)
    k.dma(tmp.v(), V(cd["ident"], (Buf(cd["ident"]),)))
    k.copy(c.ident.v(), tmp.v(), eng=k.dve)
    k.memset(c.mhalf.v(), -0.5, eng=k.dve)
    c.epscol = sb("k_epscol", [128, 1], F32)
    k.memset(c.epscol.v(), EPS, eng=k.dve)
    c.identf = sb("k_identf", [128, 128], F32)
    k.copy(c.identf.v(), tmp.v(), eng=k.dve)
    c.i4 = sb("k_i4", [128, 512], BF16)
    for h_ in range(4):
        k.copy(c.i4[:, h_ * 128:(h_ + 1) * 128], tmp.v(), eng=k.dve)
    c.ones = sb("k_ones", [128, 128], BF16)
    k.memset(c.ones.v(), 1.0, eng=k.dve)
    c.onesf = sb("k_onesf", [128, 128], F32)
    k.memset(c.onesf.v(), 1.0, eng=k.dve)
    c.tri = sb("k_tri", [128, 128], BF16)
    k.dma(tmp.v(), V(cd["tri"], (Buf(cd["tri"]),)))
    k.copy(c.tri.v(), tmp.v(), eng=k.dve)
    c.trif = sb("k_trif", [128, 128], F32)
    k.copy(c.trif.v(), tmp.v(), eng=k.dve)
    c.invf = sb("k_invf", [64, 1], F32)
    k.dma(c.invf.v(), V(cd["invf"], (Buf(cd["invf"]),)))
    c.rotm = sb("k_rotm", [64, 64], BF16)
    k.dma(tmp[0:64, 0:64], V(cd["rotm"], (Buf(cd["rotm"]),)))
    k.copy(c.rotm.v(), tmp[0:64, 0:64], eng=k.dve)
    c.negtril_d = V(cd["negtril"], (Buf(cd["negtril"]),))
    c.negbig = sb("k_negbig", [128, 1], F32)
    k.memset(c.negbig.v(), -1e29, eng=k.dve)
    c.pow2 = sb("k_pow2", [128, 32], F32)
    k.dma(c.pow2.v(), V(cd["pow2"], (Buf(cd["pow2"]),)))
    c.negtri = sb("k_negtri", [128, 128], F32)
    k.dma(c.negtri.v(), V(cd["negtri"], (Buf(cd["negtri"]),)))
    return c


def rmsnorm_tile(k, c, ph, xt, gt, hn, scr, st):
    k.actf(scr.v(), xt.v(), AF.Square, accum_out=st[:, 0:1])
    k.ts(st[:, 1:2], st[:, 0:1], 1.0 / D, ALU.mult, EPS, ALU.add)
    k.tt(st[:, 2:3], st[:, 1:2], c.mhalf.v(), ALU.pow, eng=k.pool)
    k.stt(hn.v(), xt.v(), st[:, 2:3], gt.v(), ALU.mult, ALU.mult)


def phase_mlp(k, c, x_d, xo_d, g_row, w1_d, w2_d):
    G = 256
    NG = S // G
    with k.phase() as ph:
        w1b = ph.split([128, 8, DFF], BF16, 1)
        w2b = ph.split([128, 32, D], BF16, 1)
        stg = Rot(ph.sbs([128, 2048], F32, 3))
        gt = ph.sb([128, D], F32)
        xin = Rot(ph.sbs([128, D], F32, 3))
        scr = ph.sb([128, D], BF16)
        stats = Rot(ph.sbs([128, 4], F32, 4))
        hn = Rot(ph.sbs([128, D], BF16, 2))
        hT = Rot(ph.sbs([128, 8, G], BF16, 2))
        rl = Rot(ph.sbs([128, G], BF16, 4))
        hid = Rot(ph.sbs([128, G], BF16, 5))
        xres = Rot(ph.sbs([128, D], F32, 3))
        ps = ph.psum(8)
        ps_y = ps[0:4]
        ps_h = Rot(ps[4:7])
        ps_t = Rot(ps[7:8])
        xd = Buf(x_d)
        xod = Buf(xo_d)
        w1d = Buf(w1_d)
        w2d = Buf(w2_d)
        k.dma(gt.v(), V(g_row.to_broadcast([128, D]), (Buf(g_row),)))
        for kk in range(8):
            for hf in range(2):
                s = stg.next()
                k.dma(s.v(), V(w1_d[kk * 128:(kk + 1) * 128, hf * 2048:(hf + 1) * 2048], (w1d,)))
                k.copy(w1b[kk][:, hf * 2048:(hf + 1) * 2048], s.v(), eng=(k.pool, k.dve, k.act)[(kk * 2 + hf) % 3])
        w2v = w2_d.rearrange("(j p) n -> p j n", p=128)
        for jj in range(0, 32, 2):
            s = stg.next()
            k.dma(s.v().re("p (j n) -> p j n", j=2), V(w2v[:, jj:jj + 2, :], (w2d,)))
            eng = (k.pool, k.dve, k.act)[(jj // 2) % 3]
            k.copy(w2b[jj][:, :], s[:, 0:1024], eng=eng)
            k.copy(w2b[jj + 1][:, :], s[:, 1024:2048], eng=eng)

        def norm_a(g):
            res = []
            for t in range(G // 128):
                xt = xin.next()
                r0 = g * G + t * 128
                k.dma(xt.v(), V(x_d[r0:r0 + 128, :], (xd,)))
                h = hn.next()
                rmsnorm_tile(k, c, ph, xt, gt, h, scr, stats.next())
                res.append(h)
            return res

        def norm_b(g, hs):
            hTg = hT.next()
            for t, h in enumerate(hs):
                pt = ps_t.next()
                ptb = pt.v().bitcast(BF16)
                for kk in range(8):
                    k.tr(ptb[:, kk * 128:(kk + 1) * 128], h[:, kk * 128:(kk + 1) * 128], c.ident.v())
                k.copy(hTg[:, :, t * 128:(t + 1) * 128], ptb.re("p (k t) -> p k t", k=8), eng=k.act)
            return hTg

        hs = norm_a(0)
        hT_cur = norm_b(0, hs)
        for g in range(NG):
            hs_next = None
            hT_next = None
            pend = []

            def w2(pj, phd, last):
                for t in range(2):
                    for cc in range(2):
                        k.mm(ps_y[t * 2 + cc].v(), phd[:, t * 128:(t + 1) * 128],
                             w2b[pj][:, cc * 512:(cc + 1) * 512], start=(pj == 0), stop=last)
            for j in range(32):
                ph_ = ps_h.next()
                for kk in range(8):
                    k.mm(ph_[:, 0:G], w1b[kk][:, j * 128:(j + 1) * 128], hT_cur[:, kk, :],
                         start=(kk == 0), stop=(kk == 7))
                r = rl.next()
                k.actf(r.v(), ph_[:, 0:G], AF.Relu)
                hd = hid.next()
                k.tt(hd.v(), r.v(), r.v(), ALU.mult)
                pend.append((j, hd))
                if len(pend) > 2:
                    pj, phd = pend.pop(0)
                    w2(pj, phd, False)
                if j == 4 and g + 1 < NG:
                    hs_next = norm_a(g + 1)
                if j == 20 and g + 1 < NG:
                    hT_next = norm_b(g + 1, hs_next)
            while pend:
                pj, phd = pend.pop(0)
                w2(pj, phd, pj == 31)
            for t in range(2):
                r0 = g * G + t * 128
                xr = xres.next()
                k.dma(xr.v(), V(x_d[r0:r0 + 128, :], (xd,)))
                for cc in range(2):
                    k.tt(xr[:, cc * 512:(cc + 1) * 512], ps_y[t * 2 + cc].v(), xr[:, cc * 512:(cc + 1) * 512], ALU.add)
                k.dma(V(xo_d[r0:r0 + 128, :], (xod,)), xr.v())
            hT_cur = hT_next


def xnorm_group(k, c, x_d, xd, g, gt, xin, hn, scr, stats, hTg, ps_t, ntile=4):
    G = ntile * 128
    for t in range(ntile):
        xt = xin.next()
        r0 = g * G + t * 128
        k.dma(xt.v(), V(x_d[r0:r0 + 128, :], (xd,)))
        h = hn.next()
        rmsnorm_tile(k, c, None, xt, gt, h, scr, stats.next())
        pt = ps_t.next()
        ptb = pt.v().bitcast(BF16)
        for kk in range(8):
            k.tr(ptb[:, kk * 128:(kk + 1) * 128], h[:, kk * 128:(kk + 1) * 128], c.ident.v())
        k.copy(hTg[:, :, t * 128:(t + 1) * 128], ptb.re("p (k t) -> p k t", k=8), eng=k.act)


def run_rr(gens):
    gens = list(gens)
    while gens:
        for g_ in list(gens):
            try:
                next(g_)
            except StopIteration:
                gens.remove(g_)


def xnorm_gen(k, c, x_d, xd, g, gt, xin, hn, scr, stats, hTg, ps_t, ntile=4):
    G = ntile * 128
    for t in range(ntile):
        xt = xin.next()
        r0 = g * G + t * 128
        k.dma(xt.v(), V(x_d[r0:r0 + 128, :], (xd,)))
        h = hn.next()
        rmsnorm_tile(k, c, None, xt, gt, h, scr, stats.next())
        yield
        pt = ps_t.next()
        ptb = pt.v().bitcast(BF16)
        for kk in range(8):
            k.tr(ptb[:, kk * 128:(kk + 1) * 128], h[:, kk * 128:(kk + 1) * 128], c.ident.v())
        yield
        k.copy(hTg[:, :, t * 128:(t + 1) * 128], ptb.re("p (k t) -> p k t", k=8), eng=k.act)
        yield


def load_w_bf16(k, ph, w_d, nk, ncols, stg_cols=None):
    wb = ph.split([128, nk, ncols], BF16, 1)
    stg = Rot(ph.sbs([128, ncols], F32, 2))
    wd = Buf(w_d)
    rows = w_d.shape[0]
    for kk in range(nk):
        s = stg.next()
        r = min(128, rows - kk * 128)
        k.dma(s[0:r, :], V(w_d[kk * 128:kk * 128 + r, :], (wd,)))
        k.copy(wb[kk][0:r, :], s[0:r, :], eng=(k.pool, k.dve, k.act)[kk % 3])
    return wb


def fm_qknorm(k, c, ps, M, gcol, outb, sq, lnb, rstd, ps2, hd):
    N = ps.ap.shape[-1]
    k.actf(sq[0:M, 0:N], ps, AF.Square)
    k.mm(ps2[0:M, 0:N], c.ones[0:M, 0:M], sq[0:M, 0:N])
    k.actf(lnb[0:M, 0:N], ps2[0:M, 0:N], AF.Ln, scale=1.0 / hd, bias=c.epscol[0:M, :])
    k.actf(rstd[0:M, 0:N], lnb[0:M, 0:N], AF.Exp, scale=-0.5)
    k.stt(outb, ps, gcol, rstd[0:M, 0:N], ALU.mult, ALU.mult)


EV = dict(qa=0, ka=512, va=640, qi=768, ki=1280, wi=1344, qb=1352, kb=1864, vb=2376, fb=2888)


def phase_even_proj(k, c, sc, x_d, g_row, w_d, qn_a, kn_a, qn_b, kn_b):
    with k.phase() as ph:
        wb = load_w_bf16(k, ph, w_d, 8, 2892)
        wkd = ph.sb([128, 8, 128], BF16)
        for kk in range(8):
            k.copy(wkd[:, kk, 0:64], wb[kk][:, 1280:1344], eng=k.pool)
            k.copy(wkd[:, kk, 64:128], wb[kk][:, 1280:1344], eng=k.pool)
        gt = ph.sb([128, D], F32)
        k.dma(gt.v(), V(g_row.to_broadcast([128, D]), (Buf(g_row),)))
        gcol = ph.sb([128, 4], F32)
        for i, gn in enumerate((qn_a, kn_a, qn_b, kn_b)):
            k.dma(gcol[:, i:i + 1], V(gn.rearrange("(p o) -> p o", o=1), (Buf(gn),)))
        xin = Rot(ph.sbs([128, D], F32, 3))
        scr = ph.sb([128, D], BF16)
        stats = Rot(ph.sbs([128, 4], F32, 4))
        hn = Rot(ph.sbs([128, D], BF16, 2))
        hT = Rot(ph.sbs([128, 8, 512], BF16, 2))
        sq = Rot(ph.sbs([128, 512], BF16, 2))
        lnb = Rot(ph.sbs([128, 512], F32, 2))
        rstd = Rot(ph.sbs([128, 512], F32, 2))
        ob = Rot(ph.sbs([128, 512], BF16, 4))
        of = Rot(ph.sbs([128, 512], F32, 2))
        ps = ph.psum(8)
        psA = Rot(ps[0:3])
        psB = Rot(ps[3:5])
        ps_t = Rot(ps[5:7])
        psC = Rot(ps[7:8])
        xd = Buf(x_d)
        chunks = []
        for h in range(4):
            chunks.append((wb, EV["qa"] + h * 128, 128, 0, sc["qT"][h]))
        chunks.append((wb, EV["ka"], 128, 1, sc["kT"][0]))
        for cc in range(4):
            chunks.append((wb, EV["qi"] + cc * 128, 128, None, sc["qiT"][cc]))
        chunks.append((None, 0, 128, None, sc["kiT"]))
        for h in range(4):
            chunks.append((wb, EV["qb"] + h * 128, 128, 2, sc["qT"][4 + h]))
        for h in range(4):
            chunks.append((wb, EV["kb"] + h * 128, 128, 3, sc["kT"][1 + h]))
        chunks.append((wb, EV["fb"], 4, "f32", sc["fbT"]))
        ci = 0
        for g in range(S // 512):
            hTg = hT.next()
            xnorm_group(k, c, x_d, xd, g, gt, xin, hn, scr, stats, hTg, ps_t)
            tok = slice(g * 512, (g + 1) * 512)
            for (wsrc, c0, M, nrm, dst) in chunks:
                p = psA.next()
                for kk in range(8):
                    lhsT = wkd[:, kk, :] if wsrc is None else wb[kk][:, c0:c0 + M]
                    k.mm(p[0:M, :], lhsT, hTg[:, kk, :], start=(kk == 0), stop=(kk == 7))
                if nrm == "f32":
                    o = of.next()
                    k.copy(o[0:M, :], p[0:M, :], eng=k.dve)
                    k.dma(V(dst.ap[0:M, tok], dst.bufs), o[0:M, :])
                    continue
                o = ob.next()
                if nrm is None:
                    ci += 1
                    k.copy(o[0:M, :], p[0:M, :], eng=(k.act if ci % 2 else k.dve))
                else:
                    fm_qknorm(k, c, p[0:M, :], M, gcol[:, nrm:nrm + 1], o[0:M, :], sq.next(), lnb.next(),
                              rstd.next(), psB.next(), 128)
                k.dma(V(dst.ap[0:M, tok], dst.bufs), o[0:M, :])
            for t in range(4):
                r0 = g * 512 + t * 128
                tk = slice(t * 128, (t + 1) * 128)
                p = psA.next()
                for kk in range(8):
                    k.mm(p[:, :], hTg[:, kk, tk], wb[kk][:, EV["vb"]:EV["vb"] + 512], start=(kk == 0), stop=(kk == 7))
                o = ob.next()
                k.copy(o[:, :], p[:, :], eng=k.act)
                k.dma(V(sc["vb"].ap[r0:r0 + 128, :], sc["vb"].bufs), o[:, :])
                p = psC.next()
                for kk in range(8):
                    k.mm(p[:, 0:128], hTg[:, kk, tk], wb[kk][:, EV["va"]:EV["va"] + 128], start=(kk == 0), stop=(kk == 7))
                for kk in range(8):
                    k.mm(p[:, 128:136], hTg[:, kk, tk], wb[kk][:, EV["wi"]:EV["wi"] + 8], start=(kk == 0), stop=(kk == 7))
                o = ob.next()
                k.copy(o[:, 0:128], p[:, 0:128], eng=k.dve)
                k.dma(V(sc["va"].ap[r0:r0 + 128, :], sc["va"].bufs), o[:, 0:128])
                o2 = of.next()
                k.copy(o2[:, 0:8], p[:, 128:136], eng=k.dve)
                k.dma(V(sc["wi"].ap[r0:r0 + 128, :], sc["wi"].bufs), o2[:, 0:8])


def split3(k, ph, src, n, outs):
    k.copy(outs[0][0:n, :], src[0:n, :])
    k.tt(src[0:n, :], src[0:n, :], outs[0][0:n, :], ALU.subtract)
    k.copy(outs[1][0:n, :], src[0:n, :])
    k.tt(src[0:n, :], src[0:n, :], outs[1][0:n, :], ALU.subtract)
    k.copy(outs[2][0:n, :], src[0:n, :])


def attn_core(k, c, qg, nkt_fn, qk_fn, P_rot, ps_s, ps_o, ps_d, v_fn, scale, finalize, la=2):
    nkt = 4 * qg + 4
    po = ps_o
    pd = ps_d
    issued = []

    def issue(kt):
        diag = kt >= 4 * qg
        col0 = (kt - 4 * qg) * 128 if diag else 0
        s = ps_s.next()
        qk_fn(s[:, col0:512], kt, col0)
        issued.append((s, col0, diag))
    for kt in range(min(la, nkt)):
        issue(kt)
    for kt in range(nkt):
        if kt + la < nkt:
            issue(kt + la)
        s, col0, diag = issued[kt]
        P = P_rot.next()
        k.actf(P[:, col0:512], s[:, col0:512], AF.Exp, scale=scale)
        if diag:
            k.tt(P[:, col0:col0 + 128], P[:, col0:col0 + 128], c.tri.v(), ALU.mult, eng=k.pool)
        k.mm(po[:, col0:512], v_fn(kt), P[:, col0:512], start=(kt == 0), stop=(kt == nkt - 1))
        k.mm(pd[:, col0:512], c.ones.v(), P[:, col0:512], start=(kt == 0), stop=(kt == nkt - 1))
    finalize(po, pd)


def phase_fox(k, c, sc, b_f):
    SQ = float(np.sqrt(128.0))
    with k.phase() as ph:
        with contextlib.ExitStack() as es2:
            ph2 = Phase(k, ph.name + "a")
            es2.enter_context(ph2.es)
            f0 = ph2.sb([4, S], F32)
            f1 = ph2.sb([4, S], F32)
            bcol = ph2.sb([4, 2], F32)
            one4 = ph2.sb([4, 1], F32)
            pcs = ph2.sbs([4, S], BF16, 3)
            ones4 = ph2.sb([4, S], BF16)
            k.dma(f0.v(), sc["fbT"])
            k.dma(bcol[:, 0:1], V(b_f.rearrange("(p o) -> p o", o=1), (Buf(b_f),)))
            k.ts(bcol[:, 1:2], bcol[:, 0:1], -1.0, ALU.mult)
            k.memset(one4.v(), 1.0)
            k.memset(ones4.v(), 1.0)
            k.actf(f0.v(), f0.v(), AF.Exp, scale=-1.0, bias=bcol[:, 1:2])
            k.actf(f0.v(), f0.v(), AF.Ln, bias=one4.v())
            k.ts(f0.v(), f0.v(), -SQ, ALU.mult)
            k.call(k.dve, "tensor_tensor_scan", out=f1.v(), data0=one4.v().bc([4, S]), data1=f0.v(),
                   initial=0.0, op0=ALU.mult, op1=ALU.add)
            k.copy(f0.v().re("p (b t) -> p b t", t=128), f1.v().re("p (b t) -> p b t", t=128)[:, :, 127:128].bc([4, 32, 128]))
            aug = sc["aug"]
            split3(k, ph2, f0, 4, pcs)
            for p_ in range(3):
                k.dma(V(aug.ap[:, 0, p_, :], aug.bufs), pcs[p_].v())
            k.ts(f1.v(), f1.v(), -1.0, ALU.mult)
            split3(k, ph2, f1, 4, pcs)
            for p_ in range(3):
                k.dma(V(aug.ap[:, 1, 3 + p_, :], aug.bufs), pcs[p_].v())
                k.dma(V(aug.ap[:, 1, p_, :], aug.bufs), ones4.v())
                k.dma(V(aug.ap[:, 0, 3 + p_, :], aug.bufs), ones4.v())
            k.barrier()
        qT = Rot(ph.sbs([128, S], BF16, 2))
        kT = Rot(ph.sbs([128, S], BF16, 2))
        vv = Rot(ph.sbs([128, 32, 128], BF16, 2))
        aq = Rot(ph.sbs([6, S], BF16, 2))
        ak = Rot(ph.sbs([6, S], BF16, 2))
        P_rot = Rot(ph.sbs([128, 512], BF16, 4))
        rden = Rot(ph.sbs([128, 512], F32, 2))
        ob = Rot(ph.sbs([128, 512], BF16, 2))
        ps = ph.psum(8)
        ps_s = Rot(ps[0:3])
        ps_o = Rot(ps[3:5])
        ps_d = Rot(ps[5:7])
        for h in range(4):
            q_, k_, v_, aq_, ak_ = qT.next(), kT.next(), vv.next(), aq.next(), ak.next()
            k.dma(q_.v(), sc["qT"][4 + h])
            k.dma(k_.v(), sc["kT"][1 + h])
            vsrc = sc["vb"]
            k.dma(v_.v(), V(vsrc.ap.rearrange("(t p) (h d) -> p t h d", p=128, h=4)[:, :, h, :], vsrc.bufs))
            k.dma(aq_.v(), V(sc["aug"].ap[h, 0], sc["aug"].bufs))
            k.dma(ak_.v(), V(sc["aug"].ap[h, 1], sc["aug"].bufs))
            for qg in range(8):
                def qk_fn(sv, kt, col0, q_=q_, k_=k_, aq_=aq_, ak_=ak_, qg=qg):
                    qs = slice(qg * 512 + col0, (qg + 1) * 512)
                    ks = slice(kt * 128, (kt + 1) * 128)
                    k.mm(sv, k_[:, ks], q_[:, qs], start=True, stop=False)
                    k.mm(sv, ak_[:, ks], aq_[:, qs], start=False, stop=True)

                def fin(po, pd, h=h, qg=qg):
                    r = rden.next()
                    k.call(k.dve, "reciprocal", out=r.v(), in_=pd.v())
                    o = ob.next()
                    k.tt(o.v(), po.v(), r.v(), ALU.mult)
                    dst = sc["oT"]
                    k.dma(V(dst.ap[512 + h * 128:512 + (h + 1) * 128, qg * 512:(qg + 1) * 512], dst.bufs), o.v())

                attn_core(k, c, qg, None, qk_fn, P_rot, ps_s, ps_o.next(), ps_d.next(),
                          lambda kt, v_=v_: v_[:, kt, :], 1.0 / SQ, fin)


def phase_outproj(k, c, sc, x_d, xo_d, w_d, nfeat):
    nk = nfeat // 128
    with k.phase() as ph:
        wb = load_w_bf16(k, ph, w_d, nk, D)
        oT = Rot(ph.sbs([128, nk, 512], BF16, 2))
        xres = Rot(ph.sbs([128, D], F32, 3))
        ps = ph.psum(8)
        psr = Rot(ps)
        xd = Buf(x_d)
        xod = Buf(xo_d)
        src = sc["oT"]
        for g in range(S // 512):
            o_ = oT.next()
            k.dma(o_.v(), V(src.ap[0:nfeat, g * 512:(g + 1) * 512].rearrange("(k p) s -> p k s", p=128), src.bufs))
            for t in range(4):
                r0 = g * 512 + t * 128
                xr = xres.next()
                k.dma(xr.v(), V(x_d[r0:r0 + 128, :], (xd,)))
                for cc in range(2):
                    p = psr.next()
                    for kk in range(nk):
                        k.mm(p.v(), o_[:, kk, t * 128:(t + 1) * 128], wb[kk][:, cc * 512:(cc + 1) * 512],
                             start=(kk == 0), stop=(kk == nk - 1))
                    k.tt(xr[:, cc * 512:(cc + 1) * 512], p.v(), xr[:, cc * 512:(cc + 1) * 512], ALU.add)
                k.dma(V(xo_d[r0:r0 + 128, :], (xod,)), xr.v())


def bc1(v, n):
    p, f = v.ap.shape
    return V(v.ap.unsqueeze(1).to_broadcast([p, n, f]), v.bufs)


NBIS = 12


def phase_dsa(k, c, sc):
    SCALE = float(128.0 ** -0.5)
    with k.phase() as ph:
        qi = ph.sb([128, 4, S], BF16)
        ki = ph.sb([128, S], BF16)
        qa = ph.sb([128, 4, S], BF16)
        ka = ph.sb([128, S], BF16)
        va = ph.sb([128, 32, 128], BF16)
        wi = ph.sb([128, 32, 8], F32)
        for h in range(4):
            k.dma(qi[:, h, :], sc["qiT"][h])
            k.dma(qa[:, h, :], sc["qT"][h])
        k.dma(ki.v(), sc["kiT"])
        k.dma(ka.v(), sc["kT"][0])
        k.dma(va.v(), V(sc["va"].ap.rearrange("(t p) d -> p t d", p=128), sc["va"].bufs))
        k.dma(wi.v(), V(sc["wi"].ap.rearrange("(t p) d -> p t d", p=128), sc["wi"].bufs))
        scb = ph.sbs([128, S], F32, 3)
        junk_t = ph.sbt([128, S], BF16)
        msk = ph.sbs([128, S], BF16, 3)
        rl = Rot(ph.sbs([128, 512], BF16, 6))
        dg = ph.sbs([128, 8, 128], BF16, 3)
        E = Rot(ph.sbs([128, 512], BF16, 3))
        P = Rot(ph.sbs([128, 512], BF16, 3))
        mT = Rot(ph.sbs([128, 512], BF16, 3))
        stt_ = ph.sbs([128, 64], F32, 3)
        rden = Rot(ph.sbs([128, 512], F32, 2))
        ob = Rot(ph.sbs([128, 512], BF16, 2))
        ps = ph.psum(8)
        ps_i = Rot(ps[0:4])
        ps_acc = Rot([ps[4], ps[7]])
        ps_s = Rot(ps[0:3])
        po = ps[5]
        pd = ps[6]
        thr_of = {}

        def gen_index(qt):
            W = (qt + 1) * 128
            qs = slice(qt * 128, (qt + 1) * 128)
            dgt = dg[qt % 3]
            for h in range(8):
                k.actf(dgt[:, h, :], c.identf.v(), AF.Copy, scale=wi[:, qt, h:h + 1])
            scq = scb[qt % 3]
            for kg in range((W + 511) // 512):
                cols = min(512, W - kg * 512)
                acc = ps_acc.next()
                pend = []
                for h in range(8):
                    p = ps_i.next()
                    pr = slice(64 * (h % 2), 64 * (h % 2) + 64)
                    k.mm(p[:, 0:cols], qi[pr, h // 2, qs], ki[pr, kg * 512:kg * 512 + cols])
                    r = rl.next()
                    k.actf(r[:, 0:cols], p[:, 0:cols], AF.Relu)
                    pend.append((h, r))
                    if len(pend) > 2:
                        h0, r0 = pend.pop(0)
                        k.mm(acc[:, 0:cols], dgt[:, h0, :], r0[:, 0:cols], start=(h0 == 0), stop=(h0 == 7))
                for h0, r0 in pend:
                    k.mm(acc[:, 0:cols], dgt[:, h0, :], r0[:, 0:cols], start=(h0 == 0), stop=(h0 == 7))
                k.copy(scq[:, kg * 512:kg * 512 + cols], acc[:, 0:cols], eng=k.act)
                yield
            k.tt(scq[:, qt * 128:W], scq[:, qt * 128:W], c.negtri.v(), ALU.add, eng=k.pool)
            yield

        def gen_bisect(qt):
            W = (qt + 1) * 128
            scq = scb[qt % 3]
            st = stt_[qt % 3]
            if qt >= 2:
                k.call(k.dve, "tensor_reduce", out=st[:, 0:1], in_=scq[:, 0:W], axis=AX.X, op=ALU.max)
                yield
                k.call(k.dve, "tensor_reduce", out=st[:, 1:2], in_=scq[:, 0:qt * 128], axis=AX.X, op=ALU.min)
                k.ts(st[:, 1:2], st[:, 1:2], -1.0, ALU.add)
                k.tt(st[:, 2:3], st[:, 0:1], st[:, 1:2], ALU.subtract)
                k.ts(st[:, 8:8 + NBIS + 1], c.pow2[:, 0:NBIS + 1], st[:, 2:3], ALU.mult)
                k.ts(st[:, 32:32 + NBIS + 1], st[:, 8:8 + NBIS + 1], 2.0, ALU.mult)
                k.tt(st[:, 3:4], st[:, 1:2], st[:, 8:9], ALU.add)
                yield
                for it in range(NBIS):
                    k.call(k.dve, "tensor_scalar", out=junk_t[:, 0:W], in0=scq[:, 0:W], scalar1=st[:, 3:4], scalar2=None,
                           op0=ALU.is_gt, op1=ALU.add, accum_out=st[:, 4:5])
                    yield
                    k.stt(st[:, 5:6], st[:, 4:5], 255.5, st[:, 32 + it + 1:32 + it + 2], ALU.is_gt, ALU.mult)
                    k.stt(st[:, 3:4], st[:, 5:6], st[:, 8 + it + 1:8 + it + 2], st[:, 3:4], ALU.subtract, ALU.add)
                k.tt(st[:, 6:7], st[:, 3:4], st[:, 8 + NBIS:8 + NBIS + 1], ALU.subtract)
                thr = st[:, 6:7]
            else:
                thr = c.negbig.v()
            m = msk[qt % 3]
            k.ts(m[:, 0:W], scq[:, 0:W], thr, ALU.is_le, -30000.0, ALU.mult)
            yield

        def gen_attn(qt):
            qs = slice(qt * 128, (qt + 1) * 128)
            m = msk[qt % 3]
            nkt = qt + 1
            issued = {}

            def issue(kt):
                s = ps_s.next()
                sv = s.v().re("p (h q) -> p h q", h=4)
                k.mm(sv, ka[:, kt * 128:(kt + 1) * 128], qa[:, :, qs], start=True, stop=False)
                k.mm(s.v(), m[:, kt * 128:(kt + 1) * 128], c.i4.v(), start=False, stop=True)
                issued[kt] = s
            for kt in range(min(2, nkt)):
                issue(kt)
            for kt in range(nkt):
                if kt + 2 < nkt:
                    issue(kt + 2)
                s = issued.pop(kt)
                p_ = P.next()
                k.actf(p_.v(), s.v(), AF.Exp, scale=SCALE)
                k.mm(po.v(), va[:, kt, :], p_.v(), start=(kt == 0), stop=(kt == qt))
                k.mm(pd.v(), c.ones.v(), p_.v(), start=(kt == 0), stop=(kt == qt))
                yield
            r = rden.next()
            k.call(k.dve, "reciprocal", out=r.v(), in_=pd.v())
            o = ob.next()
            k.tt(o.v(), po.v(), r.v(), ALU.mult)
            dst = sc["oT"]
            k.dma(V(dst.ap[0:512, qs].rearrange("(h d) q -> d h q", d=128), dst.bufs),
                  o.v().re("p (h q) -> p h q", h=4))
            yield

        def chain(*gs):
            for g_ in gs:
                yield from g_
        HALF = (NBIS + 4) // 2
        bgen = {}
        for step in range(-3, NT):
            tasks = []
            lane1 = []
            if 0 <= step + 3 < NT:
                lane1.append(gen_index(step + 3))
            if 0 <= step < NT:
                lane1.append(gen_attn(step))
            if lane1:
                tasks.append([chain(*lane1), None])
            if 0 <= step + 2 < NT:
                bgen[step + 2] = gen_bisect(step + 2)
                tasks.append([bgen[step + 2], HALF])
            if 0 <= step + 1 < NT:
                tasks.append([bgen.pop(step + 1), None])
            while tasks:
                for tk_ in list(tasks):
                    try:
                        next(tk_[0])
                        if tk_[1] is not None:
                            tk_[1] -= 1
                            if tk_[1] <= 0:
                                tasks.remove(tk_)
                    except StopIteration:
                        tasks.remove(tk_)


OD = dict(cq=0, ckv=384, kr=640, z=704, xs=1728, B=2752, C=3264, dt=3776)
PI = float(np.pi)


def phase_rope_tables(k, c, sc, pos_d):
    with k.phase() as ph:
        pi_ = ph.sb([64, S], I32)
        ang = ph.sb([64, S], F32)
        u = ph.sb([64, S], F32)
        ni = ph.sb([64, S], I32)
        r = ph.sb([64, S], F32)
        k.dma(pi_.v(), V(pos_d.rearrange("(o s) -> o s", o=1).to_broadcast([64, S]), (Buf(pos_d),)))
        k.copy(ang.v(), pi_.v())
        k.ts(ang.v(), ang.v(), c.invf.v(), ALU.mult)
        for name, shift in (("sin", 0.0), ("cos", PI / 2)):
            k.ts(r.v(), ang.v(), shift, ALU.add)
            k.ts(u.v(), r.v(), 1.0 / (2 * PI), ALU.mult)
            k.copy(ni.v(), u.v())
            k.copy(u.v(), ni.v())
            k.stt(r.v(), u.v(), -2 * PI, r.v(), ALU.mult, ALU.add)
            k.ts(u.v(), r.v(), PI, ALU.is_gt, 2 * PI, ALU.mult)
            k.tt(r.v(), r.v(), u.v(), ALU.subtract)
            k.ts(u.v(), r.v(), -PI, ALU.is_lt, 2 * PI, ALU.mult)
            k.tt(r.v(), r.v(), u.v(), ALU.add)
            k.ts(r.v(), r.v(), 3.1415925, ALU.min, -3.1415925, ALU.max)
            k.actf(u.v(), r.v(), AF.Sin)
            k.dma(sc[name], u.v())


def col_load(k, dst, src_ap, n):
    k.dma(dst, V(src_ap.rearrange("(p o) -> p o", o=1), (Buf(src_ap),)))


ODDLVL = [9]


def phase_odd_proj(k, c, sc, x_d, g_row, w_d, cqn, ckvn, wuq_d, wukv_d, qn_c, kn_c, conv_w, conv_b):
    with k.phase() as ph:
        stg = Rot(ph.sbs([128, 1896], F32, 3))
        ncast = [0]

        def loadw(w_ap, nk, ncols):
            wb_ = ph.split([128, nk, ncols], BF16, 1)
            wd = Buf(w_ap)
            for kk in range(nk):
                for c0 in range(0, ncols, 1896):
                    c1 = min(ncols, c0 + 1896)
                    s = stg.next()
                    k.dma(s[:, 0:c1 - c0], V(w_ap[kk * 128:(kk + 1) * 128, c0:c1], (wd,)))
                    ncast[0] += 1
                    k.copy(wb_[kk][:, c0:c1], s[:, 0:c1 - c0], eng=(k.pool, k.dve, k.act)[ncast[0] % 3])
            return wb_
        wb = loadw(w_d, 8, 3792)
        wuq = loadw(wuq_d, 3, 768)
        wukv = loadw(wukv_d, 2, 1024)
        gt = ph.sb([128, D], F32)
        k.dma(gt.v(), V(g_row.to_broadcast([128, D]), (Buf(g_row),)))
        gc = ph.sb([128, 16], F32)
        for i in range(3):
            col_load(k, gc[:, i:i + 1], cqn[i * 128:(i + 1) * 128], 128)
        for i in range(2):
            col_load(k, gc[:, 3 + i:4 + i], ckvn[i * 128:(i + 1) * 128], 128)
        col_load(k, gc[:, 5:6], qn_c[0:128], 128)
        col_load(k, gc[0:64, 6:7], qn_c[128:192], 64)
        col_load(k, gc[:, 7:8], kn_c[0:128], 128)
        col_load(k, gc[0:64, 8:9], kn_c[128:192], 64)
        cw = ph.sb([128, 16, 4], F32)
        cb = ph.sb([128, 16], F32)
        cwd = Buf(conv_w)
        cbd = Buf(conv_b)
        for j in range(16):
            k.dma(cw[:, j, :], V(conv_w[:, j * 128:(j + 1) * 128].rearrange("w p -> p w"), (cwd,)),
                  allow_slow_non_contiguous=True)
            k.dma(cb[:, j:j + 1], V(conv_b[j * 128:(j + 1) * 128].rearrange("(p o) -> p o", o=1), (cbd,)))
        hal = ph.split([128, 16, 3], F32, 1)
        for j in range(16):
            k.memset(hal[j][:, :], 0.0, eng=k.pool)
        xin = Rot(ph.sbs([128, D], F32, 3))
        scr = ph.sb([128, D], BF16)
        stats = Rot(ph.sbs([128, 4], F32, 4))
        hn = Rot(ph.sbs([128, D], BF16, 2))
        hT = Rot(ph.sbs([128, 8, 512], BF16, 2))
        sq = Rot(ph.sbs([128, 512], BF16, 6))
        lnb = Rot(ph.sbs([128, 512], F32, 2))
        rstd = Rot(ph.sbs([128, 512], F32, 2))
        ob = Rot(ph.sbs([128, 512], BF16, 4))
        of = Rot(ph.sbs([128, 16], F32, 3))
        cqraw = ph.sb([128, 3, 512], F32)
        cqn_b = ph.sb([128, 3, 512], BF16)
        ckvraw = ph.sb([128, 2, 512], F32)
        ckvn_b = ph.sb([128, 2, 512], BF16)
        krraw = ph.sb([64, 512], F32)
        sqkr = ph.sb([64, 512], BF16)
        xr = Rot(ph.sbs([128, 515], F32, 2))
        acc = Rot(ph.sbs([128, 512], F32, 2))
        xact = Rot(ph.sbs([128, 512], BF16, 3))
        xtm = Rot(ph.sbs([128, 4, 128], BF16, 2))
        cs = ph.sb([64, 2, 512], F32)
        yb = Rot(ph.sbs([64, 512], BF16, 2))
        t1 = Rot(ph.sbs([64, 512], F32, 2))
        t2 = Rot(ph.sbs([64, 512], F32, 2))
        ps = ph.psum(8)
        psB = Rot(ps[2:4])
        xd = Buf(x_d)

        def grpnorm(raws, sqs, nch, hd, gcol0, outb):
            p2 = psB.next()
            for i in range(nch):
                k.mm(p2.v(), c.ones.v(), sqs[i].v(), start=(i == 0), stop=(i == nch - 1))
            l_, r_ = lnb.next(), rstd.next()
            k.actf(l_.v(), p2.v(), AF.Ln, scale=1.0 / hd, bias=c.epscol.v())
            k.actf(r_.v(), l_.v(), AF.Exp, scale=-0.5)
            for i in range(nch):
                k.stt(outb[:, i, :], raws[:, i, :], gc[:, gcol0 + i:gcol0 + i + 1], r_.v(), ALU.mult, ALU.mult)

        def rope(ybv, dst, tok):
            p = psB.next()
            k.mm(p[0:64, :], c.rotm.v(), ybv)
            a, b = t1.next(), t2.next()
            k.tt(a.v(), ybv, cs[:, 0, :], ALU.mult)
            k.tt(b.v(), p[0:64, :], cs[:, 1, :], ALU.mult)
            o = ob.next()
            k.tt(o[0:64, :], a.v(), b.v(), ALU.add)
            k.dma(V(dst.ap[:, tok], dst.bufs), o[0:64, :])

        def headnorm(pn, sq_r, gcol_n):
            sqn = sq.next()
            k.actf(sqn.v(), pn.v(), AF.Square)
            p2 = psB.next()
            k.mm(p2.v(), c.ones.v(), sqn.v(), start=True, stop=False)
            k.mm(p2.v(), c.ones[0:64, :], sq_r, start=False, stop=True)
            l_, r_ = lnb.next(), rstd.next()
            k.actf(l_.v(), p2.v(), AF.Ln, scale=1.0 / 192, bias=c.epscol.v())
            k.actf(r_.v(), l_.v(), AF.Exp, scale=-0.5)
            return r_

        psA = Rot(ps[0:2])
        psB = Rot(ps[2:4])
        psBx = Rot(ps[4:5])
        psBt = Rot(ps[5:6])
        psC = Rot(ps[6:7])
        ps_t = Rot(ps[7:8])
        obC = Rot(ph.sbs([128, 512], BF16, 2))

        def proj(pool, hTg, c0, M):
            p = pool.next()
            for kk in range(8):
                k.mm(p[0:M, :], wb[kk][:, c0:c0 + M], hTg[:, kk, :], start=(kk == 0), stop=(kk == 7))
            return p

        def laneA(g, hTg):
            tok = slice(g * 512, (g + 1) * 512)
            sqs = []
            for i in range(3):
                p = proj(psA, hTg, OD["cq"] + i * 128, 128)
                s_ = sq.next()
                k.actf(s_.v(), p.v(), AF.Square)
                k.copy(cqraw[:, i, :], p.v(), eng=k.dve)
                sqs.append(s_)
                yield
            grpnorm(cqraw, sqs, 3, 384, 0, cqn_b)
            yield
            sqs = []
            for i in range(2):
                p = proj(psA, hTg, OD["ckv"] + i * 128, 128)
                s_ = sq.next()
                k.actf(s_.v(), p.v(), AF.Square)
                k.copy(ckvraw[:, i, :], p.v(), eng=k.dve)
                sqs.append(s_)
                yield
            grpnorm(ckvraw, sqs, 2, 256, 3, ckvn_b)
            yield
            p = proj(psA, hTg, OD["kr"], 64)
            k.copy(krraw.v(), p[0:64, :], eng=k.dve)
            yield
            for h in range(4):
                pn = psA.next()
                for kc in range(3):
                    k.mm(pn.v(), wuq[kc][:, h * 192:h * 192 + 128], cqn_b[:, kc, :], start=(kc == 0), stop=(kc == 2))
                pr = psA.next()
                for kc in range(3):
                    k.mm(pr[0:64, :], wuq[kc][:, h * 192 + 128:h * 192 + 192], cqn_b[:, kc, :], start=(kc == 0), stop=(kc == 2))
                yield
                sqr = sq.next()
                k.actf(sqr[0:64, :], pr[0:64, :], AF.Square)
                yield
                r_ = headnorm(pn, sqr[0:64, :], 5)
                yield
                o = ob.next()
                k.stt(o.v(), pn.v(), gc[:, 5:6], r_.v(), ALU.mult, ALU.mult)
                k.dma(V(sc["qT"][h].ap[:, tok], sc["qT"][h].bufs), o.v())
                y_ = yb.next()
                k.stt(y_.v(), pr[0:64, :], gc[0:64, 6:7], r_[0:64, :], ALU.mult, ALU.mult)
                yield
                rope(y_.v(), sc["qr"][h], tok)
                yield
            k.actf(sqkr.v(), krraw.v(), AF.Square)
            for h in range(4):
                pn = psA.next()
                for kc in range(2):
                    k.mm(pn.v(), wukv[kc][:, h * 256:h * 256 + 128], ckvn_b[:, kc, :], start=(kc == 0), stop=(kc == 1))
                yield
                r_ = headnorm(pn, sqkr.v(), 7)
                yield
                o = ob.next()
                k.stt(o.v(), pn.v(), gc[:, 7:8], r_.v(), ALU.mult, ALU.mult)
                k.dma(V(sc["kT"][h].ap[:, tok], sc["kT"][h].bufs), o.v())
                y_ = yb.next()
                k.stt(y_.v(), krraw.v(), gc[0:64, 8:9], r_[0:64, :], ALU.mult, ALU.mult)
                yield
                rope(y_.v(), sc["kr"][h], tok)
                yield
            for t in range(4):
                r0 = g * 512 + t * 128
                tk = slice(t * 128, (t + 1) * 128)
                p = psA.next()
                for kc in range(2):
                    k.mm(p.v().re("p (h d) -> p h d", h=4), ckvn_b[:, kc, tk],
                         wukv[kc][:, :].re("p (h d) -> p h d", h=4)[:, :, 128:256], start=(kc == 0), stop=(kc == 1))
                o = ob.next()
                k.copy(o.v(), p.v(), eng=k.act)
                k.dma(V(sc["vb"].ap[r0:r0 + 128, :], sc["vb"].bufs), o.v())
                yield

        def laneB(g, hTg):
            tok = slice(g * 512, (g + 1) * 512)

            def stage1(j):
                p = proj(psBx, hTg, OD["xs"] + j * 128, 128)
                x_ = xr.next()
                k.copy(x_[:, 0:3], hal[j][:, :], eng=k.pool)
                k.copy(x_[:, 3:515], p.v(), eng=k.act)
                k.copy(hal[j][:, :], x_[:, 512:515], eng=k.pool)
                yield
                a_ = acc.next()
                k.ts(a_.v(), x_[:, 0:512], cw[:, j, 0:1], ALU.mult)
                for w in range(1, 4):
                    k.stt(a_.v(), x_[:, w:w + 512], cw[:, j, w:w + 1], a_.v(), ALU.mult, ALU.add)
                yield
                xa_ = xact.next()
                k.actf(xa_.v(), a_.v(), AF.Silu, bias=cb[:, j:j + 1])
                if j >= 8:
                    dst = sc["BCT"]
                    k.dma(V(dst.ap[(j - 8) * 128:(j - 7) * 128, tok], dst.bufs), xa_.v())
                yield
                return xa_

            def stage2(j, xa_):
                if j >= 12:
                    return
                pt = psBt.next()
                ptb = pt.v().bitcast(BF16)
                for t in range(4):
                    k.tr(ptb[:, t * 128:(t + 1) * 128], xa_[:, t * 128:(t + 1) * 128], c.ident.v())
                yield
                xt_ = xtm.next()
                k.copy(xt_.v(), ptb[:, 0:512].re("p (t c) -> p t c", t=4), eng=k.dve)
                dst = sc["xsB"]
                k.dma(V(dst.ap[tok, j * 128:(j + 1) * 128].rearrange("(t p) c -> p t c", p=128), dst.bufs), xt_.v())
                yield
            prev = None
            for j in range(16):
                xa_ = yield from stage1(j)
                if prev is not None:
                    yield from stage2(*prev)
                prev = (j, xa_)
            yield from stage2(*prev)

        def laneC(g, hTg):
            for t in range(4):
                r0 = g * 512 + t * 128
                tk = slice(t * 128, (t + 1) * 128)
                for hf in range(2):
                    p = psC.next()
                    for kk in range(8):
                        k.mm(p.v(), hTg[:, kk, tk], wb[kk][:, OD["z"] + hf * 512:OD["z"] + (hf + 1) * 512],
                             start=(kk == 0), stop=(kk == 7))
                    yield
                    o = obC.next()
                    k.actf(o.v(), p.v(), AF.Silu)
                    k.dma(V(sc["zs"].ap[r0:r0 + 128, hf * 512:(hf + 1) * 512], sc["zs"].bufs), o.v())
                    yield
                p = psC.next()
                for kk in range(8):
                    k.mm(p[:, 0:16], hTg[:, kk, tk], wb[kk][:, OD["dt"]:OD["dt"] + 16], start=(kk == 0), stop=(kk == 7))
                yield
                o2 = of.next()
                k.copy(o2[:, 0:16], p[:, 0:16], eng=k.dve)
                k.dma(V(sc["dt"].ap[r0:r0 + 128, :], sc["dt"].bufs), o2[:, 0:16])
                yield

        NG = S // 512
        hTs = [None] * NG
        hTs[0] = hT.next()
        run_rr([xnorm_gen(k, c, x_d, xd, 0, gt, xin, hn, scr, stats, hTs[0], ps_t)])
        for g in range(NG):
            tok = slice(g * 512, (g + 1) * 512)
            k.dma(cs[:, 0, :], V(sc["cos"].ap[:, tok], sc["cos"].bufs))
            k.dma(cs[:, 1, :], V(sc["sin"].ap[:, tok], sc["sin"].bufs))
            gens = [laneA(g, hTs[g]), laneB(g, hTs[g]), laneC(g, hTs[g])]
            if g + 1 < NG:
                hTs[g + 1] = hT.next()
                gens.append(xnorm_gen(k, c, x_d, xd, g + 1, gt, xin, hn, scr, stats, hTs[g + 1], ps_t))
            run_rr(gens)


def phase_mla(k, c, sc):
    SCALE = float(192.0 ** -0.5)
    with k.phase() as ph:
        qT = Rot(ph.sbs([128, S], BF16, 2))
        kT = Rot(ph.sbs([128, S], BF16, 2))
        qr = Rot(ph.sbs([64, S], BF16, 2))
        kr = Rot(ph.sbs([64, S], BF16, 2))
        vv = Rot(ph.sbs([128, 32, 128], BF16, 2))
        P_rot = Rot(ph.sbs([128, 512], BF16, 4))
        rden = Rot(ph.sbs([128, 512], F32, 2))
        ob = Rot(ph.sbs([128, 512], BF16, 2))
        ps = ph.psum(8)
        ps_s = Rot(ps[0:3])
        ps_o = Rot(ps[3:5])
        ps_d = Rot(ps[5:7])
        for h in range(4):
            q_, k_, qr_, kr_, v_ = qT.next(), kT.next(), qr.next(), kr.next(), vv.next()
            k.dma(q_.v(), sc["qT"][h])
            k.dma(k_.v(), sc["kT"][h])
            k.dma(qr_.v(), sc["qr"][h])
            k.dma(kr_.v(), sc["kr"][h])
            vsrc = sc["vb"]
            k.dma(v_.v(), V(vsrc.ap.rearrange("(t p) (h d) -> p t h d", p=128, h=4)[:, :, h, :], vsrc.bufs))
            for qg in range(8):
                def qk_fn(sv, kt, col0, q_=q_, k_=k_, qr_=qr_, kr_=kr_, qg=qg):
                    qs = slice(qg * 512 + col0, (qg + 1) * 512)
                    ks = slice(kt * 128, (kt + 1) * 128)
                    k.mm(sv, k_[:, ks], q_[:, qs], start=True, stop=False)
                    k.mm(sv, kr_[:, ks], qr_[:, qs], start=False, stop=True)

                def fin(po, pd, h=h, qg=qg):
                    r = rden.next()
                    k.call(k.dve, "reciprocal", out=r.v(), in_=pd.v())
                    o = ob.next()
                    k.tt(o.v(), po.v(), r.v(), ALU.mult)
                    dst = sc["oT"]
                    k.dma(V(dst.ap[h * 128:(h + 1) * 128, qg * 512:(qg + 1) * 512], dst.bufs), o.v())

                attn_core(k, c, qg, None, qk_fn, P_rot, ps_s, ps_o.next(), ps_d.next(),
                          lambda kt, v_=v_: v_[:, kt, :], SCALE, fin)


def bcl(v, n):
    p, h = v.ap.shape
    return V(v.ap.unsqueeze(2).to_broadcast([p, h, n]), v.bufs)


def phase_ssd(k, c, sc, dt_bias, a_log, d_skip, gate_norm):
    with k.phase() as ph:
        rep = ph.sb([128, 64], F32)
        for i, src in enumerate((dt_bias, a_log, d_skip)):
            k.dma(rep[:, i * 16:(i + 1) * 16], V(src.rearrange("(o h) -> o h", o=1).to_broadcast([128, 16]), (Buf(src),)))
        k.actf(rep[:, 16:32], rep[:, 16:32], AF.Exp)
        k.ts(rep[:, 16:32], rep[:, 16:32], -1.0, ALU.mult)
        one = ph.sb([128, 1], F32)
        k.memset(one.v(), 1.0)
        gg = ph.sb([128, D], F32)
        k.dma(gg.v(), V(gate_norm.rearrange("(o h) -> o h", o=1).to_broadcast([128, D]), (Buf(gate_norm),)))
        negtril = ph.sb([128, 128], BF16)
        tmpf = ph.sb([128, 128], F32)
        k.dma(tmpf.v(), c.negtril_d)
        k.copy(negtril.v(), tmpf.v())
        hst = ph.sb([128, D], F32)
        hstb = ph.sb([128, D], BF16)
        k.memset(hst.v(), 0.0)
        k.memset(hstb.v(), 0.0)
        xs = Rot(ph.sbs([128, D], BF16, 2))
        Btm = Rot(ph.sbs([128, 4, 128], BF16, 2))
        BT = Rot(ph.sbs([128, 4, 128], BF16, 2))
        CT = Rot(ph.sbs([128, 4, 128], BF16, 2))
        zs = Rot(ph.sbs([128, D], BF16, 2))
        dtr = Rot(ph.sbs([128, 16], F32, 2))
        sm = Rot(ph.sbs([128, 128], F32, 2))
        LT = Rot(ph.sbs([128, 16, 128], BF16, 2))
        MT = Rot(ph.sbs([128, 16, 128], BF16, 2))
        cbt = Rot(ph.sbs([128, 4, 128], BF16, 2))
        xdr = Rot(ph.sbs([128, D], BF16, 2))
        xddr = Rot(ph.sbs([128, D], BF16, 2))
        yr = Rot(ph.sbs([128, D], F32, 2))
        t2r = Rot(ph.sbs([128, D], F32, 2))
        junk = ph.sb([128, 256], BF16)
        ynr = Rot(ph.sbs([128, D], BF16, 2))
        oTt = Rot(ph.sbs([128, 8, 128], BF16, 2))
        ps = Rot(ph.psum(8))
        ydr = Rot(ph.sbs([128, D], F32, 2))

        def part1(ci):
            r0 = ci * 128
            tok = slice(r0, r0 + 128)
            xs_, Btm_, BT_, CT_, zs_, dtr_ = xs.next(), Btm.next(), BT.next(), CT.next(), zs.next(), dtr.next()
            xsB = sc["xsB"]
            k.dma(xs_.v(), V(xsB.ap[tok, 0:1024], xsB.bufs))
            k.dma(Btm_.v().re("p g n -> p (g n)"), V(xsB.ap[tok, 1024:1536], xsB.bufs))
            bct = sc["BCT"]
            k.dma(BT_.v(), V(bct.ap[0:512, tok].rearrange("(g n) t -> n g t", n=128), bct.bufs))
            k.dma(CT_.v(), V(bct.ap[512:1024, tok].rearrange("(g n) t -> n g t", n=128), bct.bufs))
            k.dma(zs_.v(), V(sc["zs"].ap[tok, :], sc["zs"].bufs))
            k.dma(dtr_.v(), V(sc["dt"].ap[tok, :], sc["dt"].bufs))
            s_ = sm.next()
            k.tt(s_[:, 0:16], dtr_.v(), rep[:, 0:16], ALU.add)
            k.actf(s_[:, 0:16], s_[:, 0:16], AF.Exp)
            k.actf(s_[:, 0:16], s_[:, 0:16], AF.Ln, bias=one.v())
            k.tt(s_[:, 16:32], s_[:, 0:16], rep[:, 16:32], ALU.mult)
            yield
            pc = ps.next()
            k.mm(pc[:, 0:16], c.trif.v(), s_[:, 16:32])
            k.mm(pc[:, 16:32], c.onesf.v(), s_[:, 16:32])
            k.copy(s_[:, 32:48], pc[:, 0:16])
            k.ts(s_[:, 48:64], pc[:, 0:16], -1.0, ALU.mult)
            k.actf(s_[:, 64:80], pc[:, 0:16], AF.Exp)
            k.tt(s_[:, 112:128], pc[:, 16:32], s_[:, 32:48], ALU.subtract)
            k.actf(s_[:, 80:96], s_[:, 112:128], AF.Exp)
            k.actf(s_[:, 96:112], pc[:, 16:32], AF.Exp)
            yield
            LT_ = LT.next()
            for q4 in range(4):
                pl = ps.next()
                for i in range(4):
                    h = 4 * q4 + i
                    k.mm(pl[:, i * 128:(i + 1) * 128], s_[:, 16 + h:17 + h].bc([128, 128]), c.trif.v(), start=True, stop=False)
                    k.mm(pl[:, i * 128:(i + 1) * 128], c.ident.v(), negtril.v(), start=False, stop=True)
                    k.actf(LT_[:, h, :], pl[:, i * 128:(i + 1) * 128], AF.Exp, bias=s_[:, 48 + h:49 + h])
                yield
            pcb = ps.next()
            for g in range(4):
                k.mm(pcb[:, g * 128:(g + 1) * 128], BT_[:, g, :], CT_[:, g, :])
            cbt_ = cbt.next()
            k.copy(cbt_.v().re("p g n -> p (g n)"), pcb.v(), eng=k.act)
            yield
            MT_ = MT.next()
            for g in range(4):
                k.tt(MT_[:, 4 * g:4 * g + 4, :], LT_[:, 4 * g:4 * g + 4, :], bc1(cbt_[:, g, :], 4), ALU.mult)
            yield
            xd_, xdd_ = xdr.next(), xddr.next()
            k.tt(xd_.v().re("l (h p) -> l h p", p=64), xs_.v().re("l (h p) -> l h p", p=64), bcl(s_[:, 0:16], 64), ALU.mult)
            k.tt(xdd_.v().re("l (h p) -> l h p", p=64), xd_.v().re("l (h p) -> l h p", p=64), bcl(s_[:, 80:96], 64), ALU.mult)
            t2_ = t2r.next()
            k.tt(t2_.v().re("l (h p) -> l h p", p=64), xs_.v().re("l (h p) -> l h p", p=64), bcl(rep[:, 32:48], 64), ALU.mult, eng=k.pool)
            yield
            yd_ = ydr.next()
            for hf in range(2):
                hs = slice(hf * 512, (hf + 1) * 512)
                py = ps.next()
                for hh in range(8):
                    h = hf * 8 + hh
                    k.mm(py[:, hh * 64:(hh + 1) * 64], MT_[:, h, :], xd_[:, h * 64:(h + 1) * 64])
                k.tt(yd_[:, hs], py.v(), t2_[:, hs], ALU.add)
                yield
            return (tok, s_, CT_, Btm_, xdd_, zs_, yd_)

        def part2(st):
            tok, s_, CT_, Btm_, xdd_, zs_, yd_ = st
            y_ = yr.next()
            for hf in range(2):
                hs = slice(hf * 512, (hf + 1) * 512)
                po = ps.next()
                for gg_ in range(2):
                    g = hf * 2 + gg_
                    k.mm(po[:, gg_ * 256:(gg_ + 1) * 256], CT_[:, g, :], hstb[:, g * 256:(g + 1) * 256])
                k.tt(y_[:, hs].re("l (h p) -> l h p", p=64), po.v().re("l (h p) -> l h p", p=64),
                     bcl(s_[:, 64 + hf * 8:72 + hf * 8], 64), ALU.mult)
                k.tt(y_[:, hs], y_[:, hs], yd_[:, hs], ALU.add)
                yield
            for hf in range(2):
                hs = slice(hf * 512, (hf + 1) * 512)
                pst = ps.next()
                for gg_ in range(2):
                    g = hf * 2 + gg_
                    k.mm(pst[:, gg_ * 256:(gg_ + 1) * 256], Btm_[:, g, :], xdd_[:, g * 256:(g + 1) * 256])
                k.tt(hst[:, hs].re("l (h p) -> l h p", p=64), hst[:, hs].re("l (h p) -> l h p", p=64),
                     bcl(s_[:, 96 + hf * 8:104 + hf * 8], 64), ALU.mult, eng=k.pool)
                k.tt(hst[:, hs], hst[:, hs], pst.v(), ALU.add)
                k.copy(hstb[:, hs], hst[:, hs], eng=k.act)
                yield
            k.tt(y_.v(), y_.v(), zs_.v(), ALU.mult)
            for g in range(4):
                k.actf(junk.v(), y_[:, g * 256:(g + 1) * 256], AF.Square, accum_out=s_[:, 112 + g:113 + g])
            yield
            k.ts(s_[:, 116:120], s_[:, 112:116], 1.0 / 256, ALU.mult, EPS, ALU.add)
            k.tt(s_[:, 120:124], s_[:, 116:120], c.mhalf.v().bc([128, 4]), ALU.pow, eng=k.pool)
            yn_ = ynr.next()
            for g in range(4):
                gs = slice(g * 256, (g + 1) * 256)
                k.stt(yn_[:, gs], y_[:, gs], s_[:, 120 + g:121 + g], gg[:, gs], ALU.mult, ALU.mult)
            yield
            pt = ps.next()
            ptb = pt.v().bitcast(BF16)
            for j in range(8):
                k.tr(ptb[:, j * 128:(j + 1) * 128], yn_[:, j * 128:(j + 1) * 128], c.ident.v())
            o_ = oTt.next()
            k.copy(o_.v().re("p j t -> p (j t)"), ptb, eng=k.act)
            dst = sc["oT"]
            k.dma(V(dst.ap[512:1536, tok].rearrange("(j p) t -> p j t", p=128), dst.bufs), o_.v())
            yield

        states = {}

        def p1(ci):
            states[ci] = yield from part1(ci)
        run_rr([p1(0)])
        for ci in range(NT):
            gens = [part2(states.pop(ci))]
            if ci + 1 < NT:
                gens.append(p1(ci + 1))
            run_rr(gens)


def const_arrays():
    cd = {}
    cd["ident"] = np.eye(128, dtype=np.float32)
    cd["pow2"] = np.tile((2.0 ** -(np.arange(32) + 1.0)).astype(np.float32)[None, :], (128, 1))
    invf = (10000.0 ** (-np.arange(32, dtype=np.float32) / 32)).astype(np.float32)
    cd["invf"] = np.concatenate([invf, invf])[:, None].astype(np.float32)
    rot = np.zeros((64, 64), np.float32)
    for m in range(32):
        rot[m + 32, m] = -1.0
        rot[m, m + 32] = 1.0
    cd["rotm"] = rot
    cd["negtril"] = (np.tril(np.ones((128, 128), np.float32), -1) * -30000.0).astype(np.float32)
    cd["tri"] = np.triu(np.ones((128, 128), np.float32))
    cd["negtri"] = (np.triu(np.ones((128, 128), np.float32), 1) * -1e30).astype(np.float32)
    return cd


INPUT_NAMES = ["x", "positions", "ev_norm", "ev_w_in", "ev_b_f", "ev_qn_a", "ev_kn_a", "ev_qn_b", "ev_kn_b",
               "ev_w_out", "od_norm", "od_w_in", "od_cq_norm", "od_ckv_norm", "od_w_uq", "od_w_ukv", "od_qn_c",
               "od_kn_c", "od_conv_w", "od_conv_b", "od_dt_bias", "od_a_log", "od_d_skip", "od_gate_norm",
               "od_w_out", "mlp_norm", "mlp_w1", "mlp_w2"]


def build(shapes, cfg):
    nc = bass.Bass("TRN2", target_bir_lowering=False)
    din = {}
    for name, (shp, dt) in shapes.items():
        bdt = I32 if np.dtype(dt) == np.int32 else F32
        din[name] = nc.dram_tensor(name, list(shp), bdt, kind="ExternalInput").ap()
    cds = const_arrays()
    cd = {n: nc.dram_tensor("c_" + n, list(a.shape), F32, kind="ExternalInput").ap() for n, a in cds.items()}
    y = nc.dram_tensor("y", [S, D], F32, kind="ExternalOutput").ap()
    xa = nc.dram_tensor("xa", [S, D], F32, kind="Internal").ap()
    xb = nc.dram_tensor("xb", [S, D], F32, kind="Internal").ap()

    def mk(name, shape, dt):
        return nc.dram_tensor(name, list(shape), dt, kind="Internal").ap()

    def dv(ap):
        return V(ap, (Buf(ap),))
    sc = {}
    t = mk("s_qT", [8, 128, S], BF16)
    sc["qT"] = [dv(t[h]) for h in range(8)]
    t = mk("s_kT", [5, 128, S], BF16)
    sc["kT"] = [dv(t[h]) for h in range(5)]
    t = mk("s_qiT", [4, 128, S], BF16)
    sc["qiT"] = [dv(t[h]) for h in range(4)]
    sc["kiT"] = dv(mk("s_kiT", [128, S], BF16))
    sc["va"] = dv(mk("s_va", [S, 128], BF16))
    sc["vb"] = dv(mk("s_vb", [S, 512], BF16))
    sc["wi"] = dv(mk("s_wi", [S, 8], F32))
    sc["fbT"] = dv(mk("s_fbT", [4, S], F32))
    sc["aug"] = dv(mk("s_aug", [4, 2, 6, S], BF16))
    sc["oT"] = dv(mk("s_oT", [1536, S], BF16))
    t = mk("s_qr", [4, 64, S], BF16)
    sc["qr"] = [dv(t[h]) for h in range(4)]
    t = mk("s_kr", [4, 64, S], BF16)
    sc["kr"] = [dv(t[h]) for h in range(4)]
    sc["cos"] = dv(mk("s_cos", [64, S], F32))
    sc["sin"] = dv(mk("s_sin", [64, S], F32))
    sc["BCT"] = dv(mk("s_BCT", [1024, S], BF16))
    sc["xsB"] = dv(mk("s_xsB", [S, 1536], BF16))
    sc["zs"] = dv(mk("s_zs", [S, 1024], BF16))
    sc["dt"] = dv(mk("s_dt", [S, 16], F32))
    dbg = cfg.get("debug", {})
    k = K(nc)
    with k.es:
        c = setup_consts(k, cd)
        cur = din["x"]
        ropedone = [False]
        steps = cfg["steps"]
        for si, (kind, l) in enumerate(steps):
            last = si == len(steps) - 1
            dst = y if last else (xa if cur is not xa else xb)
            if kind == "mlp":
                phase_mlp(k, c, cur, dst, din["mlp_norm"][l:l + 1, :], din["mlp_w1"][l], din["mlp_w2"][l])
            elif kind == "even":
                phase_even_proj(k, c, sc, cur, din["ev_norm"][l:l + 1, :], din["ev_w_in"][l], din["ev_qn_a"][l],
                                din["ev_kn_a"][l], din["ev_qn_b"][l], din["ev_kn_b"][l])
                if "nodsa" not in dbg:
                    phase_dsa(k, c, sc)
                if "nofox" not in dbg:
                    phase_fox(k, c, sc, din["ev_b_f"][l])
                phase_outproj(k, c, sc, cur, dst, din["ev_w_out"][l], 1024)
            elif kind == "odd":
                if "oddlvl" in dbg:
                    ODDLVL[0] = dbg["oddlvl"]
                if not ropedone[0] and "norope" not in dbg:
                    phase_rope_tables(k, c, sc, din["positions"])
                    ropedone[0] = True
                phase_odd_proj(k, c, sc, cur, din["od_norm"][l:l + 1, :], din["od_w_in"][l], din["od_cq_norm"][l],
                               din["od_ckv_norm"][l], din["od_w_uq"][l], din["od_w_ukv"][l], din["od_qn_c"][l],
                               din["od_kn_c"][l], din["od_conv_w"][l], din["od_conv_b"][l])
                if "nomla" not in dbg:
                    phase_mla(k, c, sc)
                if "nossd" not in dbg:
                    phase_ssd(k, c, sc, din["od_dt_bias"][l], din["od_a_log"][l], din["od_d_skip"][l],
                              din["od_gate_norm"][l])
                phase_outproj(k, c, sc, cur, dst, din["od_w_out"][l], 1536)
            else:
                raise ValueError(kind)
            cur = dst
        k.barrier()
    return nc, cds


FULL_CFG = {"steps": [("even", 0), ("mlp", 0), ("odd", 0), ("mlp", 1), ("even", 1), ("mlp", 2), ("odd", 1), ("mlp", 3)]}


def run(inputs, cfg, cores=N_CORES):
    per_core = []
    for b in range(cores):
        m = {}
        for n in INPUT_NAMES:
            a = np.asarray(inputs[n])
            if n in ("x", "positions"):
                a = a[b]
            m[n] = np.ascontiguousarray(a)
        per_core.append(m)
    shapes = {n: (per_core[0][n].shape, per_core[0][n].dtype) for n in INPUT_NAMES}
    nc, cds = build(shapes, cfg)
    for m in per_core:
        for n, a in cds.items():
            m["c_" + n] = a
    res = run_bass_kernel_spmd(nc, per_core, core_ids=list(range(cores)))
    return np.stack([np.asarray(r["y"]) for r in res.results], axis=0)


def kernel(**inputs):
    out = run(inputs, FULL_CFG)
    return out.astype(np.float32)
```

```python
import contextlib
import numpy as np
import ml_dtypes
import concourse.bass as bass
import concourse.mybir as mybir
from concourse.bass_utils import run_bass_kernel_spmd

F32 = mybir.dt.float32
BF16 = mybir.dt.bfloat16
I32 = mybir.dt.int32
AF = mybir.ActivationFunctionType
ALU = mybir.AluOpType
AX = mybir.AxisListType

S = 4096
D = 1024
NT = S // 128
DFF = 4096
EPS = 1e-6
N_CORES = 8
WRITE_KEYS = ("out", "accum_out", "ap")


class V:
    def __init__(self, ap, bufs):
        self.ap = ap
        self.bufs = bufs

    def __getitem__(self, idx):
        return V(self.ap[idx], self.bufs)

    def bc(self, shape):
        return V(self.ap.to_broadcast(shape), self.bufs)

    def re(self, pat, **kw):
        return V(self.ap.rearrange(pat, **kw), self.bufs)

    def bitcast(self, dt):
        return V(self.ap.bitcast(dt), self.bufs)


class Buf:
    def __init__(self, ap):
        self.ap = ap
        self.w = None
        self.r = {}
        self.excl = False

    def __getitem__(self, idx):
        return V(self.ap[idx], (self,))

    def v(self):
        return V(self.ap, (self,))


def multi(*views):
    bufs = []
    for v in views:
        bufs.extend(v.bufs)
    return V(views[0].ap, tuple(bufs))


class Eng:
    def __init__(self, k, name, raw, self_sync):
        self.name = name
        self.raw = raw
        self.sem = k.new_sem("e_" + name)
        self.cnt = 0
        self.seen = {}
        self.self_sync = self_sync


class Slot:
    def __init__(self, k, key):
        self.key = key
        self.sem = k.new_sem(key)
        self.val = 0


class K:
    NSLOT = 12

    def __init__(self, nc):
        self.nc = nc
        self.es = contextlib.ExitStack()
        self.pe = Eng(self, "pe", nc.tensor, False)
        self.act = Eng(self, "act", nc.scalar, True)
        self.dve = Eng(self, "dve", nc.vector, True)
        self.pool = Eng(self, "pool", nc.gpsimd, True)
        self.sp = Eng(self, "sp", nc.sync, False)
        self.engs = [self.pe, self.act, self.dve, self.pool, self.sp]
        self.queues = {}
        for q in (self.sp, self.pool):
            self.queues[q.name] = [Slot(self, "d_%s_%d" % (q.name, i)) for i in range(self.NSLOT)]
        self.qnext = {q: 0 for q in self.queues}
        self.nph = 0

    def new_sem(self, name):
        return self.es.enter_context(self.nc.semaphore(name))

    def _wait(self, eng, tok):
        key, sem, val = tok
        if key == eng.name and not eng.self_sync:
            return
        if eng.seen.get(key, 0) >= val:
            return
        eng.raw.wait_ge(sem, val)
        eng.seen[key] = val

    def _deps(self, eng, reads, writes):
        for v in reads:
            for b in v.bufs:
                if b.w is not None:
                    self._wait(eng, b.w)
                if b.excl:
                    for t in b.r.values():
                        if t[0] != eng.name:
                            self._wait(eng, t)
        for v in writes:
            for b in v.bufs:
                if b.w is not None:
                    self._wait(eng, b.w)
                for t in b.r.values():
                    self._wait(eng, t)

    def _mark(self, tok, reads, writes):
        for v in reads:
            for b in v.bufs:
                b.r[tok[0]] = tok
        for v in writes:
            for b in v.bufs:
                b.w = tok
                b.r = {}

    def call(self, eng, method, **kw):
        reads, writes, args = [], [], {}
        for key, v in kw.items():
            if isinstance(v, V):
                (writes if key in WRITE_KEYS else reads).append(v)
                args[key] = v.ap
            else:
                args[key] = v
        self._deps(eng, reads, writes)
        inst = getattr(eng.raw, method)(**args)
        eng.cnt += 1
        inst.then_inc(eng.sem, 1)
        self._mark((eng.name, eng.sem, eng.cnt), reads, writes)
        return inst

    def dma(self, out, in_, q=None, **kw):
        q = q or self.sp
        slots = self.queues[q.name]
        slot = slots[self.qnext[q.name] % len(slots)]
        self.qnext[q.name] += 1
        if slot.val > 0:
            self._wait(q, (slot.key, slot.sem, slot.val))
        self._deps(q, [in_], [out])
        slot.val += 16
        q.raw.dma_start(out=out.ap, in_=in_.ap, **kw).then_inc(slot.sem, 16)
        self._mark((slot.key, slot.sem, slot.val), [in_], [out])

    def barrier(self):
        toks = [(e.name, e.sem, e.cnt) for e in self.engs if e.cnt > 0]
        for sl in self.queues.values():
            toks += [(s.key, s.sem, s.val) for s in sl if s.val > 0]
        for e in self.engs:
            for t in toks:
                self._wait(e, t)

    def mm(self, out, lhsT, rhs, start=True, stop=True):
        return self.call(self.pe, "matmul", out=out, lhsT=lhsT, rhs=rhs, start=start, stop=stop)

    def tr(self, out, in_, ident):
        return self.call(self.pe, "transpose", out=out, in_=in_, identity=ident)

    def actf(self, out, in_, func, **kw):
        return self.call(self.act, "activation", out=out, in_=in_, func=func, **kw)

    def tt(self, out, in0, in1, op, eng=None):
        return self.call(eng or self.dve, "tensor_tensor", out=out, in0=in0, in1=in1, op=op)

    def ts(self, out, in0, s1, op0, s2=None, op1=None, eng=None, **kw):
        if op1 is None:
            return self.call(eng or self.dve, "tensor_scalar", out=out, in0=in0, scalar1=s1, scalar2=None,
                             op0=op0, **kw)
        return self.call(eng or self.dve, "tensor_scalar", out=out, in0=in0, scalar1=s1, scalar2=s2,
                         op0=op0, op1=op1, **kw)

    def stt(self, out, in0, scalar, in1, op0, op1, **kw):
        return self.call(self.dve, "scalar_tensor_tensor", out=out, in0=in0, scalar=scalar, in1=in1,
                         op0=op0, op1=op1, **kw)

    def copy(self, out, in_, eng=None):
        eng = eng or self.dve
        if eng is self.act:
            return self.call(eng, "copy", out=out, in_=in_)
        return self.call(eng, "tensor_copy", out=out, in_=in_)

    def memset(self, ap, val, eng=None):
        return self.call(eng or self.dve, "memset", ap=ap, constant=val)

    @contextlib.contextmanager
    def phase(self):
        self.barrier()
        self.nph += 1
        ph = Phase(self, "p%d" % self.nph)
        with ph.es:
            yield ph
            self.barrier()


class Phase:
    def __init__(self, k, name):
        self.k = k
        self.name = name
        self.es = contextlib.ExitStack()
        self.n = 0

    def sbt(self, shape, dtype):
        self.n += 1
        return self.es.enter_context(self.k.nc.sbuf_tensor("%s_s%d" % (self.name, self.n), list(shape), dtype))

    def sb(self, shape, dtype):
        t = self.sbt(shape, dtype)
        return Buf(t[tuple(slice(None) for _ in shape)])

    def sbs(self, shape, dtype, n):
        return [self.sb(shape, dtype) for _ in range(n)]

    def split(self, shape, dtype, axis, step=1):
        t = self.sbt(shape, dtype)
        out = []
        for i in range(0, shape[axis], step):
            idx = [slice(None)] * len(shape)
            idx[axis] = slice(i, i + step) if step > 1 else i
            out.append(Buf(t[tuple(idx)]))
        return out

    def psum(self, n=8):
        out = []
        for i in range(n):
            self.n += 1
            t = self.es.enter_context(self.k.nc.psum_tensor("%s_ps%d" % (self.name, self.n), [128, 512], F32))
            b = Buf(t[:, :])
            b.excl = True
            out.append(b)
        return out


class Rot:
    def __init__(self, items):
        self.items = items
        self.i = 0

    def next(self):
        it = self.items[self.i % len(self.items)]
        self.i += 1
        return it


def dram_buf(ap):
    return Buf(ap)


class Ctx:
    pass


def setup_consts(k, cd):
    nc = k.nc
    c = Ctx()
    es = k.es
    def sb(name, shape, dt):
        t = es.enter_context(nc.sbuf_tensor(name, list(shape), dt))
        return Buf(t[tuple(slice(None) for _ in shape)])
    c.ident = sb("k_ident", [128, 128], BF16)
    c.mhalf = sb("k_mhalf", [128, 1], F32)
    tmp = sb("k_tmp", [128, 128], F32)
    k.dma(tmp.v(), V(cd["ident"], (Buf(cd["ident"]),)))
    k.copy(c.ident.v(), tmp.v(), eng=k.dve)
    k.memset(c.mhalf.v(), -0.5, eng=k.dve)
    c.epscol = sb("k_epscol", [128, 1], F32)
    k.memset(c.epscol.v(), EPS, eng=k.dve)
    c.identf = sb("k_identf", [128, 128], F32)
    k.copy(c.identf.v(), tmp.v(), eng=k.dve)
    c.i4 = sb("k_i4", [128, 512], BF16)
    for h_ in range(4):
        k.copy(c.i4[:, h_ * 128:(h_ + 1) * 128], tmp.v(), eng=k.dve)
    c.ones = sb("k_ones", [128, 128], BF16)
    k.memset(c.ones.v(), 1.0, eng=k.dve)
    c.onesf = sb("k_onesf", [128, 128], F32)
    k.memset(c.onesf.v(), 1.0, eng=k.dve)
    c.tri = sb("k_tri", [128, 128], BF16)
    k.dma(tmp.v(), V(cd["tri"], (Buf(cd["tri"]),)))
    k.copy(c.tri.v(), tmp.v(), eng=k.dve)
    c.trif = sb("k_trif", [128, 128], F32)
    k.copy(c.trif.v(), tmp.v(), eng=k.dve)
    c.invf = sb("k_invf", [64, 1], F32)
    k.dma(c.invf.v(), V(cd["invf"], (Buf(cd["invf"]),)))
    c.rotm = sb("k_rotm", [64, 64], BF16)
    k.dma(tmp[0:64, 0:64], V(cd["rotm"], (Buf(cd["rotm"]),)))
    k.copy(c.rotm.v(), tmp[0:64, 0:64], eng=k.dve)
    c.negtril_d = V(cd["negtril"], (Buf(cd["negtril"]),))
    c.negbig = sb("k_negbig", [128, 1], F32)
    k.memset(c.negbig.v(), -1e29, eng=k.dve)
    c.pow2 = sb("k_pow2", [128, 32], F32)
    k.dma(c.pow2.v(), V(cd["pow2"], (Buf(cd["pow2"]),)))
    c.negtri = sb("k_negtri", [128, 128], F32)
    k.dma(c.negtri.v(), V(cd["negtri"], (Buf(cd["negtri"]),)))
    return c


def rmsnorm_tile(k, c, ph, xt, gt, hn, scr, st):
    k.actf(scr.v(), xt.v(), AF.Square, accum_out=st[:, 0:1])
    k.ts(st[:, 1:2], st[:, 0:1], 1.0 / D, ALU.mult, EPS, ALU.add)
    k.tt(st[:, 2:3], st[:, 1:2], c.mhalf.v(), ALU.pow, eng=k.pool)
    k.stt(hn.v(), xt.v(), st[:, 2:3], gt.v(), ALU.mult, ALU.mult)


def phase_mlp(k, c, x_d, xo_d, g_row, w1_d, w2_d):
    G = 256
    NG = S // G
    with k.phase() as ph:
        w1b = ph.split([128, 8, DFF], BF16, 1)
        w2b = ph.split([128, 32, D], BF16, 1)
        stg = Rot(ph.sbs([128, 2048], F32, 3))
        gt = ph.sb([128, D], F32)
        xin = Rot(ph.sbs([128, D], F32, 3))
        scr = ph.sb([128, D], BF16)
        stats = Rot(ph.sbs([128, 4], F32, 4))
        hn = Rot(ph.sbs([128, D], BF16, 2))
        hT = Rot(ph.sbs([128, 8, G], BF16, 2))
        rl = Rot(ph.sbs([128, G], BF16, 4))
        hid = Rot(ph.sbs([128, G], BF16, 5))
        xres = Rot(ph.sbs([128, D], F32, 3))
        ps = ph.psum(8)
        ps_y = ps[0:4]
        ps_h = Rot(ps[4:7])
        ps_t = Rot(ps[7:8])
        xd = Buf(x_d)
        xod = Buf(xo_d)
        w1d = Buf(w1_d)
        w2d = Buf(w2_d)
        k.dma(gt.v(), V(g_row.to_broadcast([128, D]), (Buf(g_row),)))
        for kk in range(8):
            for hf in range(2):
                s = stg.next()
                k.dma(s.v(), V(w1_d[kk * 128:(kk + 1) * 128, hf * 2048:(hf + 1) * 2048], (w1d,)))
                k.copy(w1b[kk][:, hf * 2048:(hf + 1) * 2048], s.v(), eng=(k.pool, k.dve, k.act)[(kk * 2 + hf) % 3])
        w2v = w2_d.rearrange("(j p) n -> p j n", p=128)
        for jj in range(0, 32, 2):
            s = stg.next()
            k.dma(s.v().re("p (j n) -> p j n", j=2), V(w2v[:, jj:jj + 2, :], (w2d,)))
            eng = (k.pool, k.dve, k.act)[(jj // 2) % 3]
            k.copy(w2b[jj][:, :], s[:, 0:1024], eng=eng)
            k.copy(w2b[jj + 1][:, :], s[:, 1024:2048], eng=eng)

        def norm_a(g):
            res = []
            for t in range(G // 128):
                xt = xin.next()
                r0 = g * G + t * 128
                k.dma(xt.v(), V(x_d[r0:r0 + 128, :], (xd,)))
                h = hn.next()
                rmsnorm_tile(k, c, ph, xt, gt, h, scr, stats.next())
                res.append(h)
            return res

        def norm_b(g, hs):
            hTg = hT.next()
            for t, h in enumerate(hs):
                pt = ps_t.next()
                ptb = pt.v().bitcast(BF16)
                for kk in range(8):
                    k.tr(ptb[:, kk * 128:(kk + 1) * 128], h[:, kk * 128:(kk + 1) * 128], c.ident.v())
                k.copy(hTg[:, :, t * 128:(t + 1) * 128], ptb.re("p (k t) -> p k t", k=8), eng=k.act)
            return hTg

        hs = norm_a(0)
        hT_cur = norm_b(0, hs)
        for g in range(NG):
            hs_next = None
            hT_next = None
            pend = []

            def w2(pj, phd, last):
                for t in range(2):
                    for cc in range(2):
                        k.mm(ps_y[t * 2 + cc].v(), phd[:, t * 128:(t + 1) * 128],
                             w2b[pj][:, cc * 512:(cc + 1) * 512], start=(pj == 0), stop=last)
            for j in range(32):
                ph_ = ps_h.next()
                for kk in range(8):
                    k.mm(ph_[:, 0:G], w1b[kk][:, j * 128:(j + 1) * 128], hT_cur[:, kk, :],
                         start=(kk == 0), stop=(kk == 7))
                r = rl.next()
                k.actf(r.v(), ph_[:, 0:G], AF.Relu)
                hd = hid.next()
                k.tt(hd.v(), r.v(), r.v(), ALU.mult)
                pend.append((j, hd))
                if len(pend) > 2:
                    pj, phd = pend.pop(0)
                    w2(pj, phd, False)
                if j == 4 and g + 1 < NG:
                    hs_next = norm_a(g + 1)
                if j == 20 and g + 1 < NG:
                    hT_next = norm_b(g + 1, hs_next)
            while pend:
                pj, phd = pend.pop(0)
                w2(pj, phd, pj == 31)
            for t in range(2):
                r0 = g * G + t * 128
                xr = xres.next()
                k.dma(xr.v(), V(x_d[r0:r0 + 128, :], (xd,)))
                for cc in range(2):
                    k.tt(xr[:, cc * 512:(cc + 1) * 512], ps_y[t * 2 + cc].v(), xr[:, cc * 512:(cc + 1) * 512], ALU.add)
                k.dma(V(xo_d[r0:r0 + 128, :], (xod,)), xr.v())
            hT_cur = hT_next


def xnorm_group(k, c, x_d, xd, g, gt, xin, hn, scr, stats, hTg, ps_t, ntile=4):
    G = ntile * 128
    for t in range(ntile):
        xt = xin.next()
        r0 = g * G + t * 128
        k.dma(xt.v(), V(x_d[r0:r0 + 128, :], (xd,)))
        h = hn.next()
        rmsnorm_tile(k, c, None, xt, gt, h, scr, stats.next())
        pt = ps_t.next()
        ptb = pt.v().bitcast(BF16)
        for kk in range(8):
            k.tr(ptb[:, kk * 128:(kk + 1) * 128], h[:, kk * 128:(kk + 1) * 128], c.ident.v())
        k.copy(hTg[:, :, t * 128:(t + 1) * 128], ptb.re("p (k t) -> p k t", k=8), eng=k.act)


def run_rr(gens):
    gens = list(gens)
    while gens:
        for g_ in list(gens):
            try:
                next(g_)
            except StopIteration:
                gens.remove(g_)


def xnorm_gen(k, c, x_d, xd, g, gt, xin, hn, scr, stats, hTg, ps_t, ntile=4):
    G = ntile * 128
    for t in range(ntile):
        xt = xin.next()
        r0 = g * G + t * 128
        k.dma(xt.v(), V(x_d[r0:r0 + 128, :], (xd,)))
        h = hn.next()
        rmsnorm_tile(k, c, None, xt, gt, h, scr, stats.next())
        yield
        pt = ps_t.next()
        ptb = pt.v().bitcast(BF16)
        for kk in range(8):
            k.tr(ptb[:, kk * 128:(kk + 1) * 128], h[:, kk * 128:(kk + 1) * 128], c.ident.v())
        yield
        k.copy(hTg[:, :, t * 128:(t + 1) * 128], ptb.re("p (k t) -> p k t", k=8), eng=k.act)
        yield


def load_w_bf16(k, ph, w_d, nk, ncols, stg_cols=None):
    wb = ph.split([128, nk, ncols], BF16, 1)
    stg = Rot(ph.sbs([128, ncols], F32, 2))
    wd = Buf(w_d)
    rows = w_d.shape[0]
    for kk in range(nk):
        s = stg.next()
        r = min(128, rows - kk * 128)
        k.dma(s[0:r, :], V(w_d[kk * 128:kk * 128 + r, :], (wd,)))
        k.copy(wb[kk][0:r, :], s[0:r, :], eng=(k.pool, k.dve, k.act)[kk % 3])
    return wb


def fm_qknorm(k, c, ps, M, gcol, outb, sq, lnb, rstd, ps2, hd):
    N = ps.ap.shape[-1]
    k.actf(sq[0:M, 0:N], ps, AF.Square)
    k.mm(ps2[0:M, 0:N], c.ones[0:M, 0:M], sq[0:M, 0:N])
    k.actf(lnb[0:M, 0:N], ps2[0:M, 0:N], AF.Ln, scale=1.0 / hd, bias=c.epscol[0:M, :])
    k.actf(rstd[0:M, 0:N], lnb[0:M, 0:N], AF.Exp, scale=-0.5)
    k.stt(outb, ps, gcol, rstd[0:M, 0:N], ALU.mult, ALU.mult)


EV = dict(qa=0, ka=512, va=640, qi=768, ki=1280, wi=1344, qb=1352, kb=1864, vb=2376, fb=2888)


def phase_even_proj(k, c, sc, x_d, g_row, w_d, qn_a, kn_a, qn_b, kn_b):
    with k.phase() as ph:
        wb = load_w_bf16(k, ph, w_d, 8, 2892)
        wkd = ph.sb([128, 8, 128], BF16)
        for kk in range(8):
            k.copy(wkd[:, kk, 0:64], wb[kk][:, 1280:1344], eng=k.pool)
            k.copy(wkd[:, kk, 64:128], wb[kk][:, 1280:1344], eng=k.pool)
        gt = ph.sb([128, D], F32)
        k.dma(gt.v(), V(g_row.to_broadcast([128, D]), (Buf(g_row),)))
        gcol = ph.sb([128, 4], F32)
        for i, gn in enumerate((qn_a, kn_a, qn_b, kn_b)):
            k.dma(gcol[:, i:i + 1], V(gn.rearrange("(p o) -> p o", o=1), (Buf(gn),)))
        xin = Rot(ph.sbs([128, D], F32, 3))
        scr = ph.sb([128, D], BF16)
        stats = Rot(ph.sbs([128, 4], F32, 4))
        hn = Rot(ph.sbs([128, D], BF16, 2))
        hT = Rot(ph.sbs([128, 8, 512], BF16, 2))
        sq = Rot(ph.sbs([128, 512], BF16, 2))
        lnb = Rot(ph.sbs([128, 512], F32, 2))
        rstd = Rot(ph.sbs([128, 512], F32, 2))
        ob = Rot(ph.sbs([128, 512], BF16, 4))
        of = Rot(ph.sbs([128, 512], F32, 2))
        ps = ph.psum(8)
        psA = Rot(ps[0:3])
        psB = Rot(ps[3:5])
        ps_t = Rot(ps[5:7])
        psC = Rot(ps[7:8])
        xd = Buf(x_d)
        chunks = []
        for h in range(4):
            chunks.append((wb, EV["qa"] + h * 128, 128, 0, sc["qT"][h]))
        chunks.append((wb, EV["ka"], 128, 1, sc["kT"][0]))
        for cc in range(4):
            chunks.append((wb, EV["qi"] + cc * 128, 128, None, sc["qiT"][cc]))
        chunks.append((None, 0, 128, None, sc["kiT"]))
        for h in range(4):
            chunks.append((wb, EV["qb"] + h * 128, 128, 2, sc["qT"][4 + h]))
        for h in range(4):
            chunks.append((wb, EV["kb"] + h * 128, 128, 3, sc["kT"][1 + h]))
        chunks.append((wb, EV["fb"], 4, "f32", sc["fbT"]))
        ci = 0
        for g in range(S // 512):
            hTg = hT.next()
            xnorm_group(k, c, x_d, xd, g, gt, xin, hn, scr, stats, hTg, ps_t)
            tok = slice(g * 512, (g + 1) * 512)
            def stage2(p, M, nrm, dst):
                if nrm == "f32":
                    o = of.next()
                    k.copy(o[0:M, :], p[0:M, :], eng=k.dve)
                    k.dma(V(dst.ap[0:M, tok], dst.bufs), o[0:M, :])
                    return
                o = ob.next()
                if nrm is None:
                    cnt_[0] += 1
                    k.copy(o[0:M, :], p[0:M, :], eng=(k.act if cnt_[0] % 2 else k.dve))
                else:
                    fm_qknorm(k, c, p[0:M, :], M, gcol[:, nrm:nrm + 1], o[0:M, :], sq.next(), lnb.next(),
                              rstd.next(), psB.next(), 128)
                k.dma(V(dst.ap[0:M, tok], dst.bufs), o[0:M, :])
            cnt_ = [0]
            pend = None
            for (wsrc, c0, M, nrm, dst) in chunks:
                p = psA.next()
                for kk in range(8):
                    lhsT = wkd[:, kk, :] if wsrc is None else wb[kk][:, c0:c0 + M]
                    k.mm(p[0:M, :], lhsT, hTg[:, kk, :], start=(kk == 0), stop=(kk == 7))
                if pend is not None:
                    stage2(*pend)
                pend = (p, M, nrm, dst)
            stage2(*pend)
            for t in range(4):
                r0 = g * 512 + t * 128
                tk = slice(t * 128, (t + 1) * 128)
                p = psA.next()
                for kk in range(8):
                    k.mm(p[:, :], hTg[:, kk, tk], wb[kk][:, EV["vb"]:EV["vb"] + 512], start=(kk == 0), stop=(kk == 7))
                o = ob.next()
                k.copy(o[:, :], p[:, :], eng=k.act)
                k.dma(V(sc["vb"].ap[r0:r0 + 128, :], sc["vb"].bufs), o[:, :])
                p = psC.next()
                for kk in range(8):
                    k.mm(p[:, 0:128], hTg[:, kk, tk], wb[kk][:, EV["va"]:EV["va"] + 128], start=(kk == 0), stop=(kk == 7))
                for kk in range(8):
                    k.mm(p[:, 128:136], hTg[:, kk, tk], wb[kk][:, EV["wi"]:EV["wi"] + 8], start=(kk == 0), stop=(kk == 7))
                o = ob.next()
                k.copy(o[:, 0:128], p[:, 0:128], eng=k.dve)
                k.dma(V(sc["va"].ap[r0:r0 + 128, :], sc["va"].bufs), o[:, 0:128])
                o2 = of.next()
                k.copy(o2[:, 0:8], p[:, 128:136], eng=k.dve)
                k.dma(V(sc["wi"].ap[r0:r0 + 128, :], sc["wi"].bufs), o2[:, 0:8])


def split3(k, ph, src, n, outs):
    k.copy(outs[0][0:n, :], src[0:n, :])
    k.tt(src[0:n, :], src[0:n, :], outs[0][0:n, :], ALU.subtract)
    k.copy(outs[1][0:n, :], src[0:n, :])
    k.tt(src[0:n, :], src[0:n, :], outs[1][0:n, :], ALU.subtract)
    k.copy(outs[2][0:n, :], src[0:n, :])


def attn_core(k, c, qg, nkt_fn, qk_fn, P_rot, ps_s, ps_o, ps_d, v_fn, scale, finalize, la=2):
    nkt = 4 * qg + 4
    po = ps_o
    pd = ps_d
    issued = []

    def issue(kt):
        diag = kt >= 4 * qg
        col0 = (kt - 4 * qg) * 128 if diag else 0
        s = ps_s.next()
        qk_fn(s[:, col0:512], kt, col0)
        issued.append((s, col0, diag))
    for kt in range(min(la, nkt)):
        issue(kt)
    for kt in range(nkt):
        if kt + la < nkt:
            issue(kt + la)
        s, col0, diag = issued[kt]
        P = P_rot.next()
        k.actf(P[:, col0:512], s[:, col0:512], AF.Exp, scale=scale)
        if diag:
            k.tt(P[:, col0:col0 + 128], P[:, col0:col0 + 128], c.tri.v(), ALU.mult, eng=k.pool)
        k.mm(po[:, col0:512], v_fn(kt), P[:, col0:512], start=(kt == 0), stop=(kt == nkt - 1))
        k.mm(pd[:, col0:512], c.ones.v(), P[:, col0:512], start=(kt == 0), stop=(kt == nkt - 1))
    finalize(po, pd)


def phase_fox(k, c, sc, b_f):
    SQ = float(np.sqrt(128.0))
    with k.phase() as ph:
        with contextlib.ExitStack() as es2:
            ph2 = Phase(k, ph.name + "a")
            es2.enter_context(ph2.es)
            f0 = ph2.sb([4, S], F32)
            f1 = ph2.sb([4, S], F32)
            bcol = ph2.sb([4, 2], F32)
            one4 = ph2.sb([4, 1], F32)
            pcs = ph2.sbs([4, S], BF16, 3)
            ones4 = ph2.sb([4, S], BF16)
            k.dma(f0.v(), sc["fbT"])
            k.dma(bcol[:, 0:1], V(b_f.rearrange("(p o) -> p o", o=1), (Buf(b_f),)))
            k.ts(bcol[:, 1:2], bcol[:, 0:1], -1.0, ALU.mult)
            k.memset(one4.v(), 1.0)
            k.memset(ones4.v(), 1.0)
            k.actf(f0.v(), f0.v(), AF.Exp, scale=-1.0, bias=bcol[:, 1:2])
            k.actf(f0.v(), f0.v(), AF.Ln, bias=one4.v())
            k.ts(f0.v(), f0.v(), -SQ, ALU.mult)
            k.call(k.dve, "tensor_tensor_scan", out=f1.v(), data0=one4.v().bc([4, S]), data1=f0.v(),
                   initial=0.0, op0=ALU.mult, op1=ALU.add)
            k.copy(f0.v().re("p (b t) -> p b t", t=128), f1.v().re("p (b t) -> p b t", t=128)[:, :, 127:128].bc([4, 32, 128]))
            aug = sc["aug"]
            split3(k, ph2, f0, 4, pcs)
            for p_ in range(3):
                k.dma(V(aug.ap[:, 0, p_, :], aug.bufs), pcs[p_].v())
            k.ts(f1.v(), f1.v(), -1.0, ALU.mult)
            split3(k, ph2, f1, 4, pcs)
            for p_ in range(3):
                k.dma(V(aug.ap[:, 1, 3 + p_, :], aug.bufs), pcs[p_].v())
                k.dma(V(aug.ap[:, 1, p_, :], aug.bufs), ones4.v())
                k.dma(V(aug.ap[:, 0, 3 + p_, :], aug.bufs), ones4.v())
            k.barrier()
        qT = Rot(ph.sbs([128, S], BF16, 2))
        kT = Rot(ph.sbs([128, S], BF16, 2))
        vv = Rot(ph.sbs([128, 32, 128], BF16, 2))
        aq = Rot(ph.sbs([6, S], BF16, 2))
        ak = Rot(ph.sbs([6, S], BF16, 2))
        P_rot = Rot(ph.sbs([128, 512], BF16, 4))
        rden = Rot(ph.sbs([128, 512], F32, 2))
        ob = Rot(ph.sbs([128, 512], BF16, 2))
        ps = ph.psum(8)
        ps_s = Rot(ps[0:3])
        ps_o = Rot(ps[3:5])
        ps_d = Rot(ps[5:7])
        for h in range(4):
            q_, k_, v_, aq_, ak_ = qT.next(), kT.next(), vv.next(), aq.next(), ak.next()
            k.dma(q_.v(), sc["qT"][4 + h])
            k.dma(k_.v(), sc["kT"][1 + h])
            vsrc = sc["vb"]
            k.dma(v_.v(), V(vsrc.ap.rearrange("(t p) (h d) -> p t h d", p=128, h=4)[:, :, h, :], vsrc.bufs))
            k.dma(aq_.v(), V(sc["aug"].ap[h, 0], sc["aug"].bufs))
            k.dma(ak_.v(), V(sc["aug"].ap[h, 1], sc["aug"].bufs))
            for qg in range(8):
                def qk_fn(sv, kt, col0, q_=q_, k_=k_, aq_=aq_, ak_=ak_, qg=qg):
                    qs = slice(qg * 512 + col0, (qg + 1) * 512)
                    ks = slice(kt * 128, (kt + 1) * 128)
                    k.mm(sv, k_[:, ks], q_[:, qs], start=True, stop=False)
                    k.mm(sv, ak_[:, ks], aq_[:, qs], start=False, stop=True)

                def fin(po, pd, h=h, qg=qg):
                    r = rden.next()
                    k.call(k.dve, "reciprocal", out=r.v(), in_=pd.v())
                    o = ob.next()
                    k.tt(o.v(), po.v(), r.v(), ALU.mult)
                    dst = sc["oT"]
                    k.dma(V(dst.ap[512 + h * 128:512 + (h + 1) * 128, qg * 512:(qg + 1) * 512], dst.bufs), o.v())

                attn_core(k, c, qg, None, qk_fn, P_rot, ps_s, ps_o.next(), ps_d.next(),
                          lambda kt, v_=v_: v_[:, kt, :], 1.0 / SQ, fin)


def phase_outproj(k, c, sc, x_d, xo_d, w_d, nfeat):
    nk = nfeat // 128
    with k.phase() as ph:
        wb = load_w_bf16(k, ph, w_d, nk, D)
        oT = Rot(ph.sbs([128, nk, 512], BF16, 2))
        xres = Rot(ph.sbs([128, D], F32, 3))
        ps = ph.psum(8)
        psr = Rot(ps)
        xd = Buf(x_d)
        xod = Buf(xo_d)
        src = sc["oT"]
        for g in range(S // 512):
            o_ = oT.next()
            k.dma(o_.v(), V(src.ap[0:nfeat, g * 512:(g + 1) * 512].rearrange("(k p) s -> p k s", p=128), src.bufs))
            for t in range(4):
                r0 = g * 512 + t * 128
                xr = xres.next()
                k.dma(xr.v(), V(x_d[r0:r0 + 128, :], (xd,)))
                for cc in range(2):
                    p = psr.next()
                    for kk in range(nk):
                        k.mm(p.v(), o_[:, kk, t * 128:(t + 1) * 128], wb[kk][:, cc * 512:(cc + 1) * 512],
                             start=(kk == 0), stop=(kk == nk - 1))
                    k.tt(xr[:, cc * 512:(cc + 1) * 512], p.v(), xr[:, cc * 512:(cc + 1) * 512], ALU.add)
                k.dma(V(xo_d[r0:r0 + 128, :], (xod,)), xr.v())


def bc1(v, n):
    p, f = v.ap.shape
    return V(v.ap.unsqueeze(1).to_broadcast([p, n, f]), v.bufs)


NBIS = 12


def phase_dsa(k, c, sc):
    SCALE = float(128.0 ** -0.5)
    with k.phase() as ph:
        qi = ph.sb([128, 4, S], BF16)
        ki = ph.sb([128, S], BF16)
        qa = ph.sb([128, 4, S], BF16)
        ka = ph.sb([128, S], BF16)
        va = ph.sb([128, 32, 128], BF16)
        wi = ph.sb([128, 32, 8], F32)
        for h in range(4):
            k.dma(qi[:, h, :], sc["qiT"][h])
            k.dma(qa[:, h, :], sc["qT"][h])
        k.dma(ki.v(), sc["kiT"])
        k.dma(ka.v(), sc["kT"][0])
        k.dma(va.v(), V(sc["va"].ap.rearrange("(t p) d -> p t d", p=128), sc["va"].bufs))
        k.dma(wi.v(), V(sc["wi"].ap.rearrange("(t p) d -> p t d", p=128), sc["wi"].bufs))
        scb = ph.sbs([128, S], F32, 3)
        junk_t = ph.sbt([128, S], BF16)
        msk = ph.sbs([128, S], BF16, 3)
        rl = Rot(ph.sbs([128, 512], BF16, 6))
        dg = ph.sbs([128, 8, 128], BF16, 3)
        E = Rot(ph.sbs([128, 512], BF16, 3))
        P = Rot(ph.sbs([128, 512], BF16, 3))
        mT = Rot(ph.sbs([128, 512], BF16, 3))
        stt_ = ph.sbs([128, 64], F32, 3)
        rden = Rot(ph.sbs([128, 512], F32, 2))
        ob = Rot(ph.sbs([128, 512], BF16, 2))
        ps = ph.psum(8)
        ps_i = Rot(ps[0:4])
        ps_acc = Rot([ps[4], ps[7]])
        ps_s = Rot(ps[0:3])
        po = ps[5]
        pd = ps[6]
        thr_of = {}

        def gen_index(qt):
            W = (qt + 1) * 128
            qs = slice(qt * 128, (qt + 1) * 128)
            dgt = dg[qt % 3]
            for h in range(8):
                k.actf(dgt[:, h, :], c.identf.v(), AF.Copy, scale=wi[:, qt, h:h + 1])
            scq = scb[qt % 3]
            for kg in range((W + 511) // 512):
                cols = min(512, W - kg * 512)
                acc = ps_acc.next()
                pend = []
                for h in range(8):
                    p = ps_i.next()
                    pr = slice(64 * (h % 2), 64 * (h % 2) + 64)
                    k.mm(p[:, 0:cols], qi[pr, h // 2, qs], ki[pr, kg * 512:kg * 512 + cols])
                    r = rl.next()
                    k.actf(r[:, 0:cols], p[:, 0:cols], AF.Relu)
                    pend.append((h, r))
                    if len(pend) > 2:
                        h0, r0 = pend.pop(0)
                        k.mm(acc[:, 0:cols], dgt[:, h0, :], r0[:, 0:cols], start=(h0 == 0), stop=(h0 == 7))
                for h0, r0 in pend:
                    k.mm(acc[:, 0:cols], dgt[:, h0, :], r0[:, 0:cols], start=(h0 == 0), stop=(h0 == 7))
                k.copy(scq[:, kg * 512:kg * 512 + cols], acc[:, 0:cols], eng=k.act)
                yield
            k.tt(scq[:, qt * 128:W], scq[:, qt * 128:W], c.negtri.v(), ALU.add, eng=k.pool)
            yield

        def gen_bisect(qt):
            W = (qt + 1) * 128
            scq = scb[qt % 3]
            st = stt_[qt % 3]
            if qt >= 2:
                k.call(k.dve, "tensor_reduce", out=st[:, 0:1], in_=scq[:, 0:W], axis=AX.X, op=ALU.max)
                yield
                k.call(k.dve, "tensor_reduce", out=st[:, 1:2], in_=scq[:, 0:qt * 128], axis=AX.X, op=ALU.min)
                k.ts(st[:, 1:2], st[:, 1:2], -1.0, ALU.add)
                k.tt(st[:, 2:3], st[:, 0:1], st[:, 1:2], ALU.subtract)
                k.ts(st[:, 8:8 + NBIS + 1], c.pow2[:, 0:NBIS + 1], st[:, 2:3], ALU.mult)
                k.ts(st[:, 32:32 + NBIS + 1], st[:, 8:8 + NBIS + 1], 2.0, ALU.mult)
                k.tt(st[:, 3:4], st[:, 1:2], st[:, 8:9], ALU.add)
                yield
                for it in range(NBIS):
                    k.call(k.dve, "tensor_scalar", out=junk_t[:, 0:W], in0=scq[:, 0:W], scalar1=st[:, 3:4], scalar2=None,
                           op0=ALU.is_gt, op1=ALU.add, accum_out=st[:, 4:5])
                    yield
                    k.stt(st[:, 5:6], st[:, 4:5], 255.5, st[:, 32 + it + 1:32 + it + 2], ALU.is_gt, ALU.mult)
                    k.stt(st[:, 3:4], st[:, 5:6], st[:, 8 + it + 1:8 + it + 2], st[:, 3:4], ALU.subtract, ALU.add)
                k.tt(st[:, 6:7], st[:, 3:4], st[:, 8 + NBIS:8 + NBIS + 1], ALU.subtract)
                thr = st[:, 6:7]
            else:
                thr = c.negbig.v()
            m = msk[qt % 3]
            k.ts(m[:, 0:W], scq[:, 0:W], thr, ALU.is_le, -30000.0, ALU.mult)
            yield

        def gen_attn(qt):
            qs = slice(qt * 128, (qt + 1) * 128)
            m = msk[qt % 3]
            nkt = qt + 1
            issued = {}

            def issue(kt):
                s = ps_s.next()
                sv = s.v().re("p (h q) -> p h q", h=4)
                k.mm(sv, ka[:, kt * 128:(kt + 1) * 128], qa[:, :, qs], start=True, stop=False)
                k.mm(s.v(), m[:, kt * 128:(kt + 1) * 128], c.i4.v(), start=False, stop=True)
                issued[kt] = s
            for kt in range(min(2, nkt)):
                issue(kt)
            for kt in range(nkt):
                if kt + 2 < nkt:
                    issue(kt + 2)
                s = issued.pop(kt)
                p_ = P.next()
                k.actf(p_.v(), s.v(), AF.Exp, scale=SCALE)
                k.mm(po.v(), va[:, kt, :], p_.v(), start=(kt == 0), stop=(kt == qt))
                k.mm(pd.v(), c.ones.v(), p_.v(), start=(kt == 0), stop=(kt == qt))
                yield
            r = rden.next()
            k.call(k.dve, "reciprocal", out=r.v(), in_=pd.v())
            o = ob.next()
            k.tt(o.v(), po.v(), r.v(), ALU.mult)
            dst = sc["oT"]
            k.dma(V(dst.ap[0:512, qs].rearrange("(h d) q -> d h q", d=128), dst.bufs),
                  o.v().re("p (h q) -> p h q", h=4))
            yield

        def chain(*gs):
            for g_ in gs:
                yield from g_
        HALF = (NBIS + 4) // 2
        bgen = {}
        for step in range(-3, NT):
            tasks = []
            lane1 = []
            if 0 <= step + 3 < NT:
                lane1.append(gen_index(step + 3))
            if 0 <= step < NT:
                lane1.append(gen_attn(step))
            if lane1:
                tasks.append([chain(*lane1), None])
            if 0 <= step + 2 < NT:
                bgen[step + 2] = gen_bisect(step + 2)
                tasks.append([bgen[step + 2], HALF])
            if 0 <= step + 1 < NT:
                tasks.append([bgen.pop(step + 1), None])
            while tasks:
                for tk_ in list(tasks):
                    try:
                        next(tk_[0])
                        if tk_[1] is not None:
                            tk_[1] -= 1
                            if tk_[1] <= 0:
                                tasks.remove(tk_)
                    except StopIteration:
                        tasks.remove(tk_)


OD = dict(cq=0, ckv=384, kr=640, z=704, xs=1728, B=2752, C=3264, dt=3776)
PI = float(np.pi)


def phase_rope_tables(k, c, sc, pos_d):
    with k.phase() as ph:
        pi_ = ph.sb([64, S], I32)
        ang = ph.sb([64, S], F32)
        u = ph.sb([64, S], F32)
        ni = ph.sb([64, S], I32)
        r = ph.sb([64, S], F32)
        k.dma(pi_.v(), V(pos_d.rearrange("(o s) -> o s", o=1).to_broadcast([64, S]), (Buf(pos_d),)))
        k.copy(ang.v(), pi_.v())
        k.ts(ang.v(), ang.v(), c.invf.v(), ALU.mult)
        for name, shift in (("sin", 0.0), ("cos", PI / 2)):
            k.ts(r.v(), ang.v(), shift, ALU.add)
            k.ts(u.v(), r.v(), 1.0 / (2 * PI), ALU.mult)
            k.copy(ni.v(), u.v())
            k.copy(u.v(), ni.v())
            k.stt(r.v(), u.v(), -2 * PI, r.v(), ALU.mult, ALU.add)
            k.ts(u.v(), r.v(), PI, ALU.is_gt, 2 * PI, ALU.mult)
            k.tt(r.v(), r.v(), u.v(), ALU.subtract)
            k.ts(u.v(), r.v(), -PI, ALU.is_lt, 2 * PI, ALU.mult)
            k.tt(r.v(), r.v(), u.v(), ALU.add)
            k.ts(r.v(), r.v(), 3.1415925, ALU.min, -3.1415925, ALU.max)
            k.actf(u.v(), r.v(), AF.Sin)
            k.dma(sc[name], u.v())


def col_load(k, dst, src_ap, n):
    k.dma(dst, V(src_ap.rearrange("(p o) -> p o", o=1), (Buf(src_ap),)))


ODDLVL = [9]


def phase_odd_proj(k, c, sc, x_d, g_row, w_d, cqn, ckvn, wuq_d, wukv_d, qn_c, kn_c, conv_w, conv_b):
    with k.phase() as ph:
        stg = Rot(ph.sbs([128, 1896], F32, 3))
        ncast = [0]

        def loadw(w_ap, nk, ncols):
            wb_ = ph.split([128, nk, ncols], BF16, 1)
            wd = Buf(w_ap)
            for kk in range(nk):
                for c0 in range(0, ncols, 1896):
                    c1 = min(ncols, c0 + 1896)
                    s = stg.next()
                    k.dma(s[:, 0:c1 - c0], V(w_ap[kk * 128:(kk + 1) * 128, c0:c1], (wd,)))
                    ncast[0] += 1
                    k.copy(wb_[kk][:, c0:c1], s[:, 0:c1 - c0], eng=(k.pool, k.dve, k.act)[ncast[0] % 3])
            return wb_
        wb = loadw(w_d, 8, 3792)
        wuq = loadw(wuq_d, 3, 768)
        wukv = loadw(wukv_d, 2, 1024)
        gt = ph.sb([128, D], F32)
        k.dma(gt.v(), V(g_row.to_broadcast([128, D]), (Buf(g_row),)))
        gc = ph.sb([128, 16], F32)
        for i in range(3):
            col_load(k, gc[:, i:i + 1], cqn[i * 128:(i + 1) * 128], 128)
        for i in range(2):
            col_load(k, gc[:, 3 + i:4 + i], ckvn[i * 128:(i + 1) * 128], 128)
        col_load(k, gc[:, 5:6], qn_c[0:128], 128)
        col_load(k, gc[0:64, 6:7], qn_c[128:192], 64)
        col_load(k, gc[:, 7:8], kn_c[0:128], 128)
        col_load(k, gc[0:64, 8:9], kn_c[128:192], 64)
        cw = ph.sb([128, 16, 4], F32)
        cb = ph.sb([128, 16], F32)
        cwd = Buf(conv_w)
        cbd = Buf(conv_b)
        for j in range(16):
            k.dma(cw[:, j, :], V(conv_w[:, j * 128:(j + 1) * 128].rearrange("w p -> p w"), (cwd,)),
                  allow_slow_non_contiguous=True)
            k.dma(cb[:, j:j + 1], V(conv_b[j * 128:(j + 1) * 128].rearrange("(p o) -> p o", o=1), (cbd,)))
        hal = ph.split([128, 16, 3], F32, 1)
        for j in range(16):
            k.memset(hal[j][:, :], 0.0, eng=k.pool)
        xin = Rot(ph.sbs([128, D], F32, 3))
        scr = ph.sb([128, D], BF16)
        stats = Rot(ph.sbs([128, 4], F32, 4))
        hn = Rot(ph.sbs([128, D], BF16, 2))
        hT = Rot(ph.sbs([128, 8, 512], BF16, 2))
        sq = Rot(ph.sbs([128, 512], BF16, 6))
        lnb = Rot(ph.sbs([128, 512], F32, 2))
        rstd = Rot(ph.sbs([128, 512], F32, 2))
        ob = Rot(ph.sbs([128, 512], BF16, 4))
        of = Rot(ph.sbs([128, 16], F32, 3))
        cqraw = ph.sb([128, 3, 512], F32)
        cqn_b = ph.sb([128, 3, 512], BF16)
        ckvraw = ph.sb([128, 2, 512], F32)
        ckvn_b = ph.sb([128, 2, 512], BF16)
        krraw = ph.sb([64, 512], F32)
        sqkr = ph.sb([64, 512], BF16)
        xr = Rot(ph.sbs([128, 515], F32, 2))
        acc = Rot(ph.sbs([128, 512], F32, 2))
        xact = Rot(ph.sbs([128, 512], BF16, 3))
        xtm = Rot(ph.sbs([128, 4, 128], BF16, 2))
        cs = ph.sb([64, 2, 512], F32)
        yb = Rot(ph.sbs([64, 512], BF16, 2))
        t1 = Rot(ph.sbs([64, 512], F32, 2))
        t2 = Rot(ph.sbs([64, 512], F32, 2))
        ps = ph.psum(8)
        psB = Rot(ps[2:4])
        xd = Buf(x_d)

        def grpnorm(raws, sqs, nch, hd, gcol0, outb):
            p2 = psB.next()
            for i in range(nch):
                k.mm(p2.v(), c.ones.v(), sqs[i].v(), start=(i == 0), stop=(i == nch - 1))
            l_, r_ = lnb.next(), rstd.next()
            k.actf(l_.v(), p2.v(), AF.Ln, scale=1.0 / hd, bias=c.epscol.v())
            k.actf(r_.v(), l_.v(), AF.Exp, scale=-0.5)
            for i in range(nch):
                k.stt(outb[:, i, :], raws[:, i, :], gc[:, gcol0 + i:gcol0 + i + 1], r_.v(), ALU.mult, ALU.mult)

        def rope(ybv, dst, tok):
            p = psB.next()
            k.mm(p[0:64, :], c.rotm.v(), ybv)
            a, b = t1.next(), t2.next()
            k.tt(a.v(), ybv, cs[:, 0, :], ALU.mult)
            k.tt(b.v(), p[0:64, :], cs[:, 1, :], ALU.mult)
            o = ob.next()
            k.tt(o[0:64, :], a.v(), b.v(), ALU.add)
            k.dma(V(dst.ap[:, tok], dst.bufs), o[0:64, :])

        def headnorm(pn, sq_r, gcol_n):
            sqn = sq.next()
            k.actf(sqn.v(), pn.v(), AF.Square)
            p2 = psB.next()
            k.mm(p2.v(), c.ones.v(), sqn.v(), start=True, stop=False)
            k.mm(p2.v(), c.ones[0:64, :], sq_r, start=False, stop=True)
            l_, r_ = lnb.next(), rstd.next()
            k.actf(l_.v(), p2.v(), AF.Ln, scale=1.0 / 192, bias=c.epscol.v())
            k.actf(r_.v(), l_.v(), AF.Exp, scale=-0.5)
            return r_

        psA = Rot(ps[0:2])
        psB = Rot(ps[2:4])
        psBx = Rot(ps[4:5])
        psBt = Rot(ps[5:6])
        psC = Rot(ps[6:7])
        ps_t = Rot(ps[7:8])
        obC = Rot(ph.sbs([128, 512], BF16, 2))

        def proj(pool, hTg, c0, M):
            p = pool.next()
            for kk in range(8):
                k.mm(p[0:M, :], wb[kk][:, c0:c0 + M], hTg[:, kk, :], start=(kk == 0), stop=(kk == 7))
            return p

        def laneA(g, hTg):
            tok = slice(g * 512, (g + 1) * 512)
            sqs = []
            for i in range(3):
                p = proj(psA, hTg, OD["cq"] + i * 128, 128)
                s_ = sq.next()
                k.actf(s_.v(), p.v(), AF.Square)
                k.copy(cqraw[:, i, :], p.v(), eng=k.dve)
                sqs.append(s_)
                yield
            grpnorm(cqraw, sqs, 3, 384, 0, cqn_b)
            yield
            sqs = []
            for i in range(2):
                p = proj(psA, hTg, OD["ckv"] + i * 128, 128)
                s_ = sq.next()
                k.actf(s_.v(), p.v(), AF.Square)
                k.copy(ckvraw[:, i, :], p.v(), eng=k.dve)
                sqs.append(s_)
                yield
            grpnorm(ckvraw, sqs, 2, 256, 3, ckvn_b)
            yield
            p = proj(psA, hTg, OD["kr"], 64)
            k.copy(krraw.v(), p[0:64, :], eng=k.dve)
            yield
            for h in range(4):
                pn = psA.next()
                for kc in range(3):
                    k.mm(pn.v(), wuq[kc][:, h * 192:h * 192 + 128], cqn_b[:, kc, :], start=(kc == 0), stop=(kc == 2))
                pr = psA.next()
                for kc in range(3):
                    k.mm(pr[0:64, :], wuq[kc][:, h * 192 + 128:h * 192 + 192], cqn_b[:, kc, :], start=(kc == 0), stop=(kc == 2))
                yield
                sqr = sq.next()
                k.actf(sqr[0:64, :], pr[0:64, :], AF.Square)
                yield
                r_ = headnorm(pn, sqr[0:64, :], 5)
                yield
                o = ob.next()
                k.stt(o.v(), pn.v(), gc[:, 5:6], r_.v(), ALU.mult, ALU.mult)
                k.dma(V(sc["qT"][h].ap[:, tok], sc["qT"][h].bufs), o.v())
                y_ = yb.next()
                k.stt(y_.v(), pr[0:64, :], gc[0:64, 6:7], r_[0:64, :], ALU.mult, ALU.mult)
                yield
                rope(y_.v(), sc["qr"][h], tok)
                yield
            k.actf(sqkr.v(), krraw.v(), AF.Square)
            for h in range(4):
                pn = psA.next()
                for kc in range(2):
                    k.mm(pn.v(), wukv[kc][:, h * 256:h * 256 + 128], ckvn_b[:, kc, :], start=(kc == 0), stop=(kc == 1))
                yield
                r_ = headnorm(pn, sqkr.v(), 7)
                yield
                o = ob.next()
                k.stt(o.v(), pn.v(), gc[:, 7:8], r_.v(), ALU.mult, ALU.mult)
                k.dma(V(sc["kT"][h].ap[:, tok], sc["kT"][h].bufs), o.v())
                y_ = yb.next()
                k.stt(y_.v(), krraw.v(), gc[0:64, 8:9], r_[0:64, :], ALU.mult, ALU.mult)
                yield
                rope(y_.v(), sc["kr"][h], tok)
                yield
            for t in range(4):
                r0 = g * 512 + t * 128
                tk = slice(t * 128, (t + 1) * 128)
                p = psA.next()
                for kc in range(2):
                    k.mm(p.v().re("p (h d) -> p h d", h=4), ckvn_b[:, kc, tk],
                         wukv[kc][:, :].re("p (h d) -> p h d", h=4)[:, :, 128:256], start=(kc == 0), stop=(kc == 1))
                o = ob.next()
                k.copy(o.v(), p.v(), eng=k.act)
                k.dma(V(sc["vb"].ap[r0:r0 + 128, :], sc["vb"].bufs), o.v())
                yield

        def laneB(g, hTg):
            tok = slice(g * 512, (g + 1) * 512)

            def stage1(j):
                p = proj(psBx, hTg, OD["xs"] + j * 128, 128)
                x_ = xr.next()
                k.copy(x_[:, 0:3], hal[j][:, :], eng=k.pool)
                k.copy(x_[:, 3:515], p.v(), eng=k.act)
                k.copy(hal[j][:, :], x_[:, 512:515], eng=k.pool)
                yield
                a_ = acc.next()
                k.ts(a_.v(), x_[:, 0:512], cw[:, j, 0:1], ALU.mult)
                for w in range(1, 4):
                    k.stt(a_.v(), x_[:, w:w + 512], cw[:, j, w:w + 1], a_.v(), ALU.mult, ALU.add)
                yield
                xa_ = xact.next()
                k.actf(xa_.v(), a_.v(), AF.Silu, bias=cb[:, j:j + 1])
                if j >= 8:
                    dst = sc["BCT"]
                    k.dma(V(dst.ap[(j - 8) * 128:(j - 7) * 128, tok], dst.bufs), xa_.v())
                yield
                return xa_

            def stage2(j, xa_):
                if j >= 12:
                    return
                pt = psBt.next()
                ptb = pt.v().bitcast(BF16)
                for t in range(4):
                    k.tr(ptb[:, t * 128:(t + 1) * 128], xa_[:, t * 128:(t + 1) * 128], c.ident.v())
                yield
                xt_ = xtm.next()
                k.copy(xt_.v(), ptb[:, 0:512].re("p (t c) -> p t c", t=4), eng=k.dve)
                dst = sc["xsB"]
                k.dma(V(dst.ap[tok, j * 128:(j + 1) * 128].rearrange("(t p) c -> p t c", p=128), dst.bufs), xt_.v())
                yield
            prev = None
            for j in range(16):
                xa_ = yield from stage1(j)
                if prev is not None:
                    yield from stage2(*prev)
                prev = (j, xa_)
            yield from stage2(*prev)

        def laneC(g, hTg):
            for t in range(4):
                r0 = g * 512 + t * 128
                tk = slice(t * 128, (t + 1) * 128)
                for hf in range(2):
                    p = psC.next()
                    for kk in range(8):
                        k.mm(p.v(), hTg[:, kk, tk], wb[kk][:, OD["z"] + hf * 512:OD["z"] + (hf + 1) * 512],
                             start=(kk == 0), stop=(kk == 7))
                    yield
                    o = obC.next()
                    k.actf(o.v(), p.v(), AF.Silu)
                    k.dma(V(sc["zs"].ap[r0:r0 + 128, hf * 512:(hf + 1) * 512], sc["zs"].bufs), o.v())
                    yield
                p = psC.next()
                for kk in range(8):
                    k.mm(p[:, 0:16], hTg[:, kk, tk], wb[kk][:, OD["dt"]:OD["dt"] + 16], start=(kk == 0), stop=(kk == 7))
                yield
                o2 = of.next()
                k.copy(o2[:, 0:16], p[:, 0:16], eng=k.dve)
                k.dma(V(sc["dt"].ap[r0:r0 + 128, :], sc["dt"].bufs), o2[:, 0:16])
                yield

        NG = S // 512
        hTs = [None] * NG
        hTs[0] = hT.next()
        run_rr([xnorm_gen(k, c, x_d, xd, 0, gt, xin, hn, scr, stats, hTs[0], ps_t)])
        for g in range(NG):
            tok = slice(g * 512, (g + 1) * 512)
            k.dma(cs[:, 0, :], V(sc["cos"].ap[:, tok], sc["cos"].bufs))
            k.dma(cs[:, 1, :], V(sc["sin"].ap[:, tok], sc["sin"].bufs))
            gens = [laneA(g, hTs[g]), laneB(g, hTs[g]), laneC(g, hTs[g])]
            if g + 1 < NG:
                hTs[g + 1] = hT.next()
                gens.append(xnorm_gen(k, c, x_d, xd, g + 1, gt, xin, hn, scr, stats, hTs[g + 1], ps_t))
            run_rr(gens)


def phase_mla(k, c, sc):
    SCALE = float(192.0 ** -0.5)
    with k.phase() as ph:
        qT = Rot(ph.sbs([128, S], BF16, 2))
        kT = Rot(ph.sbs([128, S], BF16, 2))
        qr = Rot(ph.sbs([64, S], BF16, 2))
        kr = Rot(ph.sbs([64, S], BF16, 2))
        vv = Rot(ph.sbs([128, 32, 128], BF16, 2))
        P_rot = Rot(ph.sbs([128, 512], BF16, 4))
        rden = Rot(ph.sbs([128, 512], F32, 2))
        ob = Rot(ph.sbs([128, 512], BF16, 2))
        ps = ph.psum(8)
        ps_s = Rot(ps[0:3])
        ps_o = Rot(ps[3:5])
        ps_d = Rot(ps[5:7])
        for h in range(4):
            q_, k_, qr_, kr_, v_ = qT.next(), kT.next(), qr.next(), kr.next(), vv.next()
            k.dma(q_.v(), sc["qT"][h])
            k.dma(k_.v(), sc["kT"][h])
            k.dma(qr_.v(), sc["qr"][h])
            k.dma(kr_.v(), sc["kr"][h])
            vsrc = sc["vb"]
            k.dma(v_.v(), V(vsrc.ap.rearrange("(t p) (h d) -> p t h d", p=128, h=4)[:, :, h, :], vsrc.bufs))
            for qg in range(8):
                def qk_fn(sv, kt, col0, q_=q_, k_=k_, qr_=qr_, kr_=kr_, qg=qg):
                    qs = slice(qg * 512 + col0, (qg + 1) * 512)
                    ks = slice(kt * 128, (kt + 1) * 128)
                    k.mm(sv, k_[:, ks], q_[:, qs], start=True, stop=False)
                    k.mm(sv, kr_[:, ks], qr_[:, qs], start=False, stop=True)

                def fin(po, pd, h=h, qg=qg):
                    r = rden.next()
                    k.call(k.dve, "reciprocal", out=r.v(), in_=pd.v())
                    o = ob.next()
                    k.tt(o.v(), po.v(), r.v(), ALU.mult)
                    dst = sc["oT"]
                    k.dma(V(dst.ap[h * 128:(h + 1) * 128, qg * 512:(qg + 1) * 512], dst.bufs), o.v())

                attn_core(k, c, qg, None, qk_fn, P_rot, ps_s, ps_o.next(), ps_d.next(),
                          lambda kt, v_=v_: v_[:, kt, :], SCALE, fin)


def bcl(v, n):
    p, h = v.ap.shape
    return V(v.ap.unsqueeze(2).to_broadcast([p, h, n]), v.bufs)


def phase_ssd(k, c, sc, dt_bias, a_log, d_skip, gate_norm):
    with k.phase() as ph:
        rep = ph.sb([128, 64], F32)
        for i, src in enumerate((dt_bias, a_log, d_skip)):
            k.dma(rep[:, i * 16:(i + 1) * 16], V(src.rearrange("(o h) -> o h", o=1).to_broadcast([128, 16]), (Buf(src),)))
        k.actf(rep[:, 16:32], rep[:, 16:32], AF.Exp)
        k.ts(rep[:, 16:32], rep[:, 16:32], -1.0, ALU.mult)
        one = ph.sb([128, 1], F32)
        k.memset(one.v(), 1.0)
        gg = ph.sb([128, D], F32)
        k.dma(gg.v(), V(gate_norm.rearrange("(o h) -> o h", o=1).to_broadcast([128, D]), (Buf(gate_norm),)))
        negtril = ph.sb([128, 128], BF16)
        tmpf = ph.sb([128, 128], F32)
        k.dma(tmpf.v(), c.negtril_d)
        k.copy(negtril.v(), tmpf.v())
        hst = ph.sb([128, D], F32)
        hstb = ph.sb([128, D], BF16)
        k.memset(hst.v(), 0.0)
        k.memset(hstb.v(), 0.0)
        xs = Rot(ph.sbs([128, D], BF16, 2))
        Btm = Rot(ph.sbs([128, 4, 128], BF16, 2))
        BT = Rot(ph.sbs([128, 4, 128], BF16, 2))
        CT = Rot(ph.sbs([128, 4, 128], BF16, 2))
        zs = Rot(ph.sbs([128, D], BF16, 2))
        dtr = Rot(ph.sbs([128, 16], F32, 2))
        sm = Rot(ph.sbs([128, 128], F32, 2))
        LT = Rot(ph.sbs([128, 16, 128], BF16, 2))
        MT = Rot(ph.sbs([128, 16, 128], BF16, 2))
        cbt = Rot(ph.sbs([128, 4, 128], BF16, 2))
        xdr = Rot(ph.sbs([128, D], BF16, 2))
        xddr = Rot(ph.sbs([128, D], BF16, 2))
        yr = Rot(ph.sbs([128, D], F32, 2))
        t2r = Rot(ph.sbs([128, D], F32, 2))
        junk = ph.sb([128, 256], BF16)
        ynr = Rot(ph.sbs([128, D], BF16, 2))
        oTt = Rot(ph.sbs([128, 8, 128], BF16, 2))
        ps = Rot(ph.psum(8))
        ydr = Rot(ph.sbs([128, D], F32, 2))

        def part1(ci):
            r0 = ci * 128
            tok = slice(r0, r0 + 128)
            xs_, Btm_, BT_, CT_, zs_, dtr_ = xs.next(), Btm.next(), BT.next(), CT.next(), zs.next(), dtr.next()
            xsB = sc["xsB"]
            k.dma(xs_.v(), V(xsB.ap[tok, 0:1024], xsB.bufs))
            k.dma(Btm_.v().re("p g n -> p (g n)"), V(xsB.ap[tok, 1024:1536], xsB.bufs))
            bct = sc["BCT"]
            k.dma(BT_.v(), V(bct.ap[0:512, tok].rearrange("(g n) t -> n g t", n=128), bct.bufs))
            k.dma(CT_.v(), V(bct.ap[512:1024, tok].rearrange("(g n) t -> n g t", n=128), bct.bufs))
            k.dma(zs_.v(), V(sc["zs"].ap[tok, :], sc["zs"].bufs))
            k.dma(dtr_.v(), V(sc["dt"].ap[tok, :], sc["dt"].bufs))
            s_ = sm.next()
            k.tt(s_[:, 0:16], dtr_.v(), rep[:, 0:16], ALU.add)
            k.actf(s_[:, 0:16], s_[:, 0:16], AF.Exp)
            k.actf(s_[:, 0:16], s_[:, 0:16], AF.Ln, bias=one.v())
            k.tt(s_[:, 16:32], s_[:, 0:16], rep[:, 16:32], ALU.mult)
            yield
            pc = ps.next()
            k.mm(pc[:, 0:16], c.trif.v(), s_[:, 16:32])
            k.mm(pc[:, 16:32], c.onesf.v(), s_[:, 16:32])
            k.copy(s_[:, 32:48], pc[:, 0:16])
            k.ts(s_[:, 48:64], pc[:, 0:16], -1.0, ALU.mult)
            k.actf(s_[:, 64:80], pc[:, 0:16], AF.Exp)
            k.tt(s_[:, 112:128], pc[:, 16:32], s_[:, 32:48], ALU.subtract)
            k.actf(s_[:, 80:96], s_[:, 112:128], AF.Exp)
            k.actf(s_[:, 96:112], pc[:, 16:32], AF.Exp)
            yield
            LT_ = LT.next()
            for q4 in range(4):
                pl = ps.next()
                for i in range(4):
                    h = 4 * q4 + i
                    k.mm(pl[:, i * 128:(i + 1) * 128], s_[:, 16 + h:17 + h].bc([128, 128]), c.trif.v(), start=True, stop=False)
                    k.mm(pl[:, i * 128:(i + 1) * 128], c.ident.v(), negtril.v(), start=False, stop=True)
                    k.actf(LT_[:, h, :], pl[:, i * 128:(i + 1) * 128], AF.Exp, bias=s_[:, 48 + h:49 + h])
                yield
            pcb = ps.next()
            for g in range(4):
                k.mm(pcb[:, g * 128:(g + 1) * 128], BT_[:, g, :], CT_[:, g, :])
            cbt_ = cbt.next()
            k.copy(cbt_.v().re("p g n -> p (g n)"), pcb.v(), eng=k.act)
            yield
            MT_ = MT.next()
            for g in range(4):
                k.tt(MT_[:, 4 * g:4 * g + 4, :], LT_[:, 4 * g:4 * g + 4, :], bc1(cbt_[:, g, :], 4), ALU.mult)
            yield
            xd_, xdd_ = xdr.next(), xddr.next()
            k.tt(xd_.v().re("l (h p) -> l h p", p=64), xs_.v().re("l (h p) -> l h p", p=64), bcl(s_[:, 0:16], 64), ALU.mult)
            k.tt(xdd_.v().re("l (h p) -> l h p", p=64), xd_.v().re("l (h p) -> l h p", p=64), bcl(s_[:, 80:96], 64), ALU.mult)
            t2_ = t2r.next()
            k.tt(t2_.v().re("l (h p) -> l h p", p=64), xs_.v().re("l (h p) -> l h p", p=64), bcl(rep[:, 32:48], 64), ALU.mult, eng=k.pool)
            yield
            yd_ = ydr.next()
            for hf in range(2):
                hs = slice(hf * 512, (hf + 1) * 512)
                py = ps.next()
                for hh in range(8):
                    h = hf * 8 + hh
                    k.mm(py[:, hh * 64:(hh + 1) * 64], MT_[:, h, :], xd_[:, h * 64:(h + 1) * 64])
                k.tt(yd_[:, hs], py.v(), t2_[:, hs], ALU.add)
                yield
            return (tok, s_, CT_, Btm_, xdd_, zs_, yd_)

        def part2(st):
            tok, s_, CT_, Btm_, xdd_, zs_, yd_ = st
            y_ = yr.next()
            for hf in range(2):
                hs = slice(hf * 512, (hf + 1) * 512)
                po = ps.next()
                for gg_ in range(2):
                    g = hf * 2 + gg_
                    k.mm(po[:, gg_ * 256:(gg_ + 1) * 256], CT_[:, g, :], hstb[:, g * 256:(g + 1) * 256])
                k.tt(y_[:, hs].re("l (h p) -> l h p", p=64), po.v().re("l (h p) -> l h p", p=64),
                     bcl(s_[:, 64 + hf * 8:72 + hf * 8], 64), ALU.mult)
                k.tt(y_[:, hs], y_[:, hs], yd_[:, hs], ALU.add)
                yield
            for hf in range(2):
                hs = slice(hf * 512, (hf + 1) * 512)
                pst = ps.next()
                for gg_ in range(2):
                    g = hf * 2 + gg_
                    k.mm(pst[:, gg_ * 256:(gg_ + 1) * 256], Btm_[:, g, :], xdd_[:, g * 256:(g + 1) * 256])
                k.tt(hst[:, hs].re("l (h p) -> l h p", p=64), hst[:, hs].re("l (h p) -> l h p", p=64),
                     bcl(s_[:, 96 + hf * 8:104 + hf * 8], 64), ALU.mult, eng=k.pool)
                k.tt(hst[:, hs], hst[:, hs], pst.v(), ALU.add)
                k.copy(hstb[:, hs], hst[:, hs], eng=k.act)
                yield
            k.tt(y_.v(), y_.v(), zs_.v(), ALU.mult)
            for g in range(4):
                k.actf(junk.v(), y_[:, g * 256:(g + 1) * 256], AF.Square, accum_out=s_[:, 112 + g:113 + g])
            yield
            k.ts(s_[:, 116:120], s_[:, 112:116], 1.0 / 256, ALU.mult, EPS, ALU.add)
            k.tt(s_[:, 120:124], s_[:, 116:120], c.mhalf.v().bc([128, 4]), ALU.pow, eng=k.pool)
            yn_ = ynr.next()
            for g in range(4):
                gs = slice(g * 256, (g + 1) * 256)
                k.stt(yn_[:, gs], y_[:, gs], s_[:, 120 + g:121 + g], gg[:, gs], ALU.mult, ALU.mult)
            yield
            pt = ps.next()
            ptb = pt.v().bitcast(BF16)
            for j in range(8):
                k.tr(ptb[:, j * 128:(j + 1) * 128], yn_[:, j * 128:(j + 1) * 128], c.ident.v())
            o_ = oTt.next()
            k.copy(o_.v().re("p j t -> p (j t)"), ptb, eng=k.act)
            dst = sc["oT"]
            k.dma(V(dst.ap[512:1536, tok].rearrange("(j p) t -> p j t", p=128), dst.bufs), o_.v())
            yield

        states = {}

        def p1(ci):
            states[ci] = yield from part1(ci)
        run_rr([p1(0)])
        for ci in range(NT):
            gens = [part2(states.pop(ci))]
            if ci + 1 < NT:
                gens.append(p1(ci + 1))
            run_rr(gens)


def const_arrays():
    cd = {}
    cd["ident"] = np.eye(128, dtype=np.float32)
    cd["pow2"] = np.tile((2.0 ** -(np.arange(32) + 1.0)).astype(np.float32)[None, :], (128, 1))
    invf = (10000.0 ** (-np.arange(32, dtype=np.float32) / 32)).astype(np.float32)
    cd["invf"] = np.concatenate([invf, invf])[:, None].astype(np.float32)
    rot = np.zeros((64, 64), np.float32)
    for m in range(32):
        rot[m + 32, m] = -1.0
        rot[m, m + 32] = 1.0
    cd["rotm"] = rot
    cd["negtril"] = (np.tril(np.ones((128, 128), np.float32), -1) * -30000.0).astype(np.float32)
    cd["tri"] = np.triu(np.ones((128, 128), np.float32))
    cd["negtri"] = (np.triu(np.ones((128, 128), np.float32), 1) * -1e30).astype(np.float32)
    return cd


INPUT_NAMES = ["x", "positions", "ev_norm", "ev_w_in", "ev_b_f", "ev_qn_a", "ev_kn_a", "ev_qn_b", "ev_kn_b",
               "ev_w_out", "od_norm", "od_w_in", "od_cq_norm", "od_ckv_norm", "od_w_uq", "od_w_ukv", "od_qn_c",
               "od_kn_c", "od_conv_w", "od_conv_b", "od_dt_bias", "od_a_log", "od_d_skip", "od_gate_norm",
               "od_w_out", "mlp_norm", "mlp_w1", "mlp_w2"]


def build(shapes, cfg):
    nc = bass.Bass("TRN2", target_bir_lowering=False)
    din = {}
    for name, (shp, dt) in shapes.items():
        bdt = I32 if np.dtype(dt) == np.int32 else F32
        din[name] = nc.dram_tensor(name, list(shp), bdt, kind="ExternalInput").ap()
    cds = const_arrays()
    cd = {n: nc.dram_tensor("c_" + n, list(a.shape), F32, kind="ExternalInput").ap() for n, a in cds.items()}
    y = nc.dram_tensor("y", [S, D], F32, kind="ExternalOutput").ap()
    xa = nc.dram_tensor("xa", [S, D], F32, kind="Internal").ap()
    xb = nc.dram_tensor("xb", [S, D], F32, kind="Internal").ap()

    def mk(name, shape, dt):
        return nc.dram_tensor(name, list(shape), dt, kind="Internal").ap()

    def dv(ap):
        return V(ap, (Buf(ap),))
    sc = {}
    t = mk("s_qT", [8, 128, S], BF16)
    sc["qT"] = [dv(t[h]) for h in range(8)]
    t = mk("s_kT", [5, 128, S], BF16)
    sc["kT"] = [dv(t[h]) for h in range(5)]
    t = mk("s_qiT", [4, 128, S], BF16)
    sc["qiT"] = [dv(t[h]) for h in range(4)]
    sc["kiT"] = dv(mk("s_kiT", [128, S], BF16))
    sc["va"] = dv(mk("s_va", [S, 128], BF16))
    sc["vb"] = dv(mk("s_vb", [S, 512], BF16))
    sc["wi"] = dv(mk("s_wi", [S, 8], F32))
    sc["fbT"] = dv(mk("s_fbT", [4, S], F32))
    sc["aug"] = dv(mk("s_aug", [4, 2, 6, S], BF16))
    sc["oT"] = dv(mk("s_oT", [1536, S], BF16))
    t = mk("s_qr", [4, 64, S], BF16)
    sc["qr"] = [dv(t[h]) for h in range(4)]
    t = mk("s_kr", [4, 64, S], BF16)
    sc["kr"] = [dv(t[h]) for h in range(4)]
    sc["cos"] = dv(mk("s_cos", [64, S], F32))
    sc["sin"] = dv(mk("s_sin", [64, S], F32))
    sc["BCT"] = dv(mk("s_BCT", [1024, S], BF16))
    sc["xsB"] = dv(mk("s_xsB", [S, 1536], BF16))
    sc["zs"] = dv(mk("s_zs", [S, 1024], BF16))
    sc["dt"] = dv(mk("s_dt", [S, 16], F32))
    dbg = cfg.get("debug", {})
    k = K(nc)
    with k.es:
        c = setup_consts(k, cd)
        cur = din["x"]
        ropedone = [False]
        steps = cfg["steps"]
        for si, (kind, l) in enumerate(steps):
            last = si == len(steps) - 1
            dst = y if last else (xa if cur is not xa else xb)
            if kind == "mlp":
                phase_mlp(k, c, cur, dst, din["mlp_norm"][l:l + 1, :], din["mlp_w1"][l], din["mlp_w2"][l])
            elif kind == "even":
                phase_even_proj(k, c, sc, cur, din["ev_norm"][l:l + 1, :], din["ev_w_in"][l], din["ev_qn_a"][l],
                                din["ev_kn_a"][l], din["ev_qn_b"][l], din["ev_kn_b"][l])
                if "nodsa" not in dbg:
                    phase_dsa(k, c, sc)
                if "nofox" not in dbg:
                    phase_fox(k, c, sc, din["ev_b_f"][l])
                phase_outproj(k, c, sc, cur, dst, din["ev_w_out"][l], 1024)
            elif kind == "odd":
                if "oddlvl" in dbg:
                    ODDLVL[0] = dbg["oddlvl"]
                if not ropedone[0] and "norope" not in dbg:
                    phase_rope_tables(k, c, sc, din["positions"])
                    ropedone[0] = True
                phase_odd_proj(k, c, sc, cur, din["od_norm"][l:l + 1, :], din["od_w_in"][l], din["od_cq_norm"][l],
                               din["od_ckv_norm"][l], din["od_w_uq"][l], din["od_w_ukv"][l], din["od_qn_c"][l],
                               din["od_kn_c"][l], din["od_conv_w"][l], din["od_conv_b"][l])
                if "nomla" not in dbg:
                    phase_mla(k, c, sc)
                if "nossd" not in dbg:
                    phase_ssd(k, c, sc, din["od_dt_bias"][l], din["od_a_log"][l], din["od_d_skip"][l],
                              din["od_gate_norm"][l])
                phase_outproj(k, c, sc, cur, dst, din["od_w_out"][l], 1536)
            else:
                raise ValueError(kind)
            cur = dst
        k.barrier()
    return nc, cds


FULL_CFG = {"steps": [("even", 0), ("mlp", 0), ("odd", 0), ("mlp", 1), ("even", 1), ("mlp", 2), ("odd", 1), ("mlp", 3)]}


def run(inputs, cfg, cores=N_CORES):
    per_core = []
    for b in range(cores):
        m = {}
        for n in INPUT_NAMES:
            a = np.asarray(inputs[n])
            if n in ("x", "positions"):
                a = a[b]
            m[n] = np.ascontiguousarray(a)
        per_core.append(m)
    shapes = {n: (per_core[0][n].shape, per_core[0][n].dtype) for n in INPUT_NAMES}
    nc, cds = build(shapes, cfg)
    for m in per_core:
        for n, a in cds.items():
            m["c_" + n] = a
    res = run_bass_kernel_spmd(nc, per_core, core_ids=list(range(cores)))
    return np.stack([np.asarray(r["y"]) for r in res.results], axis=0)


def kernel(**inputs):
    out = run(inputs, FULL_CFG)
    return out.astype(np.float32)
```

```python
import contextlib
import numpy as np
import ml_dtypes
import concourse.bass as bass
import concourse.mybir as mybir
from concourse.bass_utils import run_bass_kernel_spmd

F32 = mybir.dt.float32
BF16 = mybir.dt.bfloat16
I32 = mybir.dt.int32
AF = mybir.ActivationFunctionType
ALU = mybir.AluOpType
AX = mybir.AxisListType

S = 4096
D = 1024
NT = S // 128
DFF = 4096
EPS = 1e-6
N_CORES = 8
WRITE_KEYS = ("out", "accum_out", "ap")


class V:
    def __init__(self, ap, bufs):
        self.ap = ap
        self.bufs = bufs

    def __getitem__(self, idx):
        return V(self.ap[idx], self.bufs)

    def bc(self, shape):
        return V(self.ap.to_broadcast(shape), self.bufs)

    def re(self, pat, **kw):
        return V(self.ap.rearrange(pat, **kw), self.bufs)

    def bitcast(self, dt):
        return V(self.ap.bitcast(dt), self.bufs)


class Buf:
    def __init__(self, ap):
        self.ap = ap
        self.w = None
        self.r = {}
        self.excl = False

    def __getitem__(self, idx):
        return V(self.ap[idx], (self,))

    def v(self):
        return V(self.ap, (self,))


def multi(*views):
    bufs = []
    for v in views:
        bufs.extend(v.bufs)
    return V(views[0].ap, tuple(bufs))


class Eng:
    def __init__(self, k, name, raw, self_sync):
        self.name = name
        self.raw = raw
        self.sem = k.new_sem("e_" + name)
        self.cnt = 0
        self.seen = {}
        self.self_sync = self_sync


class Slot:
    def __init__(self, k, key):
        self.key = key
        self.sem = k.new_sem(key)
        self.val = 0


class K:
    NSLOT = 12

    def __init__(self, nc):
        self.nc = nc
        self.es = contextlib.ExitStack()
        self.pe = Eng(self, "pe", nc.tensor, False)
        self.act = Eng(self, "act", nc.scalar, True)
        self.dve = Eng(self, "dve", nc.vector, True)
        self.pool = Eng(self, "pool", nc.gpsimd, True)
        self.sp = Eng(self, "sp", nc.sync, False)
        self.engs = [self.pe, self.act, self.dve, self.pool, self.sp]
        self.queues = {}
        for q in (self.sp, self.pool):
            self.queues[q.name] = [Slot(self, "d_%s_%d" % (q.name, i)) for i in range(self.NSLOT)]
        self.qnext = {q: 0 for q in self.queues}
        self.nph = 0

    def new_sem(self, name):
        return self.es.enter_context(self.nc.semaphore(name))

    def _wait(self, eng, tok):
        key, sem, val = tok
        if key == eng.name and not eng.self_sync:
            return
        if eng.seen.get(key, 0) >= val:
            return
        eng.raw.wait_ge(sem, val)
        eng.seen[key] = val

    def _deps(self, eng, reads, writes):
        for v in reads:
            for b in v.bufs:
                if b.w is not None:
                    self._wait(eng, b.w)
                if b.excl:
                    for t in b.r.values():
                        if t[0] != eng.name:
                            self._wait(eng, t)
        for v in writes:
            for b in v.bufs:
                if b.w is not None:
                    self._wait(eng, b.w)
                for t in b.r.values():
                    self._wait(eng, t)

    def _mark(self, tok, reads, writes):
        for v in reads:
            for b in v.bufs:
                b.r[tok[0]] = tok
        for v in writes:
            for b in v.bufs:
                b.w = tok
                b.r = {}

    def call(self, eng, method, **kw):
        reads, writes, args = [], [], {}
        for key, v in kw.items():
            if isinstance(v, V):
                (writes if key in WRITE_KEYS else reads).append(v)
                args[key] = v.ap
            else:
                args[key] = v
        self._deps(eng, reads, writes)
        inst = getattr(eng.raw, method)(**args)
        eng.cnt += 1
        inst.then_inc(eng.sem, 1)
        self._mark((eng.name, eng.sem, eng.cnt), reads, writes)
        return inst

    def dma(self, out, in_, q=None, **kw):
        q = q or self.sp
        slots = self.queues[q.name]
        slot = slots[self.qnext[q.name] % len(slots)]
        self.qnext[q.name] += 1
        if slot.val > 0:
            self._wait(q, (slot.key, slot.sem, slot.val))
        self._deps(q, [in_], [out])
        slot.val += 16
        q.raw.dma_start(out=out.ap, in_=in_.ap, **kw).then_inc(slot.sem, 16)
        self._mark((slot.key, slot.sem, slot.val), [in_], [out])

    def barrier(self):
        toks = [(e.name, e.sem, e.cnt) for e in self.engs if e.cnt > 0]
        for sl in self.queues.values():
            toks += [(s.key, s.sem, s.val) for s in sl if s.val > 0]
        for e in self.engs:
            for t in toks:
                self._wait(e, t)

    def mm(self, out, lhsT, rhs, start=True, stop=True):
        return self.call(self.pe, "matmul", out=out, lhsT=lhsT, rhs=rhs, start=start, stop=stop)

    def tr(self, out, in_, ident):
        return self.call(self.pe, "transpose", out=out, in_=in_, identity=ident)

    def actf(self, out, in_, func, **kw):
        return self.call(self.act, "activation", out=out, in_=in_, func=func, **kw)

    def tt(self, out, in0, in1, op, eng=None):
        return self.call(eng or self.dve, "tensor_tensor", out=out, in0=in0, in1=in1, op=op)

    def ts(self, out, in0, s1, op0, s2=None, op1=None, eng=None, **kw):
        if op1 is None:
            return self.call(eng or self.dve, "tensor_scalar", out=out, in0=in0, scalar1=s1, scalar2=None,
                             op0=op0, **kw)
        return self.call(eng or self.dve, "tensor_scalar", out=out, in0=in0, scalar1=s1, scalar2=s2,
                         op0=op0, op1=op1, **kw)

    def stt(self, out, in0, scalar, in1, op0, op1, **kw):
        return self.call(self.dve, "scalar_tensor_tensor", out=out, in0=in0, scalar=scalar, in1=in1,
                         op0=op0, op1=op1, **kw)

    def copy(self, out, in_, eng=None):
        eng = eng or self.dve
        if eng is self.act:
            return self.call(eng, "copy", out=out, in_=in_)
        return self.call(eng, "tensor_copy", out=out, in_=in_)

    def memset(self, ap, val, eng=None):
        return self.call(eng or self.dve, "memset", ap=ap, constant=val)

    @contextlib.contextmanager
    def phase(self):
        self.barrier()
        self.nph += 1
        ph = Phase(self, "p%d" % self.nph)
        with ph.es:
            yield ph
            self.barrier()


class Phase:
    def __init__(self, k, name):
        self.k = k
        self.name = name
        self.es = contextlib.ExitStack()
        self.n = 0

    def sbt(self, shape, dtype):
        self.n += 1
        return self.es.enter_context(self.k.nc.sbuf_tensor("%s_s%d" % (self.name, self.n), list(shape), dtype))

    def sb(self, shape, dtype):
        t = self.sbt(shape, dtype)
        return Buf(t[tuple(slice(None) for _ in shape)])

    def sbs(self, shape, dtype, n):
        return [self.sb(shape, dtype) for _ in range(n)]

    def split(self, shape, dtype, axis, step=1):
        t = self.sbt(shape, dtype)
        out = []
        for i in range(0, shape[axis], step):
            idx = [slice(None)] * len(shape)
            idx[axis] = slice(i, i + step) if step > 1 else i
            out.append(Buf(t[tuple(idx)]))
        return out

    def psum(self, n=8):
        out = []
        for i in range(n):
            self.n += 1
            t = self.es.enter_context(self.k.nc.psum_tensor("%s_ps%d" % (self.name, self.n), [128, 512], F32))
            b = Buf(t[:, :])
            b.excl = True
            out.append(b)
        return out


class Rot:
    def __init__(self, items):
        self.items = items
        self.i = 0

    def next(self):
        it = self.items[self.i % len(self.items)]
        self.i += 1
        return it


def dram_buf(ap):
    return Buf(ap)


class Ctx:
    pass


def setup_consts(k, cd):
    nc = k.nc
    c = Ctx()
    es = k.es
    def sb(name, shape, dt):
        t = es.enter_context(nc.sbuf_tensor(name, list(shape), dt))
        return Buf(t[tuple(slice(None) for _ in shape)])
    c.ident = sb("k_ident", [128, 128], BF16)
    c.mhalf = sb("k_mhalf", [128, 1], F32)
    tmp = sb("k_tmp", [128, 128], F32)
    k.dma(tmp.v(), V(cd["ident"], (Buf(cd["ident"]),)))
    k.copy(c.ident.v(), tmp.v(), eng=k.dve)
    k.memset(c.mhalf.v(), -0.5, eng=k.dve)
    c.epscol = sb("k_epscol", [128, 1], F32)
    k.memset(c.epscol.v(), EPS, eng=k.dve)
    c.identf = sb("k_identf", [128, 128], F32)
    k.copy(c.identf.v(), tmp.v(), eng=k.dve)
    c.i4 = sb("k_i4", [128, 512], BF16)
    for h_ in range(4):
        k.copy(c.i4[:, h_ * 128:(h_ + 1) * 128], tmp.v(), eng=k.dve)
    c.ones = sb("k_ones", [128, 128], BF16)
    k.memset(c.ones.v(), 1.0, eng=k.dve)
    c.onesf = sb("k_onesf", [128, 128], F32)
    k.memset(c.onesf.v(), 1.0, eng=k.dve)
    c.tri = sb("k_tri", [128, 128], BF16)
    k.dma(tmp.v(), V(cd["tri"], (Buf(cd["tri"]),)))
    k.copy(c.tri.v(), tmp.v(), eng=k.dve)
    c.trif = sb("k_trif", [128, 128], F32)
    k.copy(c.trif.v(), tmp.v(), eng=k.dve)
    c.invf = sb("k_invf", [64, 1], F32)
    k.dma(c.invf.v(), V(cd["invf"], (Buf(cd["invf"]),)))
    c.rotm = sb("k_rotm", [64, 64], BF16)
    k.dma(tmp[0:64, 0:64], V(cd["rotm"], (Buf(cd["rotm"]),)))
    k.copy(c.rotm.v(), tmp[0:64, 0:64], eng=k.dve)
    c.negtril_d = V(cd["negtril"], (Buf(cd["negtril"]),))
    c.negbig = sb("k_negbig", [128, 1], F32)
    k.memset(c.negbig.v(), -1e29, eng=k.dve)
    c.pow2 = sb("k_pow2", [128, 32], F32)
    k.dma(c.pow2.v(), V(cd["pow2"], (Buf(cd["pow2"]),)))
    c.negtri = sb("k_negtri", [128, 128], F32)
    k.dma(c.negtri.v(), V(cd["negtri"], (Buf(cd["negtri"]),)))
    return c


def rmsnorm_tile(k, c, ph, xt, gt, hn, scr, st):
    k.actf(scr.v(), xt.v(), AF.Square, accum_out=st[:, 0:1])
    k.ts(st[:, 1:2], st[:, 0:1], 1.0 / D, ALU.mult, EPS, ALU.add)
    k.tt(st[:, 2:3], st[:, 1:2], c.mhalf.v(), ALU.pow, eng=k.pool)
    k.stt(hn.v(), xt.v(), st[:, 2:3], gt.v(), ALU.mult, ALU.mult)


def phase_mlp(k, c, x_d, xo_d, g_row, w1_d, w2_d):
    G = 256
    NG = S // G
    with k.phase() as ph:
        w1b = ph.split([128, 8, DFF], BF16, 1)
        w2b = ph.split([128, 32, D], BF16, 1)
        stg = Rot(ph.sbs([128, 2048], F32, 3))
        gt = ph.sb([128, D], F32)
        xin = Rot(ph.sbs([128, D], F32, 3))
        scr = ph.sb([128, D], BF16)
        stats = Rot(ph.sbs([128, 4], F32, 4))
        hn = Rot(ph.sbs([128, D], BF16, 2))
        hT = Rot(ph.sbs([128, 8, G], BF16, 2))
        rl = Rot(ph.sbs([128, G], BF16, 4))
        hid = Rot(ph.sbs([128, G], BF16, 5))
        xres = Rot(ph.sbs([128, D], F32, 3))
        ps = ph.psum(8)
        ps_y = ps[0:4]
        ps_h = Rot(ps[4:7])
        ps_t = Rot(ps[7:8])
        xd = Buf(x_d)
        xod = Buf(xo_d)
        w1d = Buf(w1_d)
        w2d = Buf(w2_d)
        k.dma(gt.v(), V(g_row.to_broadcast([128, D]), (Buf(g_row),)))
        def load_weights():
            for kk in range(8):
                for hf in range(2):
                    s = stg.next()
                    k.dma(s.v(), V(w1_d[kk * 128:(kk + 1) * 128, hf * 2048:(hf + 1) * 2048], (w1d,)))
                    k.copy(w1b[kk][:, hf * 2048:(hf + 1) * 2048], s.v(), eng=(k.pool, k.dve, k.act)[(kk * 2 + hf) % 3])
            w2v = w2_d.rearrange("(j p) n -> p j n", p=128)
            for jj in range(0, 32, 2):
                s = stg.next()
                k.dma(s.v().re("p (j n) -> p j n", j=2), V(w2v[:, jj:jj + 2, :], (w2d,)))
                eng = (k.pool, k.dve, k.act)[(jj // 2) % 3]
                k.copy(w2b[jj][:, :], s[:, 0:1024], eng=eng)
                k.copy(w2b[jj + 1][:, :], s[:, 1024:2048], eng=eng)

        def norm_a(g):
            res = []
            for t in range(G // 128):
                xt = xin.next()
                r0 = g * G + t * 128
                k.dma(xt.v(), V(x_d[r0:r0 + 128, :], (xd,)))
                h = hn.next()
                rmsnorm_tile(k, c, ph, xt, gt, h, scr, stats.next())
                res.append(h)
            return res

        def norm_b(g, hs):
            hTg = hT.next()
            for t, h in enumerate(hs):
                pt = ps_t.next()
                ptb = pt.v().bitcast(BF16)
                for kk in range(8):
                    k.tr(ptb[:, kk * 128:(kk + 1) * 128], h[:, kk * 128:(kk + 1) * 128], c.ident.v())
                k.copy(hTg[:, :, t * 128:(t + 1) * 128], ptb.re("p (k t) -> p k t", k=8), eng=k.act)
            return hTg

        hs = norm_a(0)
        hT_cur = norm_b(0, hs)
        load_weights()
        for g in range(NG):
            hs_next = None
            hT_next = None
            pend = []

            def w2(pj, phd, last):
                for t in range(2):
                    for cc in range(2):
                        k.mm(ps_y[t * 2 + cc].v(), phd[:, t * 128:(t + 1) * 128],
                             w2b[pj][:, cc * 512:(cc + 1) * 512], start=(pj == 0), stop=last)
            for j in range(32):
                ph_ = ps_h.next()
                for kk in range(8):
                    k.mm(ph_[:, 0:G], w1b[kk][:, j * 128:(j + 1) * 128], hT_cur[:, kk, :],
                         start=(kk == 0), stop=(kk == 7))
                r = rl.next()
                k.actf(r.v(), ph_[:, 0:G], AF.Relu)
                hd = hid.next()
                k.tt(hd.v(), r.v(), r.v(), ALU.mult)
                pend.append((j, hd))
                if len(pend) > 2:
                    pj, phd = pend.pop(0)
                    w2(pj, phd, False)
                if j == 4 and g + 1 < NG:
                    hs_next = norm_a(g + 1)
                if j == 20 and g + 1 < NG:
                    hT_next = norm_b(g + 1, hs_next)
            while pend:
                pj, phd = pend.pop(0)
                w2(pj, phd, pj == 31)
            for t in range(2):
                r0 = g * G + t * 128
                xr = xres.next()
                k.dma(xr.v(), V(x_d[r0:r0 + 128, :], (xd,)))
                for cc in range(2):
                    k.tt(xr[:, cc * 512:(cc + 1) * 512], ps_y[t * 2 + cc].v(), xr[:, cc * 512:(cc + 1) * 512], ALU.add)
                k.dma(V(xo_d[r0:r0 + 128, :], (xod,)), xr.v())
            hT_cur = hT_next


def xnorm_group(k, c, x_d, xd, g, gt, xin, hn, scr, stats, hTg, ps_t, ntile=4):
    G = ntile * 128
    for t in range(ntile):
        xt = xin.next()
        r0 = g * G + t * 128
        k.dma(xt.v(), V(x_d[r0:r0 + 128, :], (xd,)))
        h = hn.next()
        rmsnorm_tile(k, c, None, xt, gt, h, scr, stats.next())
        pt = ps_t.next()
        ptb = pt.v().bitcast(BF16)
        for kk in range(8):
            k.tr(ptb[:, kk * 128:(kk + 1) * 128], h[:, kk * 128:(kk + 1) * 128], c.ident.v())
        k.copy(hTg[:, :, t * 128:(t + 1) * 128], ptb.re("p (k t) -> p k t", k=8), eng=k.act)


def run_rr(gens):
    gens = list(gens)
    while gens:
        for g_ in list(gens):
            try:
                next(g_)
            except StopIteration:
                gens.remove(g_)


def xnorm_gen(k, c, x_d, xd, g, gt, xin, hn, scr, stats, hTg, ps_t, ntile=4):
    G = ntile * 128
    for t in range(ntile):
        xt = xin.next()
        r0 = g * G + t * 128
        k.dma(xt.v(), V(x_d[r0:r0 + 128, :], (xd,)))
        h = hn.next()
        rmsnorm_tile(k, c, None, xt, gt, h, scr, stats.next())
        yield
        pt = ps_t.next()
        ptb = pt.v().bitcast(BF16)
        for kk in range(8):
            k.tr(ptb[:, kk * 128:(kk + 1) * 128], h[:, kk * 128:(kk + 1) * 128], c.ident.v())
        yield
        k.copy(hTg[:, :, t * 128:(t + 1) * 128], ptb.re("p (k t) -> p k t", k=8), eng=k.act)
        yield


def load_w_bf16(k, ph, w_d, nk, ncols, stg_cols=None):
    wb = ph.split([128, nk, ncols], BF16, 1)
    stg = Rot(ph.sbs([128, ncols], F32, 2))
    wd = Buf(w_d)
    rows = w_d.shape[0]
    for kk in range(nk):
        s = stg.next()
        r = min(128, rows - kk * 128)
        k.dma(s[0:r, :], V(w_d[kk * 128:kk * 128 + r, :], (wd,)))
        k.copy(wb[kk][0:r, :], s[0:r, :], eng=(k.pool, k.dve, k.act)[kk % 3])
    return wb


def fm_qknorm(k, c, ps, M, gcol, outb, sq, lnb, rstd, ps2, hd):
    N = ps.ap.shape[-1]
    k.actf(sq[0:M, 0:N], ps, AF.Square)
    k.mm(ps2[0:M, 0:N], c.ones[0:M, 0:M], sq[0:M, 0:N])
    k.actf(lnb[0:M, 0:N], ps2[0:M, 0:N], AF.Ln, scale=1.0 / hd, bias=c.epscol[0:M, :])
    k.actf(rstd[0:M, 0:N], lnb[0:M, 0:N], AF.Exp, scale=-0.5)
    k.stt(outb, ps, gcol, rstd[0:M, 0:N], ALU.mult, ALU.mult)


EV = dict(qa=0, ka=512, va=640, qi=768, ki=1280, wi=1344, qb=1352, kb=1864, vb=2376, fb=2888)


def phase_even_proj(k, c, sc, x_d, g_row, w_d, qn_a, kn_a, qn_b, kn_b):
    with k.phase() as ph:
        wb = load_w_bf16(k, ph, w_d, 8, 2892)
        wkd = ph.sb([128, 8, 128], BF16)
        for kk in range(8):
            k.copy(wkd[:, kk, 0:64], wb[kk][:, 1280:1344], eng=k.pool)
            k.copy(wkd[:, kk, 64:128], wb[kk][:, 1280:1344], eng=k.pool)
        gt = ph.sb([128, D], F32)
        k.dma(gt.v(), V(g_row.to_broadcast([128, D]), (Buf(g_row),)))
        gcol = ph.sb([128, 4], F32)
        for i, gn in enumerate((qn_a, kn_a, qn_b, kn_b)):
            k.dma(gcol[:, i:i + 1], V(gn.rearrange("(p o) -> p o", o=1), (Buf(gn),)))
        xin = Rot(ph.sbs([128, D], F32, 3))
        scr = ph.sb([128, D], BF16)
        stats = Rot(ph.sbs([128, 4], F32, 4))
        hn = Rot(ph.sbs([128, D], BF16, 2))
        hT = Rot(ph.sbs([128, 8, 512], BF16, 2))
        sq = Rot(ph.sbs([128, 512], BF16, 2))
        lnb = Rot(ph.sbs([128, 512], F32, 2))
        rstd = Rot(ph.sbs([128, 512], F32, 2))
        ob = Rot(ph.sbs([128, 512], BF16, 4))
        of = Rot(ph.sbs([128, 512], F32, 2))
        ps = ph.psum(8)
        psA = Rot(ps[0:3])
        psB = Rot(ps[3:5])
        ps_t = Rot(ps[5:7])
        psC = Rot(ps[7:8])
        xd = Buf(x_d)
        chunks = []
        for h in range(4):
            chunks.append((wb, EV["qa"] + h * 128, 128, 0, sc["qT"][h]))
        chunks.append((wb, EV["ka"], 128, 1, sc["kT"][0]))
        for cc in range(4):
            chunks.append((wb, EV["qi"] + cc * 128, 128, None, sc["qiT"][cc]))
        chunks.append((None, 0, 128, None, sc["kiT"]))
        for h in range(4):
            chunks.append((wb, EV["qb"] + h * 128, 128, 2, sc["qT"][4 + h]))
        for h in range(4):
            chunks.append((wb, EV["kb"] + h * 128, 128, 3, sc["kT"][1 + h]))
        chunks.append((wb, EV["fb"], 4, "f32", sc["fbT"]))
        ci = 0
        for g in range(S // 512):
            hTg = hT.next()
            xnorm_group(k, c, x_d, xd, g, gt, xin, hn, scr, stats, hTg, ps_t)
            tok = slice(g * 512, (g + 1) * 512)
            def stage2(p, M, nrm, dst):
                if nrm == "f32":
                    o = of.next()
                    k.copy(o[0:M, :], p[0:M, :], eng=k.dve)
                    k.dma(V(dst.ap[0:M, tok], dst.bufs), o[0:M, :])
                    return
                o = ob.next()
                if nrm is None:
                    cnt_[0] += 1
                    k.copy(o[0:M, :], p[0:M, :], eng=(k.act if cnt_[0] % 2 else k.dve))
                else:
                    fm_qknorm(k, c, p[0:M, :], M, gcol[:, nrm:nrm + 1], o[0:M, :], sq.next(), lnb.next(),
                              rstd.next(), psB.next(), 128)
                k.dma(V(dst.ap[0:M, tok], dst.bufs), o[0:M, :])
            cnt_ = [0]
            pend = None
            for (wsrc, c0, M, nrm, dst) in chunks:
                p = psA.next()
                for kk in range(8):
                    lhsT = wkd[:, kk, :] if wsrc is None else wb[kk][:, c0:c0 + M]
                    k.mm(p[0:M, :], lhsT, hTg[:, kk, :], start=(kk == 0), stop=(kk == 7))
                if pend is not None:
                    stage2(*pend)
                pend = (p, M, nrm, dst)
            stage2(*pend)
            for t in range(4):
                r0 = g * 512 + t * 128
                tk = slice(t * 128, (t + 1) * 128)
                p = psA.next()
                for kk in range(8):
                    k.mm(p[:, :], hTg[:, kk, tk], wb[kk][:, EV["vb"]:EV["vb"] + 512], start=(kk == 0), stop=(kk == 7))
                o = ob.next()
                k.copy(o[:, :], p[:, :], eng=k.act)
                k.dma(V(sc["vb"].ap[r0:r0 + 128, :], sc["vb"].bufs), o[:, :])
                p = psC.next()
                for kk in range(8):
                    k.mm(p[:, 0:128], hTg[:, kk, tk], wb[kk][:, EV["va"]:EV["va"] + 128], start=(kk == 0), stop=(kk == 7))
                for kk in range(8):
                    k.mm(p[:, 128:136], hTg[:, kk, tk], wb[kk][:, EV["wi"]:EV["wi"] + 8], start=(kk == 0), stop=(kk == 7))
                o = ob.next()
                k.copy(o[:, 0:128], p[:, 0:128], eng=k.dve)
                k.dma(V(sc["va"].ap[r0:r0 + 128, :], sc["va"].bufs), o[:, 0:128])
                o2 = of.next()
                k.copy(o2[:, 0:8], p[:, 128:136], eng=k.dve)
                k.dma(V(sc["wi"].ap[r0:r0 + 128, :], sc["wi"].bufs), o2[:, 0:8])


def split3(k, ph, src, n, outs):
    k.copy(outs[0][0:n, :], src[0:n, :])
    k.tt(src[0:n, :], src[0:n, :], outs[0][0:n, :], ALU.subtract)
    k.copy(outs[1][0:n, :], src[0:n, :])
    k.tt(src[0:n, :], src[0:n, :], outs[1][0:n, :], ALU.subtract)
    k.copy(outs[2][0:n, :], src[0:n, :])


def attn_core(k, c, qg, nkt_fn, qk_fn, P_rot, ps_s, ps_o, ps_d, v_fn, scale, finalize, la=2):
    nkt = 4 * qg + 4
    po = ps_o
    pd = ps_d
    issued = []

    def issue(kt):
        diag = kt >= 4 * qg
        col0 = (kt - 4 * qg) * 128 if diag else 0
        s = ps_s.next()
        qk_fn(s[:, col0:512], kt, col0)
        issued.append((s, col0, diag))
    for kt in range(min(la, nkt)):
        issue(kt)
    for kt in range(nkt):
        if kt + la < nkt:
            issue(kt + la)
        s, col0, diag = issued[kt]
        P = P_rot.next()
        k.actf(P[:, col0:512], s[:, col0:512], AF.Exp, scale=scale)
        if diag:
            k.tt(P[:, col0:col0 + 128], P[:, col0:col0 + 128], c.tri.v(), ALU.mult, eng=k.pool)
        k.mm(po[:, col0:512], v_fn(kt), P[:, col0:512], start=(kt == 0), stop=(kt == nkt - 1))
        k.mm(pd[:, col0:512], c.ones.v(), P[:, col0:512], start=(kt == 0), stop=(kt == nkt - 1))
    finalize(po, pd)


def phase_fox(k, c, sc, b_f):
    SQ = float(np.sqrt(128.0))
    with k.phase() as ph:
        with contextlib.ExitStack() as es2:
            ph2 = Phase(k, ph.name + "a")
            es2.enter_context(ph2.es)
            f0 = ph2.sb([4, S], F32)
            f1 = ph2.sb([4, S], F32)
            bcol = ph2.sb([4, 2], F32)
            one4 = ph2.sb([4, 1], F32)
            pcs = ph2.sbs([4, S], BF16, 3)
            ones4 = ph2.sb([4, S], BF16)
            k.dma(f0.v(), sc["fbT"])
            k.dma(bcol[:, 0:1], V(b_f.rearrange("(p o) -> p o", o=1), (Buf(b_f),)))
            k.ts(bcol[:, 1:2], bcol[:, 0:1], -1.0, ALU.mult)
            k.memset(one4.v(), 1.0)
            k.memset(ones4.v(), 1.0)
            k.actf(f0.v(), f0.v(), AF.Exp, scale=-1.0, bias=bcol[:, 1:2])
            k.actf(f0.v(), f0.v(), AF.Ln, bias=one4.v())
            k.ts(f0.v(), f0.v(), -SQ, ALU.mult)
            k.call(k.dve, "tensor_tensor_scan", out=f1.v(), data0=one4.v().bc([4, S]), data1=f0.v(),
                   initial=0.0, op0=ALU.mult, op1=ALU.add)
            k.copy(f0.v().re("p (b t) -> p b t", t=128), f1.v().re("p (b t) -> p b t", t=128)[:, :, 127:128].bc([4, 32, 128]))
            aug = sc["aug"]
            split3(k, ph2, f0, 4, pcs)
            for p_ in range(3):
                k.dma(V(aug.ap[:, 0, p_, :], aug.bufs), pcs[p_].v())
            k.ts(f1.v(), f1.v(), -1.0, ALU.mult)
            split3(k, ph2, f1, 4, pcs)
            for p_ in range(3):
                k.dma(V(aug.ap[:, 1, 3 + p_, :], aug.bufs), pcs[p_].v())
                k.dma(V(aug.ap[:, 1, p_, :], aug.bufs), ones4.v())
                k.dma(V(aug.ap[:, 0, 3 + p_, :], aug.bufs), ones4.v())
            k.barrier()
        qT = Rot(ph.sbs([128, S], BF16, 2))
        kT = Rot(ph.sbs([128, S], BF16, 2))
        vv = Rot(ph.sbs([128, 32, 128], BF16, 2))
        aq = Rot(ph.sbs([6, S], BF16, 2))
        ak = Rot(ph.sbs([6, S], BF16, 2))
        P_rot = Rot(ph.sbs([128, 512], BF16, 4))
        rden = Rot(ph.sbs([128, 512], F32, 2))
        ob = Rot(ph.sbs([128, 512], BF16, 2))
        ps = ph.psum(8)
        ps_s = Rot(ps[0:3])
        ps_o = Rot(ps[3:5])
        ps_d = Rot(ps[5:7])
        for h in range(4):
            q_, k_, v_, aq_, ak_ = qT.next(), kT.next(), vv.next(), aq.next(), ak.next()
            k.dma(q_.v(), sc["qT"][4 + h])
            k.dma(k_.v(), sc["kT"][1 + h])
            vsrc = sc["vb"]
            k.dma(v_.v(), V(vsrc.ap.rearrange("(t p) (h d) -> p t h d", p=128, h=4)[:, :, h, :], vsrc.bufs))
            k.dma(aq_.v(), V(sc["aug"].ap[h, 0], sc["aug"].bufs))
            k.dma(ak_.v(), V(sc["aug"].ap[h, 1], sc["aug"].bufs))
            for qg in range(8):
                def qk_fn(sv, kt, col0, q_=q_, k_=k_, aq_=aq_, ak_=ak_, qg=qg):
                    qs = slice(qg * 512 + col0, (qg + 1) * 512)
                    ks = slice(kt * 128, (kt + 1) * 128)
                    k.mm(sv, k_[:, ks], q_[:, qs], start=True, stop=False)
                    k.mm(sv, ak_[:, ks], aq_[:, qs], start=False, stop=True)

                def fin(po, pd, h=h, qg=qg):
                    r = rden.next()
                    k.call(k.dve, "reciprocal", out=r.v(), in_=pd.v())
                    o = ob.next()
                    k.tt(o.v(), po.v(), r.v(), ALU.mult)
                    dst = sc["oT"]
                    k.dma(V(dst.ap[512 + h * 128:512 + (h + 1) * 128, qg * 512:(qg + 1) * 512], dst.bufs), o.v())

                attn_core(k, c, qg, None, qk_fn, P_rot, ps_s, ps_o.next(), ps_d.next(),
                          lambda kt, v_=v_: v_[:, kt, :], 1.0 / SQ, fin)


def phase_outproj(k, c, sc, x_d, xo_d, w_d, nfeat):
    nk = nfeat // 128
    with k.phase() as ph:
        wb = load_w_bf16(k, ph, w_d, nk, D)
        oT = Rot(ph.sbs([128, nk, 512], BF16, 2))
        xres = Rot(ph.sbs([128, D], F32, 3))
        ps = ph.psum(8)
        psr = Rot(ps)
        xd = Buf(x_d)
        xod = Buf(xo_d)
        src = sc["oT"]
        for g in range(S // 512):
            o_ = oT.next()
            k.dma(o_.v(), V(src.ap[0:nfeat, g * 512:(g + 1) * 512].rearrange("(k p) s -> p k s", p=128), src.bufs))
            for t in range(4):
                r0 = g * 512 + t * 128
                xr = xres.next()
                k.dma(xr.v(), V(x_d[r0:r0 + 128, :], (xd,)))
                for cc in range(2):
                    p = psr.next()
                    for kk in range(nk):
                        k.mm(p.v(), o_[:, kk, t * 128:(t + 1) * 128], wb[kk][:, cc * 512:(cc + 1) * 512],
                             start=(kk == 0), stop=(kk == nk - 1))
                    k.tt(xr[:, cc * 512:(cc + 1) * 512], p.v(), xr[:, cc * 512:(cc + 1) * 512], ALU.add)
                k.dma(V(xo_d[r0:r0 + 128, :], (xod,)), xr.v())


def bc1(v, n):
    p, f = v.ap.shape
    return V(v.ap.unsqueeze(1).to_broadcast([p, n, f]), v.bufs)


NBIS = 12


def phase_dsa(k, c, sc):
    SCALE = float(128.0 ** -0.5)
    with k.phase() as ph:
        qi = ph.sb([128, 4, S], BF16)
        ki = ph.sb([128, S], BF16)
        qa = ph.sb([128, 4, S], BF16)
        ka = ph.sb([128, S], BF16)
        va = ph.sb([128, 32, 128], BF16)
        wi = ph.sb([128, 32, 8], F32)
        for h in range(4):
            k.dma(qi[:, h, :], sc["qiT"][h])
            k.dma(qa[:, h, :], sc["qT"][h])
        k.dma(ki.v(), sc["kiT"])
        k.dma(ka.v(), sc["kT"][0])
        k.dma(va.v(), V(sc["va"].ap.rearrange("(t p) d -> p t d", p=128), sc["va"].bufs))
        k.dma(wi.v(), V(sc["wi"].ap.rearrange("(t p) d -> p t d", p=128), sc["wi"].bufs))
        scb = ph.sbs([128, S], F32, 3)
        junk_t = ph.sbt([128, S], BF16)
        msk = ph.sbs([128, S], BF16, 3)
        rl = Rot(ph.sbs([128, 512], BF16, 6))
        dg = ph.sbs([128, 8, 128], BF16, 3)
        E = Rot(ph.sbs([128, 512], BF16, 3))
        P = Rot(ph.sbs([128, 512], BF16, 3))
        mT = Rot(ph.sbs([128, 512], BF16, 3))
        stt_ = ph.sbs([128, 64], F32, 3)
        rden = Rot(ph.sbs([128, 512], F32, 2))
        ob = Rot(ph.sbs([128, 512], BF16, 2))
        ps = ph.psum(8)
        ps_i = Rot(ps[0:4])
        ps_acc = Rot([ps[4], ps[7]])
        ps_s = Rot(ps[0:3])
        po = ps[5]
        pd = ps[6]
        thr_of = {}

        def gen_index(qt):
            W = (qt + 1) * 128
            qs = slice(qt * 128, (qt + 1) * 128)
            dgt = dg[qt % 3]
            for h in range(8):
                k.actf(dgt[:, h, :], c.identf.v(), AF.Copy, scale=wi[:, qt, h:h + 1])
            scq = scb[qt % 3]
            for kg in range((W + 511) // 512):
                cols = min(512, W - kg * 512)
                acc = ps_acc.next()
                pend = []
                for h in range(8):
                    p = ps_i.next()
                    pr = slice(64 * (h % 2), 64 * (h % 2) + 64)
                    k.mm(p[:, 0:cols], qi[pr, h // 2, qs], ki[pr, kg * 512:kg * 512 + cols])
                    r = rl.next()
                    k.actf(r[:, 0:cols], p[:, 0:cols], AF.Relu)
                    pend.append((h, r))
                    if len(pend) > 2:
                        h0, r0 = pend.pop(0)
                        k.mm(acc[:, 0:cols], dgt[:, h0, :], r0[:, 0:cols], start=(h0 == 0), stop=(h0 == 7))
                for h0, r0 in pend:
                    k.mm(acc[:, 0:cols], dgt[:, h0, :], r0[:, 0:cols], start=(h0 == 0), stop=(h0 == 7))
                k.copy(scq[:, kg * 512:kg * 512 + cols], acc[:, 0:cols], eng=k.act)
                yield
            k.tt(scq[:, qt * 128:W], scq[:, qt * 128:W], c.negtri.v(), ALU.add, eng=k.pool)
            yield

        def gen_bisect(qt):
            W = (qt + 1) * 128
            scq = scb[qt % 3]
            st = stt_[qt % 3]
            if qt >= 2:
                k.call(k.dve, "tensor_reduce", out=st[:, 0:1], in_=scq[:, 0:W], axis=AX.X, op=ALU.max)
                yield
                k.call(k.dve, "tensor_reduce", out=st[:, 1:2], in_=scq[:, 0:qt * 128], axis=AX.X, op=ALU.min)
                k.ts(st[:, 1:2], st[:, 1:2], -1.0, ALU.add)
                k.tt(st[:, 2:3], st[:, 0:1], st[:, 1:2], ALU.subtract)
                k.ts(st[:, 8:8 + NBIS + 1], c.pow2[:, 0:NBIS + 1], st[:, 2:3], ALU.mult)
                k.ts(st[:, 32:32 + NBIS + 1], st[:, 8:8 + NBIS + 1], 2.0, ALU.mult)
                k.tt(st[:, 3:4], st[:, 1:2], st[:, 8:9], ALU.add)
                yield
                for it in range(NBIS):
                    k.call(k.dve, "tensor_scalar", out=junk_t[:, 0:W], in0=scq[:, 0:W], scalar1=st[:, 3:4], scalar2=None,
                           op0=ALU.is_gt, op1=ALU.add, accum_out=st[:, 4:5])
                    yield
                    k.stt(st[:, 5:6], st[:, 4:5], 255.5, st[:, 32 + it + 1:32 + it + 2], ALU.is_gt, ALU.mult)
                    k.stt(st[:, 3:4], st[:, 5:6], st[:, 8 + it + 1:8 + it + 2], st[:, 3:4], ALU.subtract, ALU.add)
                k.tt(st[:, 6:7], st[:, 3:4], st[:, 8 + NBIS:8 + NBIS + 1], ALU.subtract)
                thr = st[:, 6:7]
            else:
                thr = c.negbig.v()
            m = msk[qt % 3]
            k.ts(m[:, 0:W], scq[:, 0:W], thr, ALU.is_le, -30000.0, ALU.mult)
            yield

        def gen_attn(qt):
            qs = slice(qt * 128, (qt + 1) * 128)
            m = msk[qt % 3]
            nkt = qt + 1
            issued = {}

            def issue(kt):
                s = ps_s.next()
                sv = s.v().re("p (h q) -> p h q", h=4)
                k.mm(sv, ka[:, kt * 128:(kt + 1) * 128], qa[:, :, qs], start=True, stop=False)
                k.mm(s.v(), m[:, kt * 128:(kt + 1) * 128], c.i4.v(), start=False, stop=True)
                issued[kt] = s
            for kt in range(min(2, nkt)):
                issue(kt)
            for kt in range(nkt):
                if kt + 2 < nkt:
                    issue(kt + 2)
                s = issued.pop(kt)
                p_ = P.next()
                k.actf(p_.v(), s.v(), AF.Exp, scale=SCALE)
                k.mm(po.v(), va[:, kt, :], p_.v(), start=(kt == 0), stop=(kt == qt))
                k.mm(pd.v(), c.ones.v(), p_.v(), start=(kt == 0), stop=(kt == qt))
                yield
            r = rden.next()
            k.call(k.dve, "reciprocal", out=r.v(), in_=pd.v())
            o = ob.next()
            k.tt(o.v(), po.v(), r.v(), ALU.mult)
            dst = sc["oT"]
            k.dma(V(dst.ap[0:512, qs].rearrange("(h d) q -> d h q", d=128), dst.bufs),
                  o.v().re("p (h q) -> p h q", h=4))
            yield

        def chain(*gs):
            for g_ in gs:
                yield from g_
        HALF = (NBIS + 4) // 2
        bgen = {}
        for step in range(-3, NT):
            tasks = []
            lane1 = []
            if 0 <= step + 3 < NT:
                lane1.append(gen_index(step + 3))
            if 0 <= step < NT:
                lane1.append(gen_attn(step))
            if lane1:
                tasks.append([chain(*lane1), None])
            if 0 <= step + 2 < NT:
                bgen[step + 2] = gen_bisect(step + 2)
                tasks.append([bgen[step + 2], HALF])
            if 0 <= step + 1 < NT:
                tasks.append([bgen.pop(step + 1), None])
            while tasks:
                for tk_ in list(tasks):
                    try:
                        next(tk_[0])
                        if tk_[1] is not None:
                            tk_[1] -= 1
                            if tk_[1] <= 0:
                                tasks.remove(tk_)
                    except StopIteration:
                        tasks.remove(tk_)


OD = dict(cq=0, ckv=384, kr=640, z=704, xs=1728, B=2752, C=3264, dt=3776)
PI = float(np.pi)


def phase_rope_tables(k, c, sc, pos_d):
    with k.phase() as ph:
        pi_ = ph.sb([64, S], I32)
        ang = ph.sb([64, S], F32)
        u = ph.sb([64, S], F32)
        ni = ph.sb([64, S], I32)
        r = ph.sb([64, S], F32)
        k.dma(pi_.v(), V(pos_d.rearrange("(o s) -> o s", o=1).to_broadcast([64, S]), (Buf(pos_d),)))
        k.copy(ang.v(), pi_.v())
        k.ts(ang.v(), ang.v(), c.invf.v(), ALU.mult)
        for name, shift in (("sin", 0.0), ("cos", PI / 2)):
            k.ts(r.v(), ang.v(), shift, ALU.add)
            k.ts(u.v(), r.v(), 1.0 / (2 * PI), ALU.mult)
            k.copy(ni.v(), u.v())
            k.copy(u.v(), ni.v())
            k.stt(r.v(), u.v(), -2 * PI, r.v(), ALU.mult, ALU.add)
            k.ts(u.v(), r.v(), PI, ALU.is_gt, 2 * PI, ALU.mult)
            k.tt(r.v(), r.v(), u.v(), ALU.subtract)
            k.ts(u.v(), r.v(), -PI, ALU.is_lt, 2 * PI, ALU.mult)
            k.tt(r.v(), r.v(), u.v(), ALU.add)
            k.ts(r.v(), r.v(), 3.1415925, ALU.min, -3.1415925, ALU.max)
            k.actf(u.v(), r.v(), AF.Sin)
            k.dma(sc[name], u.v())


def col_load(k, dst, src_ap, n):
    k.dma(dst, V(src_ap.rearrange("(p o) -> p o", o=1), (Buf(src_ap),)))


ODDLVL = [9]


def phase_odd_proj(k, c, sc, x_d, g_row, w_d, cqn, ckvn, wuq_d, wukv_d, qn_c, kn_c, conv_w, conv_b):
    with k.phase() as ph:
        stg = Rot(ph.sbs([128, 1896], F32, 3))
        ncast = [0]

        def loadw(w_ap, nk, ncols):
            wb_ = ph.split([128, nk, ncols], BF16, 1)
            wd = Buf(w_ap)
            for kk in range(nk):
                for c0 in range(0, ncols, 1896):
                    c1 = min(ncols, c0 + 1896)
                    s = stg.next()
                    k.dma(s[:, 0:c1 - c0], V(w_ap[kk * 128:(kk + 1) * 128, c0:c1], (wd,)))
                    ncast[0] += 1
                    k.copy(wb_[kk][:, c0:c1], s[:, 0:c1 - c0], eng=(k.pool, k.dve, k.act)[ncast[0] % 3])
            return wb_
        wb = loadw(w_d, 8, 3792)
        wuq = loadw(wuq_d, 3, 768)
        wukv = loadw(wukv_d, 2, 1024)
        gt = ph.sb([128, D], F32)
        k.dma(gt.v(), V(g_row.to_broadcast([128, D]), (Buf(g_row),)))
        gc = ph.sb([128, 16], F32)
        for i in range(3):
            col_load(k, gc[:, i:i + 1], cqn[i * 128:(i + 1) * 128], 128)
        for i in range(2):
            col_load(k, gc[:, 3 + i:4 + i], ckvn[i * 128:(i + 1) * 128], 128)
        col_load(k, gc[:, 5:6], qn_c[0:128], 128)
        col_load(k, gc[0:64, 6:7], qn_c[128:192], 64)
        col_load(k, gc[:, 7:8], kn_c[0:128], 128)
        col_load(k, gc[0:64, 8:9], kn_c[128:192], 64)
        cw = ph.sb([128, 16, 4], F32)
        cb = ph.sb([128, 16], F32)
        cwd = Buf(conv_w)
        cbd = Buf(conv_b)
        for j in range(16):
            k.dma(cw[:, j, :], V(conv_w[:, j * 128:(j + 1) * 128].rearrange("w p -> p w"), (cwd,)),
                  allow_slow_non_contiguous=True)
            k.dma(cb[:, j:j + 1], V(conv_b[j * 128:(j + 1) * 128].rearrange("(p o) -> p o", o=1), (cbd,)))
        hal = ph.split([128, 16, 3], F32, 1)
        for j in range(16):
            k.memset(hal[j][:, :], 0.0, eng=k.pool)
        xin = Rot(ph.sbs([128, D], F32, 3))
        scr = ph.sb([128, D], BF16)
        stats = Rot(ph.sbs([128, 4], F32, 4))
        hn = Rot(ph.sbs([128, D], BF16, 2))
        hT = Rot(ph.sbs([128, 8, 512], BF16, 2))
        sq = Rot(ph.sbs([128, 512], BF16, 6))
        lnb = Rot(ph.sbs([128, 512], F32, 2))
        rstd = Rot(ph.sbs([128, 512], F32, 2))
        ob = Rot(ph.sbs([128, 512], BF16, 4))
        of = Rot(ph.sbs([128, 16], F32, 3))
        cqraw = ph.sb([128, 3, 512], F32)
        cqn_b = ph.sb([128, 3, 512], BF16)
        ckvraw = ph.sb([128, 2, 512], F32)
        ckvn_b = ph.sb([128, 2, 512], BF16)
        krraw = ph.sb([64, 512], F32)
        sqkr = ph.sb([64, 512], BF16)
        xr = Rot(ph.sbs([128, 515], F32, 2))
        acc = Rot(ph.sbs([128, 512], F32, 2))
        xact = Rot(ph.sbs([128, 512], BF16, 3))
        xtm = Rot(ph.sbs([128, 4, 128], BF16, 2))
        cs = ph.sb([64, 2, 512], F32)
        yb = Rot(ph.sbs([64, 512], BF16, 2))
        t1 = Rot(ph.sbs([64, 512], F32, 2))
        t2 = Rot(ph.sbs([64, 512], F32, 2))
        ps = ph.psum(8)
        psB = Rot(ps[2:4])
        xd = Buf(x_d)

        def grpnorm(raws, sqs, nch, hd, gcol0, outb):
            p2 = psB.next()
            for i in range(nch):
                k.mm(p2.v(), c.ones.v(), sqs[i].v(), start=(i == 0), stop=(i == nch - 1))
            l_, r_ = lnb.next(), rstd.next()
            k.actf(l_.v(), p2.v(), AF.Ln, scale=1.0 / hd, bias=c.epscol.v())
            k.actf(r_.v(), l_.v(), AF.Exp, scale=-0.5)
            for i in range(nch):
                k.stt(outb[:, i, :], raws[:, i, :], gc[:, gcol0 + i:gcol0 + i + 1], r_.v(), ALU.mult, ALU.mult)

        def rope(ybv, dst, tok):
            p = psB.next()
            k.mm(p[0:64, :], c.rotm.v(), ybv)
            a, b = t1.next(), t2.next()
            k.tt(a.v(), ybv, cs[:, 0, :], ALU.mult)
            k.tt(b.v(), p[0:64, :], cs[:, 1, :], ALU.mult)
            o = ob.next()
            k.tt(o[0:64, :], a.v(), b.v(), ALU.add)
            k.dma(V(dst.ap[:, tok], dst.bufs), o[0:64, :])

        def headnorm(pn, sq_r, gcol_n):
            sqn = sq.next()
            k.actf(sqn.v(), pn.v(), AF.Square)
            p2 = psB.next()
            k.mm(p2.v(), c.ones.v(), sqn.v(), start=True, stop=False)
            k.mm(p2.v(), c.ones[0:64, :], sq_r, start=False, stop=True)
            l_, r_ = lnb.next(), rstd.next()
            k.actf(l_.v(), p2.v(), AF.Ln, scale=1.0 / 192, bias=c.epscol.v())
            k.actf(r_.v(), l_.v(), AF.Exp, scale=-0.5)
            return r_

        psA = Rot(ps[0:2])
        psB = Rot(ps[2:4])
        psBx = Rot(ps[4:5])
        psBt = Rot(ps[5:6])
        psC = Rot(ps[6:7])
        ps_t = Rot(ps[7:8])
        obC = Rot(ph.sbs([128, 512], BF16, 2))

        def proj(pool, hTg, c0, M):
            p = pool.next()
            for kk in range(8):
                k.mm(p[0:M, :], wb[kk][:, c0:c0 + M], hTg[:, kk, :], start=(kk == 0), stop=(kk == 7))
            return p

        def laneA(g, hTg):
            tok = slice(g * 512, (g + 1) * 512)
            sqs = []
            for i in range(3):
                p = proj(psA, hTg, OD["cq"] + i * 128, 128)
                s_ = sq.next()
                k.actf(s_.v(), p.v(), AF.Square)
                k.copy(cqraw[:, i, :], p.v(), eng=k.dve)
                sqs.append(s_)
                yield
            grpnorm(cqraw, sqs, 3, 384, 0, cqn_b)
            yield
            sqs = []
            for i in range(2):
                p = proj(psA, hTg, OD["ckv"] + i * 128, 128)
                s_ = sq.next()
                k.actf(s_.v(), p.v(), AF.Square)
                k.copy(ckvraw[:, i, :], p.v(), eng=k.dve)
                sqs.append(s_)
                yield
            grpnorm(ckvraw, sqs, 2, 256, 3, ckvn_b)
            yield
            p = proj(psA, hTg, OD["kr"], 64)
            k.copy(krraw.v(), p[0:64, :], eng=k.dve)
            yield
            for h in range(4):
                pn = psA.next()
                for kc in range(3):
                    k.mm(pn.v(), wuq[kc][:, h * 192:h * 192 + 128], cqn_b[:, kc, :], start=(kc == 0), stop=(kc == 2))
                pr = psA.next()
                for kc in range(3):
                    k.mm(pr[0:64, :], wuq[kc][:, h * 192 + 128:h * 192 + 192], cqn_b[:, kc, :], start=(kc == 0), stop=(kc == 2))
                yield
                sqr = sq.next()
                k.actf(sqr[0:64, :], pr[0:64, :], AF.Square)
                yield
                r_ = headnorm(pn, sqr[0:64, :], 5)
                yield
                o = ob.next()
                k.stt(o.v(), pn.v(), gc[:, 5:6], r_.v(), ALU.mult, ALU.mult)
                k.dma(V(sc["qT"][h].ap[:, tok], sc["qT"][h].bufs), o.v())
                y_ = yb.next()
                k.stt(y_.v(), pr[0:64, :], gc[0:64, 6:7], r_[0:64, :], ALU.mult, ALU.mult)
                yield
                rope(y_.v(), sc["qr"][h], tok)
                yield
            k.actf(sqkr.v(), krraw.v(), AF.Square)
            for h in range(4):
                pn = psA.next()
                for kc in range(2):
                    k.mm(pn.v(), wukv[kc][:, h * 256:h * 256 + 128], ckvn_b[:, kc, :], start=(kc == 0), stop=(kc == 1))
                yield
                r_ = headnorm(pn, sqkr.v(), 7)
                yield
                o = ob.next()
                k.stt(o.v(), pn.v(), gc[:, 7:8], r_.v(), ALU.mult, ALU.mult)
                k.dma(V(sc["kT"][h].ap[:, tok], sc["kT"][h].bufs), o.v())
                y_ = yb.next()
                k.stt(y_.v(), krraw.v(), gc[0:64, 8:9], r_[0:64, :], ALU.mult, ALU.mult)
                yield
                rope(y_.v(), sc["kr"][h], tok)
                yield
            for t in range(4):
                r0 = g * 512 + t * 128
                tk = slice(t * 128, (t + 1) * 128)
                p = psA.next()
                for kc in range(2):
                    k.mm(p.v().re("p (h d) -> p h d", h=4), ckvn_b[:, kc, tk],
                         wukv[kc][:, :].re("p (h d) -> p h d", h=4)[:, :, 128:256], start=(kc == 0), stop=(kc == 1))
                o = ob.next()
                k.copy(o.v(), p.v(), eng=k.act)
                k.dma(V(sc["vb"].ap[r0:r0 + 128, :], sc["vb"].bufs), o.v())
                yield

        def laneB(g, hTg):
            tok = slice(g * 512, (g + 1) * 512)

            def stage1(j):
                p = proj(psBx, hTg, OD["xs"] + j * 128, 128)
                x_ = xr.next()
                k.copy(x_[:, 0:3], hal[j][:, :], eng=k.pool)
                k.copy(x_[:, 3:515], p.v(), eng=k.act)
                k.copy(hal[j][:, :], x_[:, 512:515], eng=k.pool)
                yield
                a_ = acc.next()
                k.ts(a_.v(), x_[:, 0:512], cw[:, j, 0:1], ALU.mult)
                for w in range(1, 4):
                    k.stt(a_.v(), x_[:, w:w + 512], cw[:, j, w:w + 1], a_.v(), ALU.mult, ALU.add)
                yield
                xa_ = xact.next()
                k.actf(xa_.v(), a_.v(), AF.Silu, bias=cb[:, j:j + 1])
                if j >= 8:
                    dst = sc["BCT"]
                    k.dma(V(dst.ap[(j - 8) * 128:(j - 7) * 128, tok], dst.bufs), xa_.v())
                yield
                return xa_

            def stage2(j, xa_):
                if j >= 12:
                    return
                pt = psBt.next()
                ptb = pt.v().bitcast(BF16)
                for t in range(4):
                    k.tr(ptb[:, t * 128:(t + 1) * 128], xa_[:, t * 128:(t + 1) * 128], c.ident.v())
                yield
                xt_ = xtm.next()
                k.copy(xt_.v(), ptb[:, 0:512].re("p (t c) -> p t c", t=4), eng=k.dve)
                dst = sc["xsB"]
                k.dma(V(dst.ap[tok, j * 128:(j + 1) * 128].rearrange("(t p) c -> p t c", p=128), dst.bufs), xt_.v())
                yield
            prev = None
            for j in range(16):
                xa_ = yield from stage1(j)
                if prev is not None:
                    yield from stage2(*prev)
                prev = (j, xa_)
            yield from stage2(*prev)

        def laneC(g, hTg):
            for t in range(4):
                r0 = g * 512 + t * 128
                tk = slice(t * 128, (t + 1) * 128)
                for hf in range(2):
                    p = psC.next()
                    for kk in range(8):
                        k.mm(p.v(), hTg[:, kk, tk], wb[kk][:, OD["z"] + hf * 512:OD["z"] + (hf + 1) * 512],
                             start=(kk == 0), stop=(kk == 7))
                    yield
                    o = obC.next()
                    k.actf(o.v(), p.v(), AF.Silu)
                    k.dma(V(sc["zs"].ap[r0:r0 + 128, hf * 512:(hf + 1) * 512], sc["zs"].bufs), o.v())
                    yield
                p = psC.next()
                for kk in range(8):
                    k.mm(p[:, 0:16], hTg[:, kk, tk], wb[kk][:, OD["dt"]:OD["dt"] + 16], start=(kk == 0), stop=(kk == 7))
                yield
                o2 = of.next()
                k.copy(o2[:, 0:16], p[:, 0:16], eng=k.dve)
                k.dma(V(sc["dt"].ap[r0:r0 + 128, :], sc["dt"].bufs), o2[:, 0:16])
                yield

        NG = S // 512
        hTs = [None] * NG
        hTs[0] = hT.next()
        run_rr([xnorm_gen(k, c, x_d, xd, 0, gt, xin, hn, scr, stats, hTs[0], ps_t)])
        for g in range(NG):
            tok = slice(g * 512, (g + 1) * 512)
            k.dma(cs[:, 0, :], V(sc["cos"].ap[:, tok], sc["cos"].bufs))
            k.dma(cs[:, 1, :], V(sc["sin"].ap[:, tok], sc["sin"].bufs))
            gens = [laneA(g, hTs[g]), laneB(g, hTs[g]), laneC(g, hTs[g])]
            if g + 1 < NG:
                hTs[g + 1] = hT.next()
                gens.append(xnorm_gen(k, c, x_d, xd, g + 1, gt, xin, hn, scr, stats, hTs[g + 1], ps_t))
            run_rr(gens)


def phase_mla(k, c, sc):
    SCALE = float(192.0 ** -0.5)
    with k.phase() as ph:
        qT = Rot(ph.sbs([128, S], BF16, 2))
        kT = Rot(ph.sbs([128, S], BF16, 2))
        qr = Rot(ph.sbs([64, S], BF16, 2))
        kr = Rot(ph.sbs([64, S], BF16, 2))
        vv = Rot(ph.sbs([128, 32, 128], BF16, 2))
        P_rot = Rot(ph.sbs([128, 512], BF16, 4))
        rden = Rot(ph.sbs([128, 512], F32, 2))
        ob = Rot(ph.sbs([128, 512], BF16, 2))
        ps = ph.psum(8)
        ps_s = Rot(ps[0:3])
        ps_o = Rot(ps[3:5])
        ps_d = Rot(ps[5:7])
        for h in range(4):
            q_, k_, qr_, kr_, v_ = qT.next(), kT.next(), qr.next(), kr.next(), vv.next()
            k.dma(q_.v(), sc["qT"][h])
            k.dma(k_.v(), sc["kT"][h])
            k.dma(qr_.v(), sc["qr"][h])
            k.dma(kr_.v(), sc["kr"][h])
            vsrc = sc["vb"]
            k.dma(v_.v(), V(vsrc.ap.rearrange("(t p) (h d) -> p t h d", p=128, h=4)[:, :, h, :], vsrc.bufs))
            for qg in range(8):
                def qk_fn(sv, kt, col0, q_=q_, k_=k_, qr_=qr_, kr_=kr_, qg=qg):
                    qs = slice(qg * 512 + col0, (qg + 1) * 512)
                    ks = slice(kt * 128, (kt + 1) * 128)
                    k.mm(sv, k_[:, ks], q_[:, qs], start=True, stop=False)
                    k.mm(sv, kr_[:, ks], qr_[:, qs], start=False, stop=True)

                def fin(po, pd, h=h, qg=qg):
                    r = rden.next()
                    k.call(k.dve, "reciprocal", out=r.v(), in_=pd.v())
                    o = ob.next()
                    k.tt(o.v(), po.v(), r.v(), ALU.mult)
                    dst = sc["oT"]
                    k.dma(V(dst.ap[h * 128:(h + 1) * 128, qg * 512:(qg + 1) * 512], dst.bufs), o.v())

                attn_core(k, c, qg, None, qk_fn, P_rot, ps_s, ps_o.next(), ps_d.next(),
                          lambda kt, v_=v_: v_[:, kt, :], SCALE, fin)


def bcl(v, n):
    p, h = v.ap.shape
    return V(v.ap.unsqueeze(2).to_broadcast([p, h, n]), v.bufs)


def phase_ssd(k, c, sc, dt_bias, a_log, d_skip, gate_norm):
    with k.phase() as ph:
        rep = ph.sb([128, 64], F32)
        for i, src in enumerate((dt_bias, a_log, d_skip)):
            k.dma(rep[:, i * 16:(i + 1) * 16], V(src.rearrange("(o h) -> o h", o=1).to_broadcast([128, 16]), (Buf(src),)))
        k.actf(rep[:, 16:32], rep[:, 16:32], AF.Exp)
        k.ts(rep[:, 16:32], rep[:, 16:32], -1.0, ALU.mult)
        one = ph.sb([128, 1], F32)
        k.memset(one.v(), 1.0)
        gg = ph.sb([128, D], F32)
        k.dma(gg.v(), V(gate_norm.rearrange("(o h) -> o h", o=1).to_broadcast([128, D]), (Buf(gate_norm),)))
        negtril = ph.sb([128, 128], BF16)
        tmpf = ph.sb([128, 128], F32)
        k.dma(tmpf.v(), c.negtril_d)
        k.copy(negtril.v(), tmpf.v())
        hst = ph.sb([128, D], F32)
        hstb = ph.sb([128, D], BF16)
        k.memset(hst.v(), 0.0)
        k.memset(hstb.v(), 0.0)
        xs = Rot(ph.sbs([128, D], BF16, 2))
        Btm = Rot(ph.sbs([128, 4, 128], BF16, 2))
        BT = Rot(ph.sbs([128, 4, 128], BF16, 2))
        CT = Rot(ph.sbs([128, 4, 128], BF16, 2))
        zs = Rot(ph.sbs([128, D], BF16, 2))
        dtr = Rot(ph.sbs([128, 16], F32, 2))
        sm = Rot(ph.sbs([128, 128], F32, 2))
        LT = Rot(ph.sbs([128, 16, 128], BF16, 2))
        MT = Rot(ph.sbs([128, 16, 128], BF16, 2))
        cbt = Rot(ph.sbs([128, 4, 128], BF16, 2))
        xdr = Rot(ph.sbs([128, D], BF16, 2))
        xddr = Rot(ph.sbs([128, D], BF16, 2))
        yr = Rot(ph.sbs([128, D], F32, 2))
        t2r = Rot(ph.sbs([128, D], F32, 2))
        junk = ph.sb([128, 256], BF16)
        ynr = Rot(ph.sbs([128, D], BF16, 2))
        oTt = Rot(ph.sbs([128, 8, 128], BF16, 2))
        ps = Rot(ph.psum(8))
        ydr = Rot(ph.sbs([128, D], F32, 2))

        def part1(ci):
            r0 = ci * 128
            tok = slice(r0, r0 + 128)
            xs_, Btm_, BT_, CT_, zs_, dtr_ = xs.next(), Btm.next(), BT.next(), CT.next(), zs.next(), dtr.next()
            xsB = sc["xsB"]
            k.dma(xs_.v(), V(xsB.ap[tok, 0:1024], xsB.bufs))
            k.dma(Btm_.v().re("p g n -> p (g n)"), V(xsB.ap[tok, 1024:1536], xsB.bufs))
            bct = sc["BCT"]
            k.dma(BT_.v(), V(bct.ap[0:512, tok].rearrange("(g n) t -> n g t", n=128), bct.bufs))
            k.dma(CT_.v(), V(bct.ap[512:1024, tok].rearrange("(g n) t -> n g t", n=128), bct.bufs))
            k.dma(zs_.v(), V(sc["zs"].ap[tok, :], sc["zs"].bufs))
            k.dma(dtr_.v(), V(sc["dt"].ap[tok, :], sc["dt"].bufs))
            s_ = sm.next()
            k.tt(s_[:, 0:16], dtr_.v(), rep[:, 0:16], ALU.add)
            k.actf(s_[:, 0:16], s_[:, 0:16], AF.Exp)
            k.actf(s_[:, 0:16], s_[:, 0:16], AF.Ln, bias=one.v())
            k.tt(s_[:, 16:32], s_[:, 0:16], rep[:, 16:32], ALU.mult)
            yield
            pc = ps.next()
            k.mm(pc[:, 0:16], c.trif.v(), s_[:, 16:32])
            k.mm(pc[:, 16:32], c.onesf.v(), s_[:, 16:32])
            k.copy(s_[:, 32:48], pc[:, 0:16])
            k.ts(s_[:, 48:64], pc[:, 0:16], -1.0, ALU.mult)
            k.actf(s_[:, 64:80], pc[:, 0:16], AF.Exp)
            k.tt(s_[:, 112:128], pc[:, 16:32], s_[:, 32:48], ALU.subtract)
            k.actf(s_[:, 80:96], s_[:, 112:128], AF.Exp)
            k.actf(s_[:, 96:112], pc[:, 16:32], AF.Exp)
            yield
            LT_ = LT.next()
            for q4 in range(4):
                pl = ps.next()
                for i in range(4):
                    h = 4 * q4 + i
                    k.mm(pl[:, i * 128:(i + 1) * 128], s_[:, 16 + h:17 + h].bc([128, 128]), c.trif.v(), start=True, stop=False)
                    k.mm(pl[:, i * 128:(i + 1) * 128], c.ident.v(), negtril.v(), start=False, stop=True)
                    k.actf(LT_[:, h, :], pl[:, i * 128:(i + 1) * 128], AF.Exp, bias=s_[:, 48 + h:49 + h])
                yield
            pcb = ps.next()
            for g in range(4):
                k.mm(pcb[:, g * 128:(g + 1) * 128], BT_[:, g, :], CT_[:, g, :])
            cbt_ = cbt.next()
            k.copy(cbt_.v().re("p g n -> p (g n)"), pcb.v(), eng=k.act)
            yield
            MT_ = MT.next()
            for g in range(4):
                k.tt(MT_[:, 4 * g:4 * g + 4, :], LT_[:, 4 * g:4 * g + 4, :], bc1(cbt_[:, g, :], 4), ALU.mult)
            yield
            xd_, xdd_ = xdr.next(), xddr.next()
            k.tt(xd_.v().re("l (h p) -> l h p", p=64), xs_.v().re("l (h p) -> l h p", p=64), bcl(s_[:, 0:16], 64), ALU.mult)
            k.tt(xdd_.v().re("l (h p) -> l h p", p=64), xd_.v().re("l (h p) -> l h p", p=64), bcl(s_[:, 80:96], 64), ALU.mult)
            t2_ = t2r.next()
            k.tt(t2_.v().re("l (h p) -> l h p", p=64), xs_.v().re("l (h p) -> l h p", p=64), bcl(rep[:, 32:48], 64), ALU.mult, eng=k.pool)
            yield
            yd_ = ydr.next()
            for hf in range(2):
                hs = slice(hf * 512, (hf + 1) * 512)
                py = ps.next()
                for hh in range(8):
                    h = hf * 8 + hh
                    k.mm(py[:, hh * 64:(hh + 1) * 64], MT_[:, h, :], xd_[:, h * 64:(h + 1) * 64])
                k.tt(yd_[:, hs], py.v(), t2_[:, hs], ALU.add)
                yield
            return (tok, s_, CT_, Btm_, xdd_, zs_, yd_)

        def part2(st):
            tok, s_, CT_, Btm_, xdd_, zs_, yd_ = st
            y_ = yr.next()
            for hf in range(2):
                hs = slice(hf * 512, (hf + 1) * 512)
                po = ps.next()
                for gg_ in range(2):
                    g = hf * 2 + gg_
                    k.mm(po[:, gg_ * 256:(gg_ + 1) * 256], CT_[:, g, :], hstb[:, g * 256:(g + 1) * 256])
                k.tt(y_[:, hs].re("l (h p) -> l h p", p=64), po.v().re("l (h p) -> l h p", p=64),
                     bcl(s_[:, 64 + hf * 8:72 + hf * 8], 64), ALU.mult)
                k.tt(y_[:, hs], y_[:, hs], yd_[:, hs], ALU.add)
                yield
            for hf in range(2):
                hs = slice(hf * 512, (hf + 1) * 512)
                pst = ps.next()
                for gg_ in range(2):
                    g = hf * 2 + gg_
                    k.mm(pst[:, gg_ * 256:(gg_ + 1) * 256], Btm_[:, g, :], xdd_[:, g * 256:(g + 1) * 256])
                k.tt(hst[:, hs].re("l (h p) -> l h p", p=64), hst[:, hs].re("l (h p) -> l h p", p=64),
                     bcl(s_[:, 96 + hf * 8:104 + hf * 8], 64), ALU.mult, eng=k.pool)
                k.tt(hst[:, hs], hst[:, hs], pst.v(), ALU.add)
                k.copy(hstb[:, hs], hst[:, hs], eng=k.act)
                yield
            k.tt(y_.v(), y_.v(), zs_.v(), ALU.mult)
            for g in range(4):
                k.actf(junk.v(), y_[:, g * 256:(g + 1) * 256], AF.Square, accum_out=s_[:, 112 + g:113 + g])
            yield
            k.ts(s_[:, 116:120], s_[:, 112:116], 1.0 / 256, ALU.mult, EPS, ALU.add)
            k.tt(s_[:, 120:124], s_[:, 116:120], c.mhalf.v().bc([128, 4]), ALU.pow, eng=k.pool)
            yn_ = ynr.next()
            for g in range(4):
                gs = slice(g * 256, (g + 1) * 256)
                k.stt(yn_[:, gs], y_[:, gs], s_[:, 120 + g:121 + g], gg[:, gs], ALU.mult, ALU.mult)
            yield
            pt = ps.next()
            ptb = pt.v().bitcast(BF16)
            for j in range(8):
                k.tr(ptb[:, j * 128:(j + 1) * 128], yn_[:, j * 128:(j + 1) * 128], c.ident.v())
            o_ = oTt.next()
            k.copy(o_.v().re("p j t -> p (j t)"), ptb, eng=k.act)
            dst = sc["oT"]
            k.dma(V(dst.ap[512:1536, tok].rearrange("(j p) t -> p j t", p=128), dst.bufs), o_.v())
            yield

        states = {}

        def p1(ci):
            states[ci] = yield from part1(ci)
        run_rr([p1(0)])
        for ci in range(NT):
            gens = [part2(states.pop(ci))]
            if ci + 1 < NT:
                gens.append(p1(ci + 1))
            run_rr(gens)


def const_arrays():
    cd = {}
    cd["ident"] = np.eye(128, dtype=np.float32)
    cd["pow2"] = np.tile((2.0 ** -(np.arange(32) + 1.0)).astype(np.float32)[None, :], (128, 1))
    invf = (10000.0 ** (-np.arange(32, dtype=np.float32) / 32)).astype(np.float32)
    cd["invf"] = np.concatenate([invf, invf])[:, None].astype(np.float32)
    rot = np.zeros((64, 64), np.float32)
    for m in range(32):
        rot[m + 32, m] = -1.0
        rot[m, m + 32] = 1.0
    cd["rotm"] = rot
    cd["negtril"] = (np.tril(np.ones((128, 128), np.float32), -1) * -30000.0).astype(np.float32)
    cd["tri"] = np.triu(np.ones((128, 128), np.float32))
    cd["negtri"] = (np.triu(np.ones((128, 128), np.float32), 1) * -1e30).astype(np.float32)
    return cd


INPUT_NAMES = ["x", "positions", "ev_norm", "ev_w_in", "ev_b_f", "ev_qn_a", "ev_kn_a", "ev_qn_b", "ev_kn_b",
               "ev_w_out", "od_norm", "od_w_in", "od_cq_norm", "od_ckv_norm", "od_w_uq", "od_w_ukv", "od_qn_c",
               "od_kn_c", "od_conv_w", "od_conv_b", "od_dt_bias", "od_a_log", "od_d_skip", "od_gate_norm",
               "od_w_out", "mlp_norm", "mlp_w1", "mlp_w2"]


def build(shapes, cfg):
    nc = bass.Bass("TRN2", target_bir_lowering=False)
    din = {}
    for name, (shp, dt) in shapes.items():
        bdt = I32 if np.dtype(dt) == np.int32 else F32
        din[name] = nc.dram_tensor(name, list(shp), bdt, kind="ExternalInput").ap()
    cds = const_arrays()
    cd = {n: nc.dram_tensor("c_" + n, list(a.shape), F32, kind="ExternalInput").ap() for n, a in cds.items()}
    y = nc.dram_tensor("y", [S, D], F32, kind="ExternalOutput").ap()
    xa = nc.dram_tensor("xa", [S, D], F32, kind="Internal").ap()
    xb = nc.dram_tensor("xb", [S, D], F32, kind="Internal").ap()

    def mk(name, shape, dt):
        return nc.dram_tensor(name, list(shape), dt, kind="Internal").ap()

    def dv(ap):
        return V(ap, (Buf(ap),))
    sc = {}
    t = mk("s_qT", [8, 128, S], BF16)
    sc["qT"] = [dv(t[h]) for h in range(8)]
    t = mk("s_kT", [5, 128, S], BF16)
    sc["kT"] = [dv(t[h]) for h in range(5)]
    t = mk("s_qiT", [4, 128, S], BF16)
    sc["qiT"] = [dv(t[h]) for h in range(4)]
    sc["kiT"] = dv(mk("s_kiT", [128, S], BF16))
    sc["va"] = dv(mk("s_va", [S, 128], BF16))
    sc["vb"] = dv(mk("s_vb", [S, 512], BF16))
    sc["wi"] = dv(mk("s_wi", [S, 8], F32))
    sc["fbT"] = dv(mk("s_fbT", [4, S], F32))
    sc["aug"] = dv(mk("s_aug", [4, 2, 6, S], BF16))
    sc["oT"] = dv(mk("s_oT", [1536, S], BF16))
    t = mk("s_qr", [4, 64, S], BF16)
    sc["qr"] = [dv(t[h]) for h in range(4)]
    t = mk("s_kr", [4, 64, S], BF16)
    sc["kr"] = [dv(t[h]) for h in range(4)]
    sc["cos"] = dv(mk("s_cos", [64, S], F32))
    sc["sin"] = dv(mk("s_sin", [64, S], F32))
    sc["BCT"] = dv(mk("s_BCT", [1024, S], BF16))
    sc["xsB"] = dv(mk("s_xsB", [S, 1536], BF16))
    sc["zs"] = dv(mk("s_zs", [S, 1024], BF16))
    sc["dt"] = dv(mk("s_dt", [S, 16], F32))
    dbg = cfg.get("debug", {})
    k = K(nc)
    with k.es:
        c = setup_consts(k, cd)
        cur = din["x"]
        ropedone = [False]
        steps = cfg["steps"]
        for si, (kind, l) in enumerate(steps):
            last = si == len(steps) - 1
            dst = y if last else (xa if cur is not xa else xb)
            if kind == "mlp":
                phase_mlp(k, c, cur, dst, din["mlp_norm"][l:l + 1, :], din["mlp_w1"][l], din["mlp_w2"][l])
            elif kind == "even":
                phase_even_proj(k, c, sc, cur, din["ev_norm"][l:l + 1, :], din["ev_w_in"][l], din["ev_qn_a"][l],
                                din["ev_kn_a"][l], din["ev_qn_b"][l], din["ev_kn_b"][l])
                if "nodsa" not in dbg:
                    phase_dsa(k, c, sc)
                if "nofox" not in dbg:
                    phase_fox(k, c, sc, din["ev_b_f"][l])
                phase_outproj(k, c, sc, cur, dst, din["ev_w_out"][l], 1024)
            elif kind == "odd":
                if "oddlvl" in dbg:
                    ODDLVL[0] = dbg["oddlvl"]
                if not ropedone[0] and "norope" not in dbg:
                    phase_rope_tables(k, c, sc, din["positions"])
                    ropedone[0] = True
                phase_odd_proj(k, c, sc, cur, din["od_norm"][l:l + 1, :], din["od_w_in"][l], din["od_cq_norm"][l],
                               din["od_ckv_norm"][l], din["od_w_uq"][l], din["od_w_ukv"][l], din["od_qn_c"][l],
                               din["od_kn_c"][l], din["od_conv_w"][l], din["od_conv_b"][l])
                if "nomla" not in dbg:
                    phase_mla(k, c, sc)
                if "nossd" not in dbg:
                    phase_ssd(k, c, sc, din["od_dt_bias"][l], din["od_a_log"][l], din["od_d_skip"][l],
                              din["od_gate_norm"][l])
                phase_outproj(k, c, sc, cur, dst, din["od_w_out"][l], 1536)
            else:
                raise ValueError(kind)
            cur = dst
        k.barrier()
    return nc, cds


FULL_CFG = {"steps": [("even", 0), ("mlp", 0), ("odd", 0), ("mlp", 1), ("even", 1), ("mlp", 2), ("odd", 1), ("mlp", 3)]}


def run(inputs, cfg, cores=N_CORES):
    per_core = []
    for b in range(cores):
        m = {}
        for n in INPUT_NAMES:
            a = np.asarray(inputs[n])
            if n in ("x", "positions"):
                a = a[b]
            m[n] = np.ascontiguousarray(a)
        per_core.append(m)
    shapes = {n: (per_core[0][n].shape, per_core[0][n].dtype) for n in INPUT_NAMES}
    nc, cds = build(shapes, cfg)
    for m in per_core:
        for n, a in cds.items():
            m["c_" + n] = a
    res = run_bass_kernel_spmd(nc, per_core, core_ids=list(range(cores)))
    return np.stack([np.asarray(r["y"]) for r in res.results], axis=0)


def kernel(**inputs):
    out = run(inputs, FULL_CFG)
    return out.astype(np.float32)
```
